# Optimizing a Trainium2 kernel written in Bass

```python
import math
import jax, jax.numpy as jnp
from jax import lax
import numpy as np

D_MODEL = 1024
BATCH = 8
SEQ = 4096
DEPTH = 2

GRID_W = 64
CTX_LEN = 256
F32 = jnp.float32
EPS = 1e-6
CONV_CH = 512
CONV_K = 31
MLA_HEADS = 8
QK_NOPE = 64
QK_ROPE = 32
V_DIM = 64
Q_RANK = 384
KV_RANK = 256
ROPE_BASE = 10000.0
Q_BLOCK = 128
MLA_SCALE = (QK_NOPE + QK_ROPE) ** -0.5
GDN_HEADS = 8
GDN_DK = 64
GDN_DV = 64
SHORT_K = 5
CHUNK = 64
N_GROUPS = 4
EXP_PER_GROUP = 8
N_EXPERTS = N_GROUPS * EXP_PER_GROUP
TOP_K = 2
D_EXPERT = 256
ROW_BLOCK = 128
ALPHA = (2 * DEPTH) ** 0.25
BETA_INIT = (8 * DEPTH) ** -0.25
IN_SIZES = (2 * CONV_CH, Q_RANK, KV_RANK, QK_ROPE, GDN_HEADS * GDN_DK, GDN_HEADS * GDN_DK, GDN_HEADS * GDN_DV, GDN_HEADS * GDN_DV, 2 * GDN_HEADS, 2 * GDN_HEADS, 3 * D_MODEL)
P_IN = sum(IN_SIZES)

kernel_name = "hybrid_conv_mla_gdn_hmoe_diffusion_block"


def _ln(x, g, b):
    xf = x.astype(F32)
    mu = jnp.mean(xf, -1, keepdims=True)
    var = jnp.mean(jnp.square(xf - mu), -1, keepdims=True)
    return ((xf - mu) * lax.rsqrt(var + EPS)).astype(x.dtype) * g + b


def _rms(x, g):
    xf = x.astype(F32)
    return (xf * lax.rsqrt(jnp.mean(jnp.square(xf), -1, keepdims=True) + EPS)).astype(x.dtype) * g


def _l2n(x):
    xf = x.astype(F32)
    return (xf * lax.rsqrt(jnp.sum(jnp.square(xf), -1, keepdims=True) + EPS)).astype(x.dtype)


def _split(y, sizes):
    return jnp.split(y, np.cumsum(sizes)[:-1].tolist(), axis=-1)


def _dwconv(u, w):
    k = w.shape[0]
    return lax.conv_general_dilated(u, w[:, None, :], (1,), [(k // 2, k // 2)],
                                    dimension_numbers=("NWC", "WIO", "NWC"),
                                    feature_group_count=u.shape[-1])


def _axial_rope(rows):
    nf = QK_ROPE // 4
    inv = ROPE_BASE ** (-jnp.arange(nf, dtype=F32) / nf)
    r = jnp.repeat(jnp.arange(rows, dtype=F32), GRID_W)
    col = jnp.tile(jnp.arange(GRID_W, dtype=F32), rows)
    ang = jnp.stack([r[:, None] * inv, col[:, None] * inv], axis=1)
    return jnp.cos(ang), jnp.sin(ang)


def _rope(x, cos, sin):
    xs = x.reshape(x.shape[:-1] + (2, 2, QK_ROPE // 4))
    x1, x2 = xs[..., 0, :], xs[..., 1, :]
    cos = cos.astype(x.dtype)
    sin = sin.astype(x.dtype)
    out = jnp.stack([x1 * cos - x2 * sin, x2 * cos + x1 * sin], axis=-2)
    return out.reshape(x.shape)


def _conv_branch(u, p):
    a, gt = jnp.split(u, 2, axis=-1)
    u = a * jax.nn.sigmoid(gt)
    u = _dwconv(u, p["conv_a_w"]) + p["conv_a_b"]
    u = jax.nn.silu(_ln(u, p["ln_a_g"], p["ln_a_b"]))
    return u @ p["w_a_out"]


def _mla_q(qd, p, cos, sin):
    b, n = qd.shape[:2]
    cq = _rms(qd, p["g_q"])
    q_nope = (cq @ p["w_uq"]).reshape(b, n, MLA_HEADS, QK_NOPE)
    q_rope = (cq @ p["w_qr"]).reshape(b, n, MLA_HEADS, QK_ROPE)
    if cos is not None:
        q_rope = _rope(q_rope, cos[:, None], sin[:, None])
    return jnp.concatenate([q_nope, q_rope], -1) * MLA_SCALE


def _mla_kv(kvd, kr, p, cos, sin):
    b, n = kvd.shape[:2]
    ckv = _rms(kvd, p["g_kv"])
    k_nope = (ckv @ p["w_uk"]).reshape(b, n, MLA_HEADS, QK_NOPE)
    v = (ckv @ p["w_uv"]).reshape(b, n, MLA_HEADS, V_DIM)
    if cos is not None:
        kr = _rope(kr, cos, sin)
    k_rope = jnp.broadcast_to(kr[:, :, None, :], (b, n, MLA_HEADS, QK_ROPE))
    return jnp.concatenate([k_nope, k_rope], -1), v


def _attend(q, k, v):
    s = jnp.einsum("bqhd,bkhd->bhqk", q, k).astype(F32)
    pr = jax.nn.softmax(s, axis=-1).astype(v.dtype)
    return jnp.einsum("bhqk,bkhd->bqhd", pr, v)


def _attend_blocks(q, k, v):
    b, n, h, dq = q.shape
    qb = q.reshape(b, n // Q_BLOCK, Q_BLOCK, h, dq).transpose(1, 0, 2, 3, 4)
    o = lax.map(lambda qq: _attend(qq, k, v), qb)
    return o.transpose(1, 0, 2, 3, 4).reshape(b, n, h, v.shape[-1])


def _gdn_inputs(gq, gk, gv, braw, araw, p, need_q):
    qk = GDN_HEADS * GDN_DK
    b, n = gk.shape[:2]
    if need_q:
        u = jax.nn.silu(_dwconv(jnp.concatenate([gq, gk, gv], -1), p["conv_c_w"]))
        q, u = u[..., :qk], u[..., qk:]
        q = _l2n(q.reshape(b, n, GDN_HEADS, GDN_DK)) * (GDN_DK ** -0.5)
    else:
        u = jax.nn.silu(_dwconv(jnp.concatenate([gk, gv], -1), p["conv_c_w"][:, qk:]))
        q = None
    k = _l2n(u[..., :qk].reshape(b, n, GDN_HEADS, GDN_DK))
    v = u[..., qk:].reshape(b, n, GDN_HEADS, GDN_DV)
    beta = jax.nn.sigmoid(braw.astype(F32)).reshape(b, n, 2, GDN_HEADS)
    g = -jnp.exp(p["a_log"].astype(F32)) * jax.nn.softplus(araw.astype(F32).reshape(b, n, 2, GDN_HEADS) + p["dt_bias"].astype(F32))
    return q, k, v, beta, g


def _direction(inp, d):
    q, k, v, beta, g = inp
    beta, g = beta[:, :, d], g[:, :, d]
    if d == 0:
        return q, k, v, beta, g
    fl = lambda t: None if t is None else jnp.flip(t, 1)
    return fl(q), fl(k), fl(v), fl(beta), fl(g)


def _gdn_scan(q, k, v, beta, g, s0):
    b, n, h, dk = k.shape
    dv = v.shape[-1]
    nc = n // CHUNK
    out_dtype = v.dtype

    def blk(t):
        return t.astype(F32).reshape(b, nc, CHUNK, h, -1).transpose(1, 0, 3, 2, 4)

    k_, v_ = blk(k), blk(v)
    beta_ = blk(beta[..., None])[..., 0]
    gam = jnp.cumsum(blk(g[..., None])[..., 0], axis=-1)
    incl = jnp.tril(jnp.ones((CHUNK, CHUNK), dtype=bool))
    strict = jnp.tril(jnp.ones((CHUNK, CHUNK), dtype=bool), -1)
    diff = gam[..., :, None] - gam[..., None, :]
    decay = jnp.where(incl, jnp.exp(jnp.where(incl, diff, 0.0)), 0.0)
    kk = jnp.einsum("nbhid,nbhjd->nbhij", k_, k_)
    a_mat = jnp.where(strict, beta_[..., :, None] * decay * kk, 0.0) + jnp.eye(CHUNK, dtype=F32)
    rhs = jnp.concatenate([(beta_ * jnp.exp(gam))[..., None] * k_, beta_[..., None] * v_], -1)
    sol = lax.linalg.triangular_solve(a_mat, rhs, left_side=True, lower=True, unit_diagonal=True)
    w, u0 = sol[..., :dk], sol[..., dk:]
    kd = k_ * jnp.exp(gam[..., -1:] - gam)[..., None]
    gc = jnp.exp(gam[..., -1])[..., None, None]

    def advance(s, w_c, u0_c, kd_c, gc_c):
        u = u0_c - jnp.einsum("bhcd,bhde->bhce", w_c, s)
        return gc_c * s + jnp.einsum("bhcd,bhce->bhde", kd_c, u), u

    if q is None:
        def step_state(s, xs):
            s_new, _ = advance(s, *xs)
            return s_new, None
        s_fin, _ = lax.scan(step_state, s0, (w, u0, kd, gc))
        return None, s_fin

    q_ = blk(q)
    qg = q_ * jnp.exp(gam)[..., None]
    pm = decay * jnp.einsum("nbhid,nbhjd->nbhij", q_, k_)

    def step(s, xs):
        w_c, u0_c, kd_c, gc_c, qg_c, pm_c = xs
        s_new, u = advance(s, w_c, u0_c, kd_c, gc_c)
        o = jnp.einsum("bhcd,bhde->bhce", qg_c, s) + jnp.einsum("bhij,bhje->bhie", pm_c, u)
        return s_new, o

    s_fin, o = lax.scan(step, s0, (w, u0, kd, gc, qg, pm))
    o = o.transpose(1, 0, 3, 2, 4).reshape(b, n, h, dv).astype(out_dtype)
    return o, s_fin


def _gdn_bidir(gin_c, gin_l):
    b = gin_l[1].shape[0]
    s0 = jnp.zeros((b, GDN_HEADS, GDN_DK, GDN_DV), F32)
    outs_c, outs_l = [], []
    for d in range(2):
        oc, s_c = _gdn_scan(*_direction(gin_c, d), s0)
        ol, _ = _gdn_scan(*_direction(gin_l, d), s_c)
        if d == 1:
            ol = jnp.flip(ol, 1)
            oc = None if oc is None else jnp.flip(oc, 1)
        outs_c.append(oc)
        outs_l.append(ol)
    o_c = None if outs_c[0] is None else outs_c[0] + outs_c[1]
    return o_c, outs_l[0] + outs_l[1]


def _gdn_out(o, gate, p):
    b, n = o.shape[:2]
    o = _rms(o, p["g_o"]) * jax.nn.silu(gate.reshape(b, n, GDN_HEADS, GDN_DV))
    return o.reshape(b, n, -1) @ p["w_c_out"]


def _merge(gate_cols, ya, yb, yc, p):
    ga, gb, gc = jnp.split(jax.nn.sigmoid(gate_cols), 3, axis=-1)
    return (ga * ya + gb * yb + gc * yc) @ p["w_out"]


def _mixer_layer(hc, hl, p, cos, sin, ctx_out):
    pc = _split(hc @ p["w_in"] + p["b_in"], IN_SIZES)
    pl = _split(hl @ p["w_in"] + p["b_in"], IN_SIZES)
    b, n = hl.shape[:2]
    ya_l = _conv_branch(pl[0], p)
    k_c, v_c = _mla_kv(pc[2], pc[3], p, None, None)
    k_l, v_l = _mla_kv(pl[2], pl[3], p, cos, sin)
    q_l = _mla_q(pl[1], p, cos, sin)
    yb_l = _attend_blocks(q_l, jnp.concatenate([k_c, k_l], 1), jnp.concatenate([v_c, v_l], 1))
    yb_l = yb_l.reshape(b, n, -1) @ p["w_b_out"]
    gin_c = _gdn_inputs(pc[4], pc[5], pc[6], pc[8], pc[9], p, ctx_out)
    gin_l = _gdn_inputs(pl[4], pl[5], pl[6], pl[8], pl[9], p, True)
    o_c, o_l = _gdn_bidir(gin_c, gin_l)
    yc_l = _gdn_out(o_l, pl[7], p)
    out_l = _merge(pl[10], ya_l, yb_l, yc_l, p)
    if not ctx_out:
        return None, out_l
    bc, nc = hc.shape[:2]
    ya_c = _conv_branch(pc[0], p)
    yb_c = _attend(_mla_q(pc[1], p, None, None), k_c, v_c).reshape(bc, nc, -1) @ p["w_b_out"]
    yc_c = _gdn_out(o_c, pc[7], p)
    out_c = _merge(pc[10], ya_c, yb_c, yc_c, p)
    return out_c, out_l


def _moe(h, p):
    shp = h.shape
    t = h.reshape(-1, shp[-1])
    n_tok = t.shape[0]
    tf = t.astype(F32)
    grp_prob = jax.nn.softmax(tf @ p["w_rg"].astype(F32) + p["b_rg"].astype(F32), axis=-1)
    gp, gi = lax.top_k(grp_prob, 1)
    e_logits = (tf @ p["w_re"].astype(F32) + p["b_re"].astype(F32)).reshape(n_tok, N_GROUPS, EXP_PER_GROUP)
    in_grp = jnp.take_along_axis(e_logits, gi[:, :, None], axis=1)[:, 0]
    ev, ei = lax.top_k(in_grp, TOP_K)
    gate = (gp * jax.nn.softmax(ev, axis=-1)).reshape(-1)
    eid = (gi * EXP_PER_GROUP + ei).reshape(-1)
    tok = jnp.repeat(jnp.arange(n_tok, dtype=jnp.int32), TOP_K)
    order = jnp.argsort(eid)
    es, ts, gs = eid[order], tok[order], gate[order]
    n_as = es.shape[0]
    sizes = jnp.bincount(es, length=N_EXPERTS)
    psizes = (sizes + ROW_BLOCK - 1) // ROW_BLOCK * ROW_BLOCK
    starts = jnp.cumsum(sizes) - sizes
    pends = jnp.cumsum(psizes)
    pstarts = pends - psizes
    dest = pstarts[es] + jnp.arange(n_as, dtype=jnp.int32) - starts[es]
    n_blk = (n_as + ROW_BLOCK - 1) // ROW_BLOCK + N_EXPERTS
    n_pad = n_blk * ROW_BLOCK
    tok_buf = jnp.zeros((n_pad,), jnp.int32).at[dest].set(ts)
    gate_buf = jnp.zeros((n_pad,), F32).at[dest].set(gs)
    blk_exp = jnp.minimum(jnp.searchsorted(pends, jnp.arange(n_blk, dtype=pends.dtype) * ROW_BLOCK, side="right"), N_EXPERTS - 1)
    xb = t[tok_buf].reshape(n_blk, ROW_BLOCK, -1)

    def expert_block(args):
        xx, e = args
        return (jax.nn.silu(xx @ p["w1"][e]) * (xx @ p["w3"][e])) @ p["w2"][e]

    yb = lax.map(expert_block, (xb, blk_exp)).reshape(n_pad, -1)
    y = jnp.zeros_like(t).at[tok_buf].add(yb * gate_buf[:, None].astype(t.dtype))
    return y.reshape(shp)


def setup_inputs(seed: int = 0) -> dict:
    key = jax.random.key(seed)
    ks = iter(jax.random.split(key, 48))
    L, D = DEPTH, D_MODEL

    def nrm(shape, scale):
        return jax.random.normal(next(ks), shape, F32) * scale

    def gain(shape):
        return 1.0 + nrm(shape, 0.02)

    out = {}
    out["x"] = nrm((BATCH, SEQ, D), 1.0)
    out["c"] = nrm((BATCH, D), 1.0)
    out["ctx"] = nrm((BATCH, CTX_LEN, D), 1.0)
    out["c_ctx"] = nrm((D,), 1.0)
    out["w_mod"] = nrm((L, D, 6 * D), 0.5 * D ** -0.5)
    out["b_mod"] = nrm((L, 6 * D), 0.02)
    out["w_in"] = nrm((L, D, P_IN), D ** -0.5)
    out["b_in"] = nrm((L, P_IN), 0.02)
    out["conv_a_w"] = nrm((L, CONV_K, CONV_CH), CONV_K ** -0.5)
    out["conv_a_b"] = nrm((L, CONV_CH), 0.02)
    out["ln_a_g"] = gain((L, CONV_CH))
    out["ln_a_b"] = nrm((L, CONV_CH), 0.02)
    out["w_a_out"] = nrm((L, CONV_CH, D), CONV_CH ** -0.5)
    out["g_q"] = gain((L, Q_RANK))
    out["g_kv"] = gain((L, KV_RANK))
    out["w_uq"] = nrm((L, Q_RANK, MLA_HEADS * QK_NOPE), Q_RANK ** -0.5)
    out["w_qr"] = nrm((L, Q_RANK, MLA_HEADS * QK_ROPE), Q_RANK ** -0.5)
    out["w_uk"] = nrm((L, KV_RANK, MLA_HEADS * QK_NOPE), KV_RANK ** -0.5)
    out["w_uv"] = nrm((L, KV_RANK, MLA_HEADS * V_DIM), KV_RANK ** -0.5)
    out["w_b_out"] = nrm((L, MLA_HEADS * V_DIM, D), (MLA_HEADS * V_DIM) ** -0.5)
    out["conv_c_w"] = nrm((L, SHORT_K, GDN_HEADS * (2 * GDN_DK + GDN_DV)), SHORT_K ** -0.5)
    out["a_log"] = jnp.log(jax.random.uniform(next(ks), (L, 2, GDN_HEADS), F32, 1.0, 16.0))
    dt = jnp.exp(jax.random.uniform(next(ks), (L, 2, GDN_HEADS), F32, math.log(1e-3), math.log(0.1)))
    out["dt_bias"] = dt + jnp.log(-jnp.expm1(-dt))
    out["g_o"] = gain((L, GDN_DV))
    out["w_c_out"] = nrm((L, GDN_HEADS * GDN_DV, D), (GDN_HEADS * GDN_DV) ** -0.5)
    out["w_out"] = nrm((L, D, D), BETA_INIT * D ** -0.5)
    out["ln1_g"] = gain((L, D))
    out["ln1_b"] = nrm((L, D), 0.02)
    out["w_rg"] = nrm((L, D, N_GROUPS), D ** -0.5)
    out["b_rg"] = nrm((L, N_GROUPS), 0.01)
    out["w_re"] = nrm((L, D, N_EXPERTS), D ** -0.5)
    out["b_re"] = nrm((L, N_EXPERTS), 0.01)
    out["w1"] = nrm((L, N_EXPERTS, D, D_EXPERT), D ** -0.5)
    out["w3"] = nrm((L, N_EXPERTS, D, D_EXPERT), D ** -0.5)
    out["w2"] = nrm((L, N_EXPERTS, D_EXPERT, D), BETA_INIT * D_EXPERT ** -0.5)
    out["ln2_g"] = gain((L, D))
    out["ln2_b"] = nrm((L, D), 0.02)
    return out


def reference(x, c, ctx, c_ctx, w_mod, b_mod, w_in, b_in, conv_a_w, conv_a_b, ln_a_g, ln_a_b, w_a_out,
              g_q, g_kv, w_uq, w_qr, w_uk, w_uv, w_b_out, conv_c_w, a_log, dt_bias, g_o, w_c_out,
              w_out, ln1_g, ln1_b, w_rg, b_rg, w_re, b_re, w1, w3, w2, ln2_g, ln2_b):
    rows = x.shape[1] // GRID_W
    cos, sin = _axial_rope(rows)
    xl, xc = x, ctx
    n_ctx = ctx.shape[1]
    for l in range(DEPTH):
        last = l == DEPTH - 1
        p = dict(w_in=w_in[l], b_in=b_in[l], conv_a_w=conv_a_w[l], conv_a_b=conv_a_b[l], ln_a_g=ln_a_g[l],
                 ln_a_b=ln_a_b[l], w_a_out=w_a_out[l], g_q=g_q[l], g_kv=g_kv[l], w_uq=w_uq[l], w_qr=w_qr[l],
                 w_uk=w_uk[l], w_uv=w_uv[l], w_b_out=w_b_out[l], conv_c_w=conv_c_w[l], a_log=a_log[l],
                 dt_bias=dt_bias[l], g_o=g_o[l], w_c_out=w_c_out[l], w_out=w_out[l], w_rg=w_rg[l], b_rg=b_rg[l],
                 w_re=w_re[l], b_re=b_re[l], w1=w1[l], w3=w3[l], w2=w2[l])
        mod_l = (jax.nn.silu(c) @ w_mod[l] + b_mod[l])[:, None, :]
        mod_c = (jax.nn.silu(c_ctx) @ w_mod[l] + b_mod[l])[None, None, :]
        sh1l, sc1l, g1l, sh2l, sc2l, g2l = jnp.split(mod_l, 6, axis=-1)
        sh1c, sc1c, g1c, sh2c, sc2c, g2c = jnp.split(mod_c, 6, axis=-1)
        hl = xl * (1 + sc1l) + sh1l
        hc = xc * (1 + sc1c) + sh1c
        yc, yl = _mixer_layer(hc, hl, p, cos, sin, not last)
        xl = _ln(ALPHA * xl + g1l * yl, ln1_g[l], ln1_b[l])
        h2l = xl * (1 + sc2l) + sh2l
        if last:
            y2l = _moe(h2l, p)
        else:
            xc = _ln(ALPHA * xc + g1c * yc, ln1_g[l], ln1_b[l])
            h2c = xc * (1 + sc2c) + sh2c
            y2 = _moe(jnp.concatenate([h2c, h2l], axis=1), p)
            y2c, y2l = y2[:, :n_ctx], y2[:, n_ctx:]
            xc = _ln(ALPHA * xc + g2c * y2c, ln2_g[l], ln2_b[l])
        xl = _ln(ALPHA * xl + g2l * y2l, ln2_g[l], ln2_b[l])
    return xl
```

```python
from contextlib import ExitStack
import numpy as np
import concourse.bass as bass
import concourse.mybir as mybir
from concourse.bass_utils import run_bass_kernel_spmd

F32 = mybir.dt.float32
BF16 = mybir.dt.bfloat16
AF = mybir.ActivationFunctionType
ALU = mybir.AluOpType
AX = mybir.AxisListType

D = 1024
NCTX = 256
NL = 4096
T = NCTX + NL
DEPTH = 2
P_IN = 6848
EPS = 1e-6
ALPHA = (2 * DEPTH) ** 0.25
MLA_SCALE = 96 ** -0.5
BLOCKS = [(0, 256)] + [(256 + 512 * i, 512) for i in range(8)]

DMA_ENGS = ("sp", "dq_pool", "dq_act")
PHYS = {"pe": "pe", "act": "act", "dve": "dve", "pool": "pool", "sp": "sp", "dq_pool": "pool", "dq_act": "act"}


class Prog:
    def __init__(self):
        self.nc = bass.Bass("TRN2", target_bir_lowering=False)
        self.root = ExitStack()
        self.es = self.root
        self.ops = []
        self.n_dsem = 12
        self.psum_names = set()

    def sb(self, name, shape, dt=F32):
        return self.es.enter_context(self.nc.sbuf_tensor(name, list(shape), dt))

    def ps(self, name, shape, dt=F32):
        self.psum_names.add(name)
        return self.es.enter_context(self.nc.psum_tensor(name, list(shape), dt))

    def dram(self, name, shape, dt=F32, kind="Internal"):
        return self.nc.dram_tensor(name, list(shape), dt, kind=kind).ap()

    def op(self, eng, fn, r=(), w=()):
        pr = [k for k in r if k in self.psum_names]
        if pr:
            r = [k for k in r if k not in self.psum_names]
            w = list(w) + pr
        self.ops.append((eng, fn, tuple(r), tuple(w)))

    def dma(self, out, in_, r=(), w=(), q="sp", slow=False):
        nc = self.nc
        e = {"sp": nc.sync, "dq_pool": nc.gpsimd, "dq_act": nc.scalar}[q]
        if slow:
            self.op(q, lambda: e.dma_start(out=out, in_=in_, allow_slow_non_contiguous=True), r, w)
        else:
            self.op(q, lambda: e.dma_start(out=out, in_=in_), r, w)

    def mm(self, out, lhsT, rhs, start, stop, r=(), w=()):
        nc = self.nc
        self.op("pe", lambda: nc.tensor.matmul(out, lhsT=lhsT, rhs=rhs, start=start, stop=stop), r, w)

    def tr(self, out, in_, ident, r=(), w=()):
        nc = self.nc
        self.op("pe", lambda: nc.tensor.transpose(out, in_, ident), r, w)

    def act(self, out, in_, func, r=(), w=(), bias=None, scale=None, accum_out=None):
        nc = self.nc
        kw = {}
        if bias is not None:
            kw["bias"] = bias
        if scale is not None:
            kw["scale"] = scale
        if accum_out is not None:
            kw["accum_out"] = accum_out
        self.op("act", lambda: nc.scalar.activation(out=out, in_=in_, func=func, **kw), r, w)

    def _veng(self, eng):
        return self.nc.vector if eng == "dve" else self.nc.gpsimd

    def tt(self, out, in0, in1, op, r=(), w=(), eng="dve"):
        e = self._veng(eng)
        self.op(eng, lambda: e.tensor_tensor(out=out, in0=in0, in1=in1, op=op), r, w)

    def ts(self, out, in0, s1, op0, s2=None, op1=None, r=(), w=(), eng="dve", accum_out=None):
        e = self._veng(eng)
        kw = {}
        if op1 is not None:
            kw["op1"] = op1
        if accum_out is not None:
            kw["accum_out"] = accum_out
        self.op(eng, lambda: e.tensor_scalar(out=out, in0=in0, scalar1=s1, scalar2=s2, op0=op0, **kw), r, w)

    def stt(self, out, in0, scalar, in1, op0, op1, r=(), w=(), eng="dve"):
        e = self._veng(eng)
        self.op(eng, lambda: e.scalar_tensor_tensor(out=out, in0=in0, scalar=scalar, in1=in1, op0=op0, op1=op1), r, w)

    def copy(self, out, in_, r=(), w=(), eng="dve"):
        if eng == "act":
            self.act(out, in_, AF.Copy, r, w)
        else:
            e = self._veng(eng)
            self.op(eng, lambda: e.tensor_copy(out=out, in_=in_), r, w)

    def recip(self, out, in_, r=(), w=()):
        nc = self.nc
        self.op("dve", lambda: nc.vector.reciprocal(out=out, in_=in_), r, w)

    def rsqrt(self, out, in_, eps, r=(), w=(), scale=1.0):
        self.act(out, in_, AF.Sqrt, r=r, w=w, bias=eps, scale=scale)
        self.recip(out, out, r=w, w=w)

    def memset(self, ap, val, w=(), eng="dve"):
        e = self._veng(eng)
        self.op(eng, lambda: e.memset(ap, val), (), w)

    def begin_phase(self):
        self._outer = self.es
        self.es = ExitStack()
        self.es.__enter__()

    def end_phase(self):
        self.flush()
        self.es.close()
        self.es = self._outer

    def _init_sync(self):
        nc = self.nc
        self.csem = {p: self._outer_ctx(nc.semaphore("cs_" + p)) for p in ("pe", "act", "dve", "pool")}
        self.dsem = {q: [self._outer_ctx(nc.semaphore(f"ds_{q}_{k}")) for k in range(self.n_dsem)]
                     for q in DMA_ENGS}
        self.bsem = self._outer_ctx(nc.semaphore("barrier"))
        self.mc = {p: 0 for p in ("pe", "act", "dve", "pool")}
        self.dcount = {q: 0 for q in DMA_ENGS}
        self.nphase = 0
        self.n_ops = 0

    def _outer_ctx(self, cm):
        return self.root.enter_context(cm)

    def flush(self):
        nc = self.nc
        if not hasattr(self, "csem"):
            self._init_sync()
        ops = self.ops
        self.ops = []
        import os as _os
        if _os.environ.get("KMAXOPS"):
            ops = ops[:int(_os.environ["KMAXOPS"])]
        queue = {"pe": nc.tensor, "act": nc.scalar, "dve": nc.vector, "pool": nc.gpsimd,
                 "sp": nc.sync, "dq_pool": nc.gpsimd, "dq_act": nc.scalar}
        n = len(ops)
        self.n_ops += n
        last_w, readers = {}, {}
        deps = []
        for i, (eng, fn, r, w) in enumerate(ops):
            d = set()
            for k in r:
                if k in last_w:
                    d.add(last_w[k])
            for k in w:
                if k in last_w:
                    d.add(last_w[k])
                d.update(readers.get(k, ()))
            d.discard(i)
            deps.append(d)
            for k in r:
                readers.setdefault(k, []).append(i)
            for k in w:
                last_w[k] = i
                readers[k] = []
        local = [0] * n
        cnt = {}
        last_on = {}
        for i, (eng, fn, r, w) in enumerate(ops):
            if eng not in DMA_ENGS:
                p = PHYS[eng]
                cnt[p] = cnt.get(p, 0) + 1
                local[i] = cnt[p]
                last_on[p] = i
        known_c, known_d = {}, {}
        milestone = [False] * n
        waits = [None] * n
        for i, (eng, fn, r, w) in enumerate(ops):
            E = PHYS[eng]
            i_dma = eng in DMA_ENGS
            need_c = {}
            need_d = []
            for j in sorted(deps[i]):
                jeng = ops[j][0]
                if jeng in DMA_ENGS:
                    s = known_d.setdefault(E, set())
                    if j not in s:
                        s.add(j)
                        need_d.append(j)
                else:
                    Pj = PHYS[jeng]
                    if Pj == E and not i_dma and E == "pe":
                        continue
                    if known_c.get((E, Pj), 0) >= local[j]:
                        continue
                    if need_c.get(Pj, (0, -1))[0] < local[j]:
                        need_c[Pj] = (local[j], j)
            for Pj, (lj, j) in need_c.items():
                known_c[(E, Pj)] = lj
                milestone[j] = True
            waits[i] = (list(need_c.values()), need_d)
        for p, i in last_on.items():
            milestone[i] = True
        msval = [0] * n
        for i, (eng, fn, r, w) in enumerate(ops):
            if eng not in DMA_ENGS and milestone[i]:
                p = PHYS[eng]
                self.mc[p] += 1
                msval[i] = self.mc[p]
        dtoken = {}
        for i, (eng, fn, r, w) in enumerate(ops):
            q = queue[eng]
            need_c, need_d = waits[i]
            for (lj, j) in need_c:
                q.wait_ge(self.csem[PHYS[ops[j][0]]], msval[j])
            for j in need_d:
                s, v = dtoken[j]
                q.wait_ge(s, v)
            if eng in DMA_ENGS:
                k = self.dcount[eng]
                self.dcount[eng] += 1
                s = self.dsem[eng][k % self.n_dsem]
                rnd = k // self.n_dsem
                if rnd > 0:
                    q.wait_ge(s, 16 * rnd)
                ins = fn()
                ins.then_inc(s, 16)
                dtoken[i] = (s, 16 * (rnd + 1))
            else:
                ins = fn()
                if milestone[i]:
                    ins.then_inc(self.csem[PHYS[eng]], 1)
        for p in ("pe", "act", "dve", "pool"):
            if self.mc[p] > 0:
                nc.sync.wait_ge(self.csem[p], self.mc[p])
        for qn in DMA_ENGS:
            k = self.dcount[qn]
            for m in range(min(k, self.n_dsem)):
                final = 16 * ((k - 1 - m) // self.n_dsem + 1)
                nc.sync.wait_ge(self.dsem[qn][m], final)
        self.nphase += 1
        nc.sync.sem_inc(self.bsem, 1)
        for e in (nc.tensor, nc.scalar, nc.vector, nc.gpsimd):
            e.wait_ge(self.bsem, self.nphase)


C_A, C_GT, C_QD, C_KVD, C_KR = 0, 512, 1024, 1408, 1664
C_GQ, C_GK, C_GV, C_OG, C_BETA, C_AR, C_MG = 1696, 2208, 2720, 3232, 3744, 3760, 3776


def load_col(P, dst, src1d, r=(), w=()):
    P.dma(dst, src1d.rearrange("(n p) -> p n", p=128), r, w, slow=True)


class Net:
    def __init__(self, dbg=()):
        self.P = Prog()
        self.dbg = set(dbg)
        self.io = {}

    def scratch(self, name, shape, dt=F32):
        kind = "ExternalOutput" if name in self.dbg else "Internal"
        if name in getattr(self, "dbg_in", ()):
            kind = "ExternalInput"
        t = self.P.dram(name, shape, dt, kind=kind)
        self.io[name] = t
        return t

    def declare_inputs(self):
        P = self.P
        specs = dict(
            x=[NL, D], c=[D], ctx=[NCTX, D], c_ctx=[D], w_mod=[2, D, 6 * D], b_mod=[2, 6 * D],
            w_in=[2, D, P_IN], b_in=[2, P_IN], conv_a_w=[2, 31, 512], conv_a_b=[2, 512],
            ln_a_g=[2, 512], ln_a_b=[2, 512], w_a_out=[2, 512, D], g_q=[2, 384], g_kv=[2, 256],
            w_uq=[2, 384, 512], w_qr=[2, 384, 256], w_uk=[2, 256, 512], w_uv=[2, 256, 512],
            w_b_out=[2, 512, D], conv_c_w=[2, 5, 1536], a_log=[2, 16], dt_bias=[2, 16], g_o=[2, 64],
            w_c_out=[2, 512, D], w_out=[2, D, D], ln1_g=[2, D], ln1_b=[2, D], w_rg=[2, D, 4],
            b_rg=[2, 4], w_re=[2, D, 32], b_re=[2, 32], w1=[2, 32, D, 256], w3=[2, 32, D, 256],
            w2=[2, 32, 256, D], ln2_g=[2, D], ln2_b=[2, D],
            ident=[128, 128], rope_cs=[2, 32, T], perm32=[32, 32], gmask=[4, 64, 64],
        )
        self.inp = {k: P.dram(k, v, F32, kind="ExternalInput") for k, v in specs.items()}

    def phase0(self, l):
        P, I = self.P, self.inp
        L = f"p0_{l}_"
        P.begin_phase()
        scin = P.sb(L + "scin", [128, 8, 2])
        sc = P.sb(L + "sc", [128, 8, 2])
        bm = P.sb(L + "bm", [2, 6 * D])
        mr = P.sb(L + "mr", [2, 6 * D])
        wm = [P.sb(L + f"wm{i}", [128, 8, 512]) for i in range(2)]
        ps = P.ps(L + "ps", [128, 512])
        load_col(P, scin[:, :, 0], I["c"], w=[L + "scin"])
        load_col(P, scin[:, :, 1], I["c_ctx"], w=[L + "scin"])
        P.dma(bm[0:1, :], I["b_mod"][l:l + 1, :], w=[L + "bm"])
        P.dma(bm[1:2, :], I["b_mod"][l:l + 1, :], w=[L + "bm"])
        P.act(sc[:], scin[:], AF.Silu, r=[L + "scin"], w=[L + "sc"])
        for nb in range(12):
            b = wm[nb % 2]
            key = L + f"wm{nb % 2}"
            P.dma(b[:], I["w_mod"][l, :, nb * 512:(nb + 1) * 512].rearrange("(k p) n -> p k n", p=128), w=[key])
            for k in range(8):
                P.mm(ps[0:2, :], sc[:, k, :], b[:, k, :], k == 0, k == 7, r=[key, L + "sc"], w=[L + "ps"])
            P.tt(mr[0:2, nb * 512:(nb + 1) * 512], ps[0:2, :], bm[0:2, nb * 512:(nb + 1) * 512], ALU.add,
                 r=[L + "ps", L + "bm"], w=[L + "mr"])
        P.dma(self.modrow[l], mr[:], r=[L + "mr"], w=[f"modrow{l}"], q="dq_pool")
        P.end_phase()

    def phase1(self, l, xsrc_l, xsrc_c, xkey):
        P, I = self.P, self.inp
        L = f"p1_{l}_"
        P.begin_phase()
        W = P.sb(L + "W", [128, 8, P_IN], BF16)
        for k in range(8):
            for c in range(4):
                c0, c1 = c * 1712, (c + 1) * 1712
                P.dma(W[:, k, c0:c1], I["w_in"][l, k * 128:(k + 1) * 128, c0:c1], w=[L + "W"], q="dq_pool")
        ident = P.sb(L + "ident", [128, 128])
        P.dma(ident[:], I["ident"], w=[L + "ident"])
        tiles = []
        for ct in range(4):
            tiles.append(("gt", None, ct, C_GT + ct * 128, 128))
            tiles.append(("a", self.U, ct, C_A + ct * 128, 128))
        for i in range(3):
            tiles.append(("id", self.QD, i, C_QD + i * 128, 128))
        for i in range(2):
            tiles.append(("id", self.KVD, i, C_KVD + i * 128, 128))
        tiles.append(("id", self.KR, None, C_KR, 32))
        for i in range(12):
            tiles.append(("id", self.GQKV, i, C_GQ + i * 128, 128))
        for i in range(4):
            tiles.append(("silu", self.OG, i, C_OG + i * 128, 128))
        tiles.append(("sig", self.BETA, None, C_BETA, 16))
        tiles.append(("sp", self.GL, None, C_AR, 16))
        for i in range(24):
            tiles.append(("sig", self.MG, i, C_MG + i * 128, 128))
        nt_ = len(tiles)
        bcol = P.sb(L + "bcol", [128, nt_])
        for n_, (kind, dst, ti, c0, m) in enumerate(tiles):
            P.dma(bcol[0:m, n_:n_ + 1], I["b_in"][l, c0:c0 + m].rearrange("(p o) -> p o", o=1),
                  w=[L + "bcol"], slow=True)
        mcol = P.sb(L + "mcol", [128, 2, 48])
        for rr in range(2):
            load_col(P, mcol[:, rr, :], self.modrow[l][rr, :], w=[L + "mcol"])
        sc1p = P.sb(L + "sc1p", [128, 2, 8])
        P.ts(sc1p[:], mcol[:, :, 8:16], 1.0, ALU.add, r=[L + "mcol"], w=[L + "sc1p"])
        dtb = P.sb(L + "dtb", [16, 1])
        nega = P.sb(L + "nega", [16, 1])
        P.dma(dtb[:], I["dt_bias"][l, :].rearrange("(p o) -> p o", o=1), w=[L + "dtb"], slow=True)
        P.dma(nega[:], I["a_log"][l, :].rearrange("(p o) -> p o", o=1), w=[L + "nega"], slow=True)
        P.act(nega[:], nega[:], AF.Exp, r=[L + "nega"], w=[L + "nega"])
        P.ts(nega[:], nega[:], -1.0, ALU.mult, r=[L + "nega"], w=[L + "nega"])

        xt = [P.sb(L + f"xt{i}", [128, 4, D]) for i in range(2)]
        hT = [P.sb(L + f"hT{i}", [128, 8, 512], BF16) for i in range(2)]
        pT = [P.ps(L + f"pT{i}", [128, 512]) for i in range(2)]
        pp = [P.ps(L + f"pp{i}", [128, 512]) for i in range(4)]
        NS = 4
        stf = [P.sb(L + f"stf{i}", [128, 512]) for i in range(NS)]
        stb = [P.sb(L + f"stb{i}", [128, 512], BF16) for i in range(NS)]
        tmp = [P.sb(L + f"tmp{i}", [128, 512]) for i in range(2)]
        spz = P.sb(L + "spz", [16, 512])
        spa = P.sb(L + "spa", [16, 512])

        def load_x(bi):
            s, sz = BLOCKS[bi]
            nt = sz // 128
            src = xsrc_c if bi == 0 else xsrc_l[s - NCTX:s - NCTX + sz, :]
            P.dma(xt[bi % 2][:, 0:nt, :], src.rearrange("(t p) d -> p t d", p=128),
                  r=[xkey], w=[L + f"xt{bi % 2}"])

        load_x(0)
        cf = cb = cp = 0
        for bi, (s, sz) in enumerate(BLOCKS):
            if bi + 1 < len(BLOCKS):
                load_x(bi + 1)
            nt = sz // 128
            rr = 1 if bi == 0 else 0
            xb, hb = xt[bi % 2], hT[bi % 2]
            xk, hk = L + f"xt{bi % 2}", L + f"hT{bi % 2}"
            for j in range(8):
                pt = pT[j % 2]
                pk = L + f"pT{j % 2}"
                for t in range(nt):
                    P.tr(pt[:, t * 128:(t + 1) * 128], xb[:, t, j * 128:(j + 1) * 128], ident[:],
                         r=[xk, L + "ident"], w=[pk])
                P.ts(hb[:, j, 0:sz], pt[:, 0:sz], sc1p[:, rr, j:j + 1], ALU.mult,
                     mcol[:, rr, j:j + 1], ALU.add, r=[pk, L + "sc1p", L + "mcol"], w=[hk])
            for n_, (kind, dst, ti, c0, m) in enumerate(tiles):
                acc = pp[cp % 4]
                ak = L + f"pp{cp % 4}"
                cp += 1
                for k in range(8):
                    P.mm(acc[0:m, 0:sz], W[:, k, c0:c0 + m], hb[:, k, 0:sz], k == 0, k == 7,
                         r=[hk, L + "W"], w=[ak])
                bc = bcol[0:m, n_:n_ + 1]
                if kind == "gt":
                    tb = tmp[ti % 2]
                    P.act(tb[:, 0:sz], acc[:, 0:sz], AF.Sigmoid, bias=bc, r=[ak, L + "bcol"], w=[L + f"tmp{ti % 2}"])
                    continue
                if kind in ("silu", "sig") and m == 128:
                    st, sk = stb[cb % NS], L + f"stb{cb % NS}"
                    cb += 1
                else:
                    st, sk = stf[cf % NS], L + f"stf{cf % NS}"
                    cf += 1
                if kind == "a":
                    P.stt(st[:, 0:sz], acc[:, 0:sz], bc, tmp[ti % 2][:, 0:sz], ALU.add, ALU.mult,
                          r=[ak, L + "bcol", L + f"tmp{ti % 2}"], w=[sk])
                elif kind == "id":
                    if n_ % 2 == 0:
                        P.ts(st[0:m, 0:sz], acc[0:m, 0:sz], bc, ALU.add, r=[ak, L + "bcol"], w=[sk])
                    else:
                        P.act(st[0:m, 0:sz], acc[0:m, 0:sz], AF.Identity, bias=bc, r=[ak, L + "bcol"], w=[sk])
                elif kind == "silu":
                    P.act(st[0:m, 0:sz], acc[0:m, 0:sz], AF.Silu, bias=bc, r=[ak, L + "bcol"], w=[sk])
                elif kind == "sig":
                    P.act(st[0:m, 0:sz], acc[0:m, 0:sz], AF.Sigmoid, bias=bc, r=[ak, L + "bcol"], w=[sk])
                elif kind == "sp":
                    P.ts(spz[:, 0:sz], acc[0:16, 0:sz], bc, ALU.add, dtb[:, 0:1], ALU.add,
                         r=[ak, L + "bcol", L + "dtb"], w=[L + "spz"])
                    P.act(spa[:, 0:sz], spz[:, 0:sz], AF.Abs, r=[L + "spz"], w=[L + "spa"])
                    P.act(spa[:, 0:sz], spa[:, 0:sz], AF.Exp, scale=-1.0, r=[L + "spa"], w=[L + "spa"])
                    P.act(spa[:, 0:sz], spa[:, 0:sz], AF.Ln, bias=1.0, r=[L + "spa"], w=[L + "spa"])
                    P.stt(spz[:, 0:sz], spz[:, 0:sz], 0.0, spa[:, 0:sz], ALU.max, ALU.add,
                          r=[L + "spz", L + "spa"], w=[L + "spz"])
                    P.ts(st[0:16, 0:sz], spz[:, 0:sz], nega[:, 0:1], ALU.mult, r=[L + "spz", L + "nega"], w=[sk])
                d_ap = dst[ti, :, s:s + sz] if ti is not None else dst[:, s:s + sz]
                P.dma(d_ap, st[0:m, 0:sz], r=[sk], w=[("scr", id(dst))], q="dq_pool")
        P.end_phase()

    def phase2a(self, l):
        P, I = self.P, self.inp
        L = f"p2a_{l}_"
        P.begin_phase()
        cw = P.sb(L + "cw", [128, 4, 31])
        for ct in range(4):
            P.dma(cw[:, ct, :], I["conv_a_w"][l][:, ct * 128:(ct + 1) * 128].rearrange("k p -> p k"),
                  w=[L + "cw"], slow=True)
        cb = P.sb(L + "cb", [128, 4])
        lg = P.sb(L + "lg", [128, 4])
        lb = P.sb(L + "lb", [128, 4])
        load_col(P, cb[:], I["conv_a_b"][l], w=[L + "cb"])
        load_col(P, lg[:], I["ln_a_g"][l], w=[L + "lg"])
        load_col(P, lb[:], I["ln_a_b"][l], w=[L + "lb"])
        ones = P.sb(L + "ones", [128, 128])
        P.memset(ones[:], 1.0, w=[L + "ones"])
        v = P.sb(L + "v", [128, 4, T])
        UB = T + 60
        ub = [P.sb(L + f"ub{i}", [128, UB]) for i in range(2)]
        for i in range(2):
            P.memset(ub[i][:, 0:15], 0.0, w=[L + f"ub{i}"])
            P.memset(ub[i][:, 271:301], 0.0, w=[L + f"ub{i}"])
            P.memset(ub[i][:, UB - 15:UB], 0.0, w=[L + f"ub{i}"])
        for ct in range(4):
            u, uk = ub[ct % 2], L + f"ub{ct % 2}"
            P.dma(u[:, 15:271], self.U[ct, :, 0:NCTX], w=[uk])
            P.dma(u[:, 301:301 + NL], self.U[ct, :, NCTX:T], w=[uk])
            vk = L + f"v{ct}"
            for (o0, n_, base) in ((0, NCTX, 0), (NCTX, NL, 286)):
                vo = v[:, ct, o0:o0 + n_]
                P.ts(vo, u[:, base:base + n_], cw[:, ct, 0:1], ALU.mult, cb[:, ct:ct + 1], ALU.add,
                     r=[uk, L + "cw", L + "cb"], w=[vk])
                for k in range(1, 31):
                    P.stt(vo, u[:, base + k:base + k + n_], cw[:, ct, k:k + 1], vo, ALU.mult, ALU.add,
                          r=[uk, L + "cw"], w=[vk])
        ps_s = [P.ps(L + f"ps_s{i}", [128, 512]) for i in range(2)]
        ps_q = [P.ps(L + f"ps_q{i}", [128, 512]) for i in range(2)]
        sq = [P.sb(L + f"sq{i}", [128, 4, 512]) for i in range(2)]
        mean = [P.sb(L + f"mean{i}", [128, 512]) for i in range(2)]
        rstd = [P.sb(L + f"rstd{i}", [128, 512]) for i in range(2)]
        xn = [P.sb(L + f"xn{i}", [128, 512]) for i in range(3)]
        so = [P.sb(L + f"so{i}", [128, 512], BF16) for i in range(3)]
        vkeys = [L + f"v{ct}" for ct in range(4)]
        cx = 0
        for bi, (s, sz) in enumerate(BLOCKS):
            i2 = bi % 2
            for ct in range(4):
                P.act(sq[i2][:, ct, 0:sz], v[:, ct, s:s + sz], AF.Square, r=[vkeys[ct]], w=[L + f"sq{i2}"])
            for ct in range(4):
                P.mm(ps_s[i2][:, 0:sz], ones[:], v[:, ct, s:s + sz], ct == 0, ct == 3,
                     r=[vkeys[ct], L + "ones"], w=[L + f"ps_s{i2}"])
            for ct in range(4):
                P.mm(ps_q[i2][:, 0:sz], ones[:], sq[i2][:, ct, 0:sz], ct == 0, ct == 3,
                     r=[L + f"sq{i2}", L + "ones"], w=[L + f"ps_q{i2}"])
            m_, r_ = mean[i2], rstd[i2]
            mk, rk = L + f"mean{i2}", L + f"rstd{i2}"
            P.ts(m_[:, 0:sz], ps_s[i2][:, 0:sz], 1.0 / 512, ALU.mult, r=[L + f"ps_s{i2}"], w=[mk])
            P.tt(r_[:, 0:sz], m_[:, 0:sz], m_[:, 0:sz], ALU.mult, r=[mk], w=[rk])
            P.stt(r_[:, 0:sz], ps_q[i2][:, 0:sz], 1.0 / 512, r_[:, 0:sz], ALU.mult, ALU.subtract,
                  r=[L + f"ps_q{i2}", rk], w=[rk])
            P.rsqrt(r_[:, 0:sz], r_[:, 0:sz], EPS, r=[rk], w=[rk])
            for ct in range(4):
                x_, xk = xn[cx % 3], L + f"xn{cx % 3}"
                o_, ok = so[cx % 3], L + f"so{cx % 3}"
                cx += 1
                P.tt(x_[:, 0:sz], v[:, ct, s:s + sz], m_[:, 0:sz], ALU.subtract, r=[vkeys[ct], mk], w=[xk])
                P.tt(x_[:, 0:sz], x_[:, 0:sz], r_[:, 0:sz], ALU.mult, r=[xk, rk], w=[xk])
                P.act(o_[:, 0:sz], x_[:, 0:sz], AF.Silu, scale=lg[:, ct:ct + 1], bias=lb[:, ct:ct + 1],
                      r=[xk, L + "lg", L + "lb"], w=[ok])
                P.dma(self.SA[ct, :, s:s + sz], o_[:, 0:sz], r=[ok], w=["SA"], q="dq_pool")
        P.end_phase()

    def phase2b1(self, l):
        P, I = self.P, self.inp
        L = f"p2b1_{l}_"
        P.begin_phase()
        ident = P.sb(L + "ident", [128, 128])
        P.dma(ident[:], I["ident"], w=[L + "ident"])
        perm = P.sb(L + "perm", [32, 32])
        P.dma(perm[:], I["perm32"], w=[L + "perm"])
        ones = P.sb(L + "ones", [128, 128])
        P.memset(ones[:], 1.0, w=[L + "ones"])
        SH = P.sb(L + "SH", [32, 96], BF16)
        P.memset(SH[:], 0.0, w=[L + "SH"])
        P.copy(SH[:, 64:96], ident[0:32, 0:32], r=[L + "ident", L + "SH"], w=[L + "SH"])
        gq = P.sb(L + "gq", [128, 3])
        gkv = P.sb(L + "gkv", [128, 2])
        load_col(P, gq[:], I["g_q"][l], w=[L + "gq"])
        load_col(P, gkv[:], I["g_kv"][l], w=[L + "gkv"])
        WK = P.sb(L + "WK", [128, 2, 8, 96], BF16)
        WQ = P.sb(L + "WQ", [128, 3, 8, 96], BF16)
        WQS = P.sb(L + "WQS", [128, 3, 8, 96], BF16)
        WV = P.sb(L + "WV", [128, 2, 512], BF16)
        P.memset(WK[:], 0.0, w=[L + "WK"])
        P.memset(WQS[:], 0.0, w=[L + "WQS"])
        for c in range(2):
            P.dma(WK[:, c, :, 0:64], I["w_uk"][l, c * 128:(c + 1) * 128, :].rearrange("p (h d) -> p h d", d=64),
                  r=[L + "WK"], w=[L + "WK"], q="dq_pool")
            P.dma(WV[:, c, :], I["w_uv"][l, c * 128:(c + 1) * 128, :], w=[L + "WV"], q="dq_pool")
        for c in range(3):
            P.dma(WQ[:, c, :, 0:64], I["w_uq"][l, c * 128:(c + 1) * 128, :].rearrange("p (h d) -> p h d", d=64),
                  w=[L + "WQ"], q="dq_pool")
            P.dma(WQ[:, c, :, 64:96], I["w_qr"][l, c * 128:(c + 1) * 128, :].rearrange("p (h d) -> p h d", d=32),
                  w=[L + "WQ"], q="dq_pool")
            src = I["w_qr"][l, c * 128:(c + 1) * 128, :].rearrange("p (h a f) -> p h a f", a=4, f=8)
            for a in range(4):
                P.dma(WQS[:, c, :, 64 + a * 8:64 + a * 8 + 8], src[:, :, a ^ 1, :],
                      r=[L + "WQS"], w=[L + "WQS"], q="dq_pool")
        vx = [P.sb(L + f"vx{i}", [128, 8, 128], BF16) for i in range(2)]
        for i in range(2):
            P.memset(vx[i][:], 1.0, w=[L + f"vx{i}"])
        xin = [P.sb(L + f"xin{i}", [128, 5, 512]) for i in range(2)]
        krin = [P.sb(L + f"krin{i}", [32, 512]) for i in range(2)]
        rope = [P.sb(L + f"rope{i}", [96, 2, 512]) for i in range(2)]
        sq = P.sb(L + "sq", [128, 5, 512])
        rs = P.sb(L + "rs", [128, 2, 512])
        cx = P.sb(L + "cx", [128, 5, 512], BF16)
        krr = P.sb(L + "krr", [32, 512], BF16)
        krt = P.sb(L + "krt", [32, 2, 512])
        ps_n = [P.ps(L + f"ps_n{i}", [128, 512]) for i in range(2)]
        ps_a = [P.ps(L + f"ps_a{i}", [128, 512]) for i in range(2)]
        ps_b = [P.ps(L + f"ps_b{i}", [128, 512]) for i in range(2)]
        ps_v = [P.ps(L + f"ps_v{i}", [128, 512]) for i in range(2)]
        ko = [P.sb(L + f"ko{i}", [96, 512], BF16) for i in range(3)]
        qo = [P.sb(L + f"qo{i}", [96, 512], BF16) for i in range(3)]
        qt = [P.sb(L + f"qt{i}", [96, 2, 512]) for i in range(2)]

        def load_blk(bi):
            s, sz = BLOCKS[bi]
            b = bi % 2
            P.dma(xin[b][:, 0:2, 0:sz], self.KVD[:, :, s:s + sz].rearrange("c p t -> p c t"), w=[L + f"xin{b}"])
            P.dma(xin[b][:, 2:5, 0:sz], self.QD[:, :, s:s + sz].rearrange("c p t -> p c t"), w=[L + f"xin{b}"])
            P.dma(krin[b][:, 0:sz], self.KR[:, s:s + sz], w=[L + f"krin{b}"])
            for ci in range(2):
                P.dma(rope[b][64:96, ci, 0:sz], I["rope_cs"][ci, :, s:s + sz], w=[L + f"rope{b}"])
                P.dma(rope[b][0:32, ci, 0:sz], I["rope_cs"][ci, :, s:s + sz], w=[L + f"rope{b}"])

        load_blk(0)
        ck = cq_ = cv = 0
        for bi, (s, sz) in enumerate(BLOCKS):
            if bi + 1 < len(BLOCKS):
                load_blk(bi + 1)
            b = bi % 2
            xb, xk = xin[b], L + f"xin{b}"
            rb, rk = rope[b], L + f"rope{b}"
            for c in range(5):
                P.act(sq[:, c, 0:sz], xb[:, c, 0:sz], AF.Square, r=[xk], w=[L + "sq"])
            for gi, (c0, c1, nf) in enumerate(((0, 2, 256.0), (2, 5, 384.0))):
                pn = ps_n[gi]
                for c in range(c0, c1):
                    P.mm(pn[:, 0:sz], ones[:], sq[:, c, 0:sz], c == c0, c == c1 - 1,
                         r=[L + "sq", L + "ones"], w=[L + f"ps_n{gi}"])
                P.rsqrt(rs[:, gi, 0:sz], pn[:, 0:sz], EPS, r=[L + f"ps_n{gi}"], w=[L + f"rs{gi}"], scale=1.0 / nf)
            for c in range(5):
                gi = 0 if c < 2 else 1
                gcol = gkv[:, c:c + 1] if c < 2 else gq[:, c - 2:c - 1]
                P.stt(cx[:, c, 0:sz], xb[:, c, 0:sz], gcol, rs[:, gi, 0:sz], ALU.mult, ALU.mult,
                      r=[xk, L + "gq", L + "gkv", L + f"rs{gi}"], w=[L + f"cx{c}"])
            ckeys_kv = [L + "cx0", L + "cx1"]
            ckeys_q = [L + "cx2", L + "cx3", L + "cx4"]
            kb, kk = krin[b], L + f"krin{b}"
            P.mm(ps_n[0][0:32, 0:sz], perm[:], kb[:, 0:sz], True, True, r=[kk, L + "perm", L + "rs0"], w=[L + "ps_n0"])
            P.tt(krt[:, 0, 0:sz], kb[:, 0:sz], rb[0:32, 0, 0:sz], ALU.mult, r=[kk, rk], w=[L + "krt0"])
            P.tt(krt[:, 1, 0:sz], ps_n[0][0:32, 0:sz], rb[0:32, 1, 0:sz], ALU.mult, r=[L + "ps_n0", rk], w=[L + "krt1"])
            P.tt(krr[:, 0:sz], krt[:, 0, 0:sz], krt[:, 1, 0:sz], ALU.add, r=[L + "krt0", L + "krt1"], w=[L + "krr"])
            for h in range(8):
                pa, pk = ps_a[ck % 2], L + f"ps_a{ck % 2}"
                o_, ok = ko[ck % 3], L + f"ko{ck % 3}"
                ck += 1
                for c in range(2):
                    P.mm(pa[0:96, 0:sz], WK[:, c, h, :], cx[:, c, 0:sz], c == 0, False,
                         r=[L + "WK", ckeys_kv[c]], w=[pk])
                P.mm(pa[0:96, 0:sz], SH[:], krr[:, 0:sz], False, True, r=[L + "SH", L + "krr"], w=[pk])
                if h % 2 == 0:
                    P.copy(o_[:, 0:sz], pa[0:96, 0:sz], r=[pk], w=[ok])
                else:
                    P.copy(o_[:, 0:sz], pa[0:96, 0:sz], r=[pk], w=[ok], eng="act")
                P.dma(self.KT[h, :, s:s + sz], o_[:, 0:sz], r=[ok], w=["KT"], q="dq_pool")
            for t in range(sz // 128):
                pv, pvk = ps_v[cv % 2], L + f"ps_v{cv % 2}"
                v_, vk = vx[cv % 2], L + f"vx{cv % 2}"
                cv += 1
                for c in range(2):
                    P.mm(pv[:, :], cx[:, c, t * 128:(t + 1) * 128], WV[:, c, :], c == 0, c == 1,
                         r=[L + "WV", ckeys_kv[c]], w=[pvk])
                pv4 = pv[:, :].rearrange("p (hp two d) -> p hp two d", two=2, d=64)
                vx4 = v_[:].rearrange("p (hp two) c -> p hp two c", two=2)
                P.copy(vx4[:, :, 0, 0:64], pv4[:, :, 0, :], r=[pvk], w=[vk])
                P.copy(vx4[:, :, 1, 64:128], pv4[:, :, 1, :], r=[pvk], w=[vk], eng="act")
                kt = (s + t * 128) // 128
                P.dma(self.VX[:, :, kt, :].rearrange("h p c -> p h c"), v_[:], r=[vk], w=["VX"], q="dq_pool")
            for h in range(8):
                pa, pk = ps_a[ck % 2], L + f"ps_a{ck % 2}"
                ck += 1
                pb, pbk = ps_b[cq_ % 2], L + f"ps_b{cq_ % 2}"
                o_, ok = qo[cq_ % 3], L + f"qo{cq_ % 3}"
                q_, qk = qt[cq_ % 2], L + f"qt{cq_ % 2}"
                cq_ += 1
                for c in range(3):
                    P.mm(pa[0:96, 0:sz], WQ[:, c, h, :], cx[:, 2 + c, 0:sz], c == 0, c == 2,
                         r=[L + "WQ", ckeys_q[c]], w=[pk])
                for c in range(3):
                    P.mm(pb[0:96, 0:sz], WQS[:, c, h, :], cx[:, 2 + c, 0:sz], c == 0, c == 2,
                         r=[L + "WQS", ckeys_q[c]], w=[pbk])
                P.act(o_[0:64, 0:sz], pa[0:64, 0:sz], AF.Copy, scale=MLA_SCALE, r=[pk], w=[ok])
                P.tt(q_[64:96, 0, 0:sz], pa[64:96, 0:sz], rb[64:96, 0, 0:sz], ALU.mult, r=[pk, rk], w=[qk])
                P.stt(q_[64:96, 1, 0:sz], pb[64:96, 0:sz], MLA_SCALE, rb[64:96, 1, 0:sz], ALU.mult, ALU.mult,
                      r=[pbk, rk], w=[qk])
                P.stt(o_[64:96, 0:sz], q_[64:96, 0, 0:sz], MLA_SCALE, q_[64:96, 1, 0:sz], ALU.mult, ALU.add,
                      r=[qk], w=[ok])
                P.dma(self.QT[h, :, s:s + sz], o_[:, 0:sz], r=[ok], w=["QT"], q="dq_pool")
        P.end_phase()

    def phase2b2(self, l):
        P, I = self.P, self.inp
        L = f"p2b2_{l}_"
        P.begin_phase()
        kT = [P.sb(L + f"kT{i}", [96, T], BF16) for i in range(2)]
        qT = [P.sb(L + f"qT{i}", [96, T], BF16) for i in range(2)]
        vx = [P.sb(L + f"vx{i}", [128, 34, 128], BF16) for i in range(2)]
        NP_ = 4
        pT = [P.sb(L + f"pT{i}", [128, 512], BF16) for i in range(NP_)]
        ps_s = [P.ps(L + f"ps_s{i}", [128, 512]) for i in range(4)]
        ps_o = [P.ps(L + f"ps_o{i}", [128, 512]) for i in range(2)]
        rec = [P.sb(L + f"rec{i}", [128, 512]) for i in range(2)]
        oo = [P.sb(L + f"oo{i}", [128, 512], BF16) for i in range(2)]

        def load_head(h):
            b = h % 2
            P.dma(kT[b][:], self.KT[h], w=[L + f"kT{b}"])
            P.dma(qT[b][:], self.QT[h], w=[L + f"qT{b}"])
            P.dma(vx[b][:], self.VX[h], w=[L + f"vx{b}"])

        load_head(0)
        cs_ = co = 0
        for h in range(8):
            if h + 1 < 8:
                load_head(h + 1)
            b = h % 2
            kb, qb, vb = kT[b], qT[b], vx[b]
            kk, qk, vk = L + f"kT{b}", L + f"qT{b}", L + f"vx{b}"
            for bi, (s, sz) in enumerate(BLOCKS):
                nkt = 2 if bi == 0 else 34
                po, pok = ps_o[co % 2], L + f"ps_o{co % 2}"
                r_, rk = rec[co % 2], L + f"rec{co % 2}"
                o_, ok = oo[co % 2], L + f"oo{co % 2}"
                co += 1
                for kt in range(nkt):
                    ps, psk = ps_s[cs_ % 4], L + f"ps_s{cs_ % 4}"
                    p_, pk = pT[cs_ % NP_], L + f"pT{cs_ % NP_}"
                    cs_ += 1
                    P.mm(ps[:, 0:sz], kb[:, kt * 128:(kt + 1) * 128], qb[:, s:s + sz], True, True,
                         r=[kk, qk], w=[psk])
                    P.act(p_[:, 0:sz], ps[:, 0:sz], AF.Exp, r=[psk], w=[pk])
                    P.mm(po[:, 0:sz], vb[:, kt, :], p_[:, 0:sz], kt == 0, kt == nkt - 1, r=[vk, pk], w=[pok])
                if h % 2 == 0:
                    num, den, dst = po[0:64, 0:sz], po[64:128, 0:sz], slice(0, 64)
                else:
                    num, den, dst = po[64:128, 0:sz], po[0:64, 0:sz], slice(64, 128)
                P.recip(r_[dst, 0:sz], den, r=[pok], w=[rk])
                P.tt(o_[dst, 0:sz], num, r_[dst, 0:sz], ALU.mult, r=[pok, rk], w=[ok])
                P.dma(self.OB[h // 2, dst, s:s + sz], o_[dst, 0:sz], r=[ok], w=["OB"], q="dq_pool")
        P.end_phase()

    def phase2c1(self, l):
        P, I = self.P, self.inp
        L = f"p2c1_{l}_"
        P.begin_phase()
        ident = P.sb(L + "ident", [128, 128])
        P.dma(ident[:], I["ident"], w=[L + "ident"])
        bones = P.sb(L + "bones", [128, 128])
        P.memset(bones[:], 0.0, w=[L + "bones"])
        P.memset(bones[0:64, 0:64], 1.0, w=[L + "bones"])
        P.memset(bones[64:128, 64:128], 1.0, w=[L + "bones"])
        cw = P.sb(L + "cw", [128, 12, 5])
        for i in range(12):
            P.dma(cw[:, i, :], I["conv_c_w"][l][:, i * 128:(i + 1) * 128].rearrange("k p -> p k"),
                  w=[L + "cw"], slow=True)
        UB = T + 8
        ub = [P.sb(L + f"ub{i}", [128, UB]) for i in range(2)]
        for i in range(2):
            P.memset(ub[i][:, 0:2], 0.0, w=[L + f"ub{i}"])
            P.memset(ub[i][:, 258:262], 0.0, w=[L + f"ub{i}"])
            P.memset(ub[i][:, UB - 2:UB], 0.0, w=[L + f"ub{i}"])
        xc = [P.sb(L + f"xc{i}", [128, T]) for i in range(2)]
        sq = [P.sb(L + f"sq{i}", [128, 512]) for i in range(2)]
        rn = [P.sb(L + f"rn{i}", [128, 512]) for i in range(2)]
        xo = [P.sb(L + f"xo{i}", [128, 512]) for i in range(3)]
        tk = [P.sb(L + f"tk{i}", [128, 4, 128]) for i in range(2)]
        ps_n = [P.ps(L + f"ps_n{i}", [128, 512]) for i in range(2)]
        ps_t = [P.ps(L + f"ps_t{i}", [128, 512]) for i in range(2)]
        cn = ct_ = cx_ = 0
        for i in range(12):
            u, uk = ub[i % 2], L + f"ub{i % 2}"
            x_, xk = xc[i % 2], L + f"xc{i % 2}"
            P.dma(u[:, 2:258], self.GQKV[i, :, 0:NCTX], w=[uk])
            P.dma(u[:, 262:262 + NL], self.GQKV[i, :, NCTX:T], w=[uk])
            for (o0, n_, base) in ((0, NCTX, 0), (NCTX, NL, 260)):
                vo = x_[:, o0:o0 + n_]
                P.ts(vo, u[:, base:base + n_], cw[:, i, 0:1], ALU.mult, r=[uk, L + "cw"], w=[xk])
                for k in range(1, 5):
                    P.stt(vo, u[:, base + k:base + k + n_], cw[:, i, k:k + 1], vo, ALU.mult, ALU.add,
                          r=[uk, L + "cw"], w=[xk])
            P.act(x_[:], x_[:], AF.Silu, r=[xk], w=[xk])
            for bi, (s, sz) in enumerate(BLOCKS):
                if i < 8:
                    j = cn % 2
                    cn += 1
                    o_, ok = xo[cx_ % 3], L + f"xo{cx_ % 3}"
                    cx_ += 1
                    P.act(sq[j][:, 0:sz], x_[:, s:s + sz], AF.Square, r=[xk], w=[L + f"sq{j}"])
                    P.mm(ps_n[j][:, 0:sz], bones[:], sq[j][:, 0:sz], True, True,
                         r=[L + "bones", L + f"sq{j}"], w=[L + f"ps_n{j}"])
                    P.rsqrt(rn[j][:, 0:sz], ps_n[j][:, 0:sz], EPS, r=[L + f"ps_n{j}"], w=[L + f"rn{j}"])
                    if i < 4:
                        P.stt(o_[:, 0:sz], x_[:, s:s + sz], 0.125, rn[j][:, 0:sz], ALU.mult, ALU.mult,
                              r=[xk, L + f"rn{j}"], w=[ok])
                    else:
                        P.tt(o_[:, 0:sz], x_[:, s:s + sz], rn[j][:, 0:sz], ALU.mult, r=[xk, L + f"rn{j}"], w=[ok])
                    P.dma(self.QKN[i, :, s:s + sz], o_[:, 0:sz], r=[ok], w=["QKN"], q="dq_pool")
                    src, sk = o_, ok
                    soff = 0
                else:
                    src, sk = x_, xk
                    soff = s
                if i >= 4:
                    j = ct_ % 2
                    ct_ += 1
                    nt = sz // 128
                    for t in range(nt):
                        P.tr(ps_t[j][:, t * 128:(t + 1) * 128], src[:, soff + t * 128:soff + (t + 1) * 128], ident[:],
                             r=[sk, L + "ident"], w=[L + f"ps_t{j}"])
                    P.copy(tk[j][:, 0:nt, :], ps_t[j][:, 0:sz].rearrange("p (t f) -> p t f", f=128),
                           r=[L + f"ps_t{j}"], w=[L + f"tk{j}"], eng="act")
                    kv = 0 if i < 8 else 1
                    f0 = ((i - 4) % 4) * 128
                    P.dma(self.KVtok[s:s + sz, kv, f0:f0 + 128].rearrange("(t p) f -> p t f", p=128),
                          tk[j][:, 0:nt, :], r=[L + f"tk{j}"], w=["KVtok"], q="dq_pool")
        bg = [P.sb(L + f"bg{i}", [16, 2, 512]) for i in range(2)]
        bo = [P.sb(L + f"bo{i}", [128, 4, 32]) for i in range(2)]
        for bi, (s, sz) in enumerate(BLOCKS):
            j = bi % 2
            nt = sz // 128
            P.dma(bg[j][:, 0, 0:sz], self.BETA[:, s:s + sz], w=[L + f"bg{j}"])
            P.dma(bg[j][:, 1, 0:sz], self.GL[:, s:s + sz], w=[L + f"bg{j}"])
            for t in range(nt):
                for z in range(2):
                    P.tr(ps_t[j][:, t * 32 + z * 16:t * 32 + z * 16 + 16], bg[j][:, z, t * 128:(t + 1) * 128],
                         ident[0:16, 0:16], r=[L + f"bg{j}", L + "ident"], w=[L + f"ps_t{j}"])
            P.copy(bo[j][:, 0:nt, :], ps_t[j][:, 0:nt * 32].rearrange("p (t f) -> p t f", f=32),
                   r=[L + f"ps_t{j}"], w=[L + f"bo{j}"])
            P.dma(self.BGtok[s:s + sz, :].rearrange("(t p) f -> p t f", p=128), bo[j][:, 0:nt, :],
                  r=[L + f"bo{j}"], w=["BGtok"], q="dq_pool")
        P.end_phase()

    @staticmethod
    def gdn_chunk(n, d):
        return n if d == 0 else (3 - n if n < 4 else 71 - n)

    def phase2c2(self, l, nsteps=68):
        P, I = self.P, self.inp
        L = f"p2c2_{l}_"
        P.begin_phase()
        ident = P.sb(L + "ident", [128, 128])
        P.dma(ident[:], I["ident"], w=[L + "ident"])
        msk = P.sb(L + "msk", [64, 4, 64])
        P.dma(msk[:], I["gmask"].rearrange("m p f -> p m f"), w=[L + "msk"])
        ones = P.sb(L + "ones", [64, 64])
        P.memset(ones[:], 1.0, w=[L + "ones"])
        CK = [L + "ident", L + "msk", L + "ones"]
        TRI = (2, 0)
        STRICT = (1, 3)
        INCLT = (2, 0)

        def bc_u(ap2):
            return ap2.unsqueeze(2).broadcast_to([ap2.shape[0], 8, 64])

        def bc_m(mi):
            return msk[:, mi, :].unsqueeze(1).broadcast_to([64, 8, 64])

        def v3(ap, np_=64):
            return ap.rearrange("p (u f) -> p u f", f=64)

        D_ = {}
        for d in range(2):
            X = {}
            for nm, shp, dt in (("bgt", [64, 32], F32), ("ktok", [64, 512], F32), ("vtok", [64, 512], F32),
                                ("kT", [64, 8, 64], F32), ("qT", [64, 8, 64], F32)):
                X[nm] = [P.sb(L + f"{nm}{d}_{b}", shp, dt) for b in range(2)]
            for nm, shp, dt in (("gam", [64, 8], F32), ("gtot", [64, 8], F32), ("eg", [64, 8], F32),
                                ("bgv", [64, 8], F32), ("dif", [64, 8], F32), ("ekd", [64, 8], F32),
                                ("gcP", [64, 8], F32), ("nbeta", [64, 8], F32),
                                ("Y", [64, 512], F32), ("diff", [64, 512], F32), ("E", [64, 512], F32),
                                ("ET", [64, 512], F32), ("egr", [64, 512], F32), ("Dst", [64, 512], F32),
                                ("DinT", [64, 512], F32), ("A0", [64, 512], F32), ("B0", [64, 512], F32),
                                ("R", [64, 512], F32), ("Pa", [64, 512], F32), ("PTa", [64, 512], F32),
                                ("Pb", [64, 512], F32), ("PTb", [64, 512], F32),
                                ("pmT", [64, 512], BF16), ("bgK", [64, 512], BF16), ("betaV", [64, 512], BF16),
                                ("kd", [64, 512], BF16), ("TTb", [64, 512], BF16), ("wT", [64, 512], BF16),
                                ("u0", [64, 512], F32), ("qgT", [64, 512], BF16)):
                X[nm] = P.sb(L + f"{nm}{d}", shp, dt)
            X["ps0"] = P.ps(L + f"ps0{d}", [128, 512])
            for i in (1, 2, 3):
                X[f"ps{i}"] = P.ps(L + f"ps{i}{d}", [128, 512])
            D_[d] = X

        def K_(d, nm):
            return L + f"{nm}{d}"

        def loads(n, d):
            X = D_[d]
            b = n % 2
            c = self.gdn_chunk(n, d)
            t0 = 64 * c
            P.dma(X["bgt"][b][:], self.BGtok[t0:t0 + 64, :], w=[K_(d, f"bgt{b}")])
            P.dma(X["ktok"][b][:], self.KVtok[t0:t0 + 64, 0, :], w=[K_(d, f"ktok{b}")])
            P.dma(X["vtok"][b][:], self.KVtok[t0:t0 + 64, 1, :], w=[K_(d, f"vtok{b}")])
            qkn = self.QKN.rearrange("t p c -> (t p) c")
            P.dma(X["qT"][b][:], qkn[0:512, t0:t0 + 64].rearrange("(u p) c -> p u c", p=64), w=[K_(d, f"qT{b}")])
            P.dma(X["kT"][b][:], qkn[512:1024, t0:t0 + 64].rearrange("(u p) c -> p u c", p=64), w=[K_(d, f"kT{b}")])

        def stage_a(n, d):
            X = D_[d]
            b = n % 2
            k = lambda nm: K_(d, nm)
            bgt, ktok, vtok, kT, qT = (X[nm][b] for nm in ("bgt", "ktok", "vtok", "kT", "qT"))
            kb = lambda nm: K_(d, f"{nm}{b}")
            beta = bgt[:, d * 8:d * 8 + 8]
            g = bgt[:, 16 + d * 8:16 + d * 8 + 8]
            ps0, ps1, ps2, ps3 = X["ps0"], X["ps1"], X["ps2"], X["ps3"]
            P.mm(ps0[0:64, 0:8], msk[:, TRI[d], :], g, True, True, r=[kb("bgt")] + CK, w=[k("ps0")])
            P.mm(ps0[0:64, 8:16], ones[:], g, True, True, r=[kb("bgt")] + CK, w=[k("ps0")])
            P.copy(X["gam"][:], ps0[0:64, 0:8], r=[k("ps0")], w=[k("gam")])
            P.copy(X["gtot"][:], ps0[0:64, 8:16], r=[k("ps0")], w=[k("gtot")])
            P.act(X["eg"][:], X["gam"][:], AF.Exp, r=[k("gam")], w=[k("eg")])
            P.tt(X["bgv"][:], X["eg"][:], beta, ALU.mult, r=[k("eg"), kb("bgt")], w=[k("bgv")])
            P.tt(X["dif"][:], X["gtot"][:], X["gam"][:], ALU.subtract, r=[k("gtot"), k("gam")], w=[k("dif")])
            P.act(X["ekd"][:], X["dif"][:], AF.Exp, r=[k("dif")], w=[k("ekd")])
            P.act(X["gcP"][:], X["gtot"][:], AF.Exp, r=[k("gtot")], w=[k("gcP")])
            P.ts(X["nbeta"][:], beta, -1.0, ALU.mult, r=[kb("bgt")], w=[k("nbeta")])
            P.tt(v3(X["Y"][:]), bc_u(g), bc_m(TRI[d]), ALU.mult, r=[kb("bgt")] + CK, w=[k("Y")])
            P.mm(ps1[0:64, :], ones[:], X["Y"][:], True, True, r=[k("Y")] + CK, w=[k("ps1")])
            P.tt(v3(X["diff"][:]), bc_u(X["gam"][:]), v3(ps1[0:64, :]), ALU.subtract,
                 r=[k("gam"), k("ps1")], w=[k("diff")])
            P.act(X["E"][:], X["diff"][:], AF.Exp, r=[k("diff")], w=[k("E")])
            P.act(X["ET"][:], X["diff"][:], AF.Exp, scale=-1.0, r=[k("diff")], w=[k("ET")])
            P.act(X["egr"][:], ps1[0:64, :], AF.Exp, r=[k("ps1")], w=[k("egr")])
            P.tt(v3(X["Dst"][:]), v3(X["E"][:]), bc_m(STRICT[d]), ALU.min, r=[k("E")] + CK, w=[k("Dst")])
            P.tt(v3(X["DinT"][:]), v3(X["ET"][:]), bc_m(INCLT[d]), ALU.min, r=[k("ET")] + CK, w=[k("DinT")])
            for u in range(8):
                P.mm(ps2[0:64, u * 64:(u + 1) * 64], kT[:, u, :], kT[:, u, :],
                     True, True, r=[kb("kT")], w=[k("ps2")])
            for u in range(8):
                P.mm(ps3[0:64, u * 64:(u + 1) * 64], kT[:, u, :], qT[:, u, :],
                     True, True, r=[kb("kT"), kb("qT")], w=[k("ps3")])
            P.tt(X["A0"][:], ps2[0:64, :], X["Dst"][:], ALU.mult, r=[k("ps2"), k("Dst")], w=[k("A0")])
            P.tt(v3(X["A0"][:]), v3(X["A0"][:]), bc_u(X["nbeta"][:]), ALU.mult, r=[k("A0"), k("nbeta")], w=[k("A0")])
            P.tt(X["pmT"][:], ps3[0:64, :], X["DinT"][:], ALU.mult, r=[k("ps3"), k("DinT")], w=[k("pmT")])
            P.tt(v3(X["bgK"][:]), v3(ktok[:]), bc_u(X["bgv"][:]), ALU.mult, r=[kb("ktok"), k("bgv")],
                 w=[k("bgK")])
            P.tt(v3(X["betaV"][:]), v3(vtok[:]), bc_u(beta), ALU.mult, r=[kb("vtok"), kb("bgt")],
                 w=[k("betaV")])
            P.tt(v3(X["kd"][:]), v3(ktok[:]), bc_u(X["ekd"][:]), ALU.mult, r=[kb("ktok"), k("ekd")],
                 w=[k("kd")])
            P.tt(v3(X["qgT"][:]), qT[:], v3(X["egr"][:]), ALU.mult, r=[kb("qT"), k("egr")], w=[k("qgT")])
            for u in range(8):
                P.tr(ps1[0:64, u * 64:(u + 1) * 64], X["A0"][:, u * 64:(u + 1) * 64], ident[0:64, 0:64],
                     r=[k("A0")] + CK, w=[k("ps1")])
            P.copy(X["B0"][:], ps1[0:64, :], r=[k("ps1")], w=[k("B0")], eng="act")
            P.tt(v3(X["R"][:]), v3(X["B0"][:]), ident[0:64, 0:64].unsqueeze(1).broadcast_to([64, 8, 64]), ALU.add,
                 r=[k("B0")] + CK, w=[k("R")])

        def stage_lev(n, d, lev):
            X = D_[d]
            k = lambda nm: K_(d, nm)
            ps1, ps2, ps3 = X["ps1"], X["ps2"], X["ps3"]
            names = [("B0", "A0"), ("Pa", "PTa"), ("Pb", "PTb")]
            pn, ptn = names[0] if lev == 0 else names[1 + (lev - 1) % 2]
            qn, qtn = names[1 + lev % 2]
            Pm, PTm, Pn, PTn = X[pn], X[ptn], X[qn], X[qtn]
            last = lev == 4
            if not last:
                for u in range(8):
                    sl = slice(u * 64, (u + 1) * 64)
                    P.mm(ps2[0:64, sl], PTm[:, sl], Pm[:, sl], True, True, r=[k(pn), k(ptn)], w=[k("ps2")])
            for u in range(8):
                sl = slice(u * 64, (u + 1) * 64)
                P.mm(ps3[0:64, sl], Pm[:, sl], PTm[:, sl], True, True, r=[k(pn), k(ptn)], w=[k("ps3")])
            if not last:
                P.copy(Pn[:], ps2[0:64, :], r=[k("ps2")], w=[k(qn)], eng="act")
            P.copy(PTn[:], ps3[0:64, :], r=[k("ps3")], w=[k(qtn)])
            for u in range(8):
                sl = slice(u * 64, (u + 1) * 64)
                P.mm(ps1[0:64, sl], PTn[:, sl], X["R"][:, sl], True, True, r=[k(qtn), k("R")], w=[k("ps1")])
            P.tt(X["R"][:], X["R"][:], ps1[0:64, :], ALU.add, r=[k("R"), k("ps1")], w=[k("R")])

        def stage_z(n, d):
            X = D_[d]
            k = lambda nm: K_(d, nm)
            ps2, ps3 = X["ps2"], X["ps3"]
            P.copy(X["TTb"][:], X["R"][:], r=[k("R")], w=[k("TTb")], eng="act")
            for u in range(8):
                hp = u // 2
                sl = slice(u * 64, (u + 1) * 64)
                P.mm(ps3[0:64, sl], X["TTb"][:, sl], X["betaV"][:, sl], True, True,
                     r=[k("betaV"), k("TTb")], w=[k("ps3")])
                P.mm(ps2[0:64, sl], X["bgK"][:, sl], X["TTb"][:, sl], True, True,
                     r=[k("bgK"), k("TTb")], w=[k("ps2")])
            P.copy(X["wT"][:], ps2[0:64, :], r=[k("ps2")], w=[k("wT")])
            P.copy(X["u0"][:], ps3[0:64, :], r=[k("ps3")], w=[k("u0")], eng="act")
            P.dma(self.PW[n, d], X["wT"][:], r=[k("wT")], w=["PW"], q="dq_pool")
            P.dma(self.PQ[n, d], X["qgT"][:], r=[k("qgT")], w=["PQ"], q="dq_pool")
            P.dma(self.PK[n, d], X["kd"][:], r=[k("kd")], w=["PK"], q="dq_pool")
            P.dma(self.PM[n, d], X["pmT"][:], r=[k("pmT")], w=["PM"], q="dq_pool")
            P.dma(self.PU[n, d], X["u0"][:], r=[k("u0")], w=["PU"], q="dq_pool")
            P.dma(self.PG[n, d], X["gcP"][:], r=[k("gcP")], w=["PG"], q="dq_pool")

        for d in range(2):
            loads(0, d)
        for n in range(nsteps):
            if n + 1 < nsteps:
                for d in range(2):
                    loads(n + 1, d)
            for d in range(2):
                stage_a(n, d)
            for lev in range(5):
                for d in range(2):
                    stage_lev(n, d, lev)
            for d in range(2):
                stage_z(n, d)
        P.end_phase()

    def phase2c3(self, l, nsteps=68):
        P, I = self.P, self.inp
        L = f"p2c3_{l}_"
        P.begin_phase()
        NB = 3
        IN = {}
        for nm, dt in (("wT", BF16), ("qgT", BF16), ("kd", BF16), ("pmT", BF16), ("u0", F32)):
            IN[nm] = [P.sb(L + f"{nm}{b}", [64, 2, 512], dt) for b in range(NB)]
        IN["gc"] = [P.sb(L + f"gc{b}", [64, 2, 8], F32) for b in range(NB)]
        SRC = dict(wT=self.PW, qgT=self.PQ, kd=self.PK, pmT=self.PM, u0=self.PU, gc=self.PG)
        S = P.sb(L + "S", [64, 16, 64])
        Sb = P.sb(L + "Sb", [64, 16, 64], BF16)
        u = P.sb(L + "u", [64, 2, 512], BF16)
        osb = [P.sb(L + f"osb{i}", [64, 2, 512]) for i in range(2)]
        ps_u = [P.ps(L + f"ps_u{d}", [128, 512]) for d in range(2)]
        ps_o = [P.ps(L + f"ps_o{d}", [128, 512]) for d in range(2)]
        ps_S = [P.ps(L + f"ps_S{d}", [128, 512]) for d in range(2)]
        P.memset(S[:], 0.0, w=[L + "S0", L + "S1"])
        P.memset(Sb[:], 0.0, w=[L + "Sb0", L + "Sb1"])

        def loads(n):
            b = n % NB
            for nm in ("wT", "qgT", "kd", "pmT", "u0", "gc"):
                for d in range(2):
                    P.dma(IN[nm][b][:, d, :], SRC[nm][n, d], w=[L + f"{nm}{b}_{d}"])

        loads(0)
        if nsteps > 1:
            loads(1)
        for n in range(nsteps):
            if n + 2 < nsteps:
                loads(n + 2)
            b = n % NB
            kin = lambda nm, d: L + f"{nm}{b}_{d}"
            wT, qgT, kd, pmT, u0, gc = (IN[nm][b] for nm in ("wT", "qgT", "kd", "pmT", "u0", "gc"))
            ob, obk = osb[n % 2], L + f"osb{n % 2}"
            for d in range(2):
                for h in range(8):
                    sl = slice(h * 64, (h + 1) * 64)
                    P.mm(ps_u[d][0:64, sl], wT[:, d, sl], Sb[:, d * 8 + h, :], True, True,
                         r=[kin("wT", d), L + f"Sb{d}"], w=[L + f"ps_u{d}"])
            for d in range(2):
                P.tt(u[:, d, :], u0[:, d, :], ps_u[d][0:64, :], ALU.subtract,
                     r=[kin("u0", d), L + f"ps_u{d}"], w=[L + f"u{d}"])
            for d in range(2):
                for h in range(8):
                    sl = slice(h * 64, (h + 1) * 64)
                    P.mm(ps_o[d][0:64, sl], qgT[:, d, sl], Sb[:, d * 8 + h, :], True, False,
                         r=[kin("qgT", d), L + f"Sb{d}"], w=[L + f"ps_o{d}"])
                    P.mm(ps_o[d][0:64, sl], pmT[:, d, sl], u[:, d, sl], False, True,
                         r=[kin("pmT", d), L + f"u{d}"], w=[L + f"ps_o{d}"])
                for h in range(8):
                    sl = slice(h * 64, (h + 1) * 64)
                    P.mm(ps_S[d][0:64, sl], kd[:, d, sl], u[:, d, sl], True, True,
                         r=[kin("kd", d), L + f"u{d}"], w=[L + f"ps_S{d}"])
            for d in range(2):
                Sd = S[:, d * 8:(d + 1) * 8, :]
                P.tt(Sd, Sd, gc[:, d, :].unsqueeze(2).broadcast_to([64, 8, 64]), ALU.mult,
                     r=[kin("gc", d), L + f"S{d}"], w=[L + f"S{d}"])
                P.tt(Sd, Sd, ps_S[d][0:64, :].rearrange("p (u f) -> p u f", f=64), ALU.add,
                     r=[L + f"ps_S{d}", L + f"S{d}"], w=[L + f"S{d}"])
                P.copy(Sb[:, d * 8:(d + 1) * 8, :], Sd, r=[L + f"S{d}"], w=[L + f"Sb{d}"], eng="act")
                P.copy(ob[:, d, :], ps_o[d][0:64, :], r=[L + f"ps_o{d}"], w=[obk + f"_{d}"], eng="act")
                c = self.gdn_chunk(n, d)
                P.dma(self.ODIR[d, c * 64:(c + 1) * 64, :], ob[:, d, :], r=[obk + f"_{d}"], w=["ODIR"], q="dq_pool")
        P.end_phase()

    def phase2c4(self, l):
        P, I = self.P, self.inp
        L = f"p2c4_{l}_"
        P.begin_phase()
        ident = P.sb(L + "ident", [128, 128])
        P.dma(ident[:], I["ident"], w=[L + "ident"])
        go = P.sb(L + "go", [128, 1])
        for hh in range(2):
            P.dma(go[hh * 64:(hh + 1) * 64, :], I["g_o"][l, :].rearrange("(p o) -> p o", o=1), w=[L + "go"], slow=True)
        oin = [P.sb(L + f"oin{i}", [128, 2, 4, 512]) for i in range(2)]
        ogin = [P.sb(L + f"ogin{i}", [128, 4, 512], BF16) for i in range(2)]
        osum = P.sb(L + "osum", [128, 4, 512])
        sq = P.sb(L + "sq", [128, 512])
        ms = P.sb(L + "ms", [128, 4, 8])
        ps_t = [P.ps(L + f"ps_t{i}", [128, 512]) for i in range(2)]
        oc = [P.sb(L + f"oc{i}", [128, 512], BF16) for i in range(3)]

        def loads(bi):
            s, sz = BLOCKS[bi]
            nt = sz // 128
            b = bi % 2
            for d in range(2):
                P.dma(oin[b][:, d, 0:nt, :], self.ODIR[d, s:s + sz, :].rearrange("(t p) f -> p t f", p=128),
                      w=[L + f"oin{b}"])
            P.dma(ogin[b][:, :, 0:sz], self.OG[:, :, s:s + sz].rearrange("c p t -> p c t"), w=[L + f"ogin{b}"])

        loads(0)
        ct_ = co = 0
        for bi, (s, sz) in enumerate(BLOCKS):
            if bi + 1 < len(BLOCKS):
                loads(bi + 1)
            b = bi % 2
            nt = sz // 128
            P.tt(osum[:, 0:nt, :], oin[b][:, 0, 0:nt, :], oin[b][:, 1, 0:nt, :], ALU.add,
                 r=[L + f"oin{b}"], w=[L + "osum"], eng="pool")
            for t in range(nt):
                P.act(sq[:], osum[:, t, :], AF.Square, r=[L + "osum"], w=[L + "sq"])
                P.op("dve", (lambda t=t: self.P.nc.vector.tensor_reduce(
                    out=ms[:, t, :], in_=sq[:].rearrange("p (h f) -> p h f", f=64), axis=AX.X, op=ALU.add)),
                    r=[L + "sq"], w=[L + "ms"])
            P.rsqrt(ms[:, 0:nt, :], ms[:, 0:nt, :], EPS, r=[L + "ms"], w=[L + "ms"], scale=1.0 / 64)
            for t in range(nt):
                o3 = osum[:, t, :].rearrange("p (h f) -> p h f", f=64)
                P.tt(o3, o3, ms[:, t, :].unsqueeze(2).broadcast_to([128, 8, 64]), ALU.mult,
                     r=[L + "osum", L + "ms"], w=[L + "osum"])
            for ft in range(4):
                pt, ptk = ps_t[ct_ % 2], L + f"ps_t{ct_ % 2}"
                ct_ += 1
                for t in range(nt):
                    P.tr(pt[:, t * 128:(t + 1) * 128], osum[:, t, ft * 128:(ft + 1) * 128], ident[:],
                         r=[L + "osum", L + "ident"], w=[ptk])
                o_, ok = oc[co % 3], L + f"oc{co % 3}"
                co += 1
                P.stt(o_[:, 0:sz], pt[:, 0:sz], go[:, 0:1], ogin[b][:, ft, 0:sz], ALU.mult, ALU.mult,
                      r=[ptk, L + "go", L + f"ogin{b}"], w=[ok])
                P.dma(self.OC[ft, :, s:s + sz], o_[:, 0:sz], r=[ok], w=["OC"], q="dq_pool")
        P.end_phase()

    def bcast_row(self, dst, src_row, w):
        self.P.dma(dst, src_row.broadcast_to([128, src_row.shape[-1]]), w=w, slow=True)

    def phase3(self, l, xsrc_l, xsrc_c):
        P, I = self.P, self.inp
        L = f"p3_{l}_"
        P.begin_phase()
        ident = P.sb(L + "ident", [128, 128])
        P.dma(ident[:], I["ident"], w=[L + "ident"])
        WA = P.sb(L + "WA", [128, 4, D], BF16)
        WB = P.sb(L + "WB", [128, 4, D], BF16)
        WC = P.sb(L + "WC", [128, 4, D], BF16)
        WO = P.sb(L + "WO", [128, 8, D], BF16)
        for wt, nm, kc in ((WA, "w_a_out", 4), (WB, "w_b_out", 4), (WC, "w_c_out", 4), (WO, "w_out", 8)):
            for c in range(kc):
                P.dma(wt[:, c, :], I[nm][l, c * 128:(c + 1) * 128, :], w=[L + "W"], q="dq_pool")
        Wr = P.sb(L + "Wr", [128, 8, 36])
        P.dma(Wr[:, :, 0:4], I["w_rg"][l].rearrange("(k p) n -> p k n", p=128), w=[L + "Wr"], slow=True)
        P.dma(Wr[:, :, 4:36], I["w_re"][l].rearrange("(k p) n -> p k n", p=128), w=[L + "Wr"], slow=True)
        br = P.sb(L + "br", [128, 36])
        self.bcast_row(br[:, 0:4], I["b_rg"][l:l + 1, :], [L + "br"])
        self.bcast_row(br[:, 4:36], I["b_re"][l:l + 1, :], [L + "br"])
        rows = P.sb(L + "rows", [128, 2, 3, D])
        for rr in range(2):
            for i_, v_ in enumerate((2, 4, 3)):
                self.bcast_row(rows[:, rr, i_, :], self.modrow[l][rr:rr + 1, v_ * D:(v_ + 1) * D], [L + "rows"])
        P.ts(rows[:, :, 1, :], rows[:, :, 1, :], 1.0, ALU.add, r=[L + "rows"], w=[L + "rows"])
        lnr = P.sb(L + "lnr", [128, 2, D])
        self.bcast_row(lnr[:, 0, :], I["ln1_g"][l:l + 1, :], [L + "lnr"])
        self.bcast_row(lnr[:, 1, :], I["ln1_b"][l:l + 1, :], [L + "lnr"])
        CK = [L + "W", L + "Wr", L + "br", L + "rows", L + "lnr", L + "ident"]

        sa = [P.sb(L + f"sa{i}", [128, 3, 4, 512], BF16) for i in range(2)]
        mg0 = P.sb(L + "mg0", [128, 24, 512], BF16)
        mg = [mg0, mg0]
        xt0 = P.sb(L + "xt0", [128, 4, D])
        xt = [xt0, xt0]
        m = P.sb(L + "m", [128, 8, 512], BF16)
        t1 = P.sb(L + "t1", [128, 512])
        t2 = P.sb(L + "t2", [128, 512])
        z = P.sb(L + "z", [128, D])
        zz = P.sb(L + "zz", [128, D])
        x1 = [P.sb(L + f"x1{i}", [128, D]) for i in range(2)]
        h2 = P.sb(L + "h2", [128, D])
        h2Tf = P.sb(L + "h2Tf", [128, 8, 128])
        h2Tb = [P.sb(L + f"h2Tb{i}", [128, 8, 128], BF16) for i in range(2)]
        st = P.sb(L + "st", [128, 8])
        rt = P.sb(L + "rt", [128, 256])
        gate = [P.sb(L + f"gate{i}", [128, 32]) for i in range(2)]
        ps_y = [P.ps(L + f"ps_y{i}", [128, 512]) for i in range(3)]
        ps_out = [P.ps(L + f"ps_out{i}", [128, 512]) for i in range(2)]
        ps_t = [P.ps(L + f"ps_t{i}", [128, 512]) for i in range(2)]
        ps_r = P.ps(L + "ps_r", [128, 512])
        BIG = 1.0e30

        def loads(bi):
            s, sz = BLOCKS[bi]
            nt = sz // 128
            b = bi % 2
            for i_, src in enumerate((self.SA, self.OB, self.OC)):
                P.dma(sa[b][:, i_, :, 0:sz], src[:, :, s:s + sz].rearrange("c p t -> p c t"), w=[L + f"sa{b}"])

        def loads1(bi):
            s, sz = BLOCKS[bi]
            nt = sz // 128
            for g_ in range(3):
                P.dma(mg0[:, g_ * 8:(g_ + 1) * 8, 0:sz],
                      self.MG[g_ * 8:(g_ + 1) * 8, :, s:s + sz].rearrange("c p t -> p c t"), w=[L + "mg0"])
            src = xsrc_c if bi == 0 else xsrc_l[s - NCTX:s - NCTX + sz, :]
            P.dma(xt0[:, 0:nt, :], src.rearrange("(t p) d -> p t d", p=128), w=[L + "xt0"])

        loads(0)
        tix = 0
        for bi, (s, sz) in enumerate(BLOCKS):
            loads1(bi)
            if bi + 1 < len(BLOCKS):
                loads(bi + 1)
            b = bi % 2
            nt = sz // 128
            rr = 1 if bi == 0 else 0
            sak, mgk, xk = L + f"sa{b}", L + "mg0", L + "xt0"
            for j in range(8):
                for i_, wt in enumerate((WA, WB, WC)):
                    for c in range(4):
                        P.mm(ps_y[i_][:, 0:sz], wt[:, c, j * 128:(j + 1) * 128], sa[b][:, i_, c, 0:sz], c == 0, c == 3,
                             r=[sak] + CK, w=[L + f"ps_y{i_}"])
                P.tt(t1[:, 0:sz], ps_y[0][:, 0:sz], mg[b][:, j, 0:sz], ALU.mult, r=[L + "ps_y0", mgk], w=[L + "t1"])
                P.tt(t2[:, 0:sz], ps_y[1][:, 0:sz], mg[b][:, 8 + j, 0:sz], ALU.mult, r=[L + "ps_y1", mgk], w=[L + "t2"])
                P.tt(t1[:, 0:sz], t1[:, 0:sz], t2[:, 0:sz], ALU.add, r=[L + "t1", L + "t2"], w=[L + "t1"], eng="pool")
                P.tt(t2[:, 0:sz], ps_y[2][:, 0:sz], mg[b][:, 16 + j, 0:sz], ALU.mult, r=[L + "ps_y2", mgk], w=[L + "t2"])
                P.tt(m[:, j, 0:sz], t1[:, 0:sz], t2[:, 0:sz], ALU.add, r=[L + "t1", L + "t2"], w=[L + "m"], eng="pool")
            for t in range(nt):
                xo, xok = x1[tix % 2], L + f"x1{tix % 2}"
                hb, hbk = h2Tb[tix % 2], L + f"h2Tb{tix % 2}"
                gt_, gtk = gate[tix % 2], L + f"gate{tix % 2}"
                tix += 1
                tok0 = s + t * 128
                for hf in range(2):
                    for j in range(8):
                        P.mm(ps_out[hf][:, :], m[:, j, t * 128:(t + 1) * 128], WO[:, j, hf * 512:(hf + 1) * 512],
                             j == 0, j == 7, r=[L + "m"] + CK, w=[L + f"ps_out{hf}"])
                    P.tt(z[:, hf * 512:(hf + 1) * 512], ps_out[hf][:, :], rows[:, rr, 0, hf * 512:(hf + 1) * 512],
                         ALU.mult, r=[L + f"ps_out{hf}"] + CK, w=[L + "z"])
                P.stt(z[:], xt[b][:, t, :], ALPHA, z[:], ALU.mult, ALU.add, r=[xk, L + "z"], w=[L + "z"])
                self.layernorm(L, z, zz, st, lnr, xo, [L + "z"], [xok], CK)
                P.dma(self.X1[tok0:tok0 + 128, :], xo[:], r=[xok], w=["X1"], q="dq_pool")
                P.tt(h2[:], xo[:], rows[:, rr, 1, :], ALU.mult, r=[xok] + CK, w=[L + "h2"], eng="pool")
                P.tt(h2[:], h2[:], rows[:, rr, 2, :], ALU.add, r=[L + "h2"] + CK, w=[L + "h2"], eng="pool")
                for hf in range(2):
                    for jj in range(4):
                        j = hf * 4 + jj
                        P.tr(ps_t[hf][:, jj * 128:(jj + 1) * 128], h2[:, j * 128:(j + 1) * 128], ident[:],
                             r=[L + "h2"] + CK, w=[L + f"ps_t{hf}"])
                    P.copy(h2Tf[:, hf * 4:(hf + 1) * 4, :], ps_t[hf][:, :].rearrange("p (j t) -> p j t", t=128),
                           r=[L + f"ps_t{hf}"], w=[L + "h2Tf"], eng="act")
                P.copy(hb[:], h2Tf[:], r=[L + "h2Tf"], w=[hbk], eng="pool")
                P.dma(self.H2T[:, :, tok0:tok0 + 128].rearrange("j p t -> p j t"), hb[:], r=[hbk], w=["H2T"], q="dq_pool")
                for j in range(8):
                    P.mm(ps_r[:, 0:36], h2Tf[:, j, :], Wr[:, j, :], j == 0, j == 7, r=[L + "h2Tf"] + CK, w=[L + "ps_r"])
                self.router(L, ps_r, br, rt, gt_, [L + "ps_r"] + CK, [gtk], BIG)
                P.dma(self.GATE[tok0:tok0 + 128, :], gt_[:], r=[gtk], w=["GATE"], q="dq_pool")
        P.end_phase()

    def layernorm(self, L, z, zz, st, lnr, out, rk, wk, CK):
        P = self.P
        nc = P.nc
        sk = L + "st"
        P.op("dve", lambda: nc.vector.tensor_reduce(out=st[:, 0:1], in_=z[:], axis=AX.X, op=ALU.add), r=rk, w=[sk])
        P.act(zz[:], z[:], AF.Square, r=rk, w=[L + "zz"])
        P.op("dve", lambda: nc.vector.tensor_reduce(out=st[:, 1:2], in_=zz[:], axis=AX.X, op=ALU.add),
             r=[L + "zz"], w=[sk])
        P.ts(st[:, 2:3], st[:, 0:1], 1.0 / D, ALU.mult, r=[sk], w=[sk])
        P.tt(st[:, 3:4], st[:, 2:3], st[:, 2:3], ALU.mult, r=[sk], w=[sk])
        P.stt(st[:, 4:5], st[:, 1:2], 1.0 / D, st[:, 3:4], ALU.mult, ALU.subtract, r=[sk], w=[sk])
        P.rsqrt(st[:, 5:6], st[:, 4:5], EPS, r=[sk], w=[sk])
        P.ts(zz[:], z[:], st[:, 2:3], ALU.subtract, st[:, 5:6], ALU.mult, r=rk + [sk, L + "zz"], w=[L + "zz"])
        P.tt(zz[:], zz[:], lnr[:, 0, :], ALU.mult, r=[L + "zz"] + CK, w=[L + "zz"], eng="pool")
        P.tt(out[:], zz[:], lnr[:, 1, :], ALU.add, r=[L + "zz"] + CK, w=wk, eng="pool")

    def router(self, L, ps_r, br, rt, gate, rk, wk, BIG):
        P = self.P
        nc = P.nc
        k = L + "rt"
        lg = rt[:, 0:36]
        P.tt(lg, ps_r[:, 0:36], br[:, :], ALU.add, r=rk, w=[k])
        gmax, ngmax, ge, gsum, gp = rt[:, 40:41], rt[:, 41:42], rt[:, 44:48], rt[:, 48:49], rt[:, 49:50]
        ohg, m1, m2, dm, e21, p1, p2 = rt[:, 52:56], rt[:, 56:57], rt[:, 57:58], rt[:, 58:59], rt[:, 59:60], rt[:, 60:61], rt[:, 61:62]
        lem, oh1, oh2 = rt[:, 64:96], rt[:, 96:128], rt[:, 128:160]
        lem2 = rt[:, 160:192]
        P.op("dve", lambda: nc.vector.tensor_reduce(out=gmax, in_=rt[:, 0:4], axis=AX.X, op=ALU.max), r=[k], w=[k])
        P.ts(ngmax, gmax, -1.0, ALU.mult, r=[k], w=[k])
        P.act(ge, rt[:, 0:4], AF.Exp, bias=ngmax, r=[k], w=[k])
        P.op("dve", lambda: nc.vector.tensor_reduce(out=gsum, in_=ge, axis=AX.X, op=ALU.add), r=[k], w=[k])
        P.recip(gp, gsum, r=[k], w=[k])
        P.ts(ohg, rt[:, 0:4], gmax, ALU.is_equal, r=[k], w=[k])
        P.ts(ohg, ohg, -1.0, ALU.add, BIG, ALU.mult, r=[k], w=[k])
        P.tt(lem.rearrange("p (g e) -> p g e", e=8), rt[:, 4:36].rearrange("p (g e) -> p g e", e=8),
             ohg.unsqueeze(2).broadcast_to([128, 4, 8]), ALU.add, r=[k], w=[k])
        P.op("dve", lambda: nc.vector.tensor_reduce(out=m1, in_=lem, axis=AX.X, op=ALU.max), r=[k], w=[k])
        P.ts(oh1, lem, m1, ALU.is_equal, r=[k], w=[k])
        P.stt(lem2, oh1, -BIG, lem, ALU.mult, ALU.add, r=[k], w=[k])
        P.op("dve", lambda: nc.vector.tensor_reduce(out=m2, in_=lem2, axis=AX.X, op=ALU.max), r=[k], w=[k])
        P.ts(oh2, lem2, m2, ALU.is_equal, r=[k], w=[k])
        P.tt(dm, m2, m1, ALU.subtract, r=[k], w=[k])
        P.act(e21, dm, AF.Exp, r=[k], w=[k])
        P.ts(p1, e21, 1.0, ALU.add, r=[k], w=[k])
        P.recip(p1, p1, r=[k], w=[k])
        P.tt(p2, e21, p1, ALU.mult, r=[k], w=[k])
        P.tt(p1, p1, gp, ALU.mult, r=[k], w=[k])
        P.tt(p2, p2, gp, ALU.mult, r=[k], w=[k])
        P.ts(gate[:], oh1, p1, ALU.mult, r=[k], w=wk)
        P.stt(gate[:], oh2, p2, gate[:], ALU.mult, ALU.add, r=[k] + wk, w=wk)

    def phase4(self, l, dst, dst_has_ctx):
        P, I = self.P, self.inp
        L = f"p4_{l}_"
        P.begin_phase()
        rows = P.sb(L + "rows", [128, 2, D])
        for rr in range(2):
            self.bcast_row(rows[:, rr, :], self.modrow[l][rr:rr + 1, 5 * D:6 * D], [L + "rows"])
        lnr = P.sb(L + "lnr", [128, 2, D])
        self.bcast_row(lnr[:, 0, :], I["ln2_g"][l:l + 1, :], [L + "lnr"])
        self.bcast_row(lnr[:, 1, :], I["ln2_b"][l:l + 1, :], [L + "lnr"])
        CK = [L + "rows", L + "lnr"]
        NT = 17
        HT = NT * 128
        H = P.sb(L + "H", [128, 8, HT], BF16)
        G = P.sb(L + "G", [128, NT, 32])
        yacc = P.sb(L + "yacc", [128, NT, D])
        W1 = [P.sb(L + f"W1_{i}", [128, 8, 256], BF16) for i in range(2)]
        W3 = [P.sb(L + f"W3_{i}", [128, 8, 256], BF16) for i in range(2)]
        W2 = [P.sb(L + f"W2_{i}", [128, 2, D], BF16) for i in range(2)]
        aT = [P.sb(L + f"aT{i}", [128, 2, 512], BF16) for i in range(2)]
        sg = [P.sb(L + f"sg{i}", [128, 512]) for i in range(2)]
        ps_h1 = [P.ps(L + f"ps_h1{i}", [128, 512]) for i in range(2)]
        ps_h3 = [P.ps(L + f"ps_h3{i}", [128, 512]) for i in range(2)]
        ps_y = [P.ps(L + f"ps_y{i}", [128, 512]) for i in range(4)]
        x1t = [P.sb(L + f"x1t{i}", [128, D]) for i in range(2)]
        z = P.sb(L + "z", [128, D])
        zz = P.sb(L + "zz", [128, D])
        st = P.sb(L + "st", [128, 8])
        xo = [P.sb(L + f"xo{i}", [128, D]) for i in range(2)]
        blocks = [(i * 512, 512) for i in range(4)] + [(2048, 128)]

        def load_w(e):
            b = e % 2
            P.dma(W1[b][:], I["w1"][l, e].rearrange("(k p) n -> p k n", p=128), w=[L + f"W1_{b}"], q="dq_pool")
            P.dma(W3[b][:], I["w3"][l, e].rearrange("(k p) n -> p k n", p=128), w=[L + f"W3_{b}"], q="dq_pool")
            P.dma(W2[b][:], I["w2"][l, e].rearrange("(o p) n -> p o n", p=128), w=[L + f"W2_{b}"], q="dq_pool")

        ca = cy = 0
        for half in range(2):
            T0 = half * HT
            P.dma(H[:], self.H2T[:, :, T0:T0 + HT].rearrange("j p t -> p j t"), w=[L + "H"])
            P.dma(G[:], self.GATE[T0:T0 + HT, :].rearrange("(t p) e -> p t e", p=128), w=[L + "G"])
            load_w(0)
            for e in range(32):
                if e + 1 < 32:
                    load_w(e + 1)
                b = e % 2
                wk1, wk3, wk2 = L + f"W1_{b}", L + f"W3_{b}", L + f"W2_{b}"
                for (b0, bsz) in blocks:
                    a_, ak = aT[ca % 2], L + f"aT{ca % 2}"
                    ca += 1
                    for o in range(2):
                        for k in range(8):
                            P.mm(ps_h1[o][:, 0:bsz], W1[b][:, k, o * 128:(o + 1) * 128], H[:, k, b0:b0 + bsz],
                                 k == 0, k == 7, r=[wk1, L + "H"], w=[L + f"ps_h1{o}"])
                        for k in range(8):
                            P.mm(ps_h3[o][:, 0:bsz], W3[b][:, k, o * 128:(o + 1) * 128], H[:, k, b0:b0 + bsz],
                                 k == 0, k == 7, r=[wk3, L + "H"], w=[L + f"ps_h3{o}"])
                        P.act(sg[o][:, 0:bsz], ps_h1[o][:, 0:bsz], AF.Silu, r=[L + f"ps_h1{o}"], w=[L + f"sg{o}"])
                        P.tt(a_[:, o, 0:bsz], sg[o][:, 0:bsz], ps_h3[o][:, 0:bsz], ALU.mult,
                             r=[L + f"sg{o}", L + f"ps_h3{o}"], w=[ak])
                    for t in range(bsz // 128):
                        tile = b0 // 128 + t
                        for hf in range(2):
                            py, pyk = ps_y[cy % 4], L + f"ps_y{cy % 4}"
                            cy += 1
                            for o in range(2):
                                P.mm(py[:, :], a_[:, o, t * 128:(t + 1) * 128], W2[b][:, o, hf * 512:(hf + 1) * 512],
                                     o == 0, o == 1, r=[ak, wk2], w=[pyk])
                            ya = yacc[:, tile, hf * 512:(hf + 1) * 512]
                            yk = L + f"yacc{tile}_{hf}"
                            if e == 0:
                                P.ts(ya, py[:, :], G[:, tile, e:e + 1], ALU.mult, r=[pyk, L + "G"], w=[yk])
                            else:
                                P.stt(ya, py[:, :], G[:, tile, e:e + 1], ya, ALU.mult, ALU.add,
                                      r=[pyk, L + "G", yk], w=[yk])
            for t in range(NT):
                tile = half * NT + t
                tok0 = tile * 128
                rr = 1 if tile < 2 else 0
                xi, xik = x1t[t % 2], L + f"x1t{t % 2}"
                o_, ok = xo[t % 2], L + f"xo{t % 2}"
                if tile < 2 and not dst_has_ctx:
                    continue
                P.dma(xi[:], self.X1[tok0:tok0 + 128, :], w=[xik])
                yks = [L + f"yacc{t}_{hf}" for hf in range(2)]
                P.tt(z[:], yacc[:, t, :], rows[:, rr, :], ALU.mult, r=yks + CK, w=[L + "z"], eng="pool")
                P.stt(z[:], xi[:], ALPHA, z[:], ALU.mult, ALU.add, r=[xik, L + "z"], w=[L + "z"])
                self.layernorm(L, z, zz, st, lnr, o_, [L + "z"], [ok], CK)
                if dst_has_ctx:
                    P.dma(dst[tok0:tok0 + 128, :], o_[:], r=[ok], w=["dst"], q="dq_pool")
                else:
                    P.dma(dst[tok0 - NCTX:tok0 - NCTX + 128, :], o_[:], r=[ok], w=["dst"], q="dq_pool")
        P.end_phase()

    def build(self, phases=None, n_layers=2):
        P = self.P
        self.declare_inputs()
        ALL = ("p0", "p1", "p2a", "p2b1", "p2b2", "p2c1", "p2c2", "p2c3", "p2c4", "p3", "p4")
        if phases is None:
            phases = ALL
        self.modrow = [self.scratch(f"modrow{l}", [2, 6 * D]) for l in range(2)]
        self.U = self.scratch("U", [4, 128, T])
        self.QD = self.scratch("QD", [3, 128, T])
        self.KVD = self.scratch("KVD", [2, 128, T])
        self.KR = self.scratch("KR", [32, T])
        self.GQKV = self.scratch("GQKV", [12, 128, T])
        self.OG = self.scratch("OG", [4, 128, T], BF16)
        self.BETA = self.scratch("BETA", [16, T])
        self.GL = self.scratch("GL", [16, T])
        self.MG = self.scratch("MG", [24, 128, T], BF16)
        self.SA = self.scratch("SA", [4, 128, T], BF16)
        self.KT = self.scratch("KT", [8, 96, T], BF16)
        self.QT = self.scratch("QT", [8, 96, T], BF16)
        self.VX = self.scratch("VX", [8, 128, 34, 128], BF16)
        self.OB = self.scratch("OB", [4, 128, T], BF16)
        self.QKN = self.scratch("QKN", [8, 128, T])
        self.KVtok = self.scratch("KVtok", [T, 2, 512])
        self.BGtok = self.scratch("BGtok", [T, 32])
        self.PW = self.scratch("PW", [68, 2, 64, 512], BF16)
        self.PQ = self.scratch("PQ", [68, 2, 64, 512], BF16)
        self.PK = self.scratch("PK", [68, 2, 64, 512], BF16)
        self.PM = self.scratch("PM", [68, 2, 64, 512], BF16)
        self.PU = self.scratch("PU", [68, 2, 64, 512])
        self.PG = self.scratch("PG", [68, 2, 64, 8])
        self.ODIR = self.scratch("ODIR", [2, T, 512])
        self.OC = self.scratch("OC", [4, 128, T], BF16)
        self.X1 = self.scratch("X1", [T, D])
        self.H2T = self.scratch("H2T", [8, 128, T], BF16)
        self.GATE = self.scratch("GATE", [T, 32])
        self.X2 = self.scratch("X2", [T, D])
        self.out = P.dram("out", [NL, D], F32, kind="ExternalOutput")
        kw = getattr(self, "p2c2_kw", {})
        for l in range(n_layers):
            if l == 0:
                xl, xc = self.inp["x"], self.inp["ctx"]
            else:
                xl, xc = self.X2[NCTX:T, :], self.X2[0:NCTX, :]
            last = l == DEPTH - 1
            if "p0" in phases:
                self.phase0(l)
            if "p1" in phases:
                self.phase1(l, xl, xc, "xin")
            if "p2a" in phases:
                self.phase2a(l)
            if "p2b1" in phases:
                self.phase2b1(l)
            if "p2b2" in phases:
                self.phase2b2(l)
            if "p2c1" in phases:
                self.phase2c1(l)
            if "p2c2" in phases:
                self.phase2c2(l, **kw)
            if "p2c3" in phases:
                self.phase2c3(l, **kw)
            if "p2c4" in phases:
                self.phase2c4(l)
            if "p3" in phases:
                self.phase3(l, xl, xc)
            if "p4" in phases:
                if last:
                    self.phase4(l, self.out, False)
                else:
                    self.phase4(l, self.X2, True)
        return P.nc


def host_consts():
    ident = np.eye(128, dtype=np.float32)
    nf = 8
    inv = (10000.0 ** (-np.arange(nf, dtype=np.float32) / nf)).astype(np.float32)
    rows = NL // 64
    r = np.repeat(np.arange(rows, dtype=np.float32), 64)
    col = np.tile(np.arange(64, dtype=np.float32), rows)
    ang = np.stack([r[:, None] * inv, col[:, None] * inv], axis=1)
    cos = np.cos(ang).astype(np.float32)
    sin = np.sin(ang).astype(np.float32)
    cs = np.zeros((2, 32, T), np.float32)
    cs[0, :, :NCTX] = 1.0
    for a in range(2):
        for h in range(2):
            d0 = a * 16 + h * 8
            cs[0, d0:d0 + 8, NCTX:] = cos[:, a, :].T
            cs[1, d0:d0 + 8, NCTX:] = (-sin[:, a, :].T) if h == 0 else sin[:, a, :].T
    perm = np.zeros((32, 32), np.float32)
    for m_ in range(32):
        perm[m_ ^ 8, m_] = 1.0
    pp, ff = np.meshgrid(np.arange(64), np.arange(64), indexing="ij")
    gmask = np.stack([ff <= pp, ff < pp, ff >= pp, ff > pp]).astype(np.float32)
    return ident, cs, perm, gmask


def make_in_maps(inputs, cores):
    ident, cs, perm, gmask = host_consts()
    maps = []
    for b in cores:
        m = {}
        for k, v in inputs.items():
            v = np.asarray(v)
            if k in ("x", "c", "ctx"):
                m[k] = np.ascontiguousarray(v[b])
            elif k in ("a_log", "dt_bias"):
                m[k] = np.ascontiguousarray(v.reshape(2, 16))
            else:
                m[k] = np.ascontiguousarray(v)
        m["ident"] = ident
        m["rope_cs"] = cs
        m["perm32"] = perm
        m["gmask"] = gmask
        maps.append(m)
    return maps


_NC_CACHE = {}


def kernel(**inputs):
    n = 8
    if "nc" not in _NC_CACHE:
        net = Net()
        _NC_CACHE["nc"] = net.build()
    nc = _NC_CACHE["nc"]
    maps = make_in_maps(inputs, list(range(n)))
    res = run_bass_kernel_spmd(nc, maps, core_ids=list(range(n)))
    return np.stack([np.asarray(r["out"]) for r in res.results], axis=0).astype(np.float32)
```

```python
from contextlib import ExitStack
import numpy as np
import concourse.bass as bass
import concourse.mybir as mybir
from concourse.bass_utils import run_bass_kernel_spmd

F32 = mybir.dt.float32
BF16 = mybir.dt.bfloat16
AF = mybir.ActivationFunctionType
ALU = mybir.AluOpType
AX = mybir.AxisListType

D = 1024
NCTX = 256
NL = 4096
T = NCTX + NL
DEPTH = 2
P_IN = 6848
EPS = 1e-6
ALPHA = (2 * DEPTH) ** 0.25
MLA_SCALE = 96 ** -0.5
BLOCKS = [(0, 256)] + [(256 + 512 * i, 512) for i in range(8)]

DMA_ENGS = ("sp", "dq_pool", "dq_act")
PHYS = {"pe": "pe", "act": "act", "dve": "dve", "pool": "pool", "sp": "sp", "dq_pool": "pool", "dq_act": "act"}


class Prog:
    def __init__(self):
        self.nc = bass.Bass("TRN2", target_bir_lowering=False)
        self.root = ExitStack()
        self.es = self.root
        self.ops = []
        self.n_dsem = 12
        self.psum_names = set()

    def sb(self, name, shape, dt=F32):
        return self.es.enter_context(self.nc.sbuf_tensor(name, list(shape), dt))

    def ps(self, name, shape, dt=F32):
        self.psum_names.add(name)
        return self.es.enter_context(self.nc.psum_tensor(name, list(shape), dt))

    def dram(self, name, shape, dt=F32, kind="Internal"):
        return self.nc.dram_tensor(name, list(shape), dt, kind=kind).ap()

    def op(self, eng, fn, r=(), w=()):
        pr = [k for k in r if k in self.psum_names]
        if pr:
            r = [k for k in r if k not in self.psum_names]
            w = list(w) + pr
        self.ops.append((eng, fn, tuple(r), tuple(w)))

    def dma(self, out, in_, r=(), w=(), q="sp", slow=False):
        nc = self.nc
        e = {"sp": nc.sync, "dq_pool": nc.gpsimd, "dq_act": nc.scalar}[q]
        if slow:
            self.op(q, lambda: e.dma_start(out=out, in_=in_, allow_slow_non_contiguous=True), r, w)
        else:
            self.op(q, lambda: e.dma_start(out=out, in_=in_), r, w)

    def mm(self, out, lhsT, rhs, start, stop, r=(), w=()):
        nc = self.nc
        self.op("pe", lambda: nc.tensor.matmul(out, lhsT=lhsT, rhs=rhs, start=start, stop=stop), r, w)

    def tr(self, out, in_, ident, r=(), w=()):
        nc = self.nc
        self.op("pe", lambda: nc.tensor.transpose(out, in_, ident), r, w)

    def act(self, out, in_, func, r=(), w=(), bias=None, scale=None, accum_out=None):
        nc = self.nc
        kw = {}
        if bias is not None:
            kw["bias"] = bias
        if scale is not None:
            kw["scale"] = scale
        if accum_out is not None:
            kw["accum_out"] = accum_out
        self.op("act", lambda: nc.scalar.activation(out=out, in_=in_, func=func, **kw), r, w)

    def _veng(self, eng):
        return self.nc.vector if eng == "dve" else self.nc.gpsimd

    def tt(self, out, in0, in1, op, r=(), w=(), eng="dve"):
        e = self._veng(eng)
        self.op(eng, lambda: e.tensor_tensor(out=out, in0=in0, in1=in1, op=op), r, w)

    def ts(self, out, in0, s1, op0, s2=None, op1=None, r=(), w=(), eng="dve", accum_out=None):
        e = self._veng(eng)
        kw = {}
        if op1 is not None:
            kw["op1"] = op1
        if accum_out is not None:
            kw["accum_out"] = accum_out
        self.op(eng, lambda: e.tensor_scalar(out=out, in0=in0, scalar1=s1, scalar2=s2, op0=op0, **kw), r, w)

    def stt(self, out, in0, scalar, in1, op0, op1, r=(), w=(), eng="dve"):
        e = self._veng(eng)
        self.op(eng, lambda: e.scalar_tensor_tensor(out=out, in0=in0, scalar=scalar, in1=in1, op0=op0, op1=op1), r, w)

    def copy(self, out, in_, r=(), w=(), eng="dve"):
        if eng == "act":
            self.act(out, in_, AF.Copy, r, w)
        else:
            e = self._veng(eng)
            self.op(eng, lambda: e.tensor_copy(out=out, in_=in_), r, w)

    def recip(self, out, in_, r=(), w=()):
        nc = self.nc
        self.op("dve", lambda: nc.vector.reciprocal(out=out, in_=in_), r, w)

    def rsqrt(self, out, in_, eps, r=(), w=(), scale=1.0):
        self.act(out, in_, AF.Sqrt, r=r, w=w, bias=eps, scale=scale)
        self.recip(out, out, r=w, w=w)

    def memset(self, ap, val, w=(), eng="dve"):
        e = self._veng(eng)
        self.op(eng, lambda: e.memset(ap, val), (), w)

    def begin_phase(self):
        self._outer = self.es
        self.es = ExitStack()
        self.es.__enter__()

    def end_phase(self):
        self.flush()
        self.es.close()
        self.es = self._outer

    def _init_sync(self):
        nc = self.nc
        self.csem = {p: self._outer_ctx(nc.semaphore("cs_" + p)) for p in ("pe", "act", "dve", "pool")}
        self.dsem = {q: [self._outer_ctx(nc.semaphore(f"ds_{q}_{k}")) for k in range(self.n_dsem)]
                     for q in DMA_ENGS}
        self.bsem = self._outer_ctx(nc.semaphore("barrier"))
        self.mc = {p: 0 for p in ("pe", "act", "dve", "pool")}
        self.dcount = {q: 0 for q in DMA_ENGS}
        self.nphase = 0
        self.n_ops = 0

    def _outer_ctx(self, cm):
        return self.root.enter_context(cm)

    def flush(self):
        nc = self.nc
        if not hasattr(self, "csem"):
            self._init_sync()
        ops = self.ops
        self.ops = []
        import os as _os
        if _os.environ.get("KMAXOPS"):
            ops = ops[:int(_os.environ["KMAXOPS"])]
        queue = {"pe": nc.tensor, "act": nc.scalar, "dve": nc.vector, "pool": nc.gpsimd,
                 "sp": nc.sync, "dq_pool": nc.gpsimd, "dq_act": nc.scalar}
        n = len(ops)
        self.n_ops += n
        last_w, readers = {}, {}
        deps = []
        for i, (eng, fn, r, w) in enumerate(ops):
            d = set()
            for k in r:
                if k in last_w:
                    d.add(last_w[k])
            for k in w:
                if k in last_w:
                    d.add(last_w[k])
                d.update(readers.get(k, ()))
            d.discard(i)
            deps.append(d)
            for k in r:
                readers.setdefault(k, []).append(i)
            for k in w:
                last_w[k] = i
                readers[k] = []
        local = [0] * n
        cnt = {}
        last_on = {}
        for i, (eng, fn, r, w) in enumerate(ops):
            if eng not in DMA_ENGS:
                p = PHYS[eng]
                cnt[p] = cnt.get(p, 0) + 1
                local[i] = cnt[p]
                last_on[p] = i
        known_c, known_d = {}, {}
        milestone = [False] * n
        waits = [None] * n
        for i, (eng, fn, r, w) in enumerate(ops):
            E = PHYS[eng]
            i_dma = eng in DMA_ENGS
            need_c = {}
            need_d = []
            for j in sorted(deps[i]):
                jeng = ops[j][0]
                if jeng in DMA_ENGS:
                    s = known_d.setdefault(E, set())
                    if j not in s:
                        s.add(j)
                        need_d.append(j)
                else:
                    Pj = PHYS[jeng]
                    if Pj == E and not i_dma and E == "pe":
                        continue
                    if known_c.get((E, Pj), 0) >= local[j]:
                        continue
                    if need_c.get(Pj, (0, -1))[0] < local[j]:
                        need_c[Pj] = (local[j], j)
            for Pj, (lj, j) in need_c.items():
                known_c[(E, Pj)] = lj
                milestone[j] = True
            waits[i] = (list(need_c.values()), need_d)
        for p, i in last_on.items():
            milestone[i] = True
        msval = [0] * n
        for i, (eng, fn, r, w) in enumerate(ops):
            if eng not in DMA_ENGS and milestone[i]:
                p = PHYS[eng]
                self.mc[p] += 1
                msval[i] = self.mc[p]
        dtoken = {}
        for i, (eng, fn, r, w) in enumerate(ops):
            q = queue[eng]
            need_c, need_d = waits[i]
            for (lj, j) in need_c:
                q.wait_ge(self.csem[PHYS[ops[j][0]]], msval[j])
            for j in need_d:
                s, v = dtoken[j]
                q.wait_ge(s, v)
            if eng in DMA_ENGS:
                k = self.dcount[eng]
                self.dcount[eng] += 1
                s = self.dsem[eng][k % self.n_dsem]
                rnd = k // self.n_dsem
                if rnd > 0:
                    q.wait_ge(s, 16 * rnd)
                ins = fn()
                ins.then_inc(s, 16)
                dtoken[i] = (s, 16 * (rnd + 1))
            else:
                ins = fn()
                if milestone[i]:
                    ins.then_inc(self.csem[PHYS[eng]], 1)
        for p in ("pe", "act", "dve", "pool"):
            if self.mc[p] > 0:
                nc.sync.wait_ge(self.csem[p], self.mc[p])
        for qn in DMA_ENGS:
            k = self.dcount[qn]
            for m in range(min(k, self.n_dsem)):
                final = 16 * ((k - 1 - m) // self.n_dsem + 1)
                nc.sync.wait_ge(self.dsem[qn][m], final)
        self.nphase += 1
        nc.sync.sem_inc(self.bsem, 1)
        for e in (nc.tensor, nc.scalar, nc.vector, nc.gpsimd):
            e.wait_ge(self.bsem, self.nphase)


C_A, C_GT, C_QD, C_KVD, C_KR = 0, 512, 1024, 1408, 1664
C_GQ, C_GK, C_GV, C_OG, C_BETA, C_AR, C_MG = 1696, 2208, 2720, 3232, 3744, 3760, 3776


def load_col(P, dst, src1d, r=(), w=()):
    P.dma(dst, src1d.rearrange("(n p) -> p n", p=128), r, w, slow=True)


class Net:
    def __init__(self, dbg=()):
        self.P = Prog()
        self.dbg = set(dbg)
        self.io = {}

    def scratch(self, name, shape, dt=F32):
        kind = "ExternalOutput" if name in self.dbg else "Internal"
        if name in getattr(self, "dbg_in", ()):
            kind = "ExternalInput"
        t = self.P.dram(name, shape, dt, kind=kind)
        self.io[name] = t
        return t

    def declare_inputs(self):
        P = self.P
        specs = dict(
            x=[NL, D], c=[D], ctx=[NCTX, D], c_ctx=[D], w_mod=[2, D, 6 * D], b_mod=[2, 6 * D],
            w_in=[2, D, P_IN], b_in=[2, P_IN], conv_a_w=[2, 31, 512], conv_a_b=[2, 512],
            ln_a_g=[2, 512], ln_a_b=[2, 512], w_a_out=[2, 512, D], g_q=[2, 384], g_kv=[2, 256],
            w_uq=[2, 384, 512], w_qr=[2, 384, 256], w_uk=[2, 256, 512], w_uv=[2, 256, 512],
            w_b_out=[2, 512, D], conv_c_w=[2, 5, 1536], a_log=[2, 16], dt_bias=[2, 16], g_o=[2, 64],
            w_c_out=[2, 512, D], w_out=[2, D, D], ln1_g=[2, D], ln1_b=[2, D], w_rg=[2, D, 4],
            b_rg=[2, 4], w_re=[2, D, 32], b_re=[2, 32], w1=[2, 32, D, 256], w3=[2, 32, D, 256],
            w2=[2, 32, 256, D], ln2_g=[2, D], ln2_b=[2, D],
            ident=[128, 128], rope_cs=[2, 32, T], perm32=[32, 32], gmask=[4, 64, 64],
        )
        self.inp = {k: P.dram(k, v, F32, kind="ExternalInput") for k, v in specs.items()}

    def phase0(self, l):
        P, I = self.P, self.inp
        L = f"p0_{l}_"
        P.begin_phase()
        scin = P.sb(L + "scin", [128, 8, 2])
        sc = P.sb(L + "sc", [128, 8, 2])
        bm = P.sb(L + "bm", [2, 6 * D])
        mr = P.sb(L + "mr", [2, 6 * D])
        wm = [P.sb(L + f"wm{i}", [128, 8, 512]) for i in range(2)]
        ps = P.ps(L + "ps", [128, 512])
        load_col(P, scin[:, :, 0], I["c"], w=[L + "scin"])
        load_col(P, scin[:, :, 1], I["c_ctx"], w=[L + "scin"])
        P.dma(bm[0:1, :], I["b_mod"][l:l + 1, :], w=[L + "bm"])
        P.dma(bm[1:2, :], I["b_mod"][l:l + 1, :], w=[L + "bm"])
        P.act(sc[:], scin[:], AF.Silu, r=[L + "scin"], w=[L + "sc"])
        for nb in range(12):
            b = wm[nb % 2]
            key = L + f"wm{nb % 2}"
            P.dma(b[:], I["w_mod"][l, :, nb * 512:(nb + 1) * 512].rearrange("(k p) n -> p k n", p=128), w=[key])
            for k in range(8):
                P.mm(ps[0:2, :], sc[:, k, :], b[:, k, :], k == 0, k == 7, r=[key, L + "sc"], w=[L + "ps"])
            P.tt(mr[0:2, nb * 512:(nb + 1) * 512], ps[0:2, :], bm[0:2, nb * 512:(nb + 1) * 512], ALU.add,
                 r=[L + "ps", L + "bm"], w=[L + "mr"])
        P.dma(self.modrow[l], mr[:], r=[L + "mr"], w=[f"modrow{l}"], q="dq_pool")
        P.end_phase()

    def phase1(self, l, xsrc_l, xsrc_c, xkey):
        P, I = self.P, self.inp
        L = f"p1_{l}_"
        P.begin_phase()
        W = P.sb(L + "W", [128, 8, P_IN], BF16)
        for k in range(8):
            for c in range(4):
                c0, c1 = c * 1712, (c + 1) * 1712
                P.dma(W[:, k, c0:c1], I["w_in"][l, k * 128:(k + 1) * 128, c0:c1], w=[L + "W"], q="dq_pool")
        ident = P.sb(L + "ident", [128, 128])
        P.dma(ident[:], I["ident"], w=[L + "ident"])
        tiles = []
        for ct in range(4):
            tiles.append(("gt", None, ct, C_GT + ct * 128, 128))
            tiles.append(("a", self.U, ct, C_A + ct * 128, 128))
        for i in range(3):
            tiles.append(("id", self.QD, i, C_QD + i * 128, 128))
        for i in range(2):
            tiles.append(("id", self.KVD, i, C_KVD + i * 128, 128))
        tiles.append(("id", self.KR, None, C_KR, 32))
        for i in range(12):
            tiles.append(("id", self.GQKV, i, C_GQ + i * 128, 128))
        for i in range(4):
            tiles.append(("silu", self.OG, i, C_OG + i * 128, 128))
        tiles.append(("sig", self.BETA, None, C_BETA, 16))
        tiles.append(("sp", self.GL, None, C_AR, 16))
        for i in range(24):
            tiles.append(("sig", self.MG, i, C_MG + i * 128, 128))
        nt_ = len(tiles)
        bcol = P.sb(L + "bcol", [128, nt_])
        for n_, (kind, dst, ti, c0, m) in enumerate(tiles):
            P.dma(bcol[0:m, n_:n_ + 1], I["b_in"][l, c0:c0 + m].rearrange("(p o) -> p o", o=1),
                  w=[L + "bcol"], slow=True)
        mcol = P.sb(L + "mcol", [128, 2, 48])
        for rr in range(2):
            load_col(P, mcol[:, rr, :], self.modrow[l][rr, :], w=[L + "mcol"])
        sc1p = P.sb(L + "sc1p", [128, 2, 8])
        P.ts(sc1p[:], mcol[:, :, 8:16], 1.0, ALU.add, r=[L + "mcol"], w=[L + "sc1p"])
        dtb = P.sb(L + "dtb", [16, 1])
        nega = P.sb(L + "nega", [16, 1])
        P.dma(dtb[:], I["dt_bias"][l, :].rearrange("(p o) -> p o", o=1), w=[L + "dtb"], slow=True)
        P.dma(nega[:], I["a_log"][l, :].rearrange("(p o) -> p o", o=1), w=[L + "nega"], slow=True)
        P.act(nega[:], nega[:], AF.Exp, r=[L + "nega"], w=[L + "nega"])
        P.ts(nega[:], nega[:], -1.0, ALU.mult, r=[L + "nega"], w=[L + "nega"])

        xt = [P.sb(L + f"xt{i}", [128, 4, D]) for i in range(2)]
        hT = [P.sb(L + f"hT{i}", [128, 8, 512], BF16) for i in range(2)]
        pT = [P.ps(L + f"pT{i}", [128, 512]) for i in range(2)]
        pp = [P.ps(L + f"pp{i}", [128, 512]) for i in range(4)]
        NS = 4
        stf = [P.sb(L + f"stf{i}", [128, 512]) for i in range(NS)]
        stb = [P.sb(L + f"stb{i}", [128, 512], BF16) for i in range(NS)]
        tmp = [P.sb(L + f"tmp{i}", [128, 512]) for i in range(2)]
        spz = P.sb(L + "spz", [16, 512])
        spa = P.sb(L + "spa", [16, 512])

        def load_x(bi):
            s, sz = BLOCKS[bi]
            nt = sz // 128
            src = xsrc_c if bi == 0 else xsrc_l[s - NCTX:s - NCTX + sz, :]
            P.dma(xt[bi % 2][:, 0:nt, :], src.rearrange("(t p) d -> p t d", p=128),
                  r=[xkey], w=[L + f"xt{bi % 2}"])

        load_x(0)
        cf = cb = cp = 0
        for bi, (s, sz) in enumerate(BLOCKS):
            if bi + 1 < len(BLOCKS):
                load_x(bi + 1)
            nt = sz // 128
            rr = 1 if bi == 0 else 0
            xb, hb = xt[bi % 2], hT[bi % 2]
            xk, hk = L + f"xt{bi % 2}", L + f"hT{bi % 2}"
            for j in range(8):
                pt = pT[j % 2]
                pk = L + f"pT{j % 2}"
                for t in range(nt):
                    P.tr(pt[:, t * 128:(t + 1) * 128], xb[:, t, j * 128:(j + 1) * 128], ident[:],
                         r=[xk, L + "ident"], w=[pk])
                P.ts(hb[:, j, 0:sz], pt[:, 0:sz], sc1p[:, rr, j:j + 1], ALU.mult,
                     mcol[:, rr, j:j + 1], ALU.add, r=[pk, L + "sc1p", L + "mcol"], w=[hk])
            for n_, (kind, dst, ti, c0, m) in enumerate(tiles):
                acc = pp[cp % 4]
                ak = L + f"pp{cp % 4}"
                cp += 1
                for k in range(8):
                    P.mm(acc[0:m, 0:sz], W[:, k, c0:c0 + m], hb[:, k, 0:sz], k == 0, k == 7,
                         r=[hk, L + "W"], w=[ak])
                bc = bcol[0:m, n_:n_ + 1]
                if kind == "gt":
                    tb = tmp[ti % 2]
                    P.act(tb[:, 0:sz], acc[:, 0:sz], AF.Sigmoid, bias=bc, r=[ak, L + "bcol"], w=[L + f"tmp{ti % 2}"])
                    continue
                if kind in ("silu", "sig") and m == 128:
                    st, sk = stb[cb % NS], L + f"stb{cb % NS}"
                    cb += 1
                else:
                    st, sk = stf[cf % NS], L + f"stf{cf % NS}"
                    cf += 1
                if kind == "a":
                    P.stt(st[:, 0:sz], acc[:, 0:sz], bc, tmp[ti % 2][:, 0:sz], ALU.add, ALU.mult,
                          r=[ak, L + "bcol", L + f"tmp{ti % 2}"], w=[sk])
                elif kind == "id":
                    if n_ % 2 == 0:
                        P.ts(st[0:m, 0:sz], acc[0:m, 0:sz], bc, ALU.add, r=[ak, L + "bcol"], w=[sk])
                    else:
                        P.act(st[0:m, 0:sz], acc[0:m, 0:sz], AF.Identity, bias=bc, r=[ak, L + "bcol"], w=[sk])
                elif kind == "silu":
                    P.act(st[0:m, 0:sz], acc[0:m, 0:sz], AF.Silu, bias=bc, r=[ak, L + "bcol"], w=[sk])
                elif kind == "sig":
                    P.act(st[0:m, 0:sz], acc[0:m, 0:sz], AF.Sigmoid, bias=bc, r=[ak, L + "bcol"], w=[sk])
                elif kind == "sp":
                    P.ts(spz[:, 0:sz], acc[0:16, 0:sz], bc, ALU.add, dtb[:, 0:1], ALU.add,
                         r=[ak, L + "bcol", L + "dtb"], w=[L + "spz"])
                    P.act(spa[:, 0:sz], spz[:, 0:sz], AF.Abs, r=[L + "spz"], w=[L + "spa"])
                    P.act(spa[:, 0:sz], spa[:, 0:sz], AF.Exp, scale=-1.0, r=[L + "spa"], w=[L + "spa"])
                    P.act(spa[:, 0:sz], spa[:, 0:sz], AF.Ln, bias=1.0, r=[L + "spa"], w=[L + "spa"])
                    P.stt(spz[:, 0:sz], spz[:, 0:sz], 0.0, spa[:, 0:sz], ALU.max, ALU.add,
                          r=[L + "spz", L + "spa"], w=[L + "spz"])
                    P.ts(st[0:16, 0:sz], spz[:, 0:sz], nega[:, 0:1], ALU.mult, r=[L + "spz", L + "nega"], w=[sk])
                d_ap = dst[ti, :, s:s + sz] if ti is not None else dst[:, s:s + sz]
                P.dma(d_ap, st[0:m, 0:sz], r=[sk], w=[("scr", id(dst))], q="dq_pool")
        P.end_phase()

    def phase2a(self, l):
        P, I = self.P, self.inp
        L = f"p2a_{l}_"
        P.begin_phase()
        cw = P.sb(L + "cw", [128, 4, 31])
        for ct in range(4):
            P.dma(cw[:, ct, :], I["conv_a_w"][l][:, ct * 128:(ct + 1) * 128].rearrange("k p -> p k"),
                  w=[L + "cw"], slow=True)
        cb = P.sb(L + "cb", [128, 4])
        lg = P.sb(L + "lg", [128, 4])
        lb = P.sb(L + "lb", [128, 4])
        load_col(P, cb[:], I["conv_a_b"][l], w=[L + "cb"])
        load_col(P, lg[:], I["ln_a_g"][l], w=[L + "lg"])
        load_col(P, lb[:], I["ln_a_b"][l], w=[L + "lb"])
        ones = P.sb(L + "ones", [128, 128])
        P.memset(ones[:], 1.0, w=[L + "ones"])
        v = P.sb(L + "v", [128, 4, T])
        UB = T + 60
        ub = [P.sb(L + f"ub{i}", [128, UB]) for i in range(2)]
        for i in range(2):
            P.memset(ub[i][:, 0:15], 0.0, w=[L + f"ub{i}"])
            P.memset(ub[i][:, 271:301], 0.0, w=[L + f"ub{i}"])
            P.memset(ub[i][:, UB - 15:UB], 0.0, w=[L + f"ub{i}"])
        for ct in range(4):
            u, uk = ub[ct % 2], L + f"ub{ct % 2}"
            P.dma(u[:, 15:271], self.U[ct, :, 0:NCTX], w=[uk])
            P.dma(u[:, 301:301 + NL], self.U[ct, :, NCTX:T], w=[uk])
            SPL = 2944
            for (o0, n_, base, eng_, sfx) in ((0, NCTX, 0, "dve", "c"), (NCTX, SPL, 286, "dve", "a"),
                                             (NCTX + SPL, NL - SPL, 286 + SPL, "dve", "b")):
                vk = L + f"v{ct}{sfx}"
                vo = v[:, ct, o0:o0 + n_]
                P.ts(vo, u[:, base:base + n_], cw[:, ct, 0:1], ALU.mult, cb[:, ct:ct + 1], ALU.add,
                     r=[uk, L + "cw", L + "cb"], w=[vk], eng=eng_)
                for k in range(1, 31):
                    P.stt(vo, u[:, base + k:base + k + n_], cw[:, ct, k:k + 1], vo, ALU.mult, ALU.add,
                          r=[uk, L + "cw"], w=[vk], eng=eng_)
        ps_s = [P.ps(L + f"ps_s{i}", [128, 512]) for i in range(2)]
        ps_q = [P.ps(L + f"ps_q{i}", [128, 512]) for i in range(2)]
        sq = [P.sb(L + f"sq{i}", [128, 4, 512]) for i in range(2)]
        mean = [P.sb(L + f"mean{i}", [128, 512]) for i in range(2)]
        rstd = [P.sb(L + f"rstd{i}", [128, 512]) for i in range(2)]
        xn = [P.sb(L + f"xn{i}", [128, 512]) for i in range(3)]
        so = [P.sb(L + f"so{i}", [128, 512], BF16) for i in range(3)]
        vkeys = [[L + f"v{ct}{sfx}" for sfx in "cab"] for ct in range(4)]
        cx = 0
        for bi, (s, sz) in enumerate(BLOCKS):
            i2 = bi % 2
            for ct in range(4):
                P.act(sq[i2][:, ct, 0:sz], v[:, ct, s:s + sz], AF.Square, r=vkeys[ct], w=[L + f"sq{i2}"])
            for ct in range(4):
                P.mm(ps_s[i2][:, 0:sz], ones[:], v[:, ct, s:s + sz], ct == 0, ct == 3,
                     r=vkeys[ct] + [L + "ones"], w=[L + f"ps_s{i2}"])
            for ct in range(4):
                P.mm(ps_q[i2][:, 0:sz], ones[:], sq[i2][:, ct, 0:sz], ct == 0, ct == 3,
                     r=[L + f"sq{i2}", L + "ones"], w=[L + f"ps_q{i2}"])
            m_, r_ = mean[i2], rstd[i2]
            mk, rk = L + f"mean{i2}", L + f"rstd{i2}"
            P.ts(m_[:, 0:sz], ps_s[i2][:, 0:sz], 1.0 / 512, ALU.mult, r=[L + f"ps_s{i2}"], w=[mk])
            P.tt(r_[:, 0:sz], m_[:, 0:sz], m_[:, 0:sz], ALU.mult, r=[mk], w=[rk])
            P.stt(r_[:, 0:sz], ps_q[i2][:, 0:sz], 1.0 / 512, r_[:, 0:sz], ALU.mult, ALU.subtract,
                  r=[L + f"ps_q{i2}", rk], w=[rk])
            P.rsqrt(r_[:, 0:sz], r_[:, 0:sz], EPS, r=[rk], w=[rk])
            for ct in range(4):
                x_, xk = xn[cx % 3], L + f"xn{cx % 3}"
                o_, ok = so[cx % 3], L + f"so{cx % 3}"
                cx += 1
                P.tt(x_[:, 0:sz], v[:, ct, s:s + sz], m_[:, 0:sz], ALU.subtract, r=vkeys[ct] + [mk], w=[xk])
                P.tt(x_[:, 0:sz], x_[:, 0:sz], r_[:, 0:sz], ALU.mult, r=[xk, rk], w=[xk])
                P.act(o_[:, 0:sz], x_[:, 0:sz], AF.Silu, scale=lg[:, ct:ct + 1], bias=lb[:, ct:ct + 1],
                      r=[xk, L + "lg", L + "lb"], w=[ok])
                P.dma(self.SA[ct, :, s:s + sz], o_[:, 0:sz], r=[ok], w=["SA"], q="dq_pool")
        P.end_phase()

    def phase2b1(self, l):
        P, I = self.P, self.inp
        L = f"p2b1_{l}_"
        P.begin_phase()
        ident = P.sb(L + "ident", [128, 128])
        P.dma(ident[:], I["ident"], w=[L + "ident"])
        perm = P.sb(L + "perm", [32, 32])
        P.dma(perm[:], I["perm32"], w=[L + "perm"])
        ones = P.sb(L + "ones", [128, 128])
        P.memset(ones[:], 1.0, w=[L + "ones"])
        SH = P.sb(L + "SH", [32, 96], BF16)
        P.memset(SH[:], 0.0, w=[L + "SH"])
        P.copy(SH[:, 64:96], ident[0:32, 0:32], r=[L + "ident", L + "SH"], w=[L + "SH"])
        gq = P.sb(L + "gq", [128, 3])
        gkv = P.sb(L + "gkv", [128, 2])
        load_col(P, gq[:], I["g_q"][l], w=[L + "gq"])
        load_col(P, gkv[:], I["g_kv"][l], w=[L + "gkv"])
        WK = P.sb(L + "WK", [128, 2, 8, 96], BF16)
        WQ = P.sb(L + "WQ", [128, 3, 8, 96], BF16)
        WQS = P.sb(L + "WQS", [128, 3, 8, 96], BF16)
        WV = P.sb(L + "WV", [128, 2, 512], BF16)
        P.memset(WK[:], 0.0, w=[L + "WK"])
        P.memset(WQS[:], 0.0, w=[L + "WQS"])
        for c in range(2):
            P.dma(WK[:, c, :, 0:64], I["w_uk"][l, c * 128:(c + 1) * 128, :].rearrange("p (h d) -> p h d", d=64),
                  r=[L + "WK"], w=[L + "WK"], q="dq_pool")
            P.dma(WV[:, c, :], I["w_uv"][l, c * 128:(c + 1) * 128, :], w=[L + "WV"], q="dq_pool")
        for c in range(3):
            P.dma(WQ[:, c, :, 0:64], I["w_uq"][l, c * 128:(c + 1) * 128, :].rearrange("p (h d) -> p h d", d=64),
                  w=[L + "WQ"], q="dq_pool")
            P.dma(WQ[:, c, :, 64:96], I["w_qr"][l, c * 128:(c + 1) * 128, :].rearrange("p (h d) -> p h d", d=32),
                  w=[L + "WQ"], q="dq_pool")
            src = I["w_qr"][l, c * 128:(c + 1) * 128, :].rearrange("p (h a f) -> p h a f", a=4, f=8)
            for a in range(4):
                P.dma(WQS[:, c, :, 64 + a * 8:64 + a * 8 + 8], src[:, :, a ^ 1, :],
                      r=[L + "WQS"], w=[L + "WQS"], q="dq_pool")
        vx = [P.sb(L + f"vx{i}", [128, 8, 128], BF16) for i in range(2)]
        for i in range(2):
            P.memset(vx[i][:], 1.0, w=[L + f"vx{i}"])
        xin = [P.sb(L + f"xin{i}", [128, 5, 512]) for i in range(2)]
        krin = [P.sb(L + f"krin{i}", [32, 512]) for i in range(2)]
        rope = [P.sb(L + f"rope{i}", [96, 2, 512]) for i in range(2)]
        sq = P.sb(L + "sq", [128, 5, 512])
        rs = P.sb(L + "rs", [128, 2, 512])
        cx = P.sb(L + "cx", [128, 5, 512], BF16)
        krr = P.sb(L + "krr", [32, 512], BF16)
        krt = P.sb(L + "krt", [32, 2, 512])
        ps_n = [P.ps(L + f"ps_n{i}", [128, 512]) for i in range(2)]
        ps_a = [P.ps(L + f"ps_a{i}", [128, 512]) for i in range(2)]
        ps_b = [P.ps(L + f"ps_b{i}", [128, 512]) for i in range(2)]
        ps_v = [P.ps(L + f"ps_v{i}", [128, 512]) for i in range(2)]
        ko = [P.sb(L + f"ko{i}", [96, 512], BF16) for i in range(3)]
        qo = [P.sb(L + f"qo{i}", [96, 512], BF16) for i in range(3)]
        qt = [P.sb(L + f"qt{i}", [96, 2, 512]) for i in range(2)]

        def load_blk(bi):
            s, sz = BLOCKS[bi]
            b = bi % 2
            P.dma(xin[b][:, 0:2, 0:sz], self.KVD[:, :, s:s + sz].rearrange("c p t -> p c t"), w=[L + f"xin{b}"])
            P.dma(xin[b][:, 2:5, 0:sz], self.QD[:, :, s:s + sz].rearrange("c p t -> p c t"), w=[L + f"xin{b}"])
            P.dma(krin[b][:, 0:sz], self.KR[:, s:s + sz], w=[L + f"krin{b}"])
            for ci in range(2):
                P.dma(rope[b][64:96, ci, 0:sz], I["rope_cs"][ci, :, s:s + sz], w=[L + f"rope{b}"])
                P.dma(rope[b][0:32, ci, 0:sz], I["rope_cs"][ci, :, s:s + sz], w=[L + f"rope{b}"])

        load_blk(0)
        ck = cq_ = cv = 0
        for bi, (s, sz) in enumerate(BLOCKS):
            if bi + 1 < len(BLOCKS):
                load_blk(bi + 1)
            b = bi % 2
            xb, xk = xin[b], L + f"xin{b}"
            rb, rk = rope[b], L + f"rope{b}"
            for c in range(5):
                P.act(sq[:, c, 0:sz], xb[:, c, 0:sz], AF.Square, r=[xk], w=[L + "sq"])
            for gi, (c0, c1, nf) in enumerate(((0, 2, 256.0), (2, 5, 384.0))):
                pn = ps_n[gi]
                for c in range(c0, c1):
                    P.mm(pn[:, 0:sz], ones[:], sq[:, c, 0:sz], c == c0, c == c1 - 1,
                         r=[L + "sq", L + "ones"], w=[L + f"ps_n{gi}"])
                P.rsqrt(rs[:, gi, 0:sz], pn[:, 0:sz], EPS, r=[L + f"ps_n{gi}"], w=[L + f"rs{gi}"], scale=1.0 / nf)
            for c in range(5):
                gi = 0 if c < 2 else 1
                gcol = gkv[:, c:c + 1] if c < 2 else gq[:, c - 2:c - 1]
                P.stt(cx[:, c, 0:sz], xb[:, c, 0:sz], gcol, rs[:, gi, 0:sz], ALU.mult, ALU.mult,
                      r=[xk, L + "gq", L + "gkv", L + f"rs{gi}"], w=[L + f"cx{c}"])
            ckeys_kv = [L + "cx0", L + "cx1"]
            ckeys_q = [L + "cx2", L + "cx3", L + "cx4"]
            kb, kk = krin[b], L + f"krin{b}"
            P.mm(ps_n[0][0:32, 0:sz], perm[:], kb[:, 0:sz], True, True, r=[kk, L + "perm", L + "rs0"], w=[L + "ps_n0"])
            P.tt(krt[:, 0, 0:sz], kb[:, 0:sz], rb[0:32, 0, 0:sz], ALU.mult, r=[kk, rk], w=[L + "krt0"])
            P.tt(krt[:, 1, 0:sz], ps_n[0][0:32, 0:sz], rb[0:32, 1, 0:sz], ALU.mult, r=[L + "ps_n0", rk], w=[L + "krt1"])
            P.tt(krr[:, 0:sz], krt[:, 0, 0:sz], krt[:, 1, 0:sz], ALU.add, r=[L + "krt0", L + "krt1"], w=[L + "krr"])
            for h in range(8):
                pa, pk = ps_a[ck % 2], L + f"ps_a{ck % 2}"
                o_, ok = ko[ck % 3], L + f"ko{ck % 3}"
                ck += 1
                for c in range(2):
                    P.mm(pa[0:96, 0:sz], WK[:, c, h, :], cx[:, c, 0:sz], c == 0, False,
                         r=[L + "WK", ckeys_kv[c]], w=[pk])
                P.mm(pa[0:96, 0:sz], SH[:], krr[:, 0:sz], False, True, r=[L + "SH", L + "krr"], w=[pk])
                if h % 2 == 0:
                    P.copy(o_[:, 0:sz], pa[0:96, 0:sz], r=[pk], w=[ok])
                else:
                    P.copy(o_[:, 0:sz], pa[0:96, 0:sz], r=[pk], w=[ok], eng="act")
                P.dma(self.KT[h, :, s:s + sz], o_[:, 0:sz], r=[ok], w=["KT"], q="dq_pool")
            for t in range(sz // 128):
                pv, pvk = ps_v[cv % 2], L + f"ps_v{cv % 2}"
                v_, vk = vx[cv % 2], L + f"vx{cv % 2}"
                cv += 1
                for c in range(2):
                    P.mm(pv[:, :], cx[:, c, t * 128:(t + 1) * 128], WV[:, c, :], c == 0, c == 1,
                         r=[L + "WV", ckeys_kv[c]], w=[pvk])
                pv4 = pv[:, :].rearrange("p (hp two d) -> p hp two d", two=2, d=64)
                vx4 = v_[:].rearrange("p (hp two) c -> p hp two c", two=2)
                P.copy(vx4[:, :, 0, 0:64], pv4[:, :, 0, :], r=[pvk], w=[vk])
                P.copy(vx4[:, :, 1, 64:128], pv4[:, :, 1, :], r=[pvk], w=[vk], eng="act")
                kt = (s + t * 128) // 128
                P.dma(self.VX[:, :, kt, :].rearrange("h p c -> p h c"), v_[:], r=[vk], w=["VX"], q="dq_pool")
            for h in range(8):
                pa, pk = ps_a[ck % 2], L + f"ps_a{ck % 2}"
                ck += 1
                pb, pbk = ps_b[cq_ % 2], L + f"ps_b{cq_ % 2}"
                o_, ok = qo[cq_ % 3], L + f"qo{cq_ % 3}"
                q_, qk = qt[cq_ % 2], L + f"qt{cq_ % 2}"
                cq_ += 1
                for c in range(3):
                    P.mm(pa[0:96, 0:sz], WQ[:, c, h, :], cx[:, 2 + c, 0:sz], c == 0, c == 2,
                         r=[L + "WQ", ckeys_q[c]], w=[pk])
                for c in range(3):
                    P.mm(pb[0:96, 0:sz], WQS[:, c, h, :], cx[:, 2 + c, 0:sz], c == 0, c == 2,
                         r=[L + "WQS", ckeys_q[c]], w=[pbk])
                P.act(o_[0:64, 0:sz], pa[0:64, 0:sz], AF.Copy, scale=MLA_SCALE, r=[pk], w=[ok])
                P.tt(q_[64:96, 0, 0:sz], pa[64:96, 0:sz], rb[64:96, 0, 0:sz], ALU.mult, r=[pk, rk], w=[qk])
                P.stt(q_[64:96, 1, 0:sz], pb[64:96, 0:sz], MLA_SCALE, rb[64:96, 1, 0:sz], ALU.mult, ALU.mult,
                      r=[pbk, rk], w=[qk])
                P.stt(o_[64:96, 0:sz], q_[64:96, 0, 0:sz], MLA_SCALE, q_[64:96, 1, 0:sz], ALU.mult, ALU.add,
                      r=[qk], w=[ok])
                P.dma(self.QT[h, :, s:s + sz], o_[:, 0:sz], r=[ok], w=["QT"], q="dq_pool")
        P.end_phase()

    def phase2b2(self, l):
        P, I = self.P, self.inp
        L = f"p2b2_{l}_"
        P.begin_phase()
        kT = [P.sb(L + f"kT{i}", [96, T], BF16) for i in range(2)]
        qT = [P.sb(L + f"qT{i}", [96, T], BF16) for i in range(2)]
        vx = [P.sb(L + f"vx{i}", [128, 34, 128], BF16) for i in range(2)]
        NP_ = 4
        pT = [P.sb(L + f"pT{i}", [128, 512], BF16) for i in range(NP_)]
        ps_s = [P.ps(L + f"ps_s{i}", [128, 512]) for i in range(4)]
        ps_o = [P.ps(L + f"ps_o{i}", [128, 512]) for i in range(2)]
        rec = [P.sb(L + f"rec{i}", [128, 512]) for i in range(2)]
        oo = [P.sb(L + f"oo{i}", [128, 512], BF16) for i in range(2)]

        def load_head(h):
            b = h % 2
            P.dma(kT[b][:], self.KT[h], w=[L + f"kT{b}"])
            P.dma(qT[b][:], self.QT[h], w=[L + f"qT{b}"])
            P.dma(vx[b][:], self.VX[h], w=[L + f"vx{b}"])

        load_head(0)
        LA = 2
        items = []
        for h in range(8):
            for bi, (s, sz) in enumerate(BLOCKS):
                nkt = 2 if bi == 0 else 34
                for kt in range(nkt):
                    items.append((h, bi, kt, nkt))
        loaded = {0}
        co_of = {}
        co = 0
        for h in range(8):
            for bi in range(len(BLOCKS)):
                co_of[(h, bi)] = co
                co += 1

        def emit_s(i):
            h, bi, kt, nkt = items[i]
            s, sz = BLOCKS[bi]
            b = h % 2
            ps, psk = ps_s[i % 4], L + f"ps_s{i % 4}"
            p_, pk = pT[i % NP_], L + f"pT{i % NP_}"
            P.mm(ps[:, 0:sz], kT[b][:, kt * 128:(kt + 1) * 128], qT[b][:, s:s + sz], True, True,
                 r=[L + f"kT{b}", L + f"qT{b}"], w=[psk])
            P.act(p_[:, 0:sz], ps[:, 0:sz], AF.Exp, r=[psk], w=[pk])

        def emit_pv(i):
            h, bi, kt, nkt = items[i]
            s, sz = BLOCKS[bi]
            if h + 1 < 8 and (h + 1) not in loaded and bi == 0 and kt == 0:
                loaded.add(h + 1)
                load_head(h + 1)
            b = h % 2
            c_ = co_of[(h, bi)]
            po, pok = ps_o[c_ % 2], L + f"ps_o{c_ % 2}"
            p_, pk = pT[i % NP_], L + f"pT{i % NP_}"
            P.mm(po[:, 0:sz], vx[b][:, kt, :], p_[:, 0:sz], kt == 0, kt == nkt - 1, r=[L + f"vx{b}", pk], w=[pok])
            if kt == nkt - 1:
                r_, rk = rec[c_ % 2], L + f"rec{c_ % 2}"
                o_, ok = oo[c_ % 2], L + f"oo{c_ % 2}"
                if h % 2 == 0:
                    num, den, dst = po[0:64, 0:sz], po[64:128, 0:sz], slice(0, 64)
                else:
                    num, den, dst = po[64:128, 0:sz], po[0:64, 0:sz], slice(64, 128)
                P.recip(r_[dst, 0:sz], den, r=[pok], w=[rk])
                P.tt(o_[dst, 0:sz], num, r_[dst, 0:sz], ALU.mult, r=[pok, rk], w=[ok])
                P.dma(self.OB[h // 2, dst, s:s + sz], o_[dst, 0:sz], r=[ok], w=["OB"], q="dq_pool")

        n_it = len(items)
        for i in range(n_it + LA):
            if i < n_it:
                emit_s(i)
            if i >= LA:
                emit_pv(i - LA)
        P.end_phase()

    def phase2c1(self, l):
        P, I = self.P, self.inp
        L = f"p2c1_{l}_"
        P.begin_phase()
        ident = P.sb(L + "ident", [128, 128])
        P.dma(ident[:], I["ident"], w=[L + "ident"])
        bones = P.sb(L + "bones", [128, 128])
        P.memset(bones[:], 0.0, w=[L + "bones"])
        P.memset(bones[0:64, 0:64], 1.0, w=[L + "bones"])
        P.memset(bones[64:128, 64:128], 1.0, w=[L + "bones"])
        cw = P.sb(L + "cw", [128, 12, 5])
        for i in range(12):
            P.dma(cw[:, i, :], I["conv_c_w"][l][:, i * 128:(i + 1) * 128].rearrange("k p -> p k"),
                  w=[L + "cw"], slow=True)
        UB = T + 8
        ub = [P.sb(L + f"ub{i}", [128, UB]) for i in range(2)]
        for i in range(2):
            P.memset(ub[i][:, 0:2], 0.0, w=[L + f"ub{i}"])
            P.memset(ub[i][:, 258:262], 0.0, w=[L + f"ub{i}"])
            P.memset(ub[i][:, UB - 2:UB], 0.0, w=[L + f"ub{i}"])
        xc = [P.sb(L + f"xc{i}", [128, T]) for i in range(2)]
        sq = [P.sb(L + f"sq{i}", [128, 512]) for i in range(2)]
        rn = [P.sb(L + f"rn{i}", [128, 512]) for i in range(2)]
        xo = [P.sb(L + f"xo{i}", [128, 512]) for i in range(3)]
        tk = [P.sb(L + f"tk{i}", [128, 4, 128]) for i in range(2)]
        ps_n = [P.ps(L + f"ps_n{i}", [128, 512]) for i in range(2)]
        ps_t = [P.ps(L + f"ps_t{i}", [128, 512]) for i in range(2)]
        cn = ct_ = cx_ = 0
        for i in range(12):
            u, uk = ub[i % 2], L + f"ub{i % 2}"
            x_, xk = xc[i % 2], L + f"xc{i % 2}"
            P.dma(u[:, 2:258], self.GQKV[i, :, 0:NCTX], w=[uk])
            P.dma(u[:, 262:262 + NL], self.GQKV[i, :, NCTX:T], w=[uk])
            SPL = 2944
            for (o0, n_, base, eng_, sfx) in ((0, NCTX, 0, "dve", "c"), (NCTX, SPL, 260, "dve", "a"),
                                             (NCTX + SPL, NL - SPL, 260 + SPL, "dve", "b")):
                vo = x_[:, o0:o0 + n_]
                P.ts(vo, u[:, base:base + n_], cw[:, i, 0:1], ALU.mult, r=[uk, L + "cw"], w=[xk], eng=eng_)
                for k in range(1, 5):
                    P.stt(vo, u[:, base + k:base + k + n_], cw[:, i, k:k + 1], vo, ALU.mult, ALU.add,
                          r=[uk, L + "cw"], w=[xk], eng=eng_)
            P.act(x_[:], x_[:], AF.Silu, r=[xk], w=[xk])
            for bi, (s, sz) in enumerate(BLOCKS):
                if i < 8:
                    j = cn % 2
                    cn += 1
                    o_, ok = xo[cx_ % 3], L + f"xo{cx_ % 3}"
                    cx_ += 1
                    P.act(sq[j][:, 0:sz], x_[:, s:s + sz], AF.Square, r=[xk], w=[L + f"sq{j}"])
                    P.mm(ps_n[j][:, 0:sz], bones[:], sq[j][:, 0:sz], True, True,
                         r=[L + "bones", L + f"sq{j}"], w=[L + f"ps_n{j}"])
                    P.rsqrt(rn[j][:, 0:sz], ps_n[j][:, 0:sz], EPS, r=[L + f"ps_n{j}"], w=[L + f"rn{j}"])
                    if i < 4:
                        P.stt(o_[:, 0:sz], x_[:, s:s + sz], 0.125, rn[j][:, 0:sz], ALU.mult, ALU.mult,
                              r=[xk, L + f"rn{j}"], w=[ok])
                    else:
                        P.tt(o_[:, 0:sz], x_[:, s:s + sz], rn[j][:, 0:sz], ALU.mult, r=[xk, L + f"rn{j}"], w=[ok])
                    P.dma(self.QKN[i, :, s:s + sz], o_[:, 0:sz], r=[ok], w=["QKN"], q="dq_pool")
                    src, sk = o_, ok
                    soff = 0
                else:
                    src, sk = x_, xk
                    soff = s
                if i >= 4:
                    j = ct_ % 2
                    ct_ += 1
                    nt = sz // 128
                    for t in range(nt):
                        P.tr(ps_t[j][:, t * 128:(t + 1) * 128], src[:, soff + t * 128:soff + (t + 1) * 128], ident[:],
                             r=[sk, L + "ident"], w=[L + f"ps_t{j}"])
                    P.copy(tk[j][:, 0:nt, :], ps_t[j][:, 0:sz].rearrange("p (t f) -> p t f", f=128),
                           r=[L + f"ps_t{j}"], w=[L + f"tk{j}"], eng="act")
                    kv = 0 if i < 8 else 1
                    f0 = ((i - 4) % 4) * 128
                    P.dma(self.KVtok[s:s + sz, kv, f0:f0 + 128].rearrange("(t p) f -> p t f", p=128),
                          tk[j][:, 0:nt, :], r=[L + f"tk{j}"], w=["KVtok"], q="dq_pool")
        bg = [P.sb(L + f"bg{i}", [16, 2, 512]) for i in range(2)]
        bo = [P.sb(L + f"bo{i}", [128, 4, 32]) for i in range(2)]
        for bi, (s, sz) in enumerate(BLOCKS):
            j = bi % 2
            nt = sz // 128
            P.dma(bg[j][:, 0, 0:sz], self.BETA[:, s:s + sz], w=[L + f"bg{j}"])
            P.dma(bg[j][:, 1, 0:sz], self.GL[:, s:s + sz], w=[L + f"bg{j}"])
            for t in range(nt):
                for z in range(2):
                    P.tr(ps_t[j][:, t * 32 + z * 16:t * 32 + z * 16 + 16], bg[j][:, z, t * 128:(t + 1) * 128],
                         ident[0:16, 0:16], r=[L + f"bg{j}", L + "ident"], w=[L + f"ps_t{j}"])
            P.copy(bo[j][:, 0:nt, :], ps_t[j][:, 0:nt * 32].rearrange("p (t f) -> p t f", f=32),
                   r=[L + f"ps_t{j}"], w=[L + f"bo{j}"])
            P.dma(self.BGtok[s:s + sz, :].rearrange("(t p) f -> p t f", p=128), bo[j][:, 0:nt, :],
                  r=[L + f"bo{j}"], w=["BGtok"], q="dq_pool")
        P.end_phase()

    @staticmethod
    def gdn_chunk(n, d):
        return n if d == 0 else (3 - n if n < 4 else 71 - n)

    def phase2c2(self, l, nsteps=68):
        P, I = self.P, self.inp
        L = f"p2c2_{l}_"
        P.begin_phase()
        ident = P.sb(L + "ident", [128, 128])
        P.dma(ident[:], I["ident"], w=[L + "ident"])
        msk = P.sb(L + "msk", [128, 2, 64])
        P.dma(msk[0:64, 0, :], I["gmask"][2], w=[L + "msk"])
        P.dma(msk[0:64, 1, :], I["gmask"][1], w=[L + "msk"])
        P.dma(msk[64:128, 0, :], I["gmask"][0], w=[L + "msk"])
        P.dma(msk[64:128, 1, :], I["gmask"][3], w=[L + "msk"])
        ones = P.sb(L + "ones", [128, 64])
        P.memset(ones[:], 1.0, w=[L + "ones"])
        idb = P.sb(L + "idb", [128, 64])
        P.copy(idb[0:64, :], ident[0:64, 0:64], r=[L + "ident"], w=[L + "idb"])
        P.copy(idb[64:128, :], ident[64:128, 64:128], r=[L + "ident"], w=[L + "idb"])
        CK = [L + "ident", L + "msk", L + "ones", L + "idb"]

        def bc_u(ap2):
            return ap2.unsqueeze(2).broadcast_to([ap2.shape[0], 8, 64])

        def bc_m(mi, sl):
            n_ = sl.stop - sl.start
            return msk[sl, mi, :].unsqueeze(1).broadcast_to([n_, 8, 64])

        def v3(ap):
            return ap.rearrange("p (u f) -> p u f", f=64)

        X = {}
        for nm, shp, dt in (("bgt", [128, 32], F32), ("ktok", [128, 512], F32), ("vtok", [128, 512], F32),
                            ("kq", [128, 8, 128], F32)):
            X[nm] = [P.sb(L + f"{nm}_{b}", shp, dt) for b in range(2)]
        for nm, shp, dt in (("gam", [128, 8], F32), ("gtot", [128, 8], F32), ("eg", [128, 8], F32),
                            ("bgv", [128, 8], F32), ("dif", [128, 8], F32), ("ekd", [128, 8], F32),
                            ("gcP", [128, 8], F32), ("nbeta", [128, 8], F32), ("beta", [128, 8], F32),
                            ("g", [128, 8], F32),
                            ("Y", [128, 512], F32), ("diff", [128, 512], F32), ("E", [128, 512], F32),
                            ("ET", [128, 512], F32), ("egr", [128, 512], F32), ("Dst", [128, 512], F32),
                            ("DinT", [128, 512], F32), ("A0", [128, 512], F32), ("B0", [128, 512], F32),
                            ("R", [128, 512], F32), ("Pa", [128, 512], F32), ("PTa", [128, 512], F32),
                            ("Pb", [128, 512], F32), ("PTb", [128, 512], F32),
                            ("pmT", [128, 512], BF16), ("bgK", [128, 512], BF16), ("betaV", [128, 512], BF16),
                            ("kd", [128, 512], BF16), ("TTb", [128, 512], BF16), ("wT", [128, 512], BF16),
                            ("u0", [128, 512], F32), ("qgT", [128, 512], BF16)):
            X[nm] = P.sb(L + nm, shp, dt)
        PS = {d: [P.ps(L + f"ps{i}_{d}", [128, 512]) for i in range(4)] for d in range(2)}
        HS = (slice(0, 64), slice(64, 128))
        ALLP = slice(0, 128)

        def K_(d, nm):
            return L + f"{nm}@{d}"

        def KB(nm):
            return [K_(0, nm), K_(1, nm)]

        def loads(n):
            b = n % 2
            qkn = self.QKN.rearrange("t p c -> (t p) c")
            for d in range(2):
                h_ = HS[d]
                c = self.gdn_chunk(n, d)
                t0 = 64 * c
                P.dma(X["bgt"][b][h_, :], self.BGtok[t0:t0 + 64, :], w=[K_(d, f"bgt{b}")])
                P.dma(X["ktok"][b][h_, :], self.KVtok[t0:t0 + 64, 0, :], w=[K_(d, f"ktok{b}")])
                P.dma(X["vtok"][b][h_, :], self.KVtok[t0:t0 + 64, 1, :], w=[K_(d, f"vtok{b}")])
                P.dma(X["kq"][b][h_, :, 64:128], qkn[0:512, t0:t0 + 64].rearrange("(u p) c -> p u c", p=64),
                      w=[K_(d, f"kq{b}")])
                P.dma(X["kq"][b][h_, :, 0:64], qkn[512:1024, t0:t0 + 64].rearrange("(u p) c -> p u c", p=64),
                      w=[K_(d, f"kq{b}")])

        def both(fn):
            for d in range(2):
                fn(d, HS[d])

        def step(n):
            b = n % 2
            bgt, ktok, vtok, kq = (X[nm][b] for nm in ("bgt", "ktok", "vtok", "kq"))
            qT = kq[:, :, 64:128]
            kbb = lambda nm: KB(f"{nm}{b}")
            def s0(d, h_):
                P.copy(X["beta"][h_, :], bgt[h_, d * 8:d * 8 + 8], r=[K_(d, f"bgt{b}")], w=[K_(d, "beta")], eng="pool")
                P.copy(X["g"][h_, :], bgt[h_, 16 + d * 8:16 + d * 8 + 8], r=[K_(d, f"bgt{b}")], w=[K_(d, "g")],
                       eng="pool")
            both(s0)
            def s1(d, h_):
                ps0 = PS[d][0]
                P.mm(ps0[h_, 0:8], msk[h_, 0, :], X["g"][h_, :], True, True, r=[K_(d, "g")] + CK, w=[L + f"ps0_{d}"])
                P.mm(ps0[h_, 8:16], ones[h_, :], X["g"][h_, :], True, True, r=[K_(d, "g")] + CK, w=[L + f"ps0_{d}"])
                P.copy(X["gam"][h_, :], ps0[h_, 0:8], r=[L + f"ps0_{d}"], w=[K_(d, "gam")], eng="act")
                P.copy(X["gtot"][h_, :], ps0[h_, 8:16], r=[L + f"ps0_{d}"], w=[K_(d, "gtot")], eng="act")
            both(s1)
            P.act(X["eg"][:], X["gam"][:], AF.Exp, r=KB("gam"), w=KB("eg"))
            P.tt(X["bgv"][:], X["eg"][:], X["beta"][:], ALU.mult, r=KB("eg") + KB("beta"), w=KB("bgv"))
            P.tt(X["dif"][:], X["gtot"][:], X["gam"][:], ALU.subtract, r=KB("gtot") + KB("gam"), w=KB("dif"))
            P.act(X["ekd"][:], X["dif"][:], AF.Exp, r=KB("dif"), w=KB("ekd"))
            P.act(X["gcP"][:], X["gtot"][:], AF.Exp, r=KB("gtot"), w=KB("gcP"))
            P.ts(X["nbeta"][:], X["beta"][:], -1.0, ALU.mult, r=KB("beta"), w=KB("nbeta"))
            P.tt(v3(X["Y"][:]), bc_u(X["g"][:]), bc_m(0, ALLP), ALU.mult, r=KB("g") + CK, w=KB("Y"), eng="pool")
            def s2(d, h_):
                ps1, ps2, ps3 = PS[d][1], PS[d][2], PS[d][3]
                P.mm(ps1[h_, :], ones[h_, :], X["Y"][h_, :], True, True, r=[K_(d, "Y")] + CK, w=[L + f"ps1_{d}"])
                for u in range(8):
                    bank = ps2 if u < 4 else ps3
                    bk = L + (f"ps2_{d}" if u < 4 else f"ps3_{d}")
                    P.mm(bank[h_, (u % 4) * 128:(u % 4 + 1) * 128], kq[h_, u, 0:64], kq[h_, u, :], True, True,
                         r=[K_(d, f"kq{b}")], w=[bk])
                P.tt(v3(X["diff"][h_, :]), bc_u(X["gam"][h_, :]), v3(ps1[h_, :]), ALU.subtract,
                     r=[K_(d, "gam"), L + f"ps1_{d}"], w=[K_(d, "diff")])
                P.act(X["egr"][h_, :], ps1[h_, :], AF.Exp, r=[L + f"ps1_{d}"], w=[K_(d, "egr")])
            both(s2)
            P.act(X["E"][:], X["diff"][:], AF.Exp, r=KB("diff"), w=KB("E"))
            P.act(X["ET"][:], X["diff"][:], AF.Exp, scale=-1.0, r=KB("diff"), w=KB("ET"))
            P.tt(v3(X["Dst"][:]), v3(X["E"][:]), bc_m(1, ALLP), ALU.min, r=KB("E") + CK, w=KB("Dst"))
            P.tt(v3(X["DinT"][:]), v3(X["ET"][:]), bc_m(0, ALLP), ALU.min, r=KB("ET") + CK, w=KB("DinT"))
            P.tt(v3(X["bgK"][:]), v3(ktok[:]), bc_u(X["bgv"][:]), ALU.mult, r=kbb("ktok") + KB("bgv"), w=KB("bgK"),
                 eng="pool")
            P.tt(v3(X["betaV"][:]), v3(vtok[:]), bc_u(X["beta"][:]), ALU.mult, r=kbb("vtok") + KB("beta"),
                 w=KB("betaV"), eng="pool")
            P.tt(v3(X["kd"][:]), v3(ktok[:]), bc_u(X["ekd"][:]), ALU.mult, r=kbb("ktok") + KB("ekd"), w=KB("kd"),
                 eng="pool")
            P.tt(v3(X["qgT"][:]), qT, v3(X["egr"][:]), ALU.mult, r=kbb("kq") + KB("egr"), w=KB("qgT"), eng="pool")
            def s3(d, h_):
                ps2, ps3 = PS[d][2], PS[d][3]
                for hb_, bank in enumerate((ps2, ps3)):
                    bk = L + (f"ps2_{d}" if hb_ == 0 else f"ps3_{d}")
                    b4 = bank[h_, :].rearrange("p (u t f) -> p u t f", t=2, f=64)
                    csl = slice(hb_ * 256, (hb_ + 1) * 256)
                    v4 = lambda ap: ap.rearrange("p (u f) -> p u f", f=64)
                    P.tt(v4(X["A0"][h_, csl]), b4[:, :, 0, :], v4(X["Dst"][h_, csl]), ALU.mult,
                         r=[bk, K_(d, "Dst")], w=[K_(d, "A0")])
                    P.tt(v4(X["pmT"][h_, csl]), b4[:, :, 1, :], v4(X["DinT"][h_, csl]), ALU.mult,
                         r=[bk, K_(d, "DinT")], w=[K_(d, "pmT")])
            both(s3)
            P.tt(v3(X["A0"][:]), v3(X["A0"][:]), bc_u(X["nbeta"][:]), ALU.mult, r=KB("A0") + KB("nbeta"), w=KB("A0"),
                 eng="pool")
            def s4(d, h_):
                ps1 = PS[d][1]
                for u in range(8):
                    sl = slice(u * 64, (u + 1) * 64)
                    P.mm(ps1[h_, sl], X["A0"][h_, sl], idb[h_, :], True, True, r=[K_(d, "A0")] + CK, w=[L + f"ps1_{d}"])
                P.copy(X["B0"][h_, :], ps1[h_, :], r=[L + f"ps1_{d}"], w=[K_(d, "B0")], eng="act")
            both(s4)
            P.tt(v3(X["R"][:]), v3(X["B0"][:]), idb[:, :].unsqueeze(1).broadcast_to([128, 8, 64]), ALU.add,
                 r=KB("B0") + CK, w=KB("R"))
            names = [("B0", "A0"), ("Pa", "PTa"), ("Pb", "PTb")]
            for lev in range(6):
                pn, ptn = names[0] if lev == 0 else names[1 + (lev - 1) % 2]
                qn, qtn = names[1 + lev % 2]
                need_p, need_pt = lev < 4, lev < 5
                def sl_(d, h_):
                    ps1, ps2, ps3 = PS[d][1], PS[d][2], PS[d][3]
                    if lev >= 1:
                        for u in range(8):
                            sl = slice(u * 64, (u + 1) * 64)
                            P.mm(ps1[h_, sl], X[ptn][h_, sl], X["R"][h_, sl], True, True,
                                 r=[K_(d, ptn), K_(d, "R")], w=[L + f"ps1_{d}"])
                    if need_p:
                        for u in range(8):
                            sl = slice(u * 64, (u + 1) * 64)
                            P.mm(ps2[h_, sl], X[ptn][h_, sl], X[pn][h_, sl], True, True,
                                 r=[K_(d, pn), K_(d, ptn)], w=[L + f"ps2_{d}"])
                    if need_pt:
                        for u in range(8):
                            sl = slice(u * 64, (u + 1) * 64)
                            P.mm(ps3[h_, sl], X[pn][h_, sl], X[ptn][h_, sl], True, True,
                                 r=[K_(d, pn), K_(d, ptn)], w=[L + f"ps3_{d}"])
                both(sl_)
                def se_(d, h_):
                    ps1, ps2, ps3 = PS[d][1], PS[d][2], PS[d][3]
                    if lev >= 1:
                        P.tt(X["R"][h_, :], X["R"][h_, :], ps1[h_, :], ALU.add, r=[K_(d, "R"), L + f"ps1_{d}"],
                             w=[K_(d, "R")])
                    if need_p:
                        P.copy(X[qn][h_, :], ps2[h_, :], r=[L + f"ps2_{d}"], w=[K_(d, qn)], eng="act")
                    if need_pt:
                        P.copy(X[qtn][h_, :], ps3[h_, :], r=[L + f"ps3_{d}"], w=[K_(d, qtn)], eng="act")
                both(se_)
            P.copy(X["TTb"][:], X["R"][:], r=KB("R"), w=KB("TTb"), eng="pool")
            def s5(d, h_):
                ps2, ps3 = PS[d][2], PS[d][3]
                for u in range(8):
                    sl = slice(u * 64, (u + 1) * 64)
                    P.mm(ps3[h_, sl], X["TTb"][h_, sl], X["betaV"][h_, sl], True, True,
                         r=[K_(d, "betaV"), K_(d, "TTb")], w=[L + f"ps3_{d}"])
                    P.mm(ps2[h_, sl], X["bgK"][h_, sl], X["TTb"][h_, sl], True, True,
                         r=[K_(d, "bgK"), K_(d, "TTb")], w=[L + f"ps2_{d}"])
                P.copy(X["wT"][h_, :], ps2[h_, :], r=[L + f"ps2_{d}"], w=[K_(d, "wT")], eng="act")
                P.copy(X["u0"][h_, :], ps3[h_, :], r=[L + f"ps3_{d}"], w=[K_(d, "u0")])
                P.dma(self.PW[n, d], X["wT"][h_, :], r=[K_(d, "wT")], w=["PW"], q="dq_pool")
                P.dma(self.PQ[n, d], X["qgT"][h_, :], r=[K_(d, "qgT")], w=["PQ"], q="dq_pool")
                P.dma(self.PK[n, d], X["kd"][h_, :], r=[K_(d, "kd")], w=["PK"], q="dq_pool")
                P.dma(self.PM[n, d], X["pmT"][h_, :], r=[K_(d, "pmT")], w=["PM"], q="dq_pool")
                P.dma(self.PU[n, d], X["u0"][h_, :], r=[K_(d, "u0")], w=["PU"], q="dq_pool")
                P.dma(self.PG[n, d], X["gcP"][h_, :], r=[K_(d, "gcP")], w=["PG"], q="dq_pool")
            both(s5)

        loads(0)
        for n in range(nsteps):
            if n + 1 < nsteps:
                loads(n + 1)
            step(n)
        P.end_phase()

    def phase2c3(self, l, nsteps=68):
        P, I = self.P, self.inp
        L = f"p2c3_{l}_"
        P.begin_phase()
        NB = 3
        IN = {}
        for nm, dt in (("wT", BF16), ("qgT", BF16), ("kd", BF16), ("pmT", BF16), ("u0", F32)):
            IN[nm] = [P.sb(L + f"{nm}{b}", [64, 2, 512], dt) for b in range(NB)]
        IN["gc"] = [P.sb(L + f"gc{b}", [64, 2, 8], F32) for b in range(NB)]
        SRC = dict(wT=self.PW, qgT=self.PQ, kd=self.PK, pmT=self.PM, u0=self.PU, gc=self.PG)
        S = P.sb(L + "S", [64, 16, 64])
        Sb = P.sb(L + "Sb", [64, 16, 64], BF16)
        u = P.sb(L + "u", [64, 2, 512], BF16)
        osb = [P.sb(L + f"osb{i}", [64, 2, 512]) for i in range(2)]
        ps_u = [P.ps(L + f"ps_u{d}", [128, 512]) for d in range(2)]
        ps_o = [P.ps(L + f"ps_o{d}", [128, 512]) for d in range(2)]
        ps_S = [P.ps(L + f"ps_S{d}", [128, 512]) for d in range(2)]
        P.memset(S[:], 0.0, w=[L + "S0", L + "S1"])
        P.memset(Sb[:], 0.0, w=[L + "Sb0", L + "Sb1"])

        def loads(n):
            b = n % NB
            for nm in ("wT", "qgT", "kd", "pmT", "u0", "gc"):
                for d in range(2):
                    P.dma(IN[nm][b][:, d, :], SRC[nm][n, d], w=[L + f"{nm}{b}_{d}"])

        loads(0)
        if nsteps > 1:
            loads(1)
        for n in range(nsteps):
            if n + 2 < nsteps:
                loads(n + 2)
            b = n % NB
            kin = lambda nm, d: L + f"{nm}{b}_{d}"
            wT, qgT, kd, pmT, u0, gc = (IN[nm][b] for nm in ("wT", "qgT", "kd", "pmT", "u0", "gc"))
            ob, obk = osb[n % 2], L + f"osb{n % 2}"
            for d in range(2):
                for h in range(8):
                    sl = slice(h * 64, (h + 1) * 64)
                    P.mm(ps_u[d][0:64, sl], wT[:, d, sl], Sb[:, d * 8 + h, :], True, True,
                         r=[kin("wT", d), L + f"Sb{d}"], w=[L + f"ps_u{d}"])
            for d in range(2):
                P.tt(u[:, d, :], u0[:, d, :], ps_u[d][0:64, :], ALU.subtract,
                     r=[kin("u0", d), L + f"ps_u{d}"], w=[L + f"u{d}"])
            for d in range(2):
                for h in range(8):
                    sl = slice(h * 64, (h + 1) * 64)
                    P.mm(ps_o[d][0:64, sl], qgT[:, d, sl], Sb[:, d * 8 + h, :], True, False,
                         r=[kin("qgT", d), L + f"Sb{d}"], w=[L + f"ps_o{d}"])
                    P.mm(ps_o[d][0:64, sl], pmT[:, d, sl], u[:, d, sl], False, True,
                         r=[kin("pmT", d), L + f"u{d}"], w=[L + f"ps_o{d}"])
                for h in range(8):
                    sl = slice(h * 64, (h + 1) * 64)
                    P.mm(ps_S[d][0:64, sl], kd[:, d, sl], u[:, d, sl], True, True,
                         r=[kin("kd", d), L + f"u{d}"], w=[L + f"ps_S{d}"])
            for d in range(2):
                Sd = S[:, d * 8:(d + 1) * 8, :]
                P.tt(Sd, Sd, gc[:, d, :].unsqueeze(2).broadcast_to([64, 8, 64]), ALU.mult,
                     r=[kin("gc", d), L + f"S{d}"], w=[L + f"S{d}"])
                P.tt(Sd, Sd, ps_S[d][0:64, :].rearrange("p (u f) -> p u f", f=64), ALU.add,
                     r=[L + f"ps_S{d}", L + f"S{d}"], w=[L + f"S{d}"])
                P.copy(Sb[:, d * 8:(d + 1) * 8, :], Sd, r=[L + f"S{d}"], w=[L + f"Sb{d}"], eng="act")
                P.copy(ob[:, d, :], ps_o[d][0:64, :], r=[L + f"ps_o{d}"], w=[obk + f"_{d}"], eng="act")
                c = self.gdn_chunk(n, d)
                P.dma(self.ODIR[d, c * 64:(c + 1) * 64, :], ob[:, d, :], r=[obk + f"_{d}"], w=["ODIR"], q="dq_pool")
        P.end_phase()

    def phase2c4(self, l):
        P, I = self.P, self.inp
        L = f"p2c4_{l}_"
        P.begin_phase()
        ident = P.sb(L + "ident", [128, 128])
        P.dma(ident[:], I["ident"], w=[L + "ident"])
        go = P.sb(L + "go", [128, 1])
        for hh in range(2):
            P.dma(go[hh * 64:(hh + 1) * 64, :], I["g_o"][l, :].rearrange("(p o) -> p o", o=1), w=[L + "go"], slow=True)
        oin = [P.sb(L + f"oin{i}", [128, 2, 4, 512]) for i in range(2)]
        ogin = [P.sb(L + f"ogin{i}", [128, 4, 512], BF16) for i in range(2)]
        osum = P.sb(L + "osum", [128, 4, 512])
        sq = P.sb(L + "sq", [128, 512])
        ms = P.sb(L + "ms", [128, 4, 8])
        ps_t = [P.ps(L + f"ps_t{i}", [128, 512]) for i in range(2)]
        oc = [P.sb(L + f"oc{i}", [128, 512], BF16) for i in range(3)]

        def loads(bi):
            s, sz = BLOCKS[bi]
            nt = sz // 128
            b = bi % 2
            for d in range(2):
                P.dma(oin[b][:, d, 0:nt, :], self.ODIR[d, s:s + sz, :].rearrange("(t p) f -> p t f", p=128),
                      w=[L + f"oin{b}"])
            P.dma(ogin[b][:, :, 0:sz], self.OG[:, :, s:s + sz].rearrange("c p t -> p c t"), w=[L + f"ogin{b}"])

        loads(0)
        ct_ = co = 0
        for bi, (s, sz) in enumerate(BLOCKS):
            if bi + 1 < len(BLOCKS):
                loads(bi + 1)
            b = bi % 2
            nt = sz // 128
            P.tt(osum[:, 0:nt, :], oin[b][:, 0, 0:nt, :], oin[b][:, 1, 0:nt, :], ALU.add,
                 r=[L + f"oin{b}"], w=[L + "osum"], eng="pool")
            for t in range(nt):
                P.act(sq[:], osum[:, t, :], AF.Square, r=[L + "osum"], w=[L + "sq"])
                P.op("dve", (lambda t=t: self.P.nc.vector.tensor_reduce(
                    out=ms[:, t, :], in_=sq[:].rearrange("p (h f) -> p h f", f=64), axis=AX.X, op=ALU.add)),
                    r=[L + "sq"], w=[L + "ms"])
            P.rsqrt(ms[:, 0:nt, :], ms[:, 0:nt, :], EPS, r=[L + "ms"], w=[L + "ms"], scale=1.0 / 64)
            for t in range(nt):
                o3 = osum[:, t, :].rearrange("p (h f) -> p h f", f=64)
                P.tt(o3, o3, ms[:, t, :].unsqueeze(2).broadcast_to([128, 8, 64]), ALU.mult,
                     r=[L + "osum", L + "ms"], w=[L + "osum"])
            for ft in range(4):
                pt, ptk = ps_t[ct_ % 2], L + f"ps_t{ct_ % 2}"
                ct_ += 1
                for t in range(nt):
                    P.tr(pt[:, t * 128:(t + 1) * 128], osum[:, t, ft * 128:(ft + 1) * 128], ident[:],
                         r=[L + "osum", L + "ident"], w=[ptk])
                o_, ok = oc[co % 3], L + f"oc{co % 3}"
                co += 1
                P.stt(o_[:, 0:sz], pt[:, 0:sz], go[:, 0:1], ogin[b][:, ft, 0:sz], ALU.mult, ALU.mult,
                      r=[ptk, L + "go", L + f"ogin{b}"], w=[ok])
                P.dma(self.OC[ft, :, s:s + sz], o_[:, 0:sz], r=[ok], w=["OC"], q="dq_pool")
        P.end_phase()

    def bcast_row(self, dst, src_row, w):
        self.P.dma(dst, src_row.broadcast_to([128, src_row.shape[-1]]), w=w, slow=True)

    def phase3(self, l, xsrc_l, xsrc_c):
        P, I = self.P, self.inp
        L = f"p3_{l}_"
        P.begin_phase()
        ident = P.sb(L + "ident", [128, 128])
        P.dma(ident[:], I["ident"], w=[L + "ident"])
        WA = P.sb(L + "WA", [128, 4, D], BF16)
        WB = P.sb(L + "WB", [128, 4, D], BF16)
        WC = P.sb(L + "WC", [128, 4, D], BF16)
        WO = P.sb(L + "WO", [128, 8, D], BF16)
        for wt, nm, kc in ((WA, "w_a_out", 4), (WB, "w_b_out", 4), (WC, "w_c_out", 4), (WO, "w_out", 8)):
            for c in range(kc):
                P.dma(wt[:, c, :], I[nm][l, c * 128:(c + 1) * 128, :], w=[L + "W"], q="dq_pool")
        Wr = P.sb(L + "Wr", [128, 8, 36])
        P.dma(Wr[:, :, 0:4], I["w_rg"][l].rearrange("(k p) n -> p k n", p=128), w=[L + "Wr"], slow=True)
        P.dma(Wr[:, :, 4:36], I["w_re"][l].rearrange("(k p) n -> p k n", p=128), w=[L + "Wr"], slow=True)
        br = P.sb(L + "br", [128, 36])
        self.bcast_row(br[:, 0:4], I["b_rg"][l:l + 1, :], [L + "br"])
        self.bcast_row(br[:, 4:36], I["b_re"][l:l + 1, :], [L + "br"])
        rows = P.sb(L + "rows", [128, 2, 3, D])
        for rr in range(2):
            for i_, v_ in enumerate((2, 4, 3)):
                self.bcast_row(rows[:, rr, i_, :], self.modrow[l][rr:rr + 1, v_ * D:(v_ + 1) * D], [L + "rows"])
        P.ts(rows[:, :, 1, :], rows[:, :, 1, :], 1.0, ALU.add, r=[L + "rows"], w=[L + "rows"])
        lnr = P.sb(L + "lnr", [128, 2, D])
        self.bcast_row(lnr[:, 0, :], I["ln1_g"][l:l + 1, :], [L + "lnr"])
        self.bcast_row(lnr[:, 1, :], I["ln1_b"][l:l + 1, :], [L + "lnr"])
        CK = [L + "W", L + "Wr", L + "br", L + "rows", L + "lnr", L + "ident"]

        sa = [P.sb(L + f"sa{i}", [128, 3, 4, 512], BF16) for i in range(2)]
        mg0 = P.sb(L + "mg0", [128, 24, 512], BF16)
        mg = [mg0, mg0]
        xt0 = P.sb(L + "xt0", [128, 4, D])
        xt = [xt0, xt0]
        m = P.sb(L + "m", [128, 8, 512], BF16)
        t1 = P.sb(L + "t1", [128, 512])
        t2 = P.sb(L + "t2", [128, 512])
        z = P.sb(L + "z", [128, D])
        zz = P.sb(L + "zz", [128, D])
        x1 = [P.sb(L + f"x1{i}", [128, D]) for i in range(2)]
        h2 = P.sb(L + "h2", [128, D])
        h2Tf = P.sb(L + "h2Tf", [128, 8, 128])
        h2Tb = [P.sb(L + f"h2Tb{i}", [128, 8, 128], BF16) for i in range(2)]
        st = P.sb(L + "st", [128, 8])
        rt = P.sb(L + "rt", [128, 256])
        gate = [P.sb(L + f"gate{i}", [128, 32]) for i in range(2)]
        ps_y = [P.ps(L + f"ps_y{i}", [128, 512]) for i in range(3)]
        ps_out = [P.ps(L + f"ps_out{i}", [128, 512]) for i in range(2)]
        ps_t = [P.ps(L + f"ps_t{i}", [128, 512]) for i in range(2)]
        ps_r = P.ps(L + "ps_r", [128, 512])
        BIG = 1.0e30

        def loads(bi):
            s, sz = BLOCKS[bi]
            nt = sz // 128
            b = bi % 2
            for i_, src in enumerate((self.SA, self.OB, self.OC)):
                P.dma(sa[b][:, i_, :, 0:sz], src[:, :, s:s + sz].rearrange("c p t -> p c t"), w=[L + f"sa{b}"])

        def loads1(bi):
            s, sz = BLOCKS[bi]
            nt = sz // 128
            for g_ in range(3):
                P.dma(mg0[:, g_ * 8:(g_ + 1) * 8, 0:sz],
                      self.MG[g_ * 8:(g_ + 1) * 8, :, s:s + sz].rearrange("c p t -> p c t"), w=[L + "mg0"])
            src = xsrc_c if bi == 0 else xsrc_l[s - NCTX:s - NCTX + sz, :]
            P.dma(xt0[:, 0:nt, :], src.rearrange("(t p) d -> p t d", p=128), w=[L + "xt0"])

        loads(0)
        tix = 0
        for bi, (s, sz) in enumerate(BLOCKS):
            loads1(bi)
            if bi + 1 < len(BLOCKS):
                loads(bi + 1)
            b = bi % 2
            nt = sz // 128
            rr = 1 if bi == 0 else 0
            sak, mgk, xk = L + f"sa{b}", L + "mg0", L + "xt0"
            for j in range(8):
                for i_, wt in enumerate((WA, WB, WC)):
                    for c in range(4):
                        P.mm(ps_y[i_][:, 0:sz], wt[:, c, j * 128:(j + 1) * 128], sa[b][:, i_, c, 0:sz], c == 0, c == 3,
                             r=[sak] + CK, w=[L + f"ps_y{i_}"])
                P.tt(t1[:, 0:sz], ps_y[0][:, 0:sz], mg[b][:, j, 0:sz], ALU.mult, r=[L + "ps_y0", mgk], w=[L + "t1"])
                P.tt(t2[:, 0:sz], ps_y[1][:, 0:sz], mg[b][:, 8 + j, 0:sz], ALU.mult, r=[L + "ps_y1", mgk], w=[L + "t2"])
                P.tt(t1[:, 0:sz], t1[:, 0:sz], t2[:, 0:sz], ALU.add, r=[L + "t1", L + "t2"], w=[L + "t1"], eng="pool")
                P.tt(t2[:, 0:sz], ps_y[2][:, 0:sz], mg[b][:, 16 + j, 0:sz], ALU.mult, r=[L + "ps_y2", mgk], w=[L + "t2"])
                P.tt(m[:, j, 0:sz], t1[:, 0:sz], t2[:, 0:sz], ALU.add, r=[L + "t1", L + "t2"], w=[L + "m"], eng="pool")
            for t in range(nt):
                xo, xok = x1[tix % 2], L + f"x1{tix % 2}"
                hb, hbk = h2Tb[tix % 2], L + f"h2Tb{tix % 2}"
                gt_, gtk = gate[tix % 2], L + f"gate{tix % 2}"
                tix += 1
                tok0 = s + t * 128
                for hf in range(2):
                    for j in range(8):
                        P.mm(ps_out[hf][:, :], m[:, j, t * 128:(t + 1) * 128], WO[:, j, hf * 512:(hf + 1) * 512],
                             j == 0, j == 7, r=[L + "m"] + CK, w=[L + f"ps_out{hf}"])
                    P.tt(z[:, hf * 512:(hf + 1) * 512], ps_out[hf][:, :], rows[:, rr, 0, hf * 512:(hf + 1) * 512],
                         ALU.mult, r=[L + f"ps_out{hf}"] + CK, w=[L + "z"])
                P.stt(z[:], xt[b][:, t, :], ALPHA, z[:], ALU.mult, ALU.add, r=[xk, L + "z"], w=[L + "z"])
                self.layernorm(L, z, zz, st, lnr, xo, [L + "z"], [xok], CK)
                P.dma(self.X1[tok0:tok0 + 128, :], xo[:], r=[xok], w=["X1"], q="dq_pool")
                P.tt(h2[:], xo[:], rows[:, rr, 1, :], ALU.mult, r=[xok] + CK, w=[L + "h2"], eng="pool")
                P.tt(h2[:], h2[:], rows[:, rr, 2, :], ALU.add, r=[L + "h2"] + CK, w=[L + "h2"], eng="pool")
                for hf in range(2):
                    for jj in range(4):
                        j = hf * 4 + jj
                        P.tr(ps_t[hf][:, jj * 128:(jj + 1) * 128], h2[:, j * 128:(j + 1) * 128], ident[:],
                             r=[L + "h2"] + CK, w=[L + f"ps_t{hf}"])
                    P.copy(h2Tf[:, hf * 4:(hf + 1) * 4, :], ps_t[hf][:, :].rearrange("p (j t) -> p j t", t=128),
                           r=[L + f"ps_t{hf}"], w=[L + "h2Tf"], eng="act")
                P.copy(hb[:], h2Tf[:], r=[L + "h2Tf"], w=[hbk], eng="pool")
                P.dma(self.H2T[:, :, tok0:tok0 + 128].rearrange("j p t -> p j t"), hb[:], r=[hbk], w=["H2T"], q="dq_pool")
                for j in range(8):
                    P.mm(ps_r[:, 0:36], h2Tf[:, j, :], Wr[:, j, :], j == 0, j == 7, r=[L + "h2Tf"] + CK, w=[L + "ps_r"])
                self.router(L, ps_r, br, rt, gt_, [L + "ps_r"] + CK, [gtk], BIG)
                P.dma(self.GATE[tok0:tok0 + 128, :], gt_[:], r=[gtk], w=["GATE"], q="dq_pool")
        P.end_phase()

    def layernorm(self, L, z, zz, st, lnr, out, rk, wk, CK):
        P = self.P
        nc = P.nc
        sk = L + "st"
        P.op("dve", lambda: nc.vector.tensor_reduce(out=st[:, 0:1], in_=z[:], axis=AX.X, op=ALU.add), r=rk, w=[sk])
        P.act(zz[:], z[:], AF.Square, r=rk, w=[L + "zz"])
        P.op("dve", lambda: nc.vector.tensor_reduce(out=st[:, 1:2], in_=zz[:], axis=AX.X, op=ALU.add),
             r=[L + "zz"], w=[sk])
        P.ts(st[:, 2:3], st[:, 0:1], 1.0 / D, ALU.mult, r=[sk], w=[sk])
        P.tt(st[:, 3:4], st[:, 2:3], st[:, 2:3], ALU.mult, r=[sk], w=[sk])
        P.stt(st[:, 4:5], st[:, 1:2], 1.0 / D, st[:, 3:4], ALU.mult, ALU.subtract, r=[sk], w=[sk])
        P.rsqrt(st[:, 5:6], st[:, 4:5], EPS, r=[sk], w=[sk])
        P.ts(zz[:], z[:], st[:, 2:3], ALU.subtract, st[:, 5:6], ALU.mult, r=rk + [sk, L + "zz"], w=[L + "zz"])
        P.tt(zz[:], zz[:], lnr[:, 0, :], ALU.mult, r=[L + "zz"] + CK, w=[L + "zz"], eng="pool")
        P.tt(out[:], zz[:], lnr[:, 1, :], ALU.add, r=[L + "zz"] + CK, w=wk, eng="pool")

    def router(self, L, ps_r, br, rt, gate, rk, wk, BIG):
        P = self.P
        nc = P.nc
        k = L + "rt"
        lg = rt[:, 0:36]
        P.tt(lg, ps_r[:, 0:36], br[:, :], ALU.add, r=rk, w=[k])
        gmax, ngmax, ge, gsum, gp = rt[:, 40:41], rt[:, 41:42], rt[:, 44:48], rt[:, 48:49], rt[:, 49:50]
        ohg, m1, m2, dm, e21, p1, p2 = rt[:, 52:56], rt[:, 56:57], rt[:, 57:58], rt[:, 58:59], rt[:, 59:60], rt[:, 60:61], rt[:, 61:62]
        lem, oh1, oh2 = rt[:, 64:96], rt[:, 96:128], rt[:, 128:160]
        lem2 = rt[:, 160:192]
        P.op("dve", lambda: nc.vector.tensor_reduce(out=gmax, in_=rt[:, 0:4], axis=AX.X, op=ALU.max), r=[k], w=[k])
        P.ts(ngmax, gmax, -1.0, ALU.mult, r=[k], w=[k])
        P.act(ge, rt[:, 0:4], AF.Exp, bias=ngmax, r=[k], w=[k])
        P.op("dve", lambda: nc.vector.tensor_reduce(out=gsum, in_=ge, axis=AX.X, op=ALU.add), r=[k], w=[k])
        P.recip(gp, gsum, r=[k], w=[k])
        P.ts(ohg, rt[:, 0:4], gmax, ALU.is_equal, r=[k], w=[k])
        P.ts(ohg, ohg, -1.0, ALU.add, BIG, ALU.mult, r=[k], w=[k])
        P.tt(lem.rearrange("p (g e) -> p g e", e=8), rt[:, 4:36].rearrange("p (g e) -> p g e", e=8),
             ohg.unsqueeze(2).broadcast_to([128, 4, 8]), ALU.add, r=[k], w=[k])
        P.op("dve", lambda: nc.vector.tensor_reduce(out=m1, in_=lem, axis=AX.X, op=ALU.max), r=[k], w=[k])
        P.ts(oh1, lem, m1, ALU.is_equal, r=[k], w=[k])
        P.stt(lem2, oh1, -BIG, lem, ALU.mult, ALU.add, r=[k], w=[k])
        P.op("dve", lambda: nc.vector.tensor_reduce(out=m2, in_=lem2, axis=AX.X, op=ALU.max), r=[k], w=[k])
        P.ts(oh2, lem2, m2, ALU.is_equal, r=[k], w=[k])
        P.tt(dm, m2, m1, ALU.subtract, r=[k], w=[k])
        P.act(e21, dm, AF.Exp, r=[k], w=[k])
        P.ts(p1, e21, 1.0, ALU.add, r=[k], w=[k])
        P.recip(p1, p1, r=[k], w=[k])
        P.tt(p2, e21, p1, ALU.mult, r=[k], w=[k])
        P.tt(p1, p1, gp, ALU.mult, r=[k], w=[k])
        P.tt(p2, p2, gp, ALU.mult, r=[k], w=[k])
        P.ts(gate[:], oh1, p1, ALU.mult, r=[k], w=wk)
        P.stt(gate[:], oh2, p2, gate[:], ALU.mult, ALU.add, r=[k] + wk, w=wk)

    def phase4(self, l, dst, dst_has_ctx):
        P, I = self.P, self.inp
        L = f"p4_{l}_"
        P.begin_phase()
        rows = P.sb(L + "rows", [128, 2, D])
        for rr in range(2):
            self.bcast_row(rows[:, rr, :], self.modrow[l][rr:rr + 1, 5 * D:6 * D], [L + "rows"])
        lnr = P.sb(L + "lnr", [128, 2, D])
        self.bcast_row(lnr[:, 0, :], I["ln2_g"][l:l + 1, :], [L + "lnr"])
        self.bcast_row(lnr[:, 1, :], I["ln2_b"][l:l + 1, :], [L + "lnr"])
        CK = [L + "rows", L + "lnr"]
        NT = 17
        HT = NT * 128
        H = P.sb(L + "H", [128, 8, HT], BF16)
        G = P.sb(L + "G", [128, NT, 32])
        yacc = P.sb(L + "yacc", [128, NT, D])
        W1 = [P.sb(L + f"W1_{i}", [128, 8, 256], BF16) for i in range(2)]
        W3 = [P.sb(L + f"W3_{i}", [128, 8, 256], BF16) for i in range(2)]
        W2 = [P.sb(L + f"W2_{i}", [128, 2, D], BF16) for i in range(2)]
        aT = [P.sb(L + f"aT{i}", [128, 2, 512], BF16) for i in range(2)]
        sg = [P.sb(L + f"sg{i}", [128, 512]) for i in range(2)]
        ps_h1 = [P.ps(L + f"ps_h1{i}", [128, 512]) for i in range(2)]
        ps_h3 = [P.ps(L + f"ps_h3{i}", [128, 512]) for i in range(2)]
        ps_y = [P.ps(L + f"ps_y{i}", [128, 512]) for i in range(4)]
        x1t = [P.sb(L + f"x1t{i}", [128, D]) for i in range(2)]
        z = P.sb(L + "z", [128, D])
        zz = P.sb(L + "zz", [128, D])
        st = P.sb(L + "st", [128, 8])
        xo = [P.sb(L + f"xo{i}", [128, D]) for i in range(2)]
        blocks = [(i * 512, 512) for i in range(4)] + [(2048, 128)]

        def load_w(e):
            b = e % 2
            P.dma(W1[b][:], I["w1"][l, e].rearrange("(k p) n -> p k n", p=128), w=[L + f"W1_{b}"], q="dq_pool")
            P.dma(W3[b][:], I["w3"][l, e].rearrange("(k p) n -> p k n", p=128), w=[L + f"W3_{b}"], q="dq_pool")
            P.dma(W2[b][:], I["w2"][l, e].rearrange("(o p) n -> p o n", p=128), w=[L + f"W2_{b}"], q="dq_pool")

        st_ = dict(cy=0)
        a_of = {}
        pending = []

        def emit_h(e, bb):
            b0, bsz = blocks[bb]
            b = e % 2
            wk1, wk3 = L + f"W1_{b}", L + f"W3_{b}"
            idx = (e * len(blocks) + bb) % 2
            a_, ak = aT[idx], L + f"aT{idx}"
            a_of[(e, bb)] = (a_, ak)
            for o in range(2):
                for k in range(8):
                    P.mm(ps_h1[o][:, 0:bsz], W1[b][:, k, o * 128:(o + 1) * 128], H[:, k, b0:b0 + bsz],
                         k == 0, k == 7, r=[wk1, L + "H"], w=[L + f"ps_h1{o}"])
                for k in range(8):
                    P.mm(ps_h3[o][:, 0:bsz], W3[b][:, k, o * 128:(o + 1) * 128], H[:, k, b0:b0 + bsz],
                         k == 0, k == 7, r=[wk3, L + "H"], w=[L + f"ps_h3{o}"])
                P.act(sg[o][:, 0:bsz], ps_h1[o][:, 0:bsz], AF.Silu, r=[L + f"ps_h1{o}"], w=[L + f"sg{o}"])
                P.tt(a_[:, o, 0:bsz], sg[o][:, 0:bsz], ps_h3[o][:, 0:bsz], ALU.mult,
                     r=[L + f"sg{o}", L + f"ps_h3{o}"], w=[ak])

        def emit_y(e, bb):
            b0, bsz = blocks[bb]
            b = e % 2
            wk2 = L + f"W2_{b}"
            a_, ak = a_of.pop((e, bb))
            for t in range(bsz // 128):
                tile = b0 // 128 + t
                for hf in range(2):
                    cy = st_["cy"]
                    py, pyk = ps_y[cy % 4], L + f"ps_y{cy % 4}"
                    st_["cy"] = cy + 1
                    for o in range(2):
                        P.mm(py[:, :], a_[:, o, t * 128:(t + 1) * 128], W2[b][:, o, hf * 512:(hf + 1) * 512],
                             o == 0, o == 1, r=[ak, wk2], w=[pyk])
                    ya = yacc[:, tile, hf * 512:(hf + 1) * 512]
                    yk = L + f"yacc{tile}_{hf}"
                    if e == 0:
                        P.ts(ya, py[:, :], G[:, tile, e:e + 1], ALU.mult, r=[pyk, L + "G"], w=[yk])
                    else:
                        P.stt(ya, py[:, :], G[:, tile, e:e + 1], ya, ALU.mult, ALU.add,
                              r=[pyk, L + "G", yk], w=[yk])

        for half in range(2):
            T0 = half * HT
            P.dma(H[:], self.H2T[:, :, T0:T0 + HT].rearrange("j p t -> p j t"), w=[L + "H"])
            P.dma(G[:], self.GATE[T0:T0 + HT, :].rearrange("(t p) e -> p t e", p=128), w=[L + "G"])
            load_w(0)
            for e in range(32):
                for bb in range(len(blocks)):
                    pending.append((e, bb))
                    emit_h(e, bb)
                    if len(pending) > 1:
                        emit_y(*pending.pop(0))
                    if bb == 0 and e + 1 < 32:
                        load_w(e + 1)
            while pending:
                emit_y(*pending.pop(0))
            for t in range(NT):
                tile = half * NT + t
                tok0 = tile * 128
                rr = 1 if tile < 2 else 0
                xi, xik = x1t[t % 2], L + f"x1t{t % 2}"
                o_, ok = xo[t % 2], L + f"xo{t % 2}"
                if tile < 2 and not dst_has_ctx:
                    continue
                P.dma(xi[:], self.X1[tok0:tok0 + 128, :], w=[xik])
                yks = [L + f"yacc{t}_{hf}" for hf in range(2)]
                P.tt(z[:], yacc[:, t, :], rows[:, rr, :], ALU.mult, r=yks + CK, w=[L + "z"], eng="pool")
                P.stt(z[:], xi[:], ALPHA, z[:], ALU.mult, ALU.add, r=[xik, L + "z"], w=[L + "z"])
                self.layernorm(L, z, zz, st, lnr, o_, [L + "z"], [ok], CK)
                if dst_has_ctx:
                    P.dma(dst[tok0:tok0 + 128, :], o_[:], r=[ok], w=["dst"], q="dq_pool")
                else:
                    P.dma(dst[tok0 - NCTX:tok0 - NCTX + 128, :], o_[:], r=[ok], w=["dst"], q="dq_pool")
        P.end_phase()

    def build(self, phases=None, n_layers=2):
        P = self.P
        self.declare_inputs()
        ALL = ("p0", "p1", "p2a", "p2b1", "p2b2", "p2c1", "p2c2", "p2c3", "p2c4", "p3", "p4")
        if phases is None:
            phases = ALL
        self.modrow = [self.scratch(f"modrow{l}", [2, 6 * D]) for l in range(2)]
        self.U = self.scratch("U", [4, 128, T])
        self.QD = self.scratch("QD", [3, 128, T])
        self.KVD = self.scratch("KVD", [2, 128, T])
        self.KR = self.scratch("KR", [32, T])
        self.GQKV = self.scratch("GQKV", [12, 128, T])
        self.OG = self.scratch("OG", [4, 128, T], BF16)
        self.BETA = self.scratch("BETA", [16, T])
        self.GL = self.scratch("GL", [16, T])
        self.MG = self.scratch("MG", [24, 128, T], BF16)
        self.SA = self.scratch("SA", [4, 128, T], BF16)
        self.KT = self.scratch("KT", [8, 96, T], BF16)
        self.QT = self.scratch("QT", [8, 96, T], BF16)
        self.VX = self.scratch("VX", [8, 128, 34, 128], BF16)
        self.OB = self.scratch("OB", [4, 128, T], BF16)
        self.QKN = self.scratch("QKN", [8, 128, T])
        self.KVtok = self.scratch("KVtok", [T, 2, 512])
        self.BGtok = self.scratch("BGtok", [T, 32])
        self.PW = self.scratch("PW", [68, 2, 64, 512], BF16)
        self.PQ = self.scratch("PQ", [68, 2, 64, 512], BF16)
        self.PK = self.scratch("PK", [68, 2, 64, 512], BF16)
        self.PM = self.scratch("PM", [68, 2, 64, 512], BF16)
        self.PU = self.scratch("PU", [68, 2, 64, 512])
        self.PG = self.scratch("PG", [68, 2, 64, 8])
        self.ODIR = self.scratch("ODIR", [2, T, 512])
        self.OC = self.scratch("OC", [4, 128, T], BF16)
        self.X1 = self.scratch("X1", [T, D])
        self.H2T = self.scratch("H2T", [8, 128, T], BF16)
        self.GATE = self.scratch("GATE", [T, 32])
        self.X2 = self.scratch("X2", [T, D])
        self.out = P.dram("out", [NL, D], F32, kind="ExternalOutput")
        kw = getattr(self, "p2c2_kw", {})
        for l in range(n_layers):
            if l == 0:
                xl, xc = self.inp["x"], self.inp["ctx"]
            else:
                xl, xc = self.X2[NCTX:T, :], self.X2[0:NCTX, :]
            last = l == DEPTH - 1
            if "p0" in phases:
                self.phase0(l)
            if "p1" in phases:
                self.phase1(l, xl, xc, "xin")
            if "p2a" in phases:
                self.phase2a(l)
            if "p2b1" in phases:
                self.phase2b1(l)
            if "p2b2" in phases:
                self.phase2b2(l)
            if "p2c1" in phases:
                self.phase2c1(l)
            if "p2c2" in phases:
                self.phase2c2(l, **kw)
            if "p2c3" in phases:
                self.phase2c3(l, **kw)
            if "p2c4" in phases:
                self.phase2c4(l)
            if "p3" in phases:
                self.phase3(l, xl, xc)
            if "p4" in phases:
                if last:
                    self.phase4(l, self.out, False)
                else:
                    self.phase4(l, self.X2, True)
        return P.nc


def host_consts():
    ident = np.eye(128, dtype=np.float32)
    nf = 8
    inv = (10000.0 ** (-np.arange(nf, dtype=np.float32) / nf)).astype(np.float32)
    rows = NL // 64
    r = np.repeat(np.arange(rows, dtype=np.float32), 64)
    col = np.tile(np.arange(64, dtype=np.float32), rows)
    ang = np.stack([r[:, None] * inv, col[:, None] * inv], axis=1)
    cos = np.cos(ang).astype(np.float32)
    sin = np.sin(ang).astype(np.float32)
    cs = np.zeros((2, 32, T), np.float32)
    cs[0, :, :NCTX] = 1.0
    for a in range(2):
        for h in range(2):
            d0 = a * 16 + h * 8
            cs[0, d0:d0 + 8, NCTX:] = cos[:, a, :].T
            cs[1, d0:d0 + 8, NCTX:] = (-sin[:, a, :].T) if h == 0 else sin[:, a, :].T
    perm = np.zeros((32, 32), np.float32)
    for m_ in range(32):
        perm[m_ ^ 8, m_] = 1.0
    pp, ff = np.meshgrid(np.arange(64), np.arange(64), indexing="ij")
    gmask = np.stack([ff <= pp, ff < pp, ff >= pp, ff > pp]).astype(np.float32)
    return ident, cs, perm, gmask


def make_in_maps(inputs, cores):
    ident, cs, perm, gmask = host_consts()
    maps = []
    for b in cores:
        m = {}
        for k, v in inputs.items():
            v = np.asarray(v)
            if k in ("x", "c", "ctx"):
                m[k] = np.ascontiguousarray(v[b])
            elif k in ("a_log", "dt_bias"):
                m[k] = np.ascontiguousarray(v.reshape(2, 16))
            else:
                m[k] = np.ascontiguousarray(v)
        m["ident"] = ident
        m["rope_cs"] = cs
        m["perm32"] = perm
        m["gmask"] = gmask
        maps.append(m)
    return maps


_NC_CACHE = {}


def kernel(**inputs):
    n = 8
    if "nc" not in _NC_CACHE:
        net = Net()
        _NC_CACHE["nc"] = net.build()
    nc = _NC_CACHE["nc"]
    maps = make_in_maps(inputs, list(range(n)))
    res = run_bass_kernel_spmd(nc, maps, core_ids=list(range(n)))
    return np.stack([np.asarray(r["out"]) for r in res.results], axis=0).astype(np.float32)
```

```python
from contextlib import ExitStack
import numpy as np
import concourse.bass as bass
import concourse.mybir as mybir
from concourse.bass_utils import run_bass_kernel_spmd

F32 = mybir.dt.float32
BF16 = mybir.dt.bfloat16
AF = mybir.ActivationFunctionType
ALU = mybir.AluOpType
AX = mybir.AxisListType

D = 1024
NCTX = 256
NL = 4096
T = NCTX + NL
DEPTH = 2
P_IN = 6848
EPS = 1e-6
ALPHA = (2 * DEPTH) ** 0.25
MLA_SCALE = 96 ** -0.5
BLOCKS = [(0, 256)] + [(256 + 512 * i, 512) for i in range(8)]

DMA_ENGS = ("sp", "dq_pool", "dq_act")
PHYS = {"pe": "pe", "act": "act", "dve": "dve", "pool": "pool", "sp": "sp", "dq_pool": "pool", "dq_act": "act"}


class Prog:
    def __init__(self):
        self.nc = bass.Bass("TRN2", target_bir_lowering=False)
        self.root = ExitStack()
        self.es = self.root
        self.ops = []
        self.n_dsem = 12
        self.psum_names = set()

    def _next_uid(self):
        self._uid = getattr(self, "_uid", 0) + 1
        return self._uid

    def sb(self, name, shape, dt=F32):
        return self.es.enter_context(self.nc.sbuf_tensor(name, list(shape), dt))

    def ps(self, name, shape, dt=F32):
        self.psum_names.add(name)
        return self.es.enter_context(self.nc.psum_tensor(name, list(shape), dt))

    def dram(self, name, shape, dt=F32, kind="Internal"):
        return self.nc.dram_tensor(name, list(shape), dt, kind=kind).ap()

    def op(self, eng, fn, r=(), w=()):
        pr = [k for k in r if k in self.psum_names]
        if pr:
            r = [k for k in r if k not in self.psum_names]
            w = list(w) + pr
        self.ops.append((eng, fn, tuple(r), tuple(w)))

    DRAM_KEYS = {"SA", "KT", "QT", "VX", "OB", "QKN", "KVtok", "BGtok", "PW", "PQ", "PK", "PM", "PU", "PG",
                 "ODIR", "OC", "X1", "H2T", "GATE", "dst", "modrow0", "modrow1"}

    def dma(self, out, in_, r=(), w=(), q="sp", slow=False):
        nc = self.nc
        w = [((k, self._next_uid()) if (k in self.DRAM_KEYS or (isinstance(k, tuple) and k[0] == "scr")) else k)
             for k in w]
        e = {"sp": nc.sync, "dq_pool": nc.gpsimd, "dq_act": nc.scalar}[q]
        if slow:
            self.op(q, lambda: e.dma_start(out=out, in_=in_, allow_slow_non_contiguous=True), r, w)
        else:
            self.op(q, lambda: e.dma_start(out=out, in_=in_), r, w)

    def mm(self, out, lhsT, rhs, start, stop, r=(), w=()):
        nc = self.nc
        self.op("pe", lambda: nc.tensor.matmul(out, lhsT=lhsT, rhs=rhs, start=start, stop=stop), r, w)

    def tr(self, out, in_, ident, r=(), w=()):
        nc = self.nc
        self.op("pe", lambda: nc.tensor.transpose(out, in_, ident), r, w)

    def act(self, out, in_, func, r=(), w=(), bias=None, scale=None, accum_out=None):
        nc = self.nc
        kw = {}
        if bias is not None:
            kw["bias"] = bias
        if scale is not None:
            kw["scale"] = scale
        if accum_out is not None:
            kw["accum_out"] = accum_out
        self.op("act", lambda: nc.scalar.activation(out=out, in_=in_, func=func, **kw), r, w)

    def _veng(self, eng):
        return self.nc.vector if eng == "dve" else self.nc.gpsimd

    def tt(self, out, in0, in1, op, r=(), w=(), eng="dve"):
        e = self._veng(eng)
        self.op(eng, lambda: e.tensor_tensor(out=out, in0=in0, in1=in1, op=op), r, w)

    def ts(self, out, in0, s1, op0, s2=None, op1=None, r=(), w=(), eng="dve", accum_out=None):
        e = self._veng(eng)
        kw = {}
        if op1 is not None:
            kw["op1"] = op1
        if accum_out is not None:
            kw["accum_out"] = accum_out
        self.op(eng, lambda: e.tensor_scalar(out=out, in0=in0, scalar1=s1, scalar2=s2, op0=op0, **kw), r, w)

    def stt(self, out, in0, scalar, in1, op0, op1, r=(), w=(), eng="dve"):
        e = self._veng(eng)
        self.op(eng, lambda: e.scalar_tensor_tensor(out=out, in0=in0, scalar=scalar, in1=in1, op0=op0, op1=op1), r, w)

    def copy(self, out, in_, r=(), w=(), eng="dve"):
        if eng == "act":
            self.act(out, in_, AF.Copy, r, w)
        else:
            e = self._veng(eng)
            self.op(eng, lambda: e.tensor_copy(out=out, in_=in_), r, w)

    def recip(self, out, in_, r=(), w=()):
        nc = self.nc
        self.op("dve", lambda: nc.vector.reciprocal(out=out, in_=in_), r, w)

    def rsqrt(self, out, in_, eps, r=(), w=(), scale=1.0):
        self.act(out, in_, AF.Sqrt, r=r, w=w, bias=eps, scale=scale)
        self.recip(out, out, r=w, w=w)

    def memset(self, ap, val, w=(), eng="dve"):
        e = self._veng(eng)
        self.op(eng, lambda: e.memset(ap, val), (), w)

    def begin_phase(self):
        self._outer = self.es
        self.es = ExitStack()
        self.es.__enter__()

    def end_phase(self):
        self.flush()
        self.es.close()
        self.es = self._outer

    def _init_sync(self):
        nc = self.nc
        self.csem = {p: self._outer_ctx(nc.semaphore("cs_" + p)) for p in ("pe", "act", "dve", "pool")}
        self.dsem = {q: [self._outer_ctx(nc.semaphore(f"ds_{q}_{k}")) for k in range(self.n_dsem)]
                     for q in DMA_ENGS}
        self.bsem = self._outer_ctx(nc.semaphore("barrier"))
        self.mc = {p: 0 for p in ("pe", "act", "dve", "pool")}
        self.dcount = {q: 0 for q in DMA_ENGS}
        self.nphase = 0
        self.n_ops = 0

    def _outer_ctx(self, cm):
        return self.root.enter_context(cm)

    def flush(self):
        nc = self.nc
        if not hasattr(self, "csem"):
            self._init_sync()
        ops = self.ops
        self.ops = []
        import os as _os
        if _os.environ.get("KMAXOPS"):
            ops = ops[:int(_os.environ["KMAXOPS"])]
        queue = {"pe": nc.tensor, "act": nc.scalar, "dve": nc.vector, "pool": nc.gpsimd,
                 "sp": nc.sync, "dq_pool": nc.gpsimd, "dq_act": nc.scalar}
        n = len(ops)
        self.n_ops += n
        last_w, readers = {}, {}
        deps = []
        for i, (eng, fn, r, w) in enumerate(ops):
            d = set()
            for k in r:
                if k in last_w:
                    d.add(last_w[k])
            for k in w:
                if k in last_w:
                    d.add(last_w[k])
                d.update(readers.get(k, ()))
            d.discard(i)
            deps.append(d)
            for k in r:
                readers.setdefault(k, []).append(i)
            for k in w:
                last_w[k] = i
                readers[k] = []
        local = [0] * n
        cnt = {}
        last_on = {}
        for i, (eng, fn, r, w) in enumerate(ops):
            if eng not in DMA_ENGS:
                p = PHYS[eng]
                cnt[p] = cnt.get(p, 0) + 1
                local[i] = cnt[p]
                last_on[p] = i
        known_c, known_d = {}, {}
        milestone = [False] * n
        waits = [None] * n
        for i, (eng, fn, r, w) in enumerate(ops):
            E = PHYS[eng]
            i_dma = eng in DMA_ENGS
            need_c = {}
            need_d = []
            for j in sorted(deps[i]):
                jeng = ops[j][0]
                if jeng in DMA_ENGS:
                    s = known_d.setdefault(E, set())
                    if j not in s:
                        s.add(j)
                        need_d.append(j)
                else:
                    Pj = PHYS[jeng]
                    if Pj == E and not i_dma and E == "pe":
                        continue
                    if known_c.get((E, Pj), 0) >= local[j]:
                        continue
                    if need_c.get(Pj, (0, -1))[0] < local[j]:
                        need_c[Pj] = (local[j], j)
            for Pj, (lj, j) in need_c.items():
                known_c[(E, Pj)] = lj
                milestone[j] = True
            waits[i] = (list(need_c.values()), need_d)
        for p, i in last_on.items():
            milestone[i] = True
        msval = [0] * n
        for i, (eng, fn, r, w) in enumerate(ops):
            if eng not in DMA_ENGS and milestone[i]:
                p = PHYS[eng]
                self.mc[p] += 1
                msval[i] = self.mc[p]
        dtoken = {}
        for i, (eng, fn, r, w) in enumerate(ops):
            q = queue[eng]
            need_c, need_d = waits[i]
            for (lj, j) in need_c:
                q.wait_ge(self.csem[PHYS[ops[j][0]]], msval[j])
            for j in need_d:
                s, v = dtoken[j]
                q.wait_ge(s, v)
            if eng in DMA_ENGS:
                k = self.dcount[eng]
                self.dcount[eng] += 1
                s = self.dsem[eng][k % self.n_dsem]
                rnd = k // self.n_dsem
                if rnd > 0:
                    q.wait_ge(s, 16 * rnd)
                ins = fn()
                ins.then_inc(s, 16)
                dtoken[i] = (s, 16 * (rnd + 1))
            else:
                ins = fn()
                if milestone[i]:
                    ins.then_inc(self.csem[PHYS[eng]], 1)
        for p in ("pe", "act", "dve", "pool"):
            if self.mc[p] > 0:
                nc.sync.wait_ge(self.csem[p], self.mc[p])
        for qn in DMA_ENGS:
            k = self.dcount[qn]
            for m in range(min(k, self.n_dsem)):
                final = 16 * ((k - 1 - m) // self.n_dsem + 1)
                nc.sync.wait_ge(self.dsem[qn][m], final)
        self.nphase += 1
        nc.sync.sem_inc(self.bsem, 1)
        for e in (nc.tensor, nc.scalar, nc.vector, nc.gpsimd):
            e.wait_ge(self.bsem, self.nphase)


C_A, C_GT, C_QD, C_KVD, C_KR = 0, 512, 1024, 1408, 1664
C_GQ, C_GK, C_GV, C_OG, C_BETA, C_AR, C_MG = 1696, 2208, 2720, 3232, 3744, 3760, 3776


def load_col(P, dst, src1d, r=(), w=()):
    P.dma(dst, src1d.rearrange("(n p) -> p n", p=128), r, w, slow=True)


class Net:
    def __init__(self, dbg=()):
        self.P = Prog()
        self.dbg = set(dbg)
        self.io = {}

    def scratch(self, name, shape, dt=F32):
        kind = "ExternalOutput" if name in self.dbg else "Internal"
        if name in getattr(self, "dbg_in", ()):
            kind = "ExternalInput"
        t = self.P.dram(name, shape, dt, kind=kind)
        self.io[name] = t
        return t

    def declare_inputs(self):
        P = self.P
        specs = dict(
            x=[NL, D], c=[D], ctx=[NCTX, D], c_ctx=[D], w_mod=[2, D, 6 * D], b_mod=[2, 6 * D],
            w_in=[2, D, P_IN], b_in=[2, P_IN], conv_a_w=[2, 31, 512], conv_a_b=[2, 512],
            ln_a_g=[2, 512], ln_a_b=[2, 512], w_a_out=[2, 512, D], g_q=[2, 384], g_kv=[2, 256],
            w_uq=[2, 384, 512], w_qr=[2, 384, 256], w_uk=[2, 256, 512], w_uv=[2, 256, 512],
            w_b_out=[2, 512, D], conv_c_w=[2, 5, 1536], a_log=[2, 16], dt_bias=[2, 16], g_o=[2, 64],
            w_c_out=[2, 512, D], w_out=[2, D, D], ln1_g=[2, D], ln1_b=[2, D], w_rg=[2, D, 4],
            b_rg=[2, 4], w_re=[2, D, 32], b_re=[2, 32], w1=[2, 32, D, 256], w3=[2, 32, D, 256],
            w2=[2, 32, 256, D], ln2_g=[2, D], ln2_b=[2, D],
            ident=[128, 128], rope_cs=[2, 32, T], perm32=[32, 32], gmask=[4, 64, 64],
        )
        self.inp = {k: P.dram(k, v, F32, kind="ExternalInput") for k, v in specs.items()}

    def phase0(self, l):
        P, I = self.P, self.inp
        L = f"p0_{l}_"
        P.begin_phase()
        scin = P.sb(L + "scin", [128, 8, 2])
        sc = P.sb(L + "sc", [128, 8, 2])
        bm = P.sb(L + "bm", [2, 6 * D])
        mr = P.sb(L + "mr", [2, 6 * D])
        wm = [P.sb(L + f"wm{i}", [128, 8, 512]) for i in range(2)]
        ps = P.ps(L + "ps", [128, 512])
        load_col(P, scin[:, :, 0], I["c"], w=[L + "scin"])
        load_col(P, scin[:, :, 1], I["c_ctx"], w=[L + "scin"])
        P.dma(bm[0:1, :], I["b_mod"][l:l + 1, :], w=[L + "bm"])
        P.dma(bm[1:2, :], I["b_mod"][l:l + 1, :], w=[L + "bm"])
        P.act(sc[:], scin[:], AF.Silu, r=[L + "scin"], w=[L + "sc"])
        for nb in range(12):
            b = wm[nb % 2]
            key = L + f"wm{nb % 2}"
            P.dma(b[:], I["w_mod"][l, :, nb * 512:(nb + 1) * 512].rearrange("(k p) n -> p k n", p=128), w=[key])
            for k in range(8):
                P.mm(ps[0:2, :], sc[:, k, :], b[:, k, :], k == 0, k == 7, r=[key, L + "sc"], w=[L + "ps"])
            P.tt(mr[0:2, nb * 512:(nb + 1) * 512], ps[0:2, :], bm[0:2, nb * 512:(nb + 1) * 512], ALU.add,
                 r=[L + "ps", L + "bm"], w=[L + "mr"])
        P.dma(self.modrow[l], mr[:], r=[L + "mr"], w=[f"modrow{l}"], q="dq_pool")
        P.end_phase()

    def phase1(self, l, xsrc_l, xsrc_c, xkey):
        P, I = self.P, self.inp
        L = f"p1_{l}_"
        P.begin_phase()
        W = P.sb(L + "W", [128, 8, P_IN], BF16)
        for c in range(4):
            for k in range(8):
                c0, c1 = c * 1712, (c + 1) * 1712
                P.dma(W[:, k, c0:c1], I["w_in"][l, k * 128:(k + 1) * 128, c0:c1], w=[L + f"W{k}_{c}"], q="dq_pool")
        ident = P.sb(L + "ident", [128, 128])
        P.dma(ident[:], I["ident"], w=[L + "ident"])
        tiles = []
        for ct in range(4):
            tiles.append(("gt", None, ct, C_GT + ct * 128, 128))
            tiles.append(("a", self.U, ct, C_A + ct * 128, 128))
        for i in range(3):
            tiles.append(("id", self.QD, i, C_QD + i * 128, 128))
        for i in range(2):
            tiles.append(("id", self.KVD, i, C_KVD + i * 128, 128))
        tiles.append(("id", self.KR, None, C_KR, 32))
        for i in range(12):
            tiles.append(("id", self.GQKV, i, C_GQ + i * 128, 128))
        for i in range(4):
            tiles.append(("silu", self.OG, i, C_OG + i * 128, 128))
        tiles.append(("sig", self.BETA, None, C_BETA, 16))
        tiles.append(("sp", self.GL, None, C_AR, 16))
        for i in range(24):
            tiles.append(("sig", self.MG, i, C_MG + i * 128, 128))
        nt_ = len(tiles)
        bcol = P.sb(L + "bcol", [128, nt_])
        for n_, (kind, dst, ti, c0, m) in enumerate(tiles):
            P.dma(bcol[0:m, n_:n_ + 1], I["b_in"][l, c0:c0 + m].rearrange("(p o) -> p o", o=1),
                  w=[L + "bcol"], slow=True)
        mcol = P.sb(L + "mcol", [128, 2, 48])
        for rr in range(2):
            load_col(P, mcol[:, rr, :], self.modrow[l][rr, :], w=[L + "mcol"])
        sc1p = P.sb(L + "sc1p", [128, 2, 8])
        P.ts(sc1p[:], mcol[:, :, 8:16], 1.0, ALU.add, r=[L + "mcol"], w=[L + "sc1p"])
        dtb = P.sb(L + "dtb", [16, 1])
        nega = P.sb(L + "nega", [16, 1])
        P.dma(dtb[:], I["dt_bias"][l, :].rearrange("(p o) -> p o", o=1), w=[L + "dtb"], slow=True)
        P.dma(nega[:], I["a_log"][l, :].rearrange("(p o) -> p o", o=1), w=[L + "nega"], slow=True)
        P.act(nega[:], nega[:], AF.Exp, r=[L + "nega"], w=[L + "nega"])
        P.ts(nega[:], nega[:], -1.0, ALU.mult, r=[L + "nega"], w=[L + "nega"])

        xt = [P.sb(L + f"xt{i}", [128, 4, D]) for i in range(2)]
        hT = [P.sb(L + f"hT{i}", [128, 8, 512], BF16) for i in range(2)]
        pT = [P.ps(L + f"pT{i}", [128, 512]) for i in range(2)]
        pp = [P.ps(L + f"pp{i}", [128, 512]) for i in range(4)]
        NS = 4
        stf = [P.sb(L + f"stf{i}", [128, 512]) for i in range(NS)]
        stb = [P.sb(L + f"stb{i}", [128, 512], BF16) for i in range(NS)]
        tmp = [P.sb(L + f"tmp{i}", [128, 512]) for i in range(2)]
        spz = P.sb(L + "spz", [16, 512])
        spa = P.sb(L + "spa", [16, 512])

        def load_x(bi):
            s, sz = BLOCKS[bi]
            nt = sz // 128
            src = xsrc_c if bi == 0 else xsrc_l[s - NCTX:s - NCTX + sz, :]
            P.dma(xt[bi % 2][:, 0:nt, :], src.rearrange("(t p) d -> p t d", p=128),
                  r=[xkey], w=[L + f"xt{bi % 2}"])

        load_x(0)
        cf = cb = cp = 0
        for bi, (s, sz) in enumerate(BLOCKS):
            if bi + 1 < len(BLOCKS):
                load_x(bi + 1)
            nt = sz // 128
            rr = 1 if bi == 0 else 0
            xb, hb = xt[bi % 2], hT[bi % 2]
            xk, hk = L + f"xt{bi % 2}", L + f"hT{bi % 2}"
            for j in range(8):
                pt = pT[j % 2]
                pk = L + f"pT{j % 2}"
                for t in range(nt):
                    P.tr(pt[:, t * 128:(t + 1) * 128], xb[:, t, j * 128:(j + 1) * 128], ident[:],
                         r=[xk, L + "ident"], w=[pk])
                P.ts(hb[:, j, 0:sz], pt[:, 0:sz], sc1p[:, rr, j:j + 1], ALU.mult,
                     mcol[:, rr, j:j + 1], ALU.add, r=[pk, L + "sc1p", L + "mcol"], w=[hk])
            for n_, (kind, dst, ti, c0, m) in enumerate(tiles):
                acc = pp[cp % 4]
                ak = L + f"pp{cp % 4}"
                cp += 1
                for k in range(8):
                    wks = [L + f"W{k}_{c}" for c in range(c0 // 1712, (c0 + m - 1) // 1712 + 1)]
                    P.mm(acc[0:m, 0:sz], W[:, k, c0:c0 + m], hb[:, k, 0:sz], k == 0, k == 7,
                         r=[hk] + wks, w=[ak])
                bc = bcol[0:m, n_:n_ + 1]
                if kind == "gt":
                    tb = tmp[ti % 2]
                    P.act(tb[:, 0:sz], acc[:, 0:sz], AF.Sigmoid, bias=bc, r=[ak, L + "bcol"], w=[L + f"tmp{ti % 2}"])
                    continue
                if kind in ("silu", "sig") and m == 128:
                    st, sk = stb[cb % NS], L + f"stb{cb % NS}"
                    cb += 1
                else:
                    st, sk = stf[cf % NS], L + f"stf{cf % NS}"
                    cf += 1
                if kind == "a":
                    P.stt(st[:, 0:sz], acc[:, 0:sz], bc, tmp[ti % 2][:, 0:sz], ALU.add, ALU.mult,
                          r=[ak, L + "bcol", L + f"tmp{ti % 2}"], w=[sk])
                elif kind == "id":
                    if n_ % 2 == 0:
                        P.ts(st[0:m, 0:sz], acc[0:m, 0:sz], bc, ALU.add, r=[ak, L + "bcol"], w=[sk])
                    else:
                        P.act(st[0:m, 0:sz], acc[0:m, 0:sz], AF.Identity, bias=bc, r=[ak, L + "bcol"], w=[sk])
                elif kind == "silu":
                    P.act(st[0:m, 0:sz], acc[0:m, 0:sz], AF.Silu, bias=bc, r=[ak, L + "bcol"], w=[sk])
                elif kind == "sig":
                    P.act(st[0:m, 0:sz], acc[0:m, 0:sz], AF.Sigmoid, bias=bc, r=[ak, L + "bcol"], w=[sk])
                elif kind == "sp":
                    P.ts(spz[:, 0:sz], acc[0:16, 0:sz], bc, ALU.add, dtb[:, 0:1], ALU.add,
                         r=[ak, L + "bcol", L + "dtb"], w=[L + "spz"])
                    P.act(spa[:, 0:sz], spz[:, 0:sz], AF.Abs, r=[L + "spz"], w=[L + "spa"])
                    P.act(spa[:, 0:sz], spa[:, 0:sz], AF.Exp, scale=-1.0, r=[L + "spa"], w=[L + "spa"])
                    P.act(spa[:, 0:sz], spa[:, 0:sz], AF.Ln, bias=1.0, r=[L + "spa"], w=[L + "spa"])
                    P.stt(spz[:, 0:sz], spz[:, 0:sz], 0.0, spa[:, 0:sz], ALU.max, ALU.add,
                          r=[L + "spz", L + "spa"], w=[L + "spz"])
                    P.ts(st[0:16, 0:sz], spz[:, 0:sz], nega[:, 0:1], ALU.mult, r=[L + "spz", L + "nega"], w=[sk])
                d_ap = dst[ti, :, s:s + sz] if ti is not None else dst[:, s:s + sz]
                P.dma(d_ap, st[0:m, 0:sz], r=[sk], w=[("scr", id(dst))], q="dq_pool")
        P.end_phase()

    def phase2a(self, l):
        P, I = self.P, self.inp
        L = f"p2a_{l}_"
        P.begin_phase()
        cw = P.sb(L + "cw", [128, 4, 31])
        for ct in range(4):
            P.dma(cw[:, ct, :], I["conv_a_w"][l][:, ct * 128:(ct + 1) * 128].rearrange("k p -> p k"),
                  w=[L + "cw"], slow=True)
        cb = P.sb(L + "cb", [128, 4])
        lg = P.sb(L + "lg", [128, 4])
        lb = P.sb(L + "lb", [128, 4])
        load_col(P, cb[:], I["conv_a_b"][l], w=[L + "cb"])
        load_col(P, lg[:], I["ln_a_g"][l], w=[L + "lg"])
        load_col(P, lb[:], I["ln_a_b"][l], w=[L + "lb"])
        ones = P.sb(L + "ones", [128, 128])
        P.memset(ones[:], 1.0, w=[L + "ones"])
        v = P.sb(L + "v", [128, 4, T])
        UB = T + 60
        ub = [P.sb(L + f"ub{i}", [128, UB]) for i in range(2)]
        for i in range(2):
            P.memset(ub[i][:, 0:15], 0.0, w=[L + f"ub{i}"])
            P.memset(ub[i][:, 271:301], 0.0, w=[L + f"ub{i}"])
            P.memset(ub[i][:, UB - 15:UB], 0.0, w=[L + f"ub{i}"])
        for ct in range(4):
            u, uk = ub[ct % 2], L + f"ub{ct % 2}"
            P.dma(u[:, 15:271], self.U[ct, :, 0:NCTX], w=[uk])
            P.dma(u[:, 301:301 + NL], self.U[ct, :, NCTX:T], w=[uk])
            SPL = 2944
            for (o0, n_, base, eng_, sfx) in ((0, NCTX, 0, "dve", "c"), (NCTX, SPL, 286, "dve", "a"),
                                             (NCTX + SPL, NL - SPL, 286 + SPL, "dve", "b")):
                vk = L + f"v{ct}{sfx}"
                vo = v[:, ct, o0:o0 + n_]
                P.ts(vo, u[:, base:base + n_], cw[:, ct, 0:1], ALU.mult, cb[:, ct:ct + 1], ALU.add,
                     r=[uk, L + "cw", L + "cb"], w=[vk], eng=eng_)
                for k in range(1, 31):
                    P.stt(vo, u[:, base + k:base + k + n_], cw[:, ct, k:k + 1], vo, ALU.mult, ALU.add,
                          r=[uk, L + "cw"], w=[vk], eng=eng_)
        ps_s = [P.ps(L + f"ps_s{i}", [128, 512]) for i in range(2)]
        ps_q = [P.ps(L + f"ps_q{i}", [128, 512]) for i in range(2)]
        sq = [P.sb(L + f"sq{i}", [128, 4, 512]) for i in range(2)]
        mean = [P.sb(L + f"mean{i}", [128, 512]) for i in range(2)]
        rstd = [P.sb(L + f"rstd{i}", [128, 512]) for i in range(2)]
        xn = [P.sb(L + f"xn{i}", [128, 512]) for i in range(3)]
        so = [P.sb(L + f"so{i}", [128, 512], BF16) for i in range(3)]
        vkeys = [[L + f"v{ct}{sfx}" for sfx in "cab"] for ct in range(4)]
        cx = 0
        for bi, (s, sz) in enumerate(BLOCKS):
            i2 = bi % 2
            for ct in range(4):
                P.act(sq[i2][:, ct, 0:sz], v[:, ct, s:s + sz], AF.Square, r=vkeys[ct], w=[L + f"sq{i2}"])
            for ct in range(4):
                P.mm(ps_s[i2][:, 0:sz], ones[:], v[:, ct, s:s + sz], ct == 0, ct == 3,
                     r=vkeys[ct] + [L + "ones"], w=[L + f"ps_s{i2}"])
            for ct in range(4):
                P.mm(ps_q[i2][:, 0:sz], ones[:], sq[i2][:, ct, 0:sz], ct == 0, ct == 3,
                     r=[L + f"sq{i2}", L + "ones"], w=[L + f"ps_q{i2}"])
            m_, r_ = mean[i2], rstd[i2]
            mk, rk = L + f"mean{i2}", L + f"rstd{i2}"
            P.ts(m_[:, 0:sz], ps_s[i2][:, 0:sz], 1.0 / 512, ALU.mult, r=[L + f"ps_s{i2}"], w=[mk])
            P.tt(r_[:, 0:sz], m_[:, 0:sz], m_[:, 0:sz], ALU.mult, r=[mk], w=[rk])
            P.stt(r_[:, 0:sz], ps_q[i2][:, 0:sz], 1.0 / 512, r_[:, 0:sz], ALU.mult, ALU.subtract,
                  r=[L + f"ps_q{i2}", rk], w=[rk])
            P.rsqrt(r_[:, 0:sz], r_[:, 0:sz], EPS, r=[rk], w=[rk])
            for ct in range(4):
                x_, xk = xn[cx % 3], L + f"xn{cx % 3}"
                o_, ok = so[cx % 3], L + f"so{cx % 3}"
                cx += 1
                P.tt(x_[:, 0:sz], v[:, ct, s:s + sz], m_[:, 0:sz], ALU.subtract, r=vkeys[ct] + [mk], w=[xk])
                P.tt(x_[:, 0:sz], x_[:, 0:sz], r_[:, 0:sz], ALU.mult, r=[xk, rk], w=[xk])
                P.act(o_[:, 0:sz], x_[:, 0:sz], AF.Silu, scale=lg[:, ct:ct + 1], bias=lb[:, ct:ct + 1],
                      r=[xk, L + "lg", L + "lb"], w=[ok])
                P.dma(self.SA[ct, :, s:s + sz], o_[:, 0:sz], r=[ok], w=["SA"], q="dq_pool")
        P.end_phase()

    def phase2b1(self, l):
        P, I = self.P, self.inp
        L = f"p2b1_{l}_"
        P.begin_phase()
        ident = P.sb(L + "ident", [128, 128])
        P.dma(ident[:], I["ident"], w=[L + "ident"])
        perm = P.sb(L + "perm", [32, 32])
        P.dma(perm[:], I["perm32"], w=[L + "perm"])
        ones = P.sb(L + "ones", [128, 128])
        P.memset(ones[:], 1.0, w=[L + "ones"])
        SH = P.sb(L + "SH", [32, 96], BF16)
        P.memset(SH[:], 0.0, w=[L + "SH"])
        P.copy(SH[:, 64:96], ident[0:32, 0:32], r=[L + "ident", L + "SH"], w=[L + "SH"])
        gq = P.sb(L + "gq", [128, 3])
        gkv = P.sb(L + "gkv", [128, 2])
        load_col(P, gq[:], I["g_q"][l], w=[L + "gq"])
        load_col(P, gkv[:], I["g_kv"][l], w=[L + "gkv"])
        WK = P.sb(L + "WK", [128, 2, 8, 96], BF16)
        WQ = P.sb(L + "WQ", [128, 3, 8, 96], BF16)
        WQS = P.sb(L + "WQS", [128, 3, 8, 96], BF16)
        WV = P.sb(L + "WV", [128, 2, 512], BF16)
        P.memset(WK[:], 0.0, w=[L + "WK"])
        P.memset(WQS[:], 0.0, w=[L + "WQS"])
        for c in range(2):
            P.dma(WK[:, c, :, 0:64], I["w_uk"][l, c * 128:(c + 1) * 128, :].rearrange("p (h d) -> p h d", d=64),
                  r=[L + "WK"], w=[L + "WK"], q="dq_pool")
            P.dma(WV[:, c, :], I["w_uv"][l, c * 128:(c + 1) * 128, :], w=[L + "WV"], q="dq_pool")
        for c in range(3):
            P.dma(WQ[:, c, :, 0:64], I["w_uq"][l, c * 128:(c + 1) * 128, :].rearrange("p (h d) -> p h d", d=64),
                  w=[L + "WQ"], q="dq_pool")
            P.dma(WQ[:, c, :, 64:96], I["w_qr"][l, c * 128:(c + 1) * 128, :].rearrange("p (h d) -> p h d", d=32),
                  w=[L + "WQ"], q="dq_pool")
            src = I["w_qr"][l, c * 128:(c + 1) * 128, :].rearrange("p (h a f) -> p h a f", a=4, f=8)
            for a in range(4):
                P.dma(WQS[:, c, :, 64 + a * 8:64 + a * 8 + 8], src[:, :, a ^ 1, :],
                      r=[L + "WQS"], w=[L + "WQS"], q="dq_pool")
        vx = [P.sb(L + f"vx{i}", [128, 8, 128], BF16) for i in range(2)]
        for i in range(2):
            P.memset(vx[i][:], 1.0, w=[L + f"vx{i}"])
        xin = [P.sb(L + f"xin{i}", [128, 5, 512]) for i in range(2)]
        krin = [P.sb(L + f"krin{i}", [32, 512]) for i in range(2)]
        rope = [P.sb(L + f"rope{i}", [96, 2, 512]) for i in range(2)]
        sq = P.sb(L + "sq", [128, 5, 512])
        rs = P.sb(L + "rs", [128, 2, 512])
        cx = P.sb(L + "cx", [128, 5, 512], BF16)
        krr = P.sb(L + "krr", [32, 512], BF16)
        krt = P.sb(L + "krt", [32, 2, 512])
        ps_n = [P.ps(L + f"ps_n{i}", [128, 512]) for i in range(2)]
        ps_a = [P.ps(L + f"ps_a{i}", [128, 512]) for i in range(2)]
        ps_b = [P.ps(L + f"ps_b{i}", [128, 512]) for i in range(2)]
        ps_v = [P.ps(L + f"ps_v{i}", [128, 512]) for i in range(2)]
        ko = [P.sb(L + f"ko{i}", [96, 512], BF16) for i in range(3)]
        qo = [P.sb(L + f"qo{i}", [96, 512], BF16) for i in range(3)]
        qt = [P.sb(L + f"qt{i}", [96, 2, 512]) for i in range(2)]

        def load_blk(bi):
            s, sz = BLOCKS[bi]
            b = bi % 2
            P.dma(xin[b][:, 0:2, 0:sz], self.KVD[:, :, s:s + sz].rearrange("c p t -> p c t"), w=[L + f"xin{b}"])
            P.dma(xin[b][:, 2:5, 0:sz], self.QD[:, :, s:s + sz].rearrange("c p t -> p c t"), w=[L + f"xin{b}"])
            P.dma(krin[b][:, 0:sz], self.KR[:, s:s + sz], w=[L + f"krin{b}"])
            for ci in range(2):
                P.dma(rope[b][64:96, ci, 0:sz], I["rope_cs"][ci, :, s:s + sz], w=[L + f"rope{b}"])
                P.dma(rope[b][0:32, ci, 0:sz], I["rope_cs"][ci, :, s:s + sz], w=[L + f"rope{b}"])

        load_blk(0)
        ck = cq_ = cv = 0
        for bi, (s, sz) in enumerate(BLOCKS):
            if bi + 1 < len(BLOCKS):
                load_blk(bi + 1)
            b = bi % 2
            xb, xk = xin[b], L + f"xin{b}"
            rb, rk = rope[b], L + f"rope{b}"
            for c in range(5):
                P.act(sq[:, c, 0:sz], xb[:, c, 0:sz], AF.Square, r=[xk], w=[L + "sq"])
            for gi, (c0, c1, nf) in enumerate(((0, 2, 256.0), (2, 5, 384.0))):
                pn = ps_n[gi]
                for c in range(c0, c1):
                    P.mm(pn[:, 0:sz], ones[:], sq[:, c, 0:sz], c == c0, c == c1 - 1,
                         r=[L + "sq", L + "ones"], w=[L + f"ps_n{gi}"])
                P.rsqrt(rs[:, gi, 0:sz], pn[:, 0:sz], EPS, r=[L + f"ps_n{gi}"], w=[L + f"rs{gi}"], scale=1.0 / nf)
            for c in range(5):
                gi = 0 if c < 2 else 1
                gcol = gkv[:, c:c + 1] if c < 2 else gq[:, c - 2:c - 1]
                P.stt(cx[:, c, 0:sz], xb[:, c, 0:sz], gcol, rs[:, gi, 0:sz], ALU.mult, ALU.mult,
                      r=[xk, L + "gq", L + "gkv", L + f"rs{gi}"], w=[L + f"cx{c}"])
            ckeys_kv = [L + "cx0", L + "cx1"]
            ckeys_q = [L + "cx2", L + "cx3", L + "cx4"]
            kb, kk = krin[b], L + f"krin{b}"
            P.mm(ps_n[0][0:32, 0:sz], perm[:], kb[:, 0:sz], True, True, r=[kk, L + "perm", L + "rs0"], w=[L + "ps_n0"])
            P.tt(krt[:, 0, 0:sz], kb[:, 0:sz], rb[0:32, 0, 0:sz], ALU.mult, r=[kk, rk], w=[L + "krt0"])
            P.tt(krt[:, 1, 0:sz], ps_n[0][0:32, 0:sz], rb[0:32, 1, 0:sz], ALU.mult, r=[L + "ps_n0", rk], w=[L + "krt1"])
            P.tt(krr[:, 0:sz], krt[:, 0, 0:sz], krt[:, 1, 0:sz], ALU.add, r=[L + "krt0", L + "krt1"], w=[L + "krr"])
            for h in range(8):
                pa, pk = ps_a[ck % 2], L + f"ps_a{ck % 2}"
                o_, ok = ko[ck % 3], L + f"ko{ck % 3}"
                ck += 1
                for c in range(2):
                    P.mm(pa[0:96, 0:sz], WK[:, c, h, :], cx[:, c, 0:sz], c == 0, False,
                         r=[L + "WK", ckeys_kv[c]], w=[pk])
                P.mm(pa[0:96, 0:sz], SH[:], krr[:, 0:sz], False, True, r=[L + "SH", L + "krr"], w=[pk])
                if h % 2 == 0:
                    P.copy(o_[:, 0:sz], pa[0:96, 0:sz], r=[pk], w=[ok])
                else:
                    P.copy(o_[:, 0:sz], pa[0:96, 0:sz], r=[pk], w=[ok], eng="act")
                P.dma(self.KT[h, :, s:s + sz], o_[:, 0:sz], r=[ok], w=["KT"], q="dq_pool")
            for t in range(sz // 128):
                pv, pvk = ps_v[cv % 2], L + f"ps_v{cv % 2}"
                v_, vk = vx[cv % 2], L + f"vx{cv % 2}"
                cv += 1
                for c in range(2):
                    P.mm(pv[:, :], cx[:, c, t * 128:(t + 1) * 128], WV[:, c, :], c == 0, c == 1,
                         r=[L + "WV", ckeys_kv[c]], w=[pvk])
                pv4 = pv[:, :].rearrange("p (hp two d) -> p hp two d", two=2, d=64)
                vx4 = v_[:].rearrange("p (hp two) c -> p hp two c", two=2)
                P.copy(vx4[:, :, 0, 0:64], pv4[:, :, 0, :], r=[pvk], w=[vk])
                P.copy(vx4[:, :, 1, 64:128], pv4[:, :, 1, :], r=[pvk], w=[vk], eng="act")
                kt = (s + t * 128) // 128
                P.dma(self.VX[:, :, kt, :].rearrange("h p c -> p h c"), v_[:], r=[vk], w=["VX"], q="dq_pool")
            for h in range(8):
                pa, pk = ps_a[ck % 2], L + f"ps_a{ck % 2}"
                ck += 1
                pb, pbk = ps_b[cq_ % 2], L + f"ps_b{cq_ % 2}"
                o_, ok = qo[cq_ % 3], L + f"qo{cq_ % 3}"
                q_, qk = qt[cq_ % 2], L + f"qt{cq_ % 2}"
                cq_ += 1
                for c in range(3):
                    P.mm(pa[0:96, 0:sz], WQ[:, c, h, :], cx[:, 2 + c, 0:sz], c == 0, c == 2,
                         r=[L + "WQ", ckeys_q[c]], w=[pk])
                for c in range(3):
                    P.mm(pb[0:96, 0:sz], WQS[:, c, h, :], cx[:, 2 + c, 0:sz], c == 0, c == 2,
                         r=[L + "WQS", ckeys_q[c]], w=[pbk])
                P.act(o_[0:64, 0:sz], pa[0:64, 0:sz], AF.Copy, scale=MLA_SCALE, r=[pk], w=[ok])
                P.tt(q_[64:96, 0, 0:sz], pa[64:96, 0:sz], rb[64:96, 0, 0:sz], ALU.mult, r=[pk, rk], w=[qk])
                P.stt(q_[64:96, 1, 0:sz], pb[64:96, 0:sz], MLA_SCALE, rb[64:96, 1, 0:sz], ALU.mult, ALU.mult,
                      r=[pbk, rk], w=[qk])
                P.stt(o_[64:96, 0:sz], q_[64:96, 0, 0:sz], MLA_SCALE, q_[64:96, 1, 0:sz], ALU.mult, ALU.add,
                      r=[qk], w=[ok])
                P.dma(self.QT[h, :, s:s + sz], o_[:, 0:sz], r=[ok], w=["QT"], q="dq_pool")
        P.end_phase()

    def phase2b2(self, l):
        P, I = self.P, self.inp
        L = f"p2b2_{l}_"
        P.begin_phase()
        kT = [P.sb(L + f"kT{i}", [96, T], BF16) for i in range(2)]
        qT = [P.sb(L + f"qT{i}", [96, T], BF16) for i in range(2)]
        vx = [P.sb(L + f"vx{i}", [128, 34, 128], BF16) for i in range(2)]
        NP_ = 4
        pT = [P.sb(L + f"pT{i}", [128, 512], BF16) for i in range(NP_)]
        ps_s = [P.ps(L + f"ps_s{i}", [128, 512]) for i in range(4)]
        ps_o = [P.ps(L + f"ps_o{i}", [128, 512]) for i in range(2)]
        rec = [P.sb(L + f"rec{i}", [128, 512]) for i in range(2)]
        oo = [P.sb(L + f"oo{i}", [128, 512], BF16) for i in range(2)]

        def load_head(h):
            b = h % 2
            P.dma(kT[b][:], self.KT[h], w=[L + f"kT{b}"])
            P.dma(qT[b][:], self.QT[h], w=[L + f"qT{b}"])
            P.dma(vx[b][:], self.VX[h], w=[L + f"vx{b}"])

        load_head(0)
        LA = 2
        items = []
        for h in range(8):
            for bi, (s, sz) in enumerate(BLOCKS):
                nkt = 2 if bi == 0 else 34
                for kt in range(nkt):
                    items.append((h, bi, kt, nkt))
        loaded = {0}
        co_of = {}
        co = 0
        for h in range(8):
            for bi in range(len(BLOCKS)):
                co_of[(h, bi)] = co
                co += 1

        def emit_s(i):
            h, bi, kt, nkt = items[i]
            s, sz = BLOCKS[bi]
            b = h % 2
            ps, psk = ps_s[i % 4], L + f"ps_s{i % 4}"
            p_, pk = pT[i % NP_], L + f"pT{i % NP_}"
            P.mm(ps[:, 0:sz], kT[b][:, kt * 128:(kt + 1) * 128], qT[b][:, s:s + sz], True, True,
                 r=[L + f"kT{b}", L + f"qT{b}"], w=[psk])
            P.act(p_[:, 0:sz], ps[:, 0:sz], AF.Exp, r=[psk], w=[pk])

        def emit_pv(i):
            h, bi, kt, nkt = items[i]
            s, sz = BLOCKS[bi]
            if h + 1 < 8 and (h + 1) not in loaded and bi == 0 and kt == 0:
                loaded.add(h + 1)
                load_head(h + 1)
            b = h % 2
            c_ = co_of[(h, bi)]
            po, pok = ps_o[c_ % 2], L + f"ps_o{c_ % 2}"
            p_, pk = pT[i % NP_], L + f"pT{i % NP_}"
            P.mm(po[:, 0:sz], vx[b][:, kt, :], p_[:, 0:sz], kt == 0, kt == nkt - 1, r=[L + f"vx{b}", pk], w=[pok])
            if kt == nkt - 1:
                r_, rk = rec[c_ % 2], L + f"rec{c_ % 2}"
                o_, ok = oo[c_ % 2], L + f"oo{c_ % 2}"
                if h % 2 == 0:
                    num, den, dst = po[0:64, 0:sz], po[64:128, 0:sz], slice(0, 64)
                else:
                    num, den, dst = po[64:128, 0:sz], po[0:64, 0:sz], slice(64, 128)
                P.recip(r_[dst, 0:sz], den, r=[pok], w=[rk])
                P.tt(o_[dst, 0:sz], num, r_[dst, 0:sz], ALU.mult, r=[pok, rk], w=[ok])
                P.dma(self.OB[h // 2, dst, s:s + sz], o_[dst, 0:sz], r=[ok], w=["OB"], q="dq_pool")

        n_it = len(items)
        for i in range(n_it + LA):
            if i < n_it:
                emit_s(i)
            if i >= LA:
                emit_pv(i - LA)
        P.end_phase()

    def phase2c1(self, l):
        P, I = self.P, self.inp
        L = f"p2c1_{l}_"
        P.begin_phase()
        ident = P.sb(L + "ident", [128, 128])
        P.dma(ident[:], I["ident"], w=[L + "ident"])
        bones = P.sb(L + "bones", [128, 128])
        P.memset(bones[:], 0.0, w=[L + "bones"])
        P.memset(bones[0:64, 0:64], 1.0, w=[L + "bones"])
        P.memset(bones[64:128, 64:128], 1.0, w=[L + "bones"])
        cw = P.sb(L + "cw", [128, 12, 5])
        for i in range(12):
            P.dma(cw[:, i, :], I["conv_c_w"][l][:, i * 128:(i + 1) * 128].rearrange("k p -> p k"),
                  w=[L + "cw"], slow=True)
        UB = T + 8
        ub = [P.sb(L + f"ub{i}", [128, UB]) for i in range(2)]
        for i in range(2):
            P.memset(ub[i][:, 0:2], 0.0, w=[L + f"ub{i}"])
            P.memset(ub[i][:, 258:262], 0.0, w=[L + f"ub{i}"])
            P.memset(ub[i][:, UB - 2:UB], 0.0, w=[L + f"ub{i}"])
        xc = [P.sb(L + f"xc{i}", [128, T]) for i in range(2)]
        sq = [P.sb(L + f"sq{i}", [128, 512]) for i in range(2)]
        rn = [P.sb(L + f"rn{i}", [128, 512]) for i in range(2)]
        xo = [P.sb(L + f"xo{i}", [128, 512]) for i in range(3)]
        tk = [P.sb(L + f"tk{i}", [128, 4, 128]) for i in range(2)]
        ps_n = [P.ps(L + f"ps_n{i}", [128, 512]) for i in range(2)]
        ps_t = [P.ps(L + f"ps_t{i}", [128, 512]) for i in range(2)]
        cn = ct_ = cx_ = 0
        for i in range(12):
            u, uk = ub[i % 2], L + f"ub{i % 2}"
            x_, xk = xc[i % 2], L + f"xc{i % 2}"
            P.dma(u[:, 2:258], self.GQKV[i, :, 0:NCTX], w=[uk])
            P.dma(u[:, 262:262 + NL], self.GQKV[i, :, NCTX:T], w=[uk])
            SPL = 2944
            for (o0, n_, base, eng_, sfx) in ((0, NCTX, 0, "dve", "c"), (NCTX, SPL, 260, "dve", "a"),
                                             (NCTX + SPL, NL - SPL, 260 + SPL, "dve", "b")):
                vo = x_[:, o0:o0 + n_]
                P.ts(vo, u[:, base:base + n_], cw[:, i, 0:1], ALU.mult, r=[uk, L + "cw"], w=[xk], eng=eng_)
                for k in range(1, 5):
                    P.stt(vo, u[:, base + k:base + k + n_], cw[:, i, k:k + 1], vo, ALU.mult, ALU.add,
                          r=[uk, L + "cw"], w=[xk], eng=eng_)
            P.act(x_[:], x_[:], AF.Silu, r=[xk], w=[xk])
            for bi, (s, sz) in enumerate(BLOCKS):
                if i < 8:
                    j = cn % 2
                    cn += 1
                    o_, ok = xo[cx_ % 3], L + f"xo{cx_ % 3}"
                    cx_ += 1
                    P.act(sq[j][:, 0:sz], x_[:, s:s + sz], AF.Square, r=[xk], w=[L + f"sq{j}"])
                    P.mm(ps_n[j][:, 0:sz], bones[:], sq[j][:, 0:sz], True, True,
                         r=[L + "bones", L + f"sq{j}"], w=[L + f"ps_n{j}"])
                    P.rsqrt(rn[j][:, 0:sz], ps_n[j][:, 0:sz], EPS, r=[L + f"ps_n{j}"], w=[L + f"rn{j}"])
                    if i < 4:
                        P.stt(o_[:, 0:sz], x_[:, s:s + sz], 0.125, rn[j][:, 0:sz], ALU.mult, ALU.mult,
                              r=[xk, L + f"rn{j}"], w=[ok])
                    else:
                        P.tt(o_[:, 0:sz], x_[:, s:s + sz], rn[j][:, 0:sz], ALU.mult, r=[xk, L + f"rn{j}"], w=[ok])
                    P.dma(self.QKN[i, :, s:s + sz], o_[:, 0:sz], r=[ok], w=["QKN"], q="dq_pool")
                    src, sk = o_, ok
                    soff = 0
                else:
                    src, sk = x_, xk
                    soff = s
                if i >= 4:
                    j = ct_ % 2
                    ct_ += 1
                    nt = sz // 128
                    for t in range(nt):
                        P.tr(ps_t[j][:, t * 128:(t + 1) * 128], src[:, soff + t * 128:soff + (t + 1) * 128], ident[:],
                             r=[sk, L + "ident"], w=[L + f"ps_t{j}"])
                    P.copy(tk[j][:, 0:nt, :], ps_t[j][:, 0:sz].rearrange("p (t f) -> p t f", f=128),
                           r=[L + f"ps_t{j}"], w=[L + f"tk{j}"], eng="act")
                    kv = 0 if i < 8 else 1
                    f0 = ((i - 4) % 4) * 128
                    P.dma(self.KVtok[s:s + sz, kv, f0:f0 + 128].rearrange("(t p) f -> p t f", p=128),
                          tk[j][:, 0:nt, :], r=[L + f"tk{j}"], w=["KVtok"], q="dq_pool")
        bg = [P.sb(L + f"bg{i}", [16, 2, 512]) for i in range(2)]
        bo = [P.sb(L + f"bo{i}", [128, 4, 32]) for i in range(2)]
        for bi, (s, sz) in enumerate(BLOCKS):
            j = bi % 2
            nt = sz // 128
            P.dma(bg[j][:, 0, 0:sz], self.BETA[:, s:s + sz], w=[L + f"bg{j}"])
            P.dma(bg[j][:, 1, 0:sz], self.GL[:, s:s + sz], w=[L + f"bg{j}"])
            for t in range(nt):
                for z in range(2):
                    P.tr(ps_t[j][:, t * 32 + z * 16:t * 32 + z * 16 + 16], bg[j][:, z, t * 128:(t + 1) * 128],
                         ident[0:16, 0:16], r=[L + f"bg{j}", L + "ident"], w=[L + f"ps_t{j}"])
            P.copy(bo[j][:, 0:nt, :], ps_t[j][:, 0:nt * 32].rearrange("p (t f) -> p t f", f=32),
                   r=[L + f"ps_t{j}"], w=[L + f"bo{j}"])
            P.dma(self.BGtok[s:s + sz, :].rearrange("(t p) f -> p t f", p=128), bo[j][:, 0:nt, :],
                  r=[L + f"bo{j}"], w=["BGtok"], q="dq_pool")
        P.end_phase()

    @staticmethod
    def gdn_chunk(n, d):
        return n if d == 0 else (3 - n if n < 4 else 71 - n)

    def phase2c2(self, l, nsteps=68):
        P, I = self.P, self.inp
        L = f"p2c2_{l}_"
        P.begin_phase()
        ident = P.sb(L + "ident", [128, 128])
        P.dma(ident[:], I["ident"], w=[L + "ident"])
        msk = P.sb(L + "msk", [64, 4, 64])
        P.dma(msk[:], I["gmask"].rearrange("m p f -> p m f"), w=[L + "msk"])
        ones = P.sb(L + "ones", [64, 64])
        P.memset(ones[:], 1.0, w=[L + "ones"])
        CK = [L + "ident", L + "msk", L + "ones"]
        TRI = (2, 0)
        STRICT = (1, 3)
        INCLT = (2, 0)
        INCL = (0, 2)

        def bc_u(ap2):
            return ap2.unsqueeze(2).broadcast_to([ap2.shape[0], 8, 64])

        def bc_m(mi):
            return msk[:, mi, :].unsqueeze(1).broadcast_to([64, 8, 64])

        def v3(ap, np_=64):
            return ap.rearrange("p (u f) -> p u f", f=64)

        D_ = {}
        for d in range(2):
            X = {}
            for nm, shp, dt in (("bgt", [64, 32], F32), ("ktok", [64, 512], F32), ("vtok", [64, 512], F32),
                                ("kT", [64, 8, 64], F32), ("qT", [64, 8, 64], F32)):
                X[nm] = [P.sb(L + f"{nm}{d}_{b}", shp, dt) for b in range(2)]
            for nm, shp, dt in (("gam", [64, 8], F32), ("gtot", [64, 8], F32), ("eg", [64, 8], F32),
                                ("bgv", [64, 8], F32), ("dif", [64, 8], F32), ("ekd", [64, 8], F32),
                                ("gcP", [64, 8], F32), ("nbeta", [64, 8], F32),
                                ("Y", [64, 512], F32), ("diff", [64, 512], F32), ("E", [64, 512], F32),
                                ("ET", [64, 512], F32), ("egr", [64, 512], F32), ("Dst", [64, 512], F32),
                                ("DinT", [64, 512], F32), ("A0", [64, 512], F32), ("B0", [64, 512], F32),
                                ("R", [64, 512], F32), ("Pa", [64, 512], F32), ("PTa", [64, 512], F32),
                                ("Pb", [64, 512], F32), ("PTb", [64, 512], F32),
                                ("Pa16", [64, 512], BF16), ("PTa16", [64, 512], BF16),
                                ("Pb16", [64, 512], BF16), ("PTb16", [64, 512], BF16), ("R16", [64, 512], BF16),
                                ("pmT", [64, 512], BF16), ("bgK", [64, 512], BF16), ("betaV", [64, 512], BF16),
                                ("kd", [64, 512], BF16), ("TTb", [64, 512], BF16), ("wT", [64, 512], BF16),
                                ("u0", [64, 512], F32), ("qgT", [64, 512], BF16)):
                X[nm] = P.sb(L + f"{nm}{d}", shp, dt)
            X["ps0"] = P.ps(L + f"ps0{d}", [128, 512])
            for i in (1, 2, 3):
                X[f"ps{i}"] = P.ps(L + f"ps{i}{d}", [128, 512])
            D_[d] = X

        def K_(d, nm):
            return L + f"{nm}{d}"

        def loads(n, d):
            X = D_[d]
            b = n % 2
            c = self.gdn_chunk(n, d)
            t0 = 64 * c
            P.dma(X["bgt"][b][:], self.BGtok[t0:t0 + 64, :], w=[K_(d, f"bgt{b}")])
            P.dma(X["ktok"][b][:], self.KVtok[t0:t0 + 64, 0, :], w=[K_(d, f"ktok{b}")])
            P.dma(X["vtok"][b][:], self.KVtok[t0:t0 + 64, 1, :], w=[K_(d, f"vtok{b}")])
            qkn = self.QKN.rearrange("t p c -> (t p) c")
            P.dma(X["qT"][b][:], qkn[0:512, t0:t0 + 64].rearrange("(u p) c -> p u c", p=64), w=[K_(d, f"qT{b}")])
            P.dma(X["kT"][b][:], qkn[512:1024, t0:t0 + 64].rearrange("(u p) c -> p u c", p=64), w=[K_(d, f"kT{b}")])

        def stage_a(n, d):
            X = D_[d]
            b = n % 2
            k = lambda nm: K_(d, nm)
            bgt, ktok, vtok, kT, qT = (X[nm][b] for nm in ("bgt", "ktok", "vtok", "kT", "qT"))
            kb = lambda nm: K_(d, f"{nm}{b}")
            beta = bgt[:, d * 8:d * 8 + 8]
            g = bgt[:, 16 + d * 8:16 + d * 8 + 8]
            ps0, ps1, ps2, ps3 = X["ps0"], X["ps1"], X["ps2"], X["ps3"]
            P.mm(ps0[0:64, 0:8], msk[:, TRI[d], :], g, True, True, r=[kb("bgt")] + CK, w=[k("ps0")])
            P.mm(ps0[0:64, 8:16], ones[:], g, True, True, r=[kb("bgt")] + CK, w=[k("ps0")])
            P.copy(X["gam"][:], ps0[0:64, 0:8], r=[k("ps0")], w=[k("gam")], eng="act")
            P.copy(X["gtot"][:], ps0[0:64, 8:16], r=[k("ps0")], w=[k("gtot")], eng="act")
            P.act(X["eg"][:], X["gam"][:], AF.Exp, r=[k("gam")], w=[k("eg")])
            P.tt(X["bgv"][:], X["eg"][:], beta, ALU.mult, r=[k("eg"), kb("bgt")], w=[k("bgv")])
            P.tt(X["dif"][:], X["gtot"][:], X["gam"][:], ALU.subtract, r=[k("gtot"), k("gam")], w=[k("dif")])
            P.act(X["ekd"][:], X["dif"][:], AF.Exp, r=[k("dif")], w=[k("ekd")])
            P.act(X["gcP"][:], X["gtot"][:], AF.Exp, r=[k("gtot")], w=[k("gcP")])
            P.ts(X["nbeta"][:], beta, -1.0, ALU.mult, r=[kb("bgt")], w=[k("nbeta")])
            P.tt(v3(X["Y"][:]), bc_u(g), bc_m(TRI[d]), ALU.mult, r=[kb("bgt")] + CK, w=[k("Y")], eng="pool")
            P.mm(ps1[0:64, :], ones[:], X["Y"][:], True, True, r=[k("Y")] + CK, w=[k("ps1")])
            P.tt(v3(X["diff"][:]), bc_u(X["gam"][:]), v3(ps1[0:64, :]), ALU.subtract,
                 r=[k("gam"), k("ps1")], w=[k("diff")])
            P.tt(v3(X["E"][:]), v3(X["diff"][:]), bc_m(INCL[d]), ALU.mult, r=[k("diff")] + CK, w=[k("E")], eng="pool")
            P.tt(v3(X["ET"][:]), v3(X["diff"][:]), bc_m(INCLT[d]), ALU.mult, r=[k("diff")] + CK, w=[k("ET")], eng="pool")
            P.act(X["E"][:], X["E"][:], AF.Exp, r=[k("E")], w=[k("E")])
            P.act(X["ET"][:], X["ET"][:], AF.Exp, scale=-1.0, r=[k("ET")], w=[k("ET")])
            P.act(X["egr"][:], ps1[0:64, :], AF.Exp, r=[k("ps1")], w=[k("egr")])
            P.tt(v3(X["Dst"][:]), v3(X["E"][:]), bc_m(STRICT[d]), ALU.min, r=[k("E")] + CK, w=[k("Dst")])
            P.tt(v3(X["DinT"][:]), v3(X["ET"][:]), bc_m(INCLT[d]), ALU.min, r=[k("ET")] + CK, w=[k("DinT")])
            for u in range(8):
                P.mm(ps2[0:64, u * 64:(u + 1) * 64], kT[:, u, :], kT[:, u, :],
                     True, True, r=[kb("kT")], w=[k("ps2")])
            for u in range(8):
                P.mm(ps3[0:64, u * 64:(u + 1) * 64], kT[:, u, :], qT[:, u, :],
                     True, True, r=[kb("kT"), kb("qT")], w=[k("ps3")])
            P.tt(X["A0"][:], ps2[0:64, :], X["Dst"][:], ALU.mult, r=[k("ps2"), k("Dst")], w=[k("A0")])
            P.tt(v3(X["A0"][:]), v3(X["A0"][:]), bc_u(X["nbeta"][:]), ALU.mult, r=[k("A0"), k("nbeta")], w=[k("A0")],
                 eng="pool")
            P.tt(X["pmT"][:], ps3[0:64, :], X["DinT"][:], ALU.mult, r=[k("ps3"), k("DinT")], w=[k("pmT")])
            P.tt(v3(X["bgK"][:]), v3(ktok[:]), bc_u(X["bgv"][:]), ALU.mult, r=[kb("ktok"), k("bgv")],
                 w=[k("bgK")], eng="pool")
            P.tt(v3(X["betaV"][:]), v3(vtok[:]), bc_u(beta), ALU.mult, r=[kb("vtok"), kb("bgt")],
                 w=[k("betaV")], eng="pool")
            P.tt(v3(X["kd"][:]), v3(ktok[:]), bc_u(X["ekd"][:]), ALU.mult, r=[kb("ktok"), k("ekd")],
                 w=[k("kd")], eng="pool")
            P.tt(v3(X["qgT"][:]), qT[:], v3(X["egr"][:]), ALU.mult, r=[kb("qT"), k("egr")], w=[k("qgT")], eng="pool")
            for u in range(8):
                P.tr(ps1[0:64, u * 64:(u + 1) * 64], X["A0"][:, u * 64:(u + 1) * 64], ident[0:64, 0:64],
                     r=[k("A0")] + CK, w=[k("ps1")])
            P.copy(X["B0"][:], ps1[0:64, :], r=[k("ps1")], w=[k("B0")], eng="act")
            P.tt(v3(X["R"][:]), v3(X["B0"][:]), ident[0:64, 0:64].unsqueeze(1).broadcast_to([64, 8, 64]), ALU.add,
                 r=[k("B0")] + CK, w=[k("R")])

        def stage_lev(n, d, lev):
            X = D_[d]
            k = lambda nm: K_(d, nm)
            ps1, ps2, ps3 = X["ps1"], X["ps2"], X["ps3"]
            lo = False
            lo_out = False
            sfx_in = "16" if lo else ""
            sfx_out = "16" if lo_out else ""
            names = [("B0", "A0"), ("Pa", "PTa"), ("Pb", "PTb")]
            pn, ptn = names[0] if lev == 0 else names[1 + (lev - 1) % 2]
            qn, qtn = names[1 + lev % 2]
            if lev > 0:
                pn, ptn = pn + sfx_in, ptn + sfx_in
            qn, qtn = qn + sfx_out, qtn + sfx_out
            Pm, PTm, Pn, PTn = X[pn], X[ptn], X[qn], X[qtn]
            Rn = "R16" if lo else "R"
            need_p = lev < 4
            need_pt = lev < 5
            if lev >= 1:
                for u in range(8):
                    sl = slice(u * 64, (u + 1) * 64)
                    P.mm(ps1[0:64, sl], PTm[:, sl], X[Rn][:, sl], True, True, r=[k(ptn), k(Rn)], w=[k("ps1")])
            if need_p:
                for u in range(8):
                    sl = slice(u * 64, (u + 1) * 64)
                    P.mm(ps2[0:64, sl], PTm[:, sl], Pm[:, sl], True, True, r=[k(pn), k(ptn)], w=[k("ps2")])
            if need_pt:
                for u in range(8):
                    sl = slice(u * 64, (u + 1) * 64)
                    P.mm(ps3[0:64, sl], Pm[:, sl], PTm[:, sl], True, True, r=[k(pn), k(ptn)], w=[k("ps3")])
            if lev >= 1:
                P.tt(X["R"][:], X["R"][:], ps1[0:64, :], ALU.add, r=[k("R"), k("ps1")], w=[k("R")])
            if need_p:
                P.copy(Pn[:], ps2[0:64, :], r=[k("ps2")], w=[k(qn)], eng="act")
            if need_pt:
                P.copy(PTn[:], ps3[0:64, :], r=[k("ps3")], w=[k(qtn)], eng="act")

        def stage_z(n, d):
            X = D_[d]
            k = lambda nm: K_(d, nm)
            ps2, ps3 = X["ps2"], X["ps3"]
            P.copy(X["TTb"][:], X["R"][:], r=[k("R")], w=[k("TTb")], eng="act")
            for u in range(8):
                hp = u // 2
                sl = slice(u * 64, (u + 1) * 64)
                P.mm(ps3[0:64, sl], X["TTb"][:, sl], X["betaV"][:, sl], True, True,
                     r=[k("betaV"), k("TTb")], w=[k("ps3")])
                P.mm(ps2[0:64, sl], X["bgK"][:, sl], X["TTb"][:, sl], True, True,
                     r=[k("bgK"), k("TTb")], w=[k("ps2")])
            P.copy(X["wT"][:], ps2[0:64, :], r=[k("ps2")], w=[k("wT")], eng="act")
            P.copy(X["u0"][:], ps3[0:64, :], r=[k("ps3")], w=[k("u0")], eng="act")
            P.dma(self.PW[n, d], X["wT"][:], r=[k("wT")], w=["PW"], q="dq_pool")
            P.dma(self.PQ[n, d], X["qgT"][:], r=[k("qgT")], w=["PQ"], q="dq_pool")
            P.dma(self.PK[n, d], X["kd"][:], r=[k("kd")], w=["PK"], q="dq_pool")
            P.dma(self.PM[n, d], X["pmT"][:], r=[k("pmT")], w=["PM"], q="dq_pool")
            P.dma(self.PU[n, d], X["u0"][:], r=[k("u0")], w=["PU"], q="dq_pool")
            P.dma(self.PG[n, d], X["gcP"][:], r=[k("gcP")], w=["PG"], q="dq_pool")

        for d in range(2):
            loads(0, d)
        for n in range(nsteps):
            if n + 1 < nsteps:
                for d in range(2):
                    loads(n + 1, d)
            for d in range(2):
                stage_a(n, d)
            for lev in range(6):
                for d in range(2):
                    stage_lev(n, d, lev)
            for d in range(2):
                stage_z(n, d)
        P.end_phase()

    def phase2c3(self, l, nsteps=68):
        P, I = self.P, self.inp
        L = f"p2c3_{l}_"
        P.begin_phase()
        NB = 3
        IN = {}
        for nm, dt in (("wT", BF16), ("qgT", BF16), ("kd", BF16), ("pmT", BF16), ("u0", F32)):
            IN[nm] = [P.sb(L + f"{nm}{b}", [64, 2, 512], dt) for b in range(NB)]
        IN["gc"] = [P.sb(L + f"gc{b}", [64, 2, 8], F32) for b in range(NB)]
        SRC = dict(wT=self.PW, qgT=self.PQ, kd=self.PK, pmT=self.PM, u0=self.PU, gc=self.PG)
        S = P.sb(L + "S", [64, 16, 64])
        Sb = P.sb(L + "Sb", [64, 16, 64], BF16)
        u = P.sb(L + "u", [64, 2, 512], BF16)
        tmpS = P.sb(L + "tmpS", [64, 16, 64])
        osb = [P.sb(L + f"osb{i}", [64, 2, 512]) for i in range(2)]
        ps_u = [P.ps(L + f"ps_u{d}", [128, 512]) for d in range(2)]
        ps_o = [P.ps(L + f"ps_o{d}", [128, 512]) for d in range(2)]
        ps_S = [P.ps(L + f"ps_S{d}", [128, 512]) for d in range(2)]
        P.memset(S[:], 0.0, w=[L + "S0", L + "S1"])
        P.memset(Sb[:], 0.0, w=[L + "Sb0", L + "Sb1"])

        def loads(n):
            b = n % NB
            for nm in ("wT", "qgT", "kd", "pmT", "u0", "gc"):
                for d in range(2):
                    P.dma(IN[nm][b][:, d, :], SRC[nm][n, d], w=[L + f"{nm}{b}_{d}"])

        loads(0)
        if nsteps > 1:
            loads(1)
        for n in range(nsteps):
            if n + 2 < nsteps:
                loads(n + 2)
            b = n % NB
            kin = lambda nm, d: L + f"{nm}{b}_{d}"
            wT, qgT, kd, pmT, u0, gc = (IN[nm][b] for nm in ("wT", "qgT", "kd", "pmT", "u0", "gc"))
            ob, obk = osb[n % 2], L + f"osb{n % 2}"
            for d in range(2):
                for h in range(8):
                    sl = slice(h * 64, (h + 1) * 64)
                    P.mm(ps_u[d][0:64, sl], wT[:, d, sl], Sb[:, d * 8 + h, :], True, True,
                         r=[kin("wT", d), L + f"Sb{d}"], w=[L + f"ps_u{d}"])
            for d in range(2):
                P.tt(u[:, d, :], u0[:, d, :], ps_u[d][0:64, :], ALU.subtract,
                     r=[kin("u0", d), L + f"ps_u{d}"], w=[L + f"u{d}"])
            for d in range(2):
                P.tt(tmpS[:, d * 8:(d + 1) * 8, :], S[:, d * 8:(d + 1) * 8, :],
                     gc[:, d, :].unsqueeze(2).broadcast_to([64, 8, 64]), ALU.mult,
                     r=[kin("gc", d), L + f"S{d}"], w=[L + f"tmpS{d}"], eng="pool")
            for d in range(2):
                for h in range(8):
                    sl = slice(h * 64, (h + 1) * 64)
                    P.mm(ps_o[d][0:64, sl], qgT[:, d, sl], Sb[:, d * 8 + h, :], True, False,
                         r=[kin("qgT", d), L + f"Sb{d}"], w=[L + f"ps_o{d}"])
                    P.mm(ps_o[d][0:64, sl], pmT[:, d, sl], u[:, d, sl], False, True,
                         r=[kin("pmT", d), L + f"u{d}"], w=[L + f"ps_o{d}"])
                for h in range(8):
                    sl = slice(h * 64, (h + 1) * 64)
                    P.mm(ps_S[d][0:64, sl], kd[:, d, sl], u[:, d, sl], True, True,
                         r=[kin("kd", d), L + f"u{d}"], w=[L + f"ps_S{d}"])
            for d in range(2):
                Sd = S[:, d * 8:(d + 1) * 8, :]
                td = tmpS[:, d * 8:(d + 1) * 8, :]
                pS3 = ps_S[d][0:64, :].rearrange("p (u f) -> p u f", f=64)
                P.tt(Sb[:, d * 8:(d + 1) * 8, :], td, pS3, ALU.add, r=[L + f"ps_S{d}", L + f"tmpS{d}"],
                     w=[L + f"Sb{d}"])
                P.tt(Sd, td, pS3, ALU.add, r=[L + f"ps_S{d}", L + f"tmpS{d}"], w=[L + f"S{d}"])
                P.copy(ob[:, d, :], ps_o[d][0:64, :], r=[L + f"ps_o{d}"], w=[obk + f"_{d}"], eng="act")
                c = self.gdn_chunk(n, d)
                P.dma(self.ODIR[d, c * 64:(c + 1) * 64, :], ob[:, d, :], r=[obk + f"_{d}"], w=["ODIR"], q="dq_pool")
        P.end_phase()

    def phase2c4(self, l):
        P, I = self.P, self.inp
        L = f"p2c4_{l}_"
        P.begin_phase()
        ident = P.sb(L + "ident", [128, 128])
        P.dma(ident[:], I["ident"], w=[L + "ident"])
        go = P.sb(L + "go", [128, 1])
        for hh in range(2):
            P.dma(go[hh * 64:(hh + 1) * 64, :], I["g_o"][l, :].rearrange("(p o) -> p o", o=1), w=[L + "go"], slow=True)
        oin = [P.sb(L + f"oin{i}", [128, 2, 4, 512]) for i in range(2)]
        ogin = [P.sb(L + f"ogin{i}", [128, 4, 512], BF16) for i in range(2)]
        osum = P.sb(L + "osum", [128, 4, 512])
        sq = P.sb(L + "sq", [128, 512])
        ms = P.sb(L + "ms", [128, 4, 8])
        ps_t = [P.ps(L + f"ps_t{i}", [128, 512]) for i in range(2)]
        oc = [P.sb(L + f"oc{i}", [128, 512], BF16) for i in range(3)]

        def loads(bi):
            s, sz = BLOCKS[bi]
            nt = sz // 128
            b = bi % 2
            for d in range(2):
                P.dma(oin[b][:, d, 0:nt, :], self.ODIR[d, s:s + sz, :].rearrange("(t p) f -> p t f", p=128),
                      w=[L + f"oin{b}"])
            P.dma(ogin[b][:, :, 0:sz], self.OG[:, :, s:s + sz].rearrange("c p t -> p c t"), w=[L + f"ogin{b}"])

        loads(0)
        ct_ = co = 0
        for bi, (s, sz) in enumerate(BLOCKS):
            if bi + 1 < len(BLOCKS):
                loads(bi + 1)
            b = bi % 2
            nt = sz // 128
            P.tt(osum[:, 0:nt, :], oin[b][:, 0, 0:nt, :], oin[b][:, 1, 0:nt, :], ALU.add,
                 r=[L + f"oin{b}"], w=[L + "osum"], eng="pool")
            for t in range(nt):
                P.act(sq[:], osum[:, t, :], AF.Square, r=[L + "osum"], w=[L + "sq"])
                P.op("dve", (lambda t=t: self.P.nc.vector.tensor_reduce(
                    out=ms[:, t, :], in_=sq[:].rearrange("p (h f) -> p h f", f=64), axis=AX.X, op=ALU.add)),
                    r=[L + "sq"], w=[L + "ms"])
            P.rsqrt(ms[:, 0:nt, :], ms[:, 0:nt, :], EPS, r=[L + "ms"], w=[L + "ms"], scale=1.0 / 64)
            for t in range(nt):
                o3 = osum[:, t, :].rearrange("p (h f) -> p h f", f=64)
                P.tt(o3, o3, ms[:, t, :].unsqueeze(2).broadcast_to([128, 8, 64]), ALU.mult,
                     r=[L + "osum", L + "ms"], w=[L + "osum"])
            for ft in range(4):
                pt, ptk = ps_t[ct_ % 2], L + f"ps_t{ct_ % 2}"
                ct_ += 1
                for t in range(nt):
                    P.tr(pt[:, t * 128:(t + 1) * 128], osum[:, t, ft * 128:(ft + 1) * 128], ident[:],
                         r=[L + "osum", L + "ident"], w=[ptk])
                o_, ok = oc[co % 3], L + f"oc{co % 3}"
                co += 1
                P.stt(o_[:, 0:sz], pt[:, 0:sz], go[:, 0:1], ogin[b][:, ft, 0:sz], ALU.mult, ALU.mult,
                      r=[ptk, L + "go", L + f"ogin{b}"], w=[ok])
                P.dma(self.OC[ft, :, s:s + sz], o_[:, 0:sz], r=[ok], w=["OC"], q="dq_pool")
        P.end_phase()

    def bcast_row(self, dst, src_row, w):
        self.P.dma(dst, src_row.broadcast_to([128, src_row.shape[-1]]), w=w, slow=True)

    def phase3(self, l, xsrc_l, xsrc_c):
        P, I = self.P, self.inp
        L = f"p3_{l}_"
        P.begin_phase()
        ident = P.sb(L + "ident", [128, 128])
        P.dma(ident[:], I["ident"], w=[L + "ident"])
        WA = P.sb(L + "WA", [128, 4, D], BF16)
        WB = P.sb(L + "WB", [128, 4, D], BF16)
        WC = P.sb(L + "WC", [128, 4, D], BF16)
        WO = P.sb(L + "WO", [128, 8, D], BF16)
        for wt, nm, kc in ((WA, "w_a_out", 4), (WB, "w_b_out", 4), (WC, "w_c_out", 4), (WO, "w_out", 8)):
            for c in range(kc):
                P.dma(wt[:, c, :], I[nm][l, c * 128:(c + 1) * 128, :], w=[L + f"W_{nm}_{c}"], q="dq_pool")
        Wr = P.sb(L + "Wr", [128, 8, 36])
        P.dma(Wr[:, :, 0:4], I["w_rg"][l].rearrange("(k p) n -> p k n", p=128), w=[L + "Wr"], slow=True)
        P.dma(Wr[:, :, 4:36], I["w_re"][l].rearrange("(k p) n -> p k n", p=128), w=[L + "Wr"], slow=True)
        br = P.sb(L + "br", [128, 36])
        self.bcast_row(br[:, 0:4], I["b_rg"][l:l + 1, :], [L + "br"])
        self.bcast_row(br[:, 4:36], I["b_re"][l:l + 1, :], [L + "br"])
        rows = P.sb(L + "rows", [128, 2, 3, D])
        for rr in range(2):
            for i_, v_ in enumerate((2, 4, 3)):
                self.bcast_row(rows[:, rr, i_, :], self.modrow[l][rr:rr + 1, v_ * D:(v_ + 1) * D], [L + "rows"])
        P.ts(rows[:, :, 1, :], rows[:, :, 1, :], 1.0, ALU.add, r=[L + "rows"], w=[L + "rows"])
        lnr = P.sb(L + "lnr", [128, 2, D])
        self.bcast_row(lnr[:, 0, :], I["ln1_g"][l:l + 1, :], [L + "lnr"])
        self.bcast_row(lnr[:, 1, :], I["ln1_b"][l:l + 1, :], [L + "lnr"])
        CK = [L + "Wr", L + "br", L + "rows", L + "lnr", L + "ident"]

        sa = [P.sb(L + f"sa{i}", [128, 3, 4, 512], BF16) for i in range(2)]
        mg0 = P.sb(L + "mg0", [128, 24, 512], BF16)
        mg = [mg0, mg0]
        xt0 = P.sb(L + "xt0", [128, 4, D])
        xt = [xt0, xt0]
        m = P.sb(L + "m", [128, 8, 512], BF16)
        t1 = P.sb(L + "t1", [128, 512])
        t2 = P.sb(L + "t2", [128, 512])
        z = P.sb(L + "z", [128, D])
        zz = P.sb(L + "zz", [128, D])
        x1 = [P.sb(L + f"x1{i}", [128, D]) for i in range(2)]
        h2 = P.sb(L + "h2", [128, D])
        h2Tf = P.sb(L + "h2Tf", [128, 8, 128])
        h2Tb = [P.sb(L + f"h2Tb{i}", [128, 8, 128], BF16) for i in range(2)]
        st = P.sb(L + "st", [128, 8])
        rt = P.sb(L + "rt", [128, 256])
        gate = [P.sb(L + f"gate{i}", [128, 32]) for i in range(2)]
        ps_y = [P.ps(L + f"ps_y{i}", [128, 512]) for i in range(3)]
        ps_out = [P.ps(L + f"ps_out{i}", [128, 512]) for i in range(2)]
        ps_t = [P.ps(L + f"ps_t{i}", [128, 512]) for i in range(2)]
        ps_r = P.ps(L + "ps_r", [128, 512])
        BIG = 1.0e30

        def loads(bi):
            s, sz = BLOCKS[bi]
            nt = sz // 128
            b = bi % 2
            for i_, src in enumerate((self.SA, self.OB, self.OC)):
                P.dma(sa[b][:, i_, :, 0:sz], src[:, :, s:s + sz].rearrange("c p t -> p c t"), w=[L + f"sa{b}"])

        def loads1(bi):
            s, sz = BLOCKS[bi]
            nt = sz // 128
            for g_ in range(3):
                P.dma(mg0[:, g_ * 8:(g_ + 1) * 8, 0:sz],
                      self.MG[g_ * 8:(g_ + 1) * 8, :, s:s + sz].rearrange("c p t -> p c t"), w=[L + "mg0"])
            src = xsrc_c if bi == 0 else xsrc_l[s - NCTX:s - NCTX + sz, :]
            P.dma(xt0[:, 0:nt, :], src.rearrange("(t p) d -> p t d", p=128), w=[L + "xt0"])

        loads(0)
        tix = 0
        for bi, (s, sz) in enumerate(BLOCKS):
            loads1(bi)
            if bi + 1 < len(BLOCKS):
                loads(bi + 1)
            b = bi % 2
            nt = sz // 128
            rr = 1 if bi == 0 else 0
            sak, mgk, xk = L + f"sa{b}", L + "mg0", L + "xt0"
            for j in range(8):
                for i_, (wt, nm) in enumerate(((WA, "w_a_out"), (WB, "w_b_out"), (WC, "w_c_out"))):
                    for c in range(4):
                        P.mm(ps_y[i_][:, 0:sz], wt[:, c, j * 128:(j + 1) * 128], sa[b][:, i_, c, 0:sz], c == 0, c == 3,
                             r=[sak, L + f"W_{nm}_{c}"], w=[L + f"ps_y{i_}"])
                P.tt(t1[:, 0:sz], ps_y[0][:, 0:sz], mg[b][:, j, 0:sz], ALU.mult, r=[L + "ps_y0", mgk], w=[L + "t1"])
                P.tt(t2[:, 0:sz], ps_y[1][:, 0:sz], mg[b][:, 8 + j, 0:sz], ALU.mult, r=[L + "ps_y1", mgk], w=[L + "t2"])
                P.tt(t1[:, 0:sz], t1[:, 0:sz], t2[:, 0:sz], ALU.add, r=[L + "t1", L + "t2"], w=[L + "t1"], eng="pool")
                P.tt(t2[:, 0:sz], ps_y[2][:, 0:sz], mg[b][:, 16 + j, 0:sz], ALU.mult, r=[L + "ps_y2", mgk], w=[L + "t2"])
                P.tt(m[:, j, 0:sz], t1[:, 0:sz], t2[:, 0:sz], ALU.add, r=[L + "t1", L + "t2"], w=[L + "m"], eng="pool")
            for t in range(nt):
                xo, xok = x1[tix % 2], L + f"x1{tix % 2}"
                hb, hbk = h2Tb[tix % 2], L + f"h2Tb{tix % 2}"
                gt_, gtk = gate[tix % 2], L + f"gate{tix % 2}"
                tix += 1
                tok0 = s + t * 128
                for hf in range(2):
                    for j in range(8):
                        P.mm(ps_out[hf][:, :], m[:, j, t * 128:(t + 1) * 128], WO[:, j, hf * 512:(hf + 1) * 512],
                             j == 0, j == 7, r=[L + "m", L + f"W_w_out_{j}"], w=[L + f"ps_out{hf}"])
                    P.tt(z[:, hf * 512:(hf + 1) * 512], ps_out[hf][:, :], rows[:, rr, 0, hf * 512:(hf + 1) * 512],
                         ALU.mult, r=[L + f"ps_out{hf}"] + CK, w=[L + "z"])
                P.stt(z[:], xt[b][:, t, :], ALPHA, z[:], ALU.mult, ALU.add, r=[xk, L + "z"], w=[L + "z"])
                self.layernorm(L, z, zz, st, lnr, xo, [L + "z"], [xok], CK)
                P.dma(self.X1[tok0:tok0 + 128, :], xo[:], r=[xok], w=["X1"], q="dq_pool")
                P.tt(h2[:], xo[:], rows[:, rr, 1, :], ALU.mult, r=[xok] + CK, w=[L + "h2"], eng="pool")
                P.tt(h2[:], h2[:], rows[:, rr, 2, :], ALU.add, r=[L + "h2"] + CK, w=[L + "h2"], eng="pool")
                for hf in range(2):
                    for jj in range(4):
                        j = hf * 4 + jj
                        P.tr(ps_t[hf][:, jj * 128:(jj + 1) * 128], h2[:, j * 128:(j + 1) * 128], ident[:],
                             r=[L + "h2"] + CK, w=[L + f"ps_t{hf}"])
                    P.copy(h2Tf[:, hf * 4:(hf + 1) * 4, :], ps_t[hf][:, :].rearrange("p (j t) -> p j t", t=128),
                           r=[L + f"ps_t{hf}"], w=[L + "h2Tf"], eng="act")
                P.copy(hb[:], h2Tf[:], r=[L + "h2Tf"], w=[hbk], eng="pool")
                P.dma(self.H2T[:, :, tok0:tok0 + 128].rearrange("j p t -> p j t"), hb[:], r=[hbk], w=["H2T"], q="dq_pool")
                for j in range(8):
                    P.mm(ps_r[:, 0:36], h2Tf[:, j, :], Wr[:, j, :], j == 0, j == 7, r=[L + "h2Tf"] + CK, w=[L + "ps_r"])
                self.router(L, ps_r, br, rt, gt_, [L + "ps_r"] + CK, [gtk], BIG)
                P.dma(self.GATE[tok0:tok0 + 128, :], gt_[:], r=[gtk], w=["GATE"], q="dq_pool")
        P.end_phase()

    def layernorm(self, L, z, zz, st, lnr, out, rk, wk, CK):
        P = self.P
        nc = P.nc
        sk = L + "st"
        P.op("dve", lambda: nc.vector.tensor_reduce(out=st[:, 0:1], in_=z[:], axis=AX.X, op=ALU.add), r=rk, w=[sk])
        P.act(zz[:], z[:], AF.Square, r=rk, w=[L + "zz"])
        P.op("dve", lambda: nc.vector.tensor_reduce(out=st[:, 1:2], in_=zz[:], axis=AX.X, op=ALU.add),
             r=[L + "zz"], w=[sk])
        P.ts(st[:, 2:3], st[:, 0:1], 1.0 / D, ALU.mult, r=[sk], w=[sk])
        P.tt(st[:, 3:4], st[:, 2:3], st[:, 2:3], ALU.mult, r=[sk], w=[sk])
        P.stt(st[:, 4:5], st[:, 1:2], 1.0 / D, st[:, 3:4], ALU.mult, ALU.subtract, r=[sk], w=[sk])
        P.rsqrt(st[:, 5:6], st[:, 4:5], EPS, r=[sk], w=[sk])
        P.ts(zz[:], z[:], st[:, 2:3], ALU.subtract, st[:, 5:6], ALU.mult, r=rk + [sk, L + "zz"], w=[L + "zz"])
        P.tt(zz[:], zz[:], lnr[:, 0, :], ALU.mult, r=[L + "zz"] + CK, w=[L + "zz"], eng="pool")
        P.tt(out[:], zz[:], lnr[:, 1, :], ALU.add, r=[L + "zz"] + CK, w=wk, eng="pool")

    def router(self, L, ps_r, br, rt, gate, rk, wk, BIG):
        P = self.P
        nc = P.nc
        k = L + "rt"
        lg = rt[:, 0:36]
        P.tt(lg, ps_r[:, 0:36], br[:, :], ALU.add, r=rk, w=[k])
        gmax, ngmax, ge, gsum, gp = rt[:, 40:41], rt[:, 41:42], rt[:, 44:48], rt[:, 48:49], rt[:, 49:50]
        ohg, m1, m2, dm, e21, p1, p2 = rt[:, 52:56], rt[:, 56:57], rt[:, 57:58], rt[:, 58:59], rt[:, 59:60], rt[:, 60:61], rt[:, 61:62]
        lem, oh1, oh2 = rt[:, 64:96], rt[:, 96:128], rt[:, 128:160]
        lem2 = rt[:, 160:192]
        P.op("dve", lambda: nc.vector.tensor_reduce(out=gmax, in_=rt[:, 0:4], axis=AX.X, op=ALU.max), r=[k], w=[k])
        P.ts(ngmax, gmax, -1.0, ALU.mult, r=[k], w=[k])
        P.act(ge, rt[:, 0:4], AF.Exp, bias=ngmax, r=[k], w=[k])
        P.op("dve", lambda: nc.vector.tensor_reduce(out=gsum, in_=ge, axis=AX.X, op=ALU.add), r=[k], w=[k])
        P.recip(gp, gsum, r=[k], w=[k])
        P.ts(ohg, rt[:, 0:4], gmax, ALU.is_equal, r=[k], w=[k])
        P.ts(ohg, ohg, -1.0, ALU.add, BIG, ALU.mult, r=[k], w=[k])
        P.tt(lem.rearrange("p (g e) -> p g e", e=8), rt[:, 4:36].rearrange("p (g e) -> p g e", e=8),
             ohg.unsqueeze(2).broadcast_to([128, 4, 8]), ALU.add, r=[k], w=[k])
        P.op("dve", lambda: nc.vector.tensor_reduce(out=m1, in_=lem, axis=AX.X, op=ALU.max), r=[k], w=[k])
        P.ts(oh1, lem, m1, ALU.is_equal, r=[k], w=[k])
        P.stt(lem2, oh1, -BIG, lem, ALU.mult, ALU.add, r=[k], w=[k])
        P.op("dve", lambda: nc.vector.tensor_reduce(out=m2, in_=lem2, axis=AX.X, op=ALU.max), r=[k], w=[k])
        P.ts(oh2, lem2, m2, ALU.is_equal, r=[k], w=[k])
        P.tt(dm, m2, m1, ALU.subtract, r=[k], w=[k])
        P.act(e21, dm, AF.Exp, r=[k], w=[k])
        P.ts(p1, e21, 1.0, ALU.add, r=[k], w=[k])
        P.recip(p1, p1, r=[k], w=[k])
        P.tt(p2, e21, p1, ALU.mult, r=[k], w=[k])
        P.tt(p1, p1, gp, ALU.mult, r=[k], w=[k])
        P.tt(p2, p2, gp, ALU.mult, r=[k], w=[k])
        P.ts(gate[:], oh1, p1, ALU.mult, r=[k], w=wk)
        P.stt(gate[:], oh2, p2, gate[:], ALU.mult, ALU.add, r=[k] + wk, w=wk)

    def phase4(self, l, dst, dst_has_ctx):
        P, I = self.P, self.inp
        L = f"p4_{l}_"
        P.begin_phase()
        rows = P.sb(L + "rows", [128, 2, D])
        for rr in range(2):
            self.bcast_row(rows[:, rr, :], self.modrow[l][rr:rr + 1, 5 * D:6 * D], [L + "rows"])
        lnr = P.sb(L + "lnr", [128, 2, D])
        self.bcast_row(lnr[:, 0, :], I["ln2_g"][l:l + 1, :], [L + "lnr"])
        self.bcast_row(lnr[:, 1, :], I["ln2_b"][l:l + 1, :], [L + "lnr"])
        CK = [L + "rows", L + "lnr"]
        NT = 17
        HT = NT * 128
        H = P.sb(L + "H", [128, 8, HT], BF16)
        G = P.sb(L + "G", [128, NT, 32])
        yacc = P.sb(L + "yacc", [128, NT, D])
        W1 = [P.sb(L + f"W1_{i}", [128, 8, 256], BF16) for i in range(2)]
        W3 = [P.sb(L + f"W3_{i}", [128, 8, 256], BF16) for i in range(2)]
        W2 = [P.sb(L + f"W2_{i}", [128, 2, D], BF16) for i in range(2)]
        aT = [P.sb(L + f"aT{i}", [128, 2, 512], BF16) for i in range(2)]
        sg = [P.sb(L + f"sg{i}", [128, 512]) for i in range(2)]
        ps_h1 = [P.ps(L + f"ps_h1{i}", [128, 512]) for i in range(2)]
        ps_h3 = [P.ps(L + f"ps_h3{i}", [128, 512]) for i in range(2)]
        ps_y = [P.ps(L + f"ps_y{i}", [128, 512]) for i in range(4)]
        x1t = [P.sb(L + f"x1t{i}", [128, D]) for i in range(2)]
        z = P.sb(L + "z", [128, D])
        zz = P.sb(L + "zz", [128, D])
        st = P.sb(L + "st", [128, 8])
        xo = [P.sb(L + f"xo{i}", [128, D]) for i in range(2)]
        blocks = [(i * 512, 512) for i in range(4)] + [(2048, 128)]

        def load_w(e):
            b = e % 2
            P.dma(W1[b][:], I["w1"][l, e].rearrange("(k p) n -> p k n", p=128), w=[L + f"W1_{b}"], q="dq_pool")
            P.dma(W3[b][:], I["w3"][l, e].rearrange("(k p) n -> p k n", p=128), w=[L + f"W3_{b}"], q="dq_pool")
            P.dma(W2[b][:], I["w2"][l, e].rearrange("(o p) n -> p o n", p=128), w=[L + f"W2_{b}"], q="dq_pool")

        st_ = dict(cy=0)
        a_of = {}
        pending = []

        def emit_h(e, bb):
            b0, bsz = blocks[bb]
            b = e % 2
            wk1, wk3 = L + f"W1_{b}", L + f"W3_{b}"
            idx = (e * len(blocks) + bb) % 2
            a_, ak = aT[idx], L + f"aT{idx}"
            a_of[(e, bb)] = (a_, ak)
            for o in range(2):
                for k in range(8):
                    P.mm(ps_h1[o][:, 0:bsz], W1[b][:, k, o * 128:(o + 1) * 128], H[:, k, b0:b0 + bsz],
                         k == 0, k == 7, r=[wk1, L + "H"], w=[L + f"ps_h1{o}"])
                for k in range(8):
                    P.mm(ps_h3[o][:, 0:bsz], W3[b][:, k, o * 128:(o + 1) * 128], H[:, k, b0:b0 + bsz],
                         k == 0, k == 7, r=[wk3, L + "H"], w=[L + f"ps_h3{o}"])
                P.act(sg[o][:, 0:bsz], ps_h1[o][:, 0:bsz], AF.Silu, r=[L + f"ps_h1{o}"], w=[L + f"sg{o}"])
                P.tt(a_[:, o, 0:bsz], sg[o][:, 0:bsz], ps_h3[o][:, 0:bsz], ALU.mult,
                     r=[L + f"sg{o}", L + f"ps_h3{o}"], w=[ak])

        def emit_y(e, bb):
            b0, bsz = blocks[bb]
            b = e % 2
            wk2 = L + f"W2_{b}"
            a_, ak = a_of.pop((e, bb))
            for t in range(bsz // 128):
                tile = b0 // 128 + t
                for hf in range(2):
                    cy = st_["cy"]
                    py, pyk = ps_y[cy % 4], L + f"ps_y{cy % 4}"
                    st_["cy"] = cy + 1
                    for o in range(2):
                        P.mm(py[:, :], a_[:, o, t * 128:(t + 1) * 128], W2[b][:, o, hf * 512:(hf + 1) * 512],
                             o == 0, o == 1, r=[ak, wk2], w=[pyk])
                    ya = yacc[:, tile, hf * 512:(hf + 1) * 512]
                    yk = L + f"yacc{tile}_{hf}"
                    if e == 0:
                        P.ts(ya, py[:, :], G[:, tile, e:e + 1], ALU.mult, r=[pyk, L + "G"], w=[yk])
                    else:
                        P.stt(ya, py[:, :], G[:, tile, e:e + 1], ya, ALU.mult, ALU.add,
                              r=[pyk, L + "G", yk], w=[yk])

        for half in range(2):
            T0 = half * HT
            P.dma(H[:], self.H2T[:, :, T0:T0 + HT].rearrange("j p t -> p j t"), w=[L + "H"])
            P.dma(G[:], self.GATE[T0:T0 + HT, :].rearrange("(t p) e -> p t e", p=128), w=[L + "G"])
            load_w(0)
            for e in range(32):
                for bb in range(len(blocks)):
                    pending.append((e, bb))
                    emit_h(e, bb)
                    if len(pending) > 1:
                        emit_y(*pending.pop(0))
                    if bb == 0 and e + 1 < 32:
                        load_w(e + 1)
            while pending:
                emit_y(*pending.pop(0))
            for t in range(NT):
                tile = half * NT + t
                tok0 = tile * 128
                rr = 1 if tile < 2 else 0
                xi, xik = x1t[t % 2], L + f"x1t{t % 2}"
                o_, ok = xo[t % 2], L + f"xo{t % 2}"
                if tile < 2 and not dst_has_ctx:
                    continue
                P.dma(xi[:], self.X1[tok0:tok0 + 128, :], w=[xik])
                yks = [L + f"yacc{t}_{hf}" for hf in range(2)]
                P.tt(z[:], yacc[:, t, :], rows[:, rr, :], ALU.mult, r=yks + CK, w=[L + "z"], eng="pool")
                P.stt(z[:], xi[:], ALPHA, z[:], ALU.mult, ALU.add, r=[xik, L + "z"], w=[L + "z"])
                self.layernorm(L, z, zz, st, lnr, o_, [L + "z"], [ok], CK)
                if dst_has_ctx:
                    P.dma(dst[tok0:tok0 + 128, :], o_[:], r=[ok], w=["dst"], q="dq_pool")
                else:
                    P.dma(dst[tok0 - NCTX:tok0 - NCTX + 128, :], o_[:], r=[ok], w=["dst"], q="dq_pool")
        P.end_phase()

    def build(self, phases=None, n_layers=2):
        P = self.P
        self.declare_inputs()
        ALL = ("p0", "p1", "p2a", "p2b1", "p2b2", "p2c1", "p2c2", "p2c3", "p2c4", "p3", "p4")
        if phases is None:
            phases = ALL
        self.modrow = [self.scratch(f"modrow{l}", [2, 6 * D]) for l in range(2)]
        self.U = self.scratch("U", [4, 128, T])
        self.QD = self.scratch("QD", [3, 128, T])
        self.KVD = self.scratch("KVD", [2, 128, T])
        self.KR = self.scratch("KR", [32, T])
        self.GQKV = self.scratch("GQKV", [12, 128, T])
        self.OG = self.scratch("OG", [4, 128, T], BF16)
        self.BETA = self.scratch("BETA", [16, T])
        self.GL = self.scratch("GL", [16, T])
        self.MG = self.scratch("MG", [24, 128, T], BF16)
        self.SA = self.scratch("SA", [4, 128, T], BF16)
        self.KT = self.scratch("KT", [8, 96, T], BF16)
        self.QT = self.scratch("QT", [8, 96, T], BF16)
        self.VX = self.scratch("VX", [8, 128, 34, 128], BF16)
        self.OB = self.scratch("OB", [4, 128, T], BF16)
        self.QKN = self.scratch("QKN", [8, 128, T])
        self.KVtok = self.scratch("KVtok", [T, 2, 512])
        self.BGtok = self.scratch("BGtok", [T, 32])
        self.PW = self.scratch("PW", [68, 2, 64, 512], BF16)
        self.PQ = self.scratch("PQ", [68, 2, 64, 512], BF16)
        self.PK = self.scratch("PK", [68, 2, 64, 512], BF16)
        self.PM = self.scratch("PM", [68, 2, 64, 512], BF16)
        self.PU = self.scratch("PU", [68, 2, 64, 512])
        self.PG = self.scratch("PG", [68, 2, 64, 8])
        self.ODIR = self.scratch("ODIR", [2, T, 512])
        self.OC = self.scratch("OC", [4, 128, T], BF16)
        self.X1 = self.scratch("X1", [T, D])
        self.H2T = self.scratch("H2T", [8, 128, T], BF16)
        self.GATE = self.scratch("GATE", [T, 32])
        self.X2 = self.scratch("X2", [T, D])
        self.out = P.dram("out", [NL, D], F32, kind="ExternalOutput")
        kw = getattr(self, "p2c2_kw", {})
        for l in range(n_layers):
            if l == 0:
                xl, xc = self.inp["x"], self.inp["ctx"]
            else:
                xl, xc = self.X2[NCTX:T, :], self.X2[0:NCTX, :]
            last = l == DEPTH - 1
            if "p0" in phases:
                self.phase0(l)
            if "p1" in phases:
                self.phase1(l, xl, xc, "xin")
            if "p2a" in phases:
                self.phase2a(l)
            if "p2b1" in phases:
                self.phase2b1(l)
            if "p2b2" in phases:
                self.phase2b2(l)
            if "p2c1" in phases:
                self.phase2c1(l)
            if "p2c2" in phases:
                self.phase2c2(l, **kw)
            if "p2c3" in phases:
                self.phase2c3(l, **kw)
            if "p2c4" in phases:
                self.phase2c4(l)
            if "p3" in phases:
                self.phase3(l, xl, xc)
            if "p4" in phases:
                if last:
                    self.phase4(l, self.out, False)
                else:
                    self.phase4(l, self.X2, True)
        return P.nc


def host_consts():
    ident = np.eye(128, dtype=np.float32)
    nf = 8
    inv = (10000.0 ** (-np.arange(nf, dtype=np.float32) / nf)).astype(np.float32)
    rows = NL // 64
    r = np.repeat(np.arange(rows, dtype=np.float32), 64)
    col = np.tile(np.arange(64, dtype=np.float32), rows)
    ang = np.stack([r[:, None] * inv, col[:, None] * inv], axis=1)
    cos = np.cos(ang).astype(np.float32)
    sin = np.sin(ang).astype(np.float32)
    cs = np.zeros((2, 32, T), np.float32)
    cs[0, :, :NCTX] = 1.0
    for a in range(2):
        for h in range(2):
            d0 = a * 16 + h * 8
            cs[0, d0:d0 + 8, NCTX:] = cos[:, a, :].T
            cs[1, d0:d0 + 8, NCTX:] = (-sin[:, a, :].T) if h == 0 else sin[:, a, :].T
    perm = np.zeros((32, 32), np.float32)
    for m_ in range(32):
        perm[m_ ^ 8, m_] = 1.0
    pp, ff = np.meshgrid(np.arange(64), np.arange(64), indexing="ij")
    gmask = np.stack([ff <= pp, ff < pp, ff >= pp, ff > pp]).astype(np.float32)
    return ident, cs, perm, gmask


def make_in_maps(inputs, cores):
    ident, cs, perm, gmask = host_consts()
    maps = []
    for b in cores:
        m = {}
        for k, v in inputs.items():
            v = np.asarray(v)
            if k in ("x", "c", "ctx"):
                m[k] = np.ascontiguousarray(v[b])
            elif k in ("a_log", "dt_bias"):
                m[k] = np.ascontiguousarray(v.reshape(2, 16))
            else:
                m[k] = np.ascontiguousarray(v)
        m["ident"] = ident
        m["rope_cs"] = cs
        m["perm32"] = perm
        m["gmask"] = gmask
        maps.append(m)
    return maps


_NC_CACHE = {}


def kernel(**inputs):
    n = 8
    if "nc" not in _NC_CACHE:
        net = Net()
        _NC_CACHE["nc"] = net.build()
    nc = _NC_CACHE["nc"]
    maps = make_in_maps(inputs, list(range(n)))
    res = run_bass_kernel_spmd(nc, maps, core_ids=list(range(n)))
    return np.stack([np.asarray(r["out"]) for r in res.results], axis=0).astype(np.float32)
```

```python
from contextlib import ExitStack
import numpy as np
import concourse.bass as bass
import concourse.mybir as mybir
from concourse.bass_utils import run_bass_kernel_spmd

F32 = mybir.dt.float32
BF16 = mybir.dt.bfloat16
AF = mybir.ActivationFunctionType
ALU = mybir.AluOpType
AX = mybir.AxisListType

D = 1024
NCTX = 256
NL = 4096
T = NCTX + NL
DEPTH = 2
P_IN = 6848
EPS = 1e-6
ALPHA = (2 * DEPTH) ** 0.25
MLA_SCALE = 96 ** -0.5
BLOCKS = [(0, 256)] + [(256 + 512 * i, 512) for i in range(8)]

DMA_ENGS = ("sp", "dq_pool", "dq_act")
PHYS = {"pe": "pe", "act": "act", "dve": "dve", "pool": "pool", "sp": "sp", "dq_pool": "pool", "dq_act": "act"}


class Prog:
    def __init__(self):
        self.nc = bass.Bass("TRN2", target_bir_lowering=False)
        self.root = ExitStack()
        self.es = self.root
        self.ops = []
        self.n_dsem = 12
        self.psum_names = set()

    def _next_uid(self):
        self._uid = getattr(self, "_uid", 0) + 1
        return self._uid

    def sb(self, name, shape, dt=F32):
        return self.es.enter_context(self.nc.sbuf_tensor(name, list(shape), dt))

    def ps(self, name, shape, dt=F32):
        self.psum_names.add(name)
        return self.es.enter_context(self.nc.psum_tensor(name, list(shape), dt))

    def dram(self, name, shape, dt=F32, kind="Internal"):
        return self.nc.dram_tensor(name, list(shape), dt, kind=kind).ap()

    def op(self, eng, fn, r=(), w=()):
        pr = [k for k in r if k in self.psum_names]
        if pr:
            r = [k for k in r if k not in self.psum_names]
            w = list(w) + pr
        self.ops.append((eng, fn, tuple(r), tuple(w)))

    DRAM_KEYS = {"SA", "KT", "QT", "VX", "OB", "QKN", "KVtok", "BGtok", "PW", "PQ", "PK", "PM", "PU", "PG",
                 "ODIR", "OC", "X1", "H2T", "GATE", "dst", "modrow0", "modrow1"}

    def dma(self, out, in_, r=(), w=(), q="sp", slow=False):
        nc = self.nc
        w = [((k, self._next_uid()) if (k in self.DRAM_KEYS or (isinstance(k, tuple) and k[0] == "scr")) else k)
             for k in w]
        e = {"sp": nc.sync, "dq_pool": nc.gpsimd, "dq_act": nc.scalar}[q]
        if slow:
            self.op(q, lambda: e.dma_start(out=out, in_=in_, allow_slow_non_contiguous=True), r, w)
        else:
            self.op(q, lambda: e.dma_start(out=out, in_=in_), r, w)

    def mm(self, out, lhsT, rhs, start, stop, r=(), w=()):
        nc = self.nc
        self.op("pe", lambda: nc.tensor.matmul(out, lhsT=lhsT, rhs=rhs, start=start, stop=stop), r, w)

    def tr(self, out, in_, ident, r=(), w=()):
        nc = self.nc
        self.op("pe", lambda: nc.tensor.transpose(out, in_, ident), r, w)

    def act(self, out, in_, func, r=(), w=(), bias=None, scale=None, accum_out=None):
        nc = self.nc
        kw = {}
        if bias is not None:
            kw["bias"] = bias
        if scale is not None:
            kw["scale"] = scale
        if accum_out is not None:
            kw["accum_out"] = accum_out
        self.op("act", lambda: nc.scalar.activation(out=out, in_=in_, func=func, **kw), r, w)

    def _veng(self, eng):
        return self.nc.vector if eng == "dve" else self.nc.gpsimd

    def tt(self, out, in0, in1, op, r=(), w=(), eng="dve"):
        e = self._veng(eng)
        self.op(eng, lambda: e.tensor_tensor(out=out, in0=in0, in1=in1, op=op), r, w)

    def ts(self, out, in0, s1, op0, s2=None, op1=None, r=(), w=(), eng="dve", accum_out=None):
        e = self._veng(eng)
        kw = {}
        if op1 is not None:
            kw["op1"] = op1
        if accum_out is not None:
            kw["accum_out"] = accum_out
        self.op(eng, lambda: e.tensor_scalar(out=out, in0=in0, scalar1=s1, scalar2=s2, op0=op0, **kw), r, w)

    def stt(self, out, in0, scalar, in1, op0, op1, r=(), w=(), eng="dve"):
        e = self._veng(eng)
        self.op(eng, lambda: e.scalar_tensor_tensor(out=out, in0=in0, scalar=scalar, in1=in1, op0=op0, op1=op1), r, w)

    def copy(self, out, in_, r=(), w=(), eng="dve"):
        if eng == "act":
            self.act(out, in_, AF.Copy, r, w)
        else:
            e = self._veng(eng)
            self.op(eng, lambda: e.tensor_copy(out=out, in_=in_), r, w)

    def recip(self, out, in_, r=(), w=()):
        nc = self.nc
        self.op("dve", lambda: nc.vector.reciprocal(out=out, in_=in_), r, w)

    def rsqrt(self, out, in_, eps, r=(), w=(), scale=1.0):
        self.act(out, in_, AF.Sqrt, r=r, w=w, bias=eps, scale=scale)
        self.recip(out, out, r=w, w=w)

    def memset(self, ap, val, w=(), eng="dve"):
        e = self._veng(eng)
        self.op(eng, lambda: e.memset(ap, val), (), w)

    def begin_phase(self):
        self._outer = self.es
        self.es = ExitStack()
        self.es.__enter__()

    def end_phase(self):
        self.flush()
        self.es.close()
        self.es = self._outer

    def _init_sync(self):
        nc = self.nc
        self.csem = {p: self._outer_ctx(nc.semaphore("cs_" + p)) for p in ("pe", "act", "dve", "pool")}
        self.dsem = {q: [self._outer_ctx(nc.semaphore(f"ds_{q}_{k}")) for k in range(self.n_dsem)]
                     for q in DMA_ENGS}
        self.bsem = self._outer_ctx(nc.semaphore("barrier"))
        self.mc = {p: 0 for p in ("pe", "act", "dve", "pool")}
        self.dcount = {q: 0 for q in DMA_ENGS}
        self.nphase = 0
        self.n_ops = 0

    def _outer_ctx(self, cm):
        return self.root.enter_context(cm)

    def flush(self):
        nc = self.nc
        if not hasattr(self, "csem"):
            self._init_sync()
        ops = self.ops
        self.ops = []
        import os as _os
        if _os.environ.get("KMAXOPS"):
            ops = ops[:int(_os.environ["KMAXOPS"])]
        queue = {"pe": nc.tensor, "act": nc.scalar, "dve": nc.vector, "pool": nc.gpsimd,
                 "sp": nc.sync, "dq_pool": nc.gpsimd, "dq_act": nc.scalar}
        n = len(ops)
        self.n_ops += n
        last_w, readers = {}, {}
        deps = []
        for i, (eng, fn, r, w) in enumerate(ops):
            d = set()
            for k in r:
                if k in last_w:
                    d.add(last_w[k])
            for k in w:
                if k in last_w:
                    d.add(last_w[k])
                d.update(readers.get(k, ()))
            d.discard(i)
            deps.append(d)
            for k in r:
                readers.setdefault(k, []).append(i)
            for k in w:
                last_w[k] = i
                readers[k] = []
        local = [0] * n
        cnt = {}
        last_on = {}
        for i, (eng, fn, r, w) in enumerate(ops):
            if eng not in DMA_ENGS:
                p = PHYS[eng]
                cnt[p] = cnt.get(p, 0) + 1
                local[i] = cnt[p]
                last_on[p] = i
        known_c, known_d = {}, {}
        milestone = [False] * n
        waits = [None] * n
        for i, (eng, fn, r, w) in enumerate(ops):
            E = PHYS[eng]
            i_dma = eng in DMA_ENGS
            need_c = {}
            need_d = []
            for j in sorted(deps[i]):
                jeng = ops[j][0]
                if jeng in DMA_ENGS:
                    s = known_d.setdefault(E, set())
                    if j not in s:
                        s.add(j)
                        need_d.append(j)
                else:
                    Pj = PHYS[jeng]
                    if Pj == E and not i_dma and E == "pe":
                        continue
                    if known_c.get((E, Pj), 0) >= local[j]:
                        continue
                    if need_c.get(Pj, (0, -1))[0] < local[j]:
                        need_c[Pj] = (local[j], j)
            for Pj, (lj, j) in need_c.items():
                known_c[(E, Pj)] = lj
                milestone[j] = True
            waits[i] = (list(need_c.values()), need_d)
        for p, i in last_on.items():
            milestone[i] = True
        msval = [0] * n
        for i, (eng, fn, r, w) in enumerate(ops):
            if eng not in DMA_ENGS and milestone[i]:
                p = PHYS[eng]
                self.mc[p] += 1
                msval[i] = self.mc[p]
        dtoken = {}
        for i, (eng, fn, r, w) in enumerate(ops):
            q = queue[eng]
            need_c, need_d = waits[i]
            for (lj, j) in need_c:
                q.wait_ge(self.csem[PHYS[ops[j][0]]], msval[j])
            for j in need_d:
                s, v = dtoken[j]
                q.wait_ge(s, v)
            if eng in DMA_ENGS:
                k = self.dcount[eng]
                self.dcount[eng] += 1
                s = self.dsem[eng][k % self.n_dsem]
                rnd = k // self.n_dsem
                if rnd > 0:
                    q.wait_ge(s, 16 * rnd)
                ins = fn()
                ins.then_inc(s, 16)
                dtoken[i] = (s, 16 * (rnd + 1))
            else:
                ins = fn()
                if milestone[i]:
                    ins.then_inc(self.csem[PHYS[eng]], 1)
        for p in ("pe", "act", "dve", "pool"):
            if self.mc[p] > 0:
                nc.sync.wait_ge(self.csem[p], self.mc[p])
        for qn in DMA_ENGS:
            k = self.dcount[qn]
            for m in range(min(k, self.n_dsem)):
                final = 16 * ((k - 1 - m) // self.n_dsem + 1)
                nc.sync.wait_ge(self.dsem[qn][m], final)
        self.nphase += 1
        nc.sync.sem_inc(self.bsem, 1)
        for e in (nc.tensor, nc.scalar, nc.vector, nc.gpsimd):
            e.wait_ge(self.bsem, self.nphase)


C_A, C_GT, C_QD, C_KVD, C_KR = 0, 512, 1024, 1408, 1664
C_GQ, C_GK, C_GV, C_OG, C_BETA, C_AR, C_MG = 1696, 2208, 2720, 3232, 3744, 3760, 3776


def load_col(P, dst, src1d, r=(), w=()):
    P.dma(dst, src1d.rearrange("(n p) -> p n", p=128), r, w, slow=True)


class Net:
    def __init__(self, dbg=()):
        self.P = Prog()
        self.dbg = set(dbg)
        self.io = {}

    def scratch(self, name, shape, dt=F32):
        kind = "ExternalOutput" if name in self.dbg else "Internal"
        if name in getattr(self, "dbg_in", ()):
            kind = "ExternalInput"
        t = self.P.dram(name, shape, dt, kind=kind)
        self.io[name] = t
        return t

    def declare_inputs(self):
        P = self.P
        specs = dict(
            x=[NL, D], c=[D], ctx=[NCTX, D], c_ctx=[D], w_mod=[2, D, 6 * D], b_mod=[2, 6 * D],
            w_in=[2, D, P_IN], b_in=[2, P_IN], conv_a_w=[2, 31, 512], conv_a_b=[2, 512],
            ln_a_g=[2, 512], ln_a_b=[2, 512], w_a_out=[2, 512, D], g_q=[2, 384], g_kv=[2, 256],
            w_uq=[2, 384, 512], w_qr=[2, 384, 256], w_uk=[2, 256, 512], w_uv=[2, 256, 512],
            w_b_out=[2, 512, D], conv_c_w=[2, 5, 1536], a_log=[2, 16], dt_bias=[2, 16], g_o=[2, 64],
            w_c_out=[2, 512, D], w_out=[2, D, D], ln1_g=[2, D], ln1_b=[2, D], w_rg=[2, D, 4],
            b_rg=[2, 4], w_re=[2, D, 32], b_re=[2, 32], w1=[2, 32, D, 256], w3=[2, 32, D, 256],
            w2=[2, 32, 256, D], ln2_g=[2, D], ln2_b=[2, D],
            ident=[128, 128], rope_cs=[2, 32, T], perm32=[32, 32], gmask=[4, 64, 64],
        )
        self.inp = {k: P.dram(k, v, F32, kind="ExternalInput") for k, v in specs.items()}

    def phase0(self, l):
        P, I = self.P, self.inp
        L = f"p0_{l}_"
        P.begin_phase()
        scin = P.sb(L + "scin", [128, 8, 2])
        sc = P.sb(L + "sc", [128, 8, 2])
        bm = P.sb(L + "bm", [2, 6 * D])
        mr = P.sb(L + "mr", [2, 6 * D])
        wm = [P.sb(L + f"wm{i}", [128, 8, 512]) for i in range(2)]
        ps = P.ps(L + "ps", [128, 512])
        load_col(P, scin[:, :, 0], I["c"], w=[L + "scin"])
        load_col(P, scin[:, :, 1], I["c_ctx"], w=[L + "scin"])
        P.dma(bm[0:1, :], I["b_mod"][l:l + 1, :], w=[L + "bm"])
        P.dma(bm[1:2, :], I["b_mod"][l:l + 1, :], w=[L + "bm"])
        P.act(sc[:], scin[:], AF.Silu, r=[L + "scin"], w=[L + "sc"])
        for nb in range(12):
            b = wm[nb % 2]
            key = L + f"wm{nb % 2}"
            P.dma(b[:], I["w_mod"][l, :, nb * 512:(nb + 1) * 512].rearrange("(k p) n -> p k n", p=128), w=[key])
            for k in range(8):
                P.mm(ps[0:2, :], sc[:, k, :], b[:, k, :], k == 0, k == 7, r=[key, L + "sc"], w=[L + "ps"])
            P.tt(mr[0:2, nb * 512:(nb + 1) * 512], ps[0:2, :], bm[0:2, nb * 512:(nb + 1) * 512], ALU.add,
                 r=[L + "ps", L + "bm"], w=[L + "mr"])
        P.dma(self.modrow[l], mr[:], r=[L + "mr"], w=[f"modrow{l}"], q="dq_pool")
        P.end_phase()

    def phase1(self, l, xsrc_l, xsrc_c, xkey):
        P, I = self.P, self.inp
        L = f"p1_{l}_"
        P.begin_phase()
        W = P.sb(L + "W", [128, 8, P_IN], BF16)
        for c in range(4):
            for k in range(8):
                c0, c1 = c * 1712, (c + 1) * 1712
                P.dma(W[:, k, c0:c1], I["w_in"][l, k * 128:(k + 1) * 128, c0:c1], w=[L + f"W{k}_{c}"], q="dq_pool")
        ident = P.sb(L + "ident", [128, 128])
        P.dma(ident[:], I["ident"], w=[L + "ident"])
        tiles = []
        for ct in range(4):
            tiles.append(("gt", None, ct, C_GT + ct * 128, 128))
            tiles.append(("a", self.U, ct, C_A + ct * 128, 128))
        for i in range(3):
            tiles.append(("id", self.QD, i, C_QD + i * 128, 128))
        for i in range(2):
            tiles.append(("id", self.KVD, i, C_KVD + i * 128, 128))
        tiles.append(("id", self.KR, None, C_KR, 32))
        for i in range(12):
            tiles.append(("id", self.GQKV, i, C_GQ + i * 128, 128))
        for i in range(4):
            tiles.append(("silu", self.OG, i, C_OG + i * 128, 128))
        tiles.append(("sig", self.BETA, None, C_BETA, 16))
        tiles.append(("sp", self.GL, None, C_AR, 16))
        for i in range(24):
            tiles.append(("sig", self.MG, i, C_MG + i * 128, 128))
        nt_ = len(tiles)
        bcol = P.sb(L + "bcol", [128, nt_])
        for n_, (kind, dst, ti, c0, m) in enumerate(tiles):
            P.dma(bcol[0:m, n_:n_ + 1], I["b_in"][l, c0:c0 + m].rearrange("(p o) -> p o", o=1),
                  w=[L + "bcol"], slow=True)
        mcol = P.sb(L + "mcol", [128, 2, 48])
        for rr in range(2):
            load_col(P, mcol[:, rr, :], self.modrow[l][rr, :], w=[L + "mcol"])
        sc1p = P.sb(L + "sc1p", [128, 2, 8])
        P.ts(sc1p[:], mcol[:, :, 8:16], 1.0, ALU.add, r=[L + "mcol"], w=[L + "sc1p"])
        dtb = P.sb(L + "dtb", [16, 1])
        nega = P.sb(L + "nega", [16, 1])
        P.dma(dtb[:], I["dt_bias"][l, :].rearrange("(p o) -> p o", o=1), w=[L + "dtb"], slow=True)
        P.dma(nega[:], I["a_log"][l, :].rearrange("(p o) -> p o", o=1), w=[L + "nega"], slow=True)
        P.act(nega[:], nega[:], AF.Exp, r=[L + "nega"], w=[L + "nega"])
        P.ts(nega[:], nega[:], -1.0, ALU.mult, r=[L + "nega"], w=[L + "nega"])

        xt = [P.sb(L + f"xt{i}", [128, 4, D]) for i in range(2)]
        hT = [P.sb(L + f"hT{i}", [128, 8, 512], BF16) for i in range(2)]
        pT = [P.ps(L + f"pT{i}", [128, 512]) for i in range(2)]
        pp = [P.ps(L + f"pp{i}", [128, 512]) for i in range(4)]
        NS = 4
        stf = [P.sb(L + f"stf{i}", [128, 512]) for i in range(NS)]
        stb = [P.sb(L + f"stb{i}", [128, 512], BF16) for i in range(NS)]
        tmp = [P.sb(L + f"tmp{i}", [128, 512]) for i in range(2)]
        spz = P.sb(L + "spz", [16, 512])
        spa = P.sb(L + "spa", [16, 512])

        def load_x(bi):
            s, sz = BLOCKS[bi]
            nt = sz // 128
            src = xsrc_c if bi == 0 else xsrc_l[s - NCTX:s - NCTX + sz, :]
            P.dma(xt[bi % 2][:, 0:nt, :], src.rearrange("(t p) d -> p t d", p=128),
                  r=[xkey], w=[L + f"xt{bi % 2}"])

        load_x(0)
        cf = cb = cp = 0
        for bi, (s, sz) in enumerate(BLOCKS):
            if bi + 1 < len(BLOCKS):
                load_x(bi + 1)
            nt = sz // 128
            rr = 1 if bi == 0 else 0
            xb, hb = xt[bi % 2], hT[bi % 2]
            xk, hk = L + f"xt{bi % 2}", L + f"hT{bi % 2}"
            for j in range(8):
                pt = pT[j % 2]
                pk = L + f"pT{j % 2}"
                for t in range(nt):
                    P.tr(pt[:, t * 128:(t + 1) * 128], xb[:, t, j * 128:(j + 1) * 128], ident[:],
                         r=[xk, L + "ident"], w=[pk])
                P.ts(hb[:, j, 0:sz], pt[:, 0:sz], sc1p[:, rr, j:j + 1], ALU.mult,
                     mcol[:, rr, j:j + 1], ALU.add, r=[pk, L + "sc1p", L + "mcol"], w=[hk])
            for n_, (kind, dst, ti, c0, m) in enumerate(tiles):
                acc = pp[cp % 4]
                ak = L + f"pp{cp % 4}"
                cp += 1
                for k in range(8):
                    wks = [L + f"W{k}_{c}" for c in range(c0 // 1712, (c0 + m - 1) // 1712 + 1)]
                    P.mm(acc[0:m, 0:sz], W[:, k, c0:c0 + m], hb[:, k, 0:sz], k == 0, k == 7,
                         r=[hk] + wks, w=[ak])
                bc = bcol[0:m, n_:n_ + 1]
                if kind == "gt":
                    tb = tmp[ti % 2]
                    P.act(tb[:, 0:sz], acc[:, 0:sz], AF.Sigmoid, bias=bc, r=[ak, L + "bcol"], w=[L + f"tmp{ti % 2}"])
                    continue
                if kind in ("silu", "sig") and m == 128:
                    st, sk = stb[cb % NS], L + f"stb{cb % NS}"
                    cb += 1
                else:
                    st, sk = stf[cf % NS], L + f"stf{cf % NS}"
                    cf += 1
                if kind == "a":
                    P.stt(st[:, 0:sz], acc[:, 0:sz], bc, tmp[ti % 2][:, 0:sz], ALU.add, ALU.mult,
                          r=[ak, L + "bcol", L + f"tmp{ti % 2}"], w=[sk])
                elif kind == "id":
                    if n_ % 2 == 0:
                        P.ts(st[0:m, 0:sz], acc[0:m, 0:sz], bc, ALU.add, r=[ak, L + "bcol"], w=[sk])
                    else:
                        P.act(st[0:m, 0:sz], acc[0:m, 0:sz], AF.Identity, bias=bc, r=[ak, L + "bcol"], w=[sk])
                elif kind == "silu":
                    P.act(st[0:m, 0:sz], acc[0:m, 0:sz], AF.Silu, bias=bc, r=[ak, L + "bcol"], w=[sk])
                elif kind == "sig":
                    P.act(st[0:m, 0:sz], acc[0:m, 0:sz], AF.Sigmoid, bias=bc, r=[ak, L + "bcol"], w=[sk])
                elif kind == "sp":
                    P.ts(spz[:, 0:sz], acc[0:16, 0:sz], bc, ALU.add, dtb[:, 0:1], ALU.add,
                         r=[ak, L + "bcol", L + "dtb"], w=[L + "spz"])
                    P.act(spa[:, 0:sz], spz[:, 0:sz], AF.Abs, r=[L + "spz"], w=[L + "spa"])
                    P.act(spa[:, 0:sz], spa[:, 0:sz], AF.Exp, scale=-1.0, r=[L + "spa"], w=[L + "spa"])
                    P.act(spa[:, 0:sz], spa[:, 0:sz], AF.Ln, bias=1.0, r=[L + "spa"], w=[L + "spa"])
                    P.stt(spz[:, 0:sz], spz[:, 0:sz], 0.0, spa[:, 0:sz], ALU.max, ALU.add,
                          r=[L + "spz", L + "spa"], w=[L + "spz"])
                    P.ts(st[0:16, 0:sz], spz[:, 0:sz], nega[:, 0:1], ALU.mult, r=[L + "spz", L + "nega"], w=[sk])
                d_ap = dst[ti, :, s:s + sz] if ti is not None else dst[:, s:s + sz]
                P.dma(d_ap, st[0:m, 0:sz], r=[sk], w=[("scr", id(dst))], q="dq_pool")
        P.end_phase()

    def phase2a(self, l):
        P, I = self.P, self.inp
        L = f"p2a_{l}_"
        P.begin_phase()
        cw = P.sb(L + "cw", [128, 4, 31])
        for ct in range(4):
            P.dma(cw[:, ct, :], I["conv_a_w"][l][:, ct * 128:(ct + 1) * 128].rearrange("k p -> p k"),
                  w=[L + "cw"], slow=True)
        cb = P.sb(L + "cb", [128, 4])
        lg = P.sb(L + "lg", [128, 4])
        lb = P.sb(L + "lb", [128, 4])
        load_col(P, cb[:], I["conv_a_b"][l], w=[L + "cb"])
        load_col(P, lg[:], I["ln_a_g"][l], w=[L + "lg"])
        load_col(P, lb[:], I["ln_a_b"][l], w=[L + "lb"])
        ones = P.sb(L + "ones", [128, 128])
        P.memset(ones[:], 1.0, w=[L + "ones"])
        v = P.sb(L + "v", [128, 4, T])
        UB = T + 60
        ub = [P.sb(L + f"ub{i}", [128, UB]) for i in range(2)]
        for i in range(2):
            P.memset(ub[i][:, 0:15], 0.0, w=[L + f"ub{i}"])
            P.memset(ub[i][:, 271:301], 0.0, w=[L + f"ub{i}"])
            P.memset(ub[i][:, UB - 15:UB], 0.0, w=[L + f"ub{i}"])
        for ct in range(4):
            u, uk = ub[ct % 2], L + f"ub{ct % 2}"
            P.dma(u[:, 15:271], self.U[ct, :, 0:NCTX], w=[uk])
            P.dma(u[:, 301:301 + NL], self.U[ct, :, NCTX:T], w=[uk])
            SPL = 2944
            for (o0, n_, base, eng_, sfx) in ((0, NCTX, 0, "dve", "c"), (NCTX, SPL, 286, "dve", "a"),
                                             (NCTX + SPL, NL - SPL, 286 + SPL, "dve", "b")):
                vk = L + f"v{ct}{sfx}"
                vo = v[:, ct, o0:o0 + n_]
                P.ts(vo, u[:, base:base + n_], cw[:, ct, 0:1], ALU.mult, cb[:, ct:ct + 1], ALU.add,
                     r=[uk, L + "cw", L + "cb"], w=[vk], eng=eng_)
                for k in range(1, 31):
                    P.stt(vo, u[:, base + k:base + k + n_], cw[:, ct, k:k + 1], vo, ALU.mult, ALU.add,
                          r=[uk, L + "cw"], w=[vk], eng=eng_)
        ps_s = [P.ps(L + f"ps_s{i}", [128, 512]) for i in range(2)]
        ps_q = [P.ps(L + f"ps_q{i}", [128, 512]) for i in range(2)]
        sq = [P.sb(L + f"sq{i}", [128, 4, 512]) for i in range(2)]
        mean = [P.sb(L + f"mean{i}", [128, 512]) for i in range(2)]
        rstd = [P.sb(L + f"rstd{i}", [128, 512]) for i in range(2)]
        xn = [P.sb(L + f"xn{i}", [128, 512]) for i in range(3)]
        so = [P.sb(L + f"so{i}", [128, 512], BF16) for i in range(3)]
        vkeys = [[L + f"v{ct}{sfx}" for sfx in "cab"] for ct in range(4)]
        cx = 0
        for bi, (s, sz) in enumerate(BLOCKS):
            i2 = bi % 2
            for ct in range(4):
                P.act(sq[i2][:, ct, 0:sz], v[:, ct, s:s + sz], AF.Square, r=vkeys[ct], w=[L + f"sq{i2}"])
            for ct in range(4):
                P.mm(ps_s[i2][:, 0:sz], ones[:], v[:, ct, s:s + sz], ct == 0, ct == 3,
                     r=vkeys[ct] + [L + "ones"], w=[L + f"ps_s{i2}"])
            for ct in range(4):
                P.mm(ps_q[i2][:, 0:sz], ones[:], sq[i2][:, ct, 0:sz], ct == 0, ct == 3,
                     r=[L + f"sq{i2}", L + "ones"], w=[L + f"ps_q{i2}"])
            m_, r_ = mean[i2], rstd[i2]
            mk, rk = L + f"mean{i2}", L + f"rstd{i2}"
            P.ts(m_[:, 0:sz], ps_s[i2][:, 0:sz], 1.0 / 512, ALU.mult, r=[L + f"ps_s{i2}"], w=[mk])
            P.tt(r_[:, 0:sz], m_[:, 0:sz], m_[:, 0:sz], ALU.mult, r=[mk], w=[rk])
            P.stt(r_[:, 0:sz], ps_q[i2][:, 0:sz], 1.0 / 512, r_[:, 0:sz], ALU.mult, ALU.subtract,
                  r=[L + f"ps_q{i2}", rk], w=[rk])
            P.rsqrt(r_[:, 0:sz], r_[:, 0:sz], EPS, r=[rk], w=[rk])
            for ct in range(4):
                x_, xk = xn[cx % 3], L + f"xn{cx % 3}"
                o_, ok = so[cx % 3], L + f"so{cx % 3}"
                cx += 1
                P.tt(x_[:, 0:sz], v[:, ct, s:s + sz], m_[:, 0:sz], ALU.subtract, r=vkeys[ct] + [mk], w=[xk])
                P.tt(x_[:, 0:sz], x_[:, 0:sz], r_[:, 0:sz], ALU.mult, r=[xk, rk], w=[xk])
                P.act(o_[:, 0:sz], x_[:, 0:sz], AF.Silu, scale=lg[:, ct:ct + 1], bias=lb[:, ct:ct + 1],
                      r=[xk, L + "lg", L + "lb"], w=[ok])
                P.dma(self.SA[ct, :, s:s + sz], o_[:, 0:sz], r=[ok], w=["SA"], q="dq_pool")
        P.end_phase()

    def phase2b1(self, l):
        P, I = self.P, self.inp
        L = f"p2b1_{l}_"
        P.begin_phase()
        ident = P.sb(L + "ident", [128, 128])
        P.dma(ident[:], I["ident"], w=[L + "ident"])
        perm = P.sb(L + "perm", [32, 32])
        P.dma(perm[:], I["perm32"], w=[L + "perm"])
        ones = P.sb(L + "ones", [128, 128])
        P.memset(ones[:], 1.0, w=[L + "ones"])
        SH = P.sb(L + "SH", [32, 96], BF16)
        P.memset(SH[:], 0.0, w=[L + "SH"])
        P.copy(SH[:, 64:96], ident[0:32, 0:32], r=[L + "ident", L + "SH"], w=[L + "SH"])
        gq = P.sb(L + "gq", [128, 3])
        gkv = P.sb(L + "gkv", [128, 2])
        load_col(P, gq[:], I["g_q"][l], w=[L + "gq"])
        load_col(P, gkv[:], I["g_kv"][l], w=[L + "gkv"])
        WK = P.sb(L + "WK", [128, 2, 8, 96], BF16)
        WQ = P.sb(L + "WQ", [128, 3, 8, 96], BF16)
        WQS = P.sb(L + "WQS", [128, 3, 8, 96], BF16)
        WV = P.sb(L + "WV", [128, 2, 512], BF16)
        P.memset(WK[:], 0.0, w=[L + "WK"])
        P.memset(WQS[:], 0.0, w=[L + "WQS"])
        for c in range(2):
            P.dma(WK[:, c, :, 0:64], I["w_uk"][l, c * 128:(c + 1) * 128, :].rearrange("p (h d) -> p h d", d=64),
                  r=[L + "WK"], w=[L + "WK"], q="dq_pool")
            P.dma(WV[:, c, :], I["w_uv"][l, c * 128:(c + 1) * 128, :], w=[L + "WV"], q="dq_pool")
        for c in range(3):
            P.dma(WQ[:, c, :, 0:64], I["w_uq"][l, c * 128:(c + 1) * 128, :].rearrange("p (h d) -> p h d", d=64),
                  w=[L + "WQ"], q="dq_pool")
            P.dma(WQ[:, c, :, 64:96], I["w_qr"][l, c * 128:(c + 1) * 128, :].rearrange("p (h d) -> p h d", d=32),
                  w=[L + "WQ"], q="dq_pool")
            src = I["w_qr"][l, c * 128:(c + 1) * 128, :].rearrange("p (h a f) -> p h a f", a=4, f=8)
            for a in range(4):
                P.dma(WQS[:, c, :, 64 + a * 8:64 + a * 8 + 8], src[:, :, a ^ 1, :],
                      r=[L + "WQS"], w=[L + "WQS"], q="dq_pool")
        vx = [P.sb(L + f"vx{i}", [128, 8, 128], BF16) for i in range(2)]
        for i in range(2):
            P.memset(vx[i][:], 1.0, w=[L + f"vx{i}"])
        xin = [P.sb(L + f"xin{i}", [128, 5, 512]) for i in range(2)]
        krin = [P.sb(L + f"krin{i}", [32, 512]) for i in range(2)]
        rope = [P.sb(L + f"rope{i}", [96, 2, 512]) for i in range(2)]
        sq = P.sb(L + "sq", [128, 5, 512])
        rs = P.sb(L + "rs", [128, 2, 512])
        cx = P.sb(L + "cx", [128, 5, 512], BF16)
        krr = P.sb(L + "krr", [32, 512], BF16)
        krt = P.sb(L + "krt", [32, 2, 512])
        ps_n = [P.ps(L + f"ps_n{i}", [128, 512]) for i in range(2)]
        ps_a = [P.ps(L + f"ps_a{i}", [128, 512]) for i in range(2)]
        ps_b = [P.ps(L + f"ps_b{i}", [128, 512]) for i in range(2)]
        ps_v = [P.ps(L + f"ps_v{i}", [128, 512]) for i in range(2)]
        ko = [P.sb(L + f"ko{i}", [96, 512], BF16) for i in range(3)]
        qo = [P.sb(L + f"qo{i}", [96, 512], BF16) for i in range(3)]
        qt = [P.sb(L + f"qt{i}", [96, 2, 512]) for i in range(2)]

        def load_blk(bi):
            s, sz = BLOCKS[bi]
            b = bi % 2
            P.dma(xin[b][:, 0:2, 0:sz], self.KVD[:, :, s:s + sz].rearrange("c p t -> p c t"), w=[L + f"xin{b}"])
            P.dma(xin[b][:, 2:5, 0:sz], self.QD[:, :, s:s + sz].rearrange("c p t -> p c t"), w=[L + f"xin{b}"])
            P.dma(krin[b][:, 0:sz], self.KR[:, s:s + sz], w=[L + f"krin{b}"])
            for ci in range(2):
                P.dma(rope[b][64:96, ci, 0:sz], I["rope_cs"][ci, :, s:s + sz], w=[L + f"rope{b}"])
                P.dma(rope[b][0:32, ci, 0:sz], I["rope_cs"][ci, :, s:s + sz], w=[L + f"rope{b}"])

        load_blk(0)
        ck = cq_ = cv = 0
        for bi, (s, sz) in enumerate(BLOCKS):
            if bi + 1 < len(BLOCKS):
                load_blk(bi + 1)
            b = bi % 2
            xb, xk = xin[b], L + f"xin{b}"
            rb, rk = rope[b], L + f"rope{b}"
            for c in range(5):
                P.act(sq[:, c, 0:sz], xb[:, c, 0:sz], AF.Square, r=[xk], w=[L + "sq"])
            for gi, (c0, c1, nf) in enumerate(((0, 2, 256.0), (2, 5, 384.0))):
                pn = ps_n[gi]
                for c in range(c0, c1):
                    P.mm(pn[:, 0:sz], ones[:], sq[:, c, 0:sz], c == c0, c == c1 - 1,
                         r=[L + "sq", L + "ones"], w=[L + f"ps_n{gi}"])
                P.rsqrt(rs[:, gi, 0:sz], pn[:, 0:sz], EPS, r=[L + f"ps_n{gi}"], w=[L + f"rs{gi}"], scale=1.0 / nf)
            for c in range(5):
                gi = 0 if c < 2 else 1
                gcol = gkv[:, c:c + 1] if c < 2 else gq[:, c - 2:c - 1]
                P.stt(cx[:, c, 0:sz], xb[:, c, 0:sz], gcol, rs[:, gi, 0:sz], ALU.mult, ALU.mult,
                      r=[xk, L + "gq", L + "gkv", L + f"rs{gi}"], w=[L + f"cx{c}"])
            ckeys_kv = [L + "cx0", L + "cx1"]
            ckeys_q = [L + "cx2", L + "cx3", L + "cx4"]
            kb, kk = krin[b], L + f"krin{b}"
            P.mm(ps_n[0][0:32, 0:sz], perm[:], kb[:, 0:sz], True, True, r=[kk, L + "perm", L + "rs0"], w=[L + "ps_n0"])
            P.tt(krt[:, 0, 0:sz], kb[:, 0:sz], rb[0:32, 0, 0:sz], ALU.mult, r=[kk, rk], w=[L + "krt0"])
            P.tt(krt[:, 1, 0:sz], ps_n[0][0:32, 0:sz], rb[0:32, 1, 0:sz], ALU.mult, r=[L + "ps_n0", rk], w=[L + "krt1"])
            P.tt(krr[:, 0:sz], krt[:, 0, 0:sz], krt[:, 1, 0:sz], ALU.add, r=[L + "krt0", L + "krt1"], w=[L + "krr"])
            for h in range(8):
                pa, pk = ps_a[ck % 2], L + f"ps_a{ck % 2}"
                o_, ok = ko[ck % 3], L + f"ko{ck % 3}"
                ck += 1
                for c in range(2):
                    P.mm(pa[0:96, 0:sz], WK[:, c, h, :], cx[:, c, 0:sz], c == 0, False,
                         r=[L + "WK", ckeys_kv[c]], w=[pk])
                P.mm(pa[0:96, 0:sz], SH[:], krr[:, 0:sz], False, True, r=[L + "SH", L + "krr"], w=[pk])
                if h % 2 == 0:
                    P.copy(o_[:, 0:sz], pa[0:96, 0:sz], r=[pk], w=[ok])
                else:
                    P.copy(o_[:, 0:sz], pa[0:96, 0:sz], r=[pk], w=[ok], eng="act")
                P.dma(self.KT[h, :, s:s + sz], o_[:, 0:sz], r=[ok], w=["KT"], q="dq_pool")
            for t in range(sz // 128):
                pv, pvk = ps_v[cv % 2], L + f"ps_v{cv % 2}"
                v_, vk = vx[cv % 2], L + f"vx{cv % 2}"
                cv += 1
                for c in range(2):
                    P.mm(pv[:, :], cx[:, c, t * 128:(t + 1) * 128], WV[:, c, :], c == 0, c == 1,
                         r=[L + "WV", ckeys_kv[c]], w=[pvk])
                pv4 = pv[:, :].rearrange("p (hp two d) -> p hp two d", two=2, d=64)
                vx4 = v_[:].rearrange("p (hp two) c -> p hp two c", two=2)
                P.copy(vx4[:, :, 0, 0:64], pv4[:, :, 0, :], r=[pvk], w=[vk])
                P.copy(vx4[:, :, 1, 64:128], pv4[:, :, 1, :], r=[pvk], w=[vk], eng="act")
                kt = (s + t * 128) // 128
                P.dma(self.VX[:, :, kt, :].rearrange("h p c -> p h c"), v_[:], r=[vk], w=["VX"], q="dq_pool")
            for h in range(8):
                pa, pk = ps_a[ck % 2], L + f"ps_a{ck % 2}"
                ck += 1
                pb, pbk = ps_b[cq_ % 2], L + f"ps_b{cq_ % 2}"
                o_, ok = qo[cq_ % 3], L + f"qo{cq_ % 3}"
                q_, qk = qt[cq_ % 2], L + f"qt{cq_ % 2}"
                cq_ += 1
                for c in range(3):
                    P.mm(pa[0:96, 0:sz], WQ[:, c, h, :], cx[:, 2 + c, 0:sz], c == 0, c == 2,
                         r=[L + "WQ", ckeys_q[c]], w=[pk])
                for c in range(3):
                    P.mm(pb[0:96, 0:sz], WQS[:, c, h, :], cx[:, 2 + c, 0:sz], c == 0, c == 2,
                         r=[L + "WQS", ckeys_q[c]], w=[pbk])
                P.act(o_[0:64, 0:sz], pa[0:64, 0:sz], AF.Copy, scale=MLA_SCALE, r=[pk], w=[ok])
                P.tt(q_[64:96, 0, 0:sz], pa[64:96, 0:sz], rb[64:96, 0, 0:sz], ALU.mult, r=[pk, rk], w=[qk])
                P.stt(q_[64:96, 1, 0:sz], pb[64:96, 0:sz], MLA_SCALE, rb[64:96, 1, 0:sz], ALU.mult, ALU.mult,
                      r=[pbk, rk], w=[qk])
                P.stt(o_[64:96, 0:sz], q_[64:96, 0, 0:sz], MLA_SCALE, q_[64:96, 1, 0:sz], ALU.mult, ALU.add,
                      r=[qk], w=[ok])
                P.dma(self.QT[h, :, s:s + sz], o_[:, 0:sz], r=[ok], w=["QT"], q="dq_pool")
        P.end_phase()

    def phase2b2(self, l):
        P, I = self.P, self.inp
        L = f"p2b2_{l}_"
        P.begin_phase()
        kT = [P.sb(L + f"kT{i}", [96, T], BF16) for i in range(2)]
        qT = [P.sb(L + f"qT{i}", [96, T], BF16) for i in range(2)]
        vx = [P.sb(L + f"vx{i}", [128, 34, 128], BF16) for i in range(2)]
        NP_ = 4
        pT = [P.sb(L + f"pT{i}", [128, 512], BF16) for i in range(NP_)]
        ps_s = [P.ps(L + f"ps_s{i}", [128, 512]) for i in range(4)]
        ps_o = [P.ps(L + f"ps_o{i}", [128, 512]) for i in range(2)]
        rec = [P.sb(L + f"rec{i}", [128, 512]) for i in range(2)]
        oo = [P.sb(L + f"oo{i}", [128, 512], BF16) for i in range(2)]

        def load_head(h):
            b = h % 2
            P.dma(kT[b][:], self.KT[h], w=[L + f"kT{b}"])
            P.dma(qT[b][:], self.QT[h], w=[L + f"qT{b}"])
            P.dma(vx[b][:], self.VX[h], w=[L + f"vx{b}"])

        load_head(0)
        LA = 2
        items = []
        for h in range(8):
            for bi, (s, sz) in enumerate(BLOCKS):
                nkt = 2 if bi == 0 else 34
                for kt in range(nkt):
                    items.append((h, bi, kt, nkt))
        loaded = {0}
        co_of = {}
        co = 0
        for h in range(8):
            for bi in range(len(BLOCKS)):
                co_of[(h, bi)] = co
                co += 1

        def emit_s(i):
            h, bi, kt, nkt = items[i]
            s, sz = BLOCKS[bi]
            b = h % 2
            ps, psk = ps_s[i % 4], L + f"ps_s{i % 4}"
            p_, pk = pT[i % NP_], L + f"pT{i % NP_}"
            P.mm(ps[:, 0:sz], kT[b][:, kt * 128:(kt + 1) * 128], qT[b][:, s:s + sz], True, True,
                 r=[L + f"kT{b}", L + f"qT{b}"], w=[psk])
            P.act(p_[:, 0:sz], ps[:, 0:sz], AF.Exp, r=[psk], w=[pk])

        def emit_pv(i):
            h, bi, kt, nkt = items[i]
            s, sz = BLOCKS[bi]
            if h + 1 < 8 and (h + 1) not in loaded and bi == 0 and kt == 0:
                loaded.add(h + 1)
                load_head(h + 1)
            b = h % 2
            c_ = co_of[(h, bi)]
            po, pok = ps_o[c_ % 2], L + f"ps_o{c_ % 2}"
            p_, pk = pT[i % NP_], L + f"pT{i % NP_}"
            P.mm(po[:, 0:sz], vx[b][:, kt, :], p_[:, 0:sz], kt == 0, kt == nkt - 1, r=[L + f"vx{b}", pk], w=[pok])
            if kt == nkt - 1:
                r_, rk = rec[c_ % 2], L + f"rec{c_ % 2}"
                o_, ok = oo[c_ % 2], L + f"oo{c_ % 2}"
                if h % 2 == 0:
                    num, den, dst = po[0:64, 0:sz], po[64:128, 0:sz], slice(0, 64)
                else:
                    num, den, dst = po[64:128, 0:sz], po[0:64, 0:sz], slice(64, 128)
                P.recip(r_[dst, 0:sz], den, r=[pok], w=[rk])
                P.tt(o_[dst, 0:sz], num, r_[dst, 0:sz], ALU.mult, r=[pok, rk], w=[ok])
                P.dma(self.OB[h // 2, dst, s:s + sz], o_[dst, 0:sz], r=[ok], w=["OB"], q="dq_pool")

        n_it = len(items)
        for i in range(n_it + LA):
            if i < n_it:
                emit_s(i)
            if i >= LA:
                emit_pv(i - LA)
        P.end_phase()

    def phase2c1(self, l):
        P, I = self.P, self.inp
        L = f"p2c1_{l}_"
        P.begin_phase()
        ident = P.sb(L + "ident", [128, 128])
        P.dma(ident[:], I["ident"], w=[L + "ident"])
        bones = P.sb(L + "bones", [128, 128])
        P.memset(bones[:], 0.0, w=[L + "bones"])
        P.memset(bones[0:64, 0:64], 1.0, w=[L + "bones"])
        P.memset(bones[64:128, 64:128], 1.0, w=[L + "bones"])
        cw = P.sb(L + "cw", [128, 12, 5])
        for i in range(12):
            P.dma(cw[:, i, :], I["conv_c_w"][l][:, i * 128:(i + 1) * 128].rearrange("k p -> p k"),
                  w=[L + "cw"], slow=True)
        UB = T + 8
        ub = [P.sb(L + f"ub{i}", [128, UB]) for i in range(2)]
        for i in range(2):
            P.memset(ub[i][:, 0:2], 0.0, w=[L + f"ub{i}"])
            P.memset(ub[i][:, 258:262], 0.0, w=[L + f"ub{i}"])
            P.memset(ub[i][:, UB - 2:UB], 0.0, w=[L + f"ub{i}"])
        xc = [P.sb(L + f"xc{i}", [128, T]) for i in range(2)]
        sq = [P.sb(L + f"sq{i}", [128, 512]) for i in range(2)]
        rn = [P.sb(L + f"rn{i}", [128, 512]) for i in range(2)]
        xo = [P.sb(L + f"xo{i}", [128, 512]) for i in range(3)]
        tk = [P.sb(L + f"tk{i}", [128, 4, 128]) for i in range(2)]
        ps_n = [P.ps(L + f"ps_n{i}", [128, 512]) for i in range(2)]
        ps_t = [P.ps(L + f"ps_t{i}", [128, 512]) for i in range(2)]
        cn = ct_ = cx_ = 0
        for i in range(12):
            u, uk = ub[i % 2], L + f"ub{i % 2}"
            x_, xk = xc[i % 2], L + f"xc{i % 2}"
            P.dma(u[:, 2:258], self.GQKV[i, :, 0:NCTX], w=[uk])
            P.dma(u[:, 262:262 + NL], self.GQKV[i, :, NCTX:T], w=[uk])
            SPL = 2944
            for (o0, n_, base, eng_, sfx) in ((0, NCTX, 0, "dve", "c"), (NCTX, SPL, 260, "dve", "a"),
                                             (NCTX + SPL, NL - SPL, 260 + SPL, "dve", "b")):
                vo = x_[:, o0:o0 + n_]
                P.ts(vo, u[:, base:base + n_], cw[:, i, 0:1], ALU.mult, r=[uk, L + "cw"], w=[xk], eng=eng_)
                for k in range(1, 5):
                    P.stt(vo, u[:, base + k:base + k + n_], cw[:, i, k:k + 1], vo, ALU.mult, ALU.add,
                          r=[uk, L + "cw"], w=[xk], eng=eng_)
            P.act(x_[:], x_[:], AF.Silu, r=[xk], w=[xk])
            for bi, (s, sz) in enumerate(BLOCKS):
                if i < 8:
                    j = cn % 2
                    cn += 1
                    o_, ok = xo[cx_ % 3], L + f"xo{cx_ % 3}"
                    cx_ += 1
                    P.act(sq[j][:, 0:sz], x_[:, s:s + sz], AF.Square, r=[xk], w=[L + f"sq{j}"])
                    P.mm(ps_n[j][:, 0:sz], bones[:], sq[j][:, 0:sz], True, True,
                         r=[L + "bones", L + f"sq{j}"], w=[L + f"ps_n{j}"])
                    P.rsqrt(rn[j][:, 0:sz], ps_n[j][:, 0:sz], EPS, r=[L + f"ps_n{j}"], w=[L + f"rn{j}"])
                    if i < 4:
                        P.stt(o_[:, 0:sz], x_[:, s:s + sz], 0.125, rn[j][:, 0:sz], ALU.mult, ALU.mult,
                              r=[xk, L + f"rn{j}"], w=[ok])
                    else:
                        P.tt(o_[:, 0:sz], x_[:, s:s + sz], rn[j][:, 0:sz], ALU.mult, r=[xk, L + f"rn{j}"], w=[ok])
                    P.dma(self.QKN[i, :, s:s + sz], o_[:, 0:sz], r=[ok], w=["QKN"], q="dq_pool")
                    src, sk = o_, ok
                    soff = 0
                else:
                    src, sk = x_, xk
                    soff = s
                if i >= 4:
                    j = ct_ % 2
                    ct_ += 1
                    nt = sz // 128
                    for t in range(nt):
                        P.tr(ps_t[j][:, t * 128:(t + 1) * 128], src[:, soff + t * 128:soff + (t + 1) * 128], ident[:],
                             r=[sk, L + "ident"], w=[L + f"ps_t{j}"])
                    P.copy(tk[j][:, 0:nt, :], ps_t[j][:, 0:sz].rearrange("p (t f) -> p t f", f=128),
                           r=[L + f"ps_t{j}"], w=[L + f"tk{j}"], eng="act")
                    kv = 0 if i < 8 else 1
                    f0 = ((i - 4) % 4) * 128
                    P.dma(self.KVtok[s:s + sz, kv, f0:f0 + 128].rearrange("(t p) f -> p t f", p=128),
                          tk[j][:, 0:nt, :], r=[L + f"tk{j}"], w=["KVtok"], q="dq_pool")
        bg = [P.sb(L + f"bg{i}", [16, 2, 512]) for i in range(2)]
        bo = [P.sb(L + f"bo{i}", [128, 4, 32]) for i in range(2)]
        for bi, (s, sz) in enumerate(BLOCKS):
            j = bi % 2
            nt = sz // 128
            P.dma(bg[j][:, 0, 0:sz], self.BETA[:, s:s + sz], w=[L + f"bg{j}"])
            P.dma(bg[j][:, 1, 0:sz], self.GL[:, s:s + sz], w=[L + f"bg{j}"])
            for t in range(nt):
                for z in range(2):
                    P.tr(ps_t[j][:, t * 32 + z * 16:t * 32 + z * 16 + 16], bg[j][:, z, t * 128:(t + 1) * 128],
                         ident[0:16, 0:16], r=[L + f"bg{j}", L + "ident"], w=[L + f"ps_t{j}"])
            P.copy(bo[j][:, 0:nt, :], ps_t[j][:, 0:nt * 32].rearrange("p (t f) -> p t f", f=32),
                   r=[L + f"ps_t{j}"], w=[L + f"bo{j}"])
            P.dma(self.BGtok[s:s + sz, :].rearrange("(t p) f -> p t f", p=128), bo[j][:, 0:nt, :],
                  r=[L + f"bo{j}"], w=["BGtok"], q="dq_pool")
        P.end_phase()

    @staticmethod
    def gdn_chunk(n, d):
        return n if d == 0 else (3 - n if n < 4 else 71 - n)

    def phase2c2(self, l, nsteps=68):
        P, I = self.P, self.inp
        L = f"p2c2_{l}_"
        P.begin_phase()
        ident = P.sb(L + "ident", [128, 128])
        P.dma(ident[:], I["ident"], w=[L + "ident"])
        msk = P.sb(L + "msk", [64, 4, 64])
        P.dma(msk[:], I["gmask"].rearrange("m p f -> p m f"), w=[L + "msk"])
        ones = P.sb(L + "ones", [64, 64])
        P.memset(ones[:], 1.0, w=[L + "ones"])
        CK = [L + "ident", L + "msk", L + "ones"]
        TRI = (2, 0)
        STRICT = (1, 3)
        INCLT = (2, 0)
        INCL = (0, 2)

        def bc_u(ap2):
            return ap2.unsqueeze(2).broadcast_to([ap2.shape[0], 8, 64])

        def bc_m(mi):
            return msk[:, mi, :].unsqueeze(1).broadcast_to([64, 8, 64])

        def v3(ap, np_=64):
            return ap.rearrange("p (u f) -> p u f", f=64)

        D_ = {}
        for d in range(2):
            X = {}
            for nm, shp, dt in (("bgt", [64, 32], F32), ("ktok", [64, 512], F32), ("vtok", [64, 512], F32),
                                ("kT", [64, 8, 64], F32), ("qT", [64, 8, 64], F32)):
                X[nm] = [P.sb(L + f"{nm}{d}_{b}", shp, dt) for b in range(2)]
            for nm, shp, dt in (("gam", [64, 8], F32), ("gtot", [64, 8], F32), ("eg", [64, 8], F32),
                                ("bgv", [64, 8], F32), ("dif", [64, 8], F32), ("ekd", [64, 8], F32),
                                ("gcP", [64, 8], F32), ("nbeta", [64, 8], F32),
                                ("Y", [64, 512], F32), ("diff", [64, 512], F32), ("E", [64, 512], F32),
                                ("ET", [64, 512], F32), ("egr", [64, 512], F32), ("Dst", [64, 512], F32),
                                ("DinT", [64, 512], F32), ("A0", [64, 512], F32), ("B0", [64, 512], F32),
                                ("R", [64, 512], F32), ("Pa", [64, 512], F32), ("PTa", [64, 512], F32),
                                ("Pb", [64, 512], F32), ("PTb", [64, 512], F32),
                                ("Pa16", [64, 512], BF16), ("PTa16", [64, 512], BF16),
                                ("Pb16", [64, 512], BF16), ("PTb16", [64, 512], BF16), ("R16", [64, 512], BF16),
                                ("pmT", [64, 512], BF16), ("bgK", [64, 512], BF16), ("betaV", [64, 512], BF16),
                                ("kd", [64, 512], BF16), ("TTb", [64, 512], BF16), ("wT", [64, 512], BF16),
                                ("u0", [64, 512], F32), ("qgT", [64, 512], BF16)):
                X[nm] = P.sb(L + f"{nm}{d}", shp, dt)
            X["ps0"] = P.ps(L + f"ps0{d}", [128, 512])
            for i in (1, 2, 3):
                X[f"ps{i}"] = P.ps(L + f"ps{i}{d}", [128, 512])
            D_[d] = X

        def K_(d, nm):
            return L + f"{nm}{d}"

        def loads(n, d):
            X = D_[d]
            b = n % 2
            c = self.gdn_chunk(n, d)
            t0 = 64 * c
            P.dma(X["bgt"][b][:], self.BGtok[t0:t0 + 64, :], w=[K_(d, f"bgt{b}")])
            P.dma(X["ktok"][b][:], self.KVtok[t0:t0 + 64, 0, :], w=[K_(d, f"ktok{b}")])
            P.dma(X["vtok"][b][:], self.KVtok[t0:t0 + 64, 1, :], w=[K_(d, f"vtok{b}")])
            qkn = self.QKN.rearrange("t p c -> (t p) c")
            P.dma(X["qT"][b][:], qkn[0:512, t0:t0 + 64].rearrange("(u p) c -> p u c", p=64), w=[K_(d, f"qT{b}")])
            P.dma(X["kT"][b][:], qkn[512:1024, t0:t0 + 64].rearrange("(u p) c -> p u c", p=64), w=[K_(d, f"kT{b}")])

        def stage_a(n, d):
            X = D_[d]
            b = n % 2
            k = lambda nm: K_(d, nm)
            bgt, ktok, vtok, kT, qT = (X[nm][b] for nm in ("bgt", "ktok", "vtok", "kT", "qT"))
            kb = lambda nm: K_(d, f"{nm}{b}")
            beta = bgt[:, d * 8:d * 8 + 8]
            g = bgt[:, 16 + d * 8:16 + d * 8 + 8]
            ps0, ps1, ps2, ps3 = X["ps0"], X["ps1"], X["ps2"], X["ps3"]
            P.mm(ps0[0:64, 0:8], msk[:, TRI[d], :], g, True, True, r=[kb("bgt")] + CK, w=[k("ps0")])
            P.mm(ps0[0:64, 8:16], ones[:], g, True, True, r=[kb("bgt")] + CK, w=[k("ps0")])
            P.copy(X["gam"][:], ps0[0:64, 0:8], r=[k("ps0")], w=[k("gam")], eng="act")
            P.copy(X["gtot"][:], ps0[0:64, 8:16], r=[k("ps0")], w=[k("gtot")], eng="act")
            P.act(X["eg"][:], X["gam"][:], AF.Exp, r=[k("gam")], w=[k("eg")])
            P.tt(X["bgv"][:], X["eg"][:], beta, ALU.mult, r=[k("eg"), kb("bgt")], w=[k("bgv")])
            P.tt(X["dif"][:], X["gtot"][:], X["gam"][:], ALU.subtract, r=[k("gtot"), k("gam")], w=[k("dif")])
            P.act(X["ekd"][:], X["dif"][:], AF.Exp, r=[k("dif")], w=[k("ekd")])
            P.act(X["gcP"][:], X["gtot"][:], AF.Exp, r=[k("gtot")], w=[k("gcP")])
            P.ts(X["nbeta"][:], beta, -1.0, ALU.mult, r=[kb("bgt")], w=[k("nbeta")])
            P.tt(v3(X["Y"][:]), bc_u(g), bc_m(TRI[d]), ALU.mult, r=[kb("bgt")] + CK, w=[k("Y")], eng="pool")
            P.mm(ps1[0:64, :], ones[:], X["Y"][:], True, True, r=[k("Y")] + CK, w=[k("ps1")])
            P.tt(v3(X["diff"][:]), bc_u(X["gam"][:]), v3(ps1[0:64, :]), ALU.subtract,
                 r=[k("gam"), k("ps1")], w=[k("diff")])
            P.tt(v3(X["E"][:]), v3(X["diff"][:]), bc_m(INCL[d]), ALU.mult, r=[k("diff")] + CK, w=[k("E")], eng="pool")
            P.tt(v3(X["ET"][:]), v3(X["diff"][:]), bc_m(INCLT[d]), ALU.mult, r=[k("diff")] + CK, w=[k("ET")], eng="pool")
            P.act(X["E"][:], X["E"][:], AF.Exp, r=[k("E")], w=[k("E")])
            P.act(X["ET"][:], X["ET"][:], AF.Exp, scale=-1.0, r=[k("ET")], w=[k("ET")])
            P.act(X["egr"][:], ps1[0:64, :], AF.Exp, r=[k("ps1")], w=[k("egr")])
            P.tt(v3(X["Dst"][:]), v3(X["E"][:]), bc_m(STRICT[d]), ALU.min, r=[k("E")] + CK, w=[k("Dst")])
            P.tt(v3(X["DinT"][:]), v3(X["ET"][:]), bc_m(INCLT[d]), ALU.min, r=[k("ET")] + CK, w=[k("DinT")])
            for u in range(8):
                P.mm(ps2[0:64, u * 64:(u + 1) * 64], kT[:, u, :], kT[:, u, :],
                     True, True, r=[kb("kT")], w=[k("ps2")])
            for u in range(8):
                P.mm(ps3[0:64, u * 64:(u + 1) * 64], kT[:, u, :], qT[:, u, :],
                     True, True, r=[kb("kT"), kb("qT")], w=[k("ps3")])
            P.tt(X["A0"][:], ps2[0:64, :], X["Dst"][:], ALU.mult, r=[k("ps2"), k("Dst")], w=[k("A0")])
            P.tt(v3(X["A0"][:]), v3(X["A0"][:]), bc_u(X["nbeta"][:]), ALU.mult, r=[k("A0"), k("nbeta")], w=[k("A0")],
                 eng="pool")
            P.tt(X["pmT"][:], ps3[0:64, :], X["DinT"][:], ALU.mult, r=[k("ps3"), k("DinT")], w=[k("pmT")])
            P.tt(v3(X["bgK"][:]), v3(ktok[:]), bc_u(X["bgv"][:]), ALU.mult, r=[kb("ktok"), k("bgv")],
                 w=[k("bgK")], eng="pool")
            P.tt(v3(X["betaV"][:]), v3(vtok[:]), bc_u(beta), ALU.mult, r=[kb("vtok"), kb("bgt")],
                 w=[k("betaV")], eng="pool")
            P.tt(v3(X["kd"][:]), v3(ktok[:]), bc_u(X["ekd"][:]), ALU.mult, r=[kb("ktok"), k("ekd")],
                 w=[k("kd")], eng="pool")
            P.tt(v3(X["qgT"][:]), qT[:], v3(X["egr"][:]), ALU.mult, r=[kb("qT"), k("egr")], w=[k("qgT")], eng="pool")
            for u in range(8):
                P.tr(ps1[0:64, u * 64:(u + 1) * 64], X["A0"][:, u * 64:(u + 1) * 64], ident[0:64, 0:64],
                     r=[k("A0")] + CK, w=[k("ps1")])
            P.copy(X["B0"][:], ps1[0:64, :], r=[k("ps1")], w=[k("B0")], eng="act")
            P.tt(v3(X["R"][:]), v3(X["B0"][:]), ident[0:64, 0:64].unsqueeze(1).broadcast_to([64, 8, 64]), ALU.add,
                 r=[k("B0")] + CK, w=[k("R")])

        def stage_lev(n, d, lev):
            X = D_[d]
            k = lambda nm: K_(d, nm)
            ps1, ps2, ps3 = X["ps1"], X["ps2"], X["ps3"]
            lo = False
            lo_out = False
            sfx_in = "16" if lo else ""
            sfx_out = "16" if lo_out else ""
            names = [("B0", "A0"), ("Pa", "PTa"), ("Pb", "PTb")]
            pn, ptn = names[0] if lev == 0 else names[1 + (lev - 1) % 2]
            qn, qtn = names[1 + lev % 2]
            if lev > 0:
                pn, ptn = pn + sfx_in, ptn + sfx_in
            qn, qtn = qn + sfx_out, qtn + sfx_out
            Pm, PTm, Pn, PTn = X[pn], X[ptn], X[qn], X[qtn]
            Rn = "R16" if lo else "R"
            need_p = lev < 4
            need_pt = lev < 5
            if lev >= 1:
                for u in range(8):
                    sl = slice(u * 64, (u + 1) * 64)
                    P.mm(ps1[0:64, sl], PTm[:, sl], X[Rn][:, sl], True, True, r=[k(ptn), k(Rn)], w=[k("ps1")])
            if need_p:
                for u in range(8):
                    sl = slice(u * 64, (u + 1) * 64)
                    P.mm(ps2[0:64, sl], PTm[:, sl], Pm[:, sl], True, True, r=[k(pn), k(ptn)], w=[k("ps2")])
            if need_pt:
                for u in range(8):
                    sl = slice(u * 64, (u + 1) * 64)
                    P.mm(ps3[0:64, sl], Pm[:, sl], PTm[:, sl], True, True, r=[k(pn), k(ptn)], w=[k("ps3")])
            if lev >= 1:
                P.tt(X["R"][:], X["R"][:], ps1[0:64, :], ALU.add, r=[k("R"), k("ps1")], w=[k("R")])
            if need_p:
                P.copy(Pn[:], ps2[0:64, :], r=[k("ps2")], w=[k(qn)], eng="act")
            if need_pt:
                P.copy(PTn[:], ps3[0:64, :], r=[k("ps3")], w=[k(qtn)], eng="act")

        def stage_z(n, d):
            X = D_[d]
            k = lambda nm: K_(d, nm)
            ps2, ps3 = X["ps2"], X["ps3"]
            P.copy(X["TTb"][:], X["R"][:], r=[k("R")], w=[k("TTb")], eng="act")
            for u in range(8):
                hp = u // 2
                sl = slice(u * 64, (u + 1) * 64)
                P.mm(ps3[0:64, sl], X["TTb"][:, sl], X["betaV"][:, sl], True, True,
                     r=[k("betaV"), k("TTb")], w=[k("ps3")])
                P.mm(ps2[0:64, sl], X["bgK"][:, sl], X["TTb"][:, sl], True, True,
                     r=[k("bgK"), k("TTb")], w=[k("ps2")])
            P.copy(X["wT"][:], ps2[0:64, :], r=[k("ps2")], w=[k("wT")], eng="act")
            P.copy(X["u0"][:], ps3[0:64, :], r=[k("ps3")], w=[k("u0")], eng="act")
            P.dma(self.PW[n, d], X["wT"][:], r=[k("wT")], w=["PW"], q="dq_pool")
            P.dma(self.PQ[n, d], X["qgT"][:], r=[k("qgT")], w=["PQ"], q="dq_pool")
            P.dma(self.PK[n, d], X["kd"][:], r=[k("kd")], w=["PK"], q="dq_pool")
            P.dma(self.PM[n, d], X["pmT"][:], r=[k("pmT")], w=["PM"], q="dq_pool")
            P.dma(self.PU[n, d], X["u0"][:], r=[k("u0")], w=["PU"], q="dq_pool")
            P.dma(self.PG[n, d], X["gcP"][:], r=[k("gcP")], w=["PG"], q="dq_pool")

        for d in range(2):
            loads(0, d)
        for n in range(nsteps):
            if n + 1 < nsteps:
                for d in range(2):
                    loads(n + 1, d)
            for d in range(2):
                stage_a(n, d)
            for lev in range(6):
                for d in range(2):
                    stage_lev(n, d, lev)
            for d in range(2):
                stage_z(n, d)
        P.end_phase()

    def phase2c3(self, l, nsteps=68):
        P, I = self.P, self.inp
        L = f"p2c3_{l}_"
        P.begin_phase()
        NB = 3
        IN = {}
        for nm, dt in (("wT", BF16), ("qgT", BF16), ("kd", BF16), ("pmT", BF16), ("u0", F32)):
            IN[nm] = [P.sb(L + f"{nm}{b}", [64, 2, 512], dt) for b in range(NB)]
        IN["gc"] = [P.sb(L + f"gc{b}", [64, 2, 8], F32) for b in range(NB)]
        SRC = dict(wT=self.PW, qgT=self.PQ, kd=self.PK, pmT=self.PM, u0=self.PU, gc=self.PG)
        S = P.sb(L + "S", [64, 16, 64])
        Sb = P.sb(L + "Sb", [64, 16, 64], BF16)
        u = P.sb(L + "u", [64, 2, 512], BF16)
        tmpS = P.sb(L + "tmpS", [64, 16, 64])
        osb = [P.sb(L + f"osb{i}", [64, 2, 512]) for i in range(2)]
        ps_u = [P.ps(L + f"ps_u{d}", [128, 512]) for d in range(2)]
        ps_o = [P.ps(L + f"ps_o{d}", [128, 512]) for d in range(2)]
        ps_S = [P.ps(L + f"ps_S{d}", [128, 512]) for d in range(2)]
        P.memset(S[:], 0.0, w=[L + "S0", L + "S1"])
        P.memset(Sb[:], 0.0, w=[L + "Sb0", L + "Sb1"])

        def loads(n):
            b = n % NB
            for nm in ("wT", "qgT", "kd", "pmT", "u0", "gc"):
                for d in range(2):
                    P.dma(IN[nm][b][:, d, :], SRC[nm][n, d], w=[L + f"{nm}{b}_{d}"])

        loads(0)
        if nsteps > 1:
            loads(1)
        for n in range(nsteps):
            if n + 2 < nsteps:
                loads(n + 2)
            b = n % NB
            kin = lambda nm, d: L + f"{nm}{b}_{d}"
            wT, qgT, kd, pmT, u0, gc = (IN[nm][b] for nm in ("wT", "qgT", "kd", "pmT", "u0", "gc"))
            ob, obk = osb[n % 2], L + f"osb{n % 2}"
            for d in range(2):
                for h in range(8):
                    sl = slice(h * 64, (h + 1) * 64)
                    P.mm(ps_u[d][0:64, sl], wT[:, d, sl], Sb[:, d * 8 + h, :], True, True,
                         r=[kin("wT", d), L + f"Sb{d}"], w=[L + f"ps_u{d}"])
            for d in range(2):
                P.tt(u[:, d, :], u0[:, d, :], ps_u[d][0:64, :], ALU.subtract,
                     r=[kin("u0", d), L + f"ps_u{d}"], w=[L + f"u{d}"])
            for d in range(2):
                P.tt(tmpS[:, d * 8:(d + 1) * 8, :], S[:, d * 8:(d + 1) * 8, :],
                     gc[:, d, :].unsqueeze(2).broadcast_to([64, 8, 64]), ALU.mult,
                     r=[kin("gc", d), L + f"S{d}"], w=[L + f"tmpS{d}"], eng="pool")
            for d in range(2):
                for h in range(8):
                    sl = slice(h * 64, (h + 1) * 64)
                    P.mm(ps_o[d][0:64, sl], qgT[:, d, sl], Sb[:, d * 8 + h, :], True, False,
                         r=[kin("qgT", d), L + f"Sb{d}"], w=[L + f"ps_o{d}"])
                    P.mm(ps_o[d][0:64, sl], pmT[:, d, sl], u[:, d, sl], False, True,
                         r=[kin("pmT", d), L + f"u{d}"], w=[L + f"ps_o{d}"])
                for h in range(8):
                    sl = slice(h * 64, (h + 1) * 64)
                    P.mm(ps_S[d][0:64, sl], kd[:, d, sl], u[:, d, sl], True, True,
                         r=[kin("kd", d), L + f"u{d}"], w=[L + f"ps_S{d}"])
            for d in range(2):
                Sd = S[:, d * 8:(d + 1) * 8, :]
                td = tmpS[:, d * 8:(d + 1) * 8, :]
                pS3 = ps_S[d][0:64, :].rearrange("p (u f) -> p u f", f=64)
                P.tt(Sb[:, d * 8:(d + 1) * 8, :], td, pS3, ALU.add, r=[L + f"ps_S{d}", L + f"tmpS{d}"],
                     w=[L + f"Sb{d}"])
                P.tt(Sd, td, pS3, ALU.add, r=[L + f"ps_S{d}", L + f"tmpS{d}"], w=[L + f"S{d}"])
                P.copy(ob[:, d, :], ps_o[d][0:64, :], r=[L + f"ps_o{d}"], w=[obk + f"_{d}"], eng="act")
                c = self.gdn_chunk(n, d)
                P.dma(self.ODIR[d, c * 64:(c + 1) * 64, :], ob[:, d, :], r=[obk + f"_{d}"], w=["ODIR"], q="dq_pool")
        P.end_phase()

    def phase2c4(self, l):
        P, I = self.P, self.inp
        L = f"p2c4_{l}_"
        P.begin_phase()
        ident = P.sb(L + "ident", [128, 128])
        P.dma(ident[:], I["ident"], w=[L + "ident"])
        go = P.sb(L + "go", [128, 1])
        for hh in range(2):
            P.dma(go[hh * 64:(hh + 1) * 64, :], I["g_o"][l, :].rearrange("(p o) -> p o", o=1), w=[L + "go"], slow=True)
        oin = [P.sb(L + f"oin{i}", [128, 2, 4, 512]) for i in range(2)]
        ogin = [P.sb(L + f"ogin{i}", [128, 4, 512], BF16) for i in range(2)]
        osum = P.sb(L + "osum", [128, 4, 512])
        sq = P.sb(L + "sq", [128, 512])
        ms = P.sb(L + "ms", [128, 4, 8])
        ps_t = [P.ps(L + f"ps_t{i}", [128, 512]) for i in range(2)]
        oc = [P.sb(L + f"oc{i}", [128, 512], BF16) for i in range(3)]

        def loads(bi):
            s, sz = BLOCKS[bi]
            nt = sz // 128
            b = bi % 2
            for d in range(2):
                P.dma(oin[b][:, d, 0:nt, :], self.ODIR[d, s:s + sz, :].rearrange("(t p) f -> p t f", p=128),
                      w=[L + f"oin{b}"])
            P.dma(ogin[b][:, :, 0:sz], self.OG[:, :, s:s + sz].rearrange("c p t -> p c t"), w=[L + f"ogin{b}"])

        loads(0)
        ct_ = co = 0
        for bi, (s, sz) in enumerate(BLOCKS):
            if bi + 1 < len(BLOCKS):
                loads(bi + 1)
            b = bi % 2
            nt = sz // 128
            P.tt(osum[:, 0:nt, :], oin[b][:, 0, 0:nt, :], oin[b][:, 1, 0:nt, :], ALU.add,
                 r=[L + f"oin{b}"], w=[L + "osum"], eng="pool")
            for t in range(nt):
                P.act(sq[:], osum[:, t, :], AF.Square, r=[L + "osum"], w=[L + "sq"])
                P.op("dve", (lambda t=t: self.P.nc.vector.tensor_reduce(
                    out=ms[:, t, :], in_=sq[:].rearrange("p (h f) -> p h f", f=64), axis=AX.X, op=ALU.add)),
                    r=[L + "sq"], w=[L + "ms"])
            P.rsqrt(ms[:, 0:nt, :], ms[:, 0:nt, :], EPS, r=[L + "ms"], w=[L + "ms"], scale=1.0 / 64)
            for t in range(nt):
                o3 = osum[:, t, :].rearrange("p (h f) -> p h f", f=64)
                P.tt(o3, o3, ms[:, t, :].unsqueeze(2).broadcast_to([128, 8, 64]), ALU.mult,
                     r=[L + "osum", L + "ms"], w=[L + "osum"])
            for ft in range(4):
                pt, ptk = ps_t[ct_ % 2], L + f"ps_t{ct_ % 2}"
                ct_ += 1
                for t in range(nt):
                    P.tr(pt[:, t * 128:(t + 1) * 128], osum[:, t, ft * 128:(ft + 1) * 128], ident[:],
                         r=[L + "osum", L + "ident"], w=[ptk])
                o_, ok = oc[co % 3], L + f"oc{co % 3}"
                co += 1
                P.stt(o_[:, 0:sz], pt[:, 0:sz], go[:, 0:1], ogin[b][:, ft, 0:sz], ALU.mult, ALU.mult,
                      r=[ptk, L + "go", L + f"ogin{b}"], w=[ok])
                P.dma(self.OC[ft, :, s:s + sz], o_[:, 0:sz], r=[ok], w=["OC"], q="dq_pool")
        P.end_phase()

    def bcast_row(self, dst, src_row, w):
        self.P.dma(dst, src_row.broadcast_to([128, src_row.shape[-1]]), w=w, slow=True)

    def phase3(self, l, xsrc_l, xsrc_c):
        P, I = self.P, self.inp
        L = f"p3_{l}_"
        P.begin_phase()
        ident = P.sb(L + "ident", [128, 128])
        P.dma(ident[:], I["ident"], w=[L + "ident"])
        WA = P.sb(L + "WA", [128, 4, D], BF16)
        WB = P.sb(L + "WB", [128, 4, D], BF16)
        WC = P.sb(L + "WC", [128, 4, D], BF16)
        WO = P.sb(L + "WO", [128, 8, D], BF16)
        for wt, nm, kc in ((WA, "w_a_out", 4), (WB, "w_b_out", 4), (WC, "w_c_out", 4), (WO, "w_out", 8)):
            for c in range(kc):
                P.dma(wt[:, c, :], I[nm][l, c * 128:(c + 1) * 128, :], w=[L + f"W_{nm}_{c}"], q="dq_pool")
        Wr = P.sb(L + "Wr", [128, 8, 36])
        P.dma(Wr[:, :, 0:4], I["w_rg"][l].rearrange("(k p) n -> p k n", p=128), w=[L + "Wr"], slow=True)
        P.dma(Wr[:, :, 4:36], I["w_re"][l].rearrange("(k p) n -> p k n", p=128), w=[L + "Wr"], slow=True)
        br = P.sb(L + "br", [128, 36])
        self.bcast_row(br[:, 0:4], I["b_rg"][l:l + 1, :], [L + "br"])
        self.bcast_row(br[:, 4:36], I["b_re"][l:l + 1, :], [L + "br"])
        rows = P.sb(L + "rows", [128, 2, 3, D])
        for rr in range(2):
            for i_, v_ in enumerate((2, 4, 3)):
                self.bcast_row(rows[:, rr, i_, :], self.modrow[l][rr:rr + 1, v_ * D:(v_ + 1) * D], [L + "rows"])
        P.ts(rows[:, :, 1, :], rows[:, :, 1, :], 1.0, ALU.add, r=[L + "rows"], w=[L + "rows"])
        lnr = P.sb(L + "lnr", [128, 2, D])
        self.bcast_row(lnr[:, 0, :], I["ln1_g"][l:l + 1, :], [L + "lnr"])
        self.bcast_row(lnr[:, 1, :], I["ln1_b"][l:l + 1, :], [L + "lnr"])
        CK = [L + "Wr", L + "br", L + "rows", L + "lnr", L + "ident"]

        sa = [P.sb(L + f"sa{i}", [128, 3, 4, 512], BF16) for i in range(2)]
        mg0 = P.sb(L + "mg0", [128, 24, 512], BF16)
        mg = [mg0, mg0]
        xt0 = P.sb(L + "xt0", [128, 4, D])
        xt = [xt0, xt0]
        m = P.sb(L + "m", [128, 8, 512], BF16)
        t1 = P.sb(L + "t1", [128, 512])
        t2 = P.sb(L + "t2", [128, 512])
        z = P.sb(L + "z", [128, D])
        zz = P.sb(L + "zz", [128, D])
        x1 = [P.sb(L + f"x1{i}", [128, D]) for i in range(2)]
        h2 = P.sb(L + "h2", [128, D])
        h2Tf = P.sb(L + "h2Tf", [128, 8, 128])
        h2Tb = [P.sb(L + f"h2Tb{i}", [128, 8, 128], BF16) for i in range(2)]
        st = P.sb(L + "st", [128, 8])
        rt = P.sb(L + "rt", [128, 256])
        gate = [P.sb(L + f"gate{i}", [128, 32]) for i in range(2)]
        ps_y = [P.ps(L + f"ps_y{i}", [128, 512]) for i in range(3)]
        ps_out = [P.ps(L + f"ps_out{i}", [128, 512]) for i in range(2)]
        ps_t = [P.ps(L + f"ps_t{i}", [128, 512]) for i in range(2)]
        ps_r = P.ps(L + "ps_r", [128, 512])
        BIG = 1.0e30

        def loads(bi):
            s, sz = BLOCKS[bi]
            nt = sz // 128
            b = bi % 2
            for i_, src in enumerate((self.SA, self.OB, self.OC)):
                P.dma(sa[b][:, i_, :, 0:sz], src[:, :, s:s + sz].rearrange("c p t -> p c t"), w=[L + f"sa{b}"])

        def loads1(bi):
            s, sz = BLOCKS[bi]
            nt = sz // 128
            for g_ in range(3):
                P.dma(mg0[:, g_ * 8:(g_ + 1) * 8, 0:sz],
                      self.MG[g_ * 8:(g_ + 1) * 8, :, s:s + sz].rearrange("c p t -> p c t"), w=[L + "mg0"])
            src = xsrc_c if bi == 0 else xsrc_l[s - NCTX:s - NCTX + sz, :]
            P.dma(xt0[:, 0:nt, :], src.rearrange("(t p) d -> p t d", p=128), w=[L + "xt0"])

        loads(0)
        tix = 0
        for bi, (s, sz) in enumerate(BLOCKS):
            loads1(bi)
            if bi + 1 < len(BLOCKS):
                loads(bi + 1)
            b = bi % 2
            nt = sz // 128
            rr = 1 if bi == 0 else 0
            sak, mgk, xk = L + f"sa{b}", L + "mg0", L + "xt0"
            for j in range(8):
                for i_, (wt, nm) in enumerate(((WA, "w_a_out"), (WB, "w_b_out"), (WC, "w_c_out"))):
                    for c in range(4):
                        P.mm(ps_y[i_][:, 0:sz], wt[:, c, j * 128:(j + 1) * 128], sa[b][:, i_, c, 0:sz], c == 0, c == 3,
                             r=[sak, L + f"W_{nm}_{c}"], w=[L + f"ps_y{i_}"])
                P.tt(t1[:, 0:sz], ps_y[0][:, 0:sz], mg[b][:, j, 0:sz], ALU.mult, r=[L + "ps_y0", mgk], w=[L + "t1"])
                P.tt(t2[:, 0:sz], ps_y[1][:, 0:sz], mg[b][:, 8 + j, 0:sz], ALU.mult, r=[L + "ps_y1", mgk], w=[L + "t2"])
                P.tt(t1[:, 0:sz], t1[:, 0:sz], t2[:, 0:sz], ALU.add, r=[L + "t1", L + "t2"], w=[L + "t1"], eng="pool")
                P.tt(t2[:, 0:sz], ps_y[2][:, 0:sz], mg[b][:, 16 + j, 0:sz], ALU.mult, r=[L + "ps_y2", mgk], w=[L + "t2"])
                P.tt(m[:, j, 0:sz], t1[:, 0:sz], t2[:, 0:sz], ALU.add, r=[L + "t1", L + "t2"], w=[L + "m"], eng="pool")
            for t in range(nt):
                xo, xok = x1[tix % 2], L + f"x1{tix % 2}"
                hb, hbk = h2Tb[tix % 2], L + f"h2Tb{tix % 2}"
                gt_, gtk = gate[tix % 2], L + f"gate{tix % 2}"
                tix += 1
                tok0 = s + t * 128
                for hf in range(2):
                    for j in range(8):
                        P.mm(ps_out[hf][:, :], m[:, j, t * 128:(t + 1) * 128], WO[:, j, hf * 512:(hf + 1) * 512],
                             j == 0, j == 7, r=[L + "m", L + f"W_w_out_{j}"], w=[L + f"ps_out{hf}"])
                    P.tt(z[:, hf * 512:(hf + 1) * 512], ps_out[hf][:, :], rows[:, rr, 0, hf * 512:(hf + 1) * 512],
                         ALU.mult, r=[L + f"ps_out{hf}"] + CK, w=[L + "z"])
                P.stt(z[:], xt[b][:, t, :], ALPHA, z[:], ALU.mult, ALU.add, r=[xk, L + "z"], w=[L + "z"])
                self.layernorm(L, z, zz, st, lnr, xo, [L + "z"], [xok], CK)
                P.dma(self.X1[tok0:tok0 + 128, :], xo[:], r=[xok], w=["X1"], q="dq_pool")
                P.tt(h2[:], xo[:], rows[:, rr, 1, :], ALU.mult, r=[xok] + CK, w=[L + "h2"], eng="pool")
                P.tt(h2[:], h2[:], rows[:, rr, 2, :], ALU.add, r=[L + "h2"] + CK, w=[L + "h2"], eng="pool")
                for hf in range(2):
                    for jj in range(4):
                        j = hf * 4 + jj
                        P.tr(ps_t[hf][:, jj * 128:(jj + 1) * 128], h2[:, j * 128:(j + 1) * 128], ident[:],
                             r=[L + "h2"] + CK, w=[L + f"ps_t{hf}"])
                    P.copy(h2Tf[:, hf * 4:(hf + 1) * 4, :], ps_t[hf][:, :].rearrange("p (j t) -> p j t", t=128),
                           r=[L + f"ps_t{hf}"], w=[L + "h2Tf"], eng="act")
                P.copy(hb[:], h2Tf[:], r=[L + "h2Tf"], w=[hbk], eng="pool")
                P.dma(self.H2T[:, :, tok0:tok0 + 128].rearrange("j p t -> p j t"), hb[:], r=[hbk], w=["H2T"], q="dq_pool")
                for j in range(8):
                    P.mm(ps_r[:, 0:36], h2Tf[:, j, :], Wr[:, j, :], j == 0, j == 7, r=[L + "h2Tf"] + CK, w=[L + "ps_r"])
                self.router(L, ps_r, br, rt, gt_, [L + "ps_r"] + CK, [gtk], BIG)
                P.dma(self.GATE[tok0:tok0 + 128, :], gt_[:], r=[gtk], w=["GATE"], q="dq_pool")
        P.end_phase()

    def layernorm(self, L, z, zz, st, lnr, out, rk, wk, CK):
        P = self.P
        nc = P.nc
        sk = L + "st"
        P.op("dve", lambda: nc.vector.tensor_reduce(out=st[:, 0:1], in_=z[:], axis=AX.X, op=ALU.add), r=rk, w=[sk])
        P.act(zz[:], z[:], AF.Square, r=rk, w=[L + "zz"])
        P.op("dve", lambda: nc.vector.tensor_reduce(out=st[:, 1:2], in_=zz[:], axis=AX.X, op=ALU.add),
             r=[L + "zz"], w=[sk])
        P.ts(st[:, 2:3], st[:, 0:1], 1.0 / D, ALU.mult, r=[sk], w=[sk])
        P.tt(st[:, 3:4], st[:, 2:3], st[:, 2:3], ALU.mult, r=[sk], w=[sk])
        P.stt(st[:, 4:5], st[:, 1:2], 1.0 / D, st[:, 3:4], ALU.mult, ALU.subtract, r=[sk], w=[sk])
        P.rsqrt(st[:, 5:6], st[:, 4:5], EPS, r=[sk], w=[sk])
        P.ts(zz[:], z[:], st[:, 2:3], ALU.subtract, st[:, 5:6], ALU.mult, r=rk + [sk, L + "zz"], w=[L + "zz"])
        P.tt(zz[:], zz[:], lnr[:, 0, :], ALU.mult, r=[L + "zz"] + CK, w=[L + "zz"], eng="pool")
        P.tt(out[:], zz[:], lnr[:, 1, :], ALU.add, r=[L + "zz"] + CK, w=wk, eng="pool")

    def router(self, L, ps_r, br, rt, gate, rk, wk, BIG):
        P = self.P
        nc = P.nc
        k = L + "rt"
        lg = rt[:, 0:36]
        P.tt(lg, ps_r[:, 0:36], br[:, :], ALU.add, r=rk, w=[k])
        gmax, ngmax, ge, gsum, gp = rt[:, 40:41], rt[:, 41:42], rt[:, 44:48], rt[:, 48:49], rt[:, 49:50]
        ohg, m1, m2, dm, e21, p1, p2 = rt[:, 52:56], rt[:, 56:57], rt[:, 57:58], rt[:, 58:59], rt[:, 59:60], rt[:, 60:61], rt[:, 61:62]
        lem, oh1, oh2 = rt[:, 64:96], rt[:, 96:128], rt[:, 128:160]
        lem2 = rt[:, 160:192]
        P.op("dve", lambda: nc.vector.tensor_reduce(out=gmax, in_=rt[:, 0:4], axis=AX.X, op=ALU.max), r=[k], w=[k])
        P.ts(ngmax, gmax, -1.0, ALU.mult, r=[k], w=[k])
        P.act(ge, rt[:, 0:4], AF.Exp, bias=ngmax, r=[k], w=[k])
        P.op("dve", lambda: nc.vector.tensor_reduce(out=gsum, in_=ge, axis=AX.X, op=ALU.add), r=[k], w=[k])
        P.recip(gp, gsum, r=[k], w=[k])
        P.ts(ohg, rt[:, 0:4], gmax, ALU.is_equal, r=[k], w=[k])
        P.ts(ohg, ohg, -1.0, ALU.add, BIG, ALU.mult, r=[k], w=[k])
        P.tt(lem.rearrange("p (g e) -> p g e", e=8), rt[:, 4:36].rearrange("p (g e) -> p g e", e=8),
             ohg.unsqueeze(2).broadcast_to([128, 4, 8]), ALU.add, r=[k], w=[k])
        P.op("dve", lambda: nc.vector.tensor_reduce(out=m1, in_=lem, axis=AX.X, op=ALU.max), r=[k], w=[k])
        P.ts(oh1, lem, m1, ALU.is_equal, r=[k], w=[k])
        P.stt(lem2, oh1, -BIG, lem, ALU.mult, ALU.add, r=[k], w=[k])
        P.op("dve", lambda: nc.vector.tensor_reduce(out=m2, in_=lem2, axis=AX.X, op=ALU.max), r=[k], w=[k])
        P.ts(oh2, lem2, m2, ALU.is_equal, r=[k], w=[k])
        P.tt(dm, m2, m1, ALU.subtract, r=[k], w=[k])
        P.act(e21, dm, AF.Exp, r=[k], w=[k])
        P.ts(p1, e21, 1.0, ALU.add, r=[k], w=[k])
        P.recip(p1, p1, r=[k], w=[k])
        P.tt(p2, e21, p1, ALU.mult, r=[k], w=[k])
        P.tt(p1, p1, gp, ALU.mult, r=[k], w=[k])
        P.tt(p2, p2, gp, ALU.mult, r=[k], w=[k])
        P.ts(gate[:], oh1, p1, ALU.mult, r=[k], w=wk)
        P.stt(gate[:], oh2, p2, gate[:], ALU.mult, ALU.add, r=[k] + wk, w=wk)

    def phase4(self, l, dst, dst_has_ctx):
        P, I = self.P, self.inp
        L = f"p4_{l}_"
        P.begin_phase()
        rows = P.sb(L + "rows", [128, 2, D])
        for rr in range(2):
            self.bcast_row(rows[:, rr, :], self.modrow[l][rr:rr + 1, 5 * D:6 * D], [L + "rows"])
        lnr = P.sb(L + "lnr", [128, 2, D])
        self.bcast_row(lnr[:, 0, :], I["ln2_g"][l:l + 1, :], [L + "lnr"])
        self.bcast_row(lnr[:, 1, :], I["ln2_b"][l:l + 1, :], [L + "lnr"])
        CK = [L + "rows", L + "lnr"]
        NT = 17 if dst_has_ctx else 16
        TBASE = 0 if dst_has_ctx else NCTX
        HT = NT * 128
        H = P.sb(L + "H", [128, 8, HT], BF16)
        G = P.sb(L + "G", [128, NT, 32])
        yacc = P.sb(L + "yacc", [128, NT, D])
        W1 = [P.sb(L + f"W1_{i}", [128, 8, 256], BF16) for i in range(2)]
        W3 = [P.sb(L + f"W3_{i}", [128, 8, 256], BF16) for i in range(2)]
        W2 = [P.sb(L + f"W2_{i}", [128, 2, D], BF16) for i in range(2)]
        aT = [P.sb(L + f"aT{i}", [128, 2, 512], BF16) for i in range(2)]
        sg = [P.sb(L + f"sg{i}", [128, 512]) for i in range(2)]
        ps_h1 = [P.ps(L + f"ps_h1{i}", [128, 512]) for i in range(2)]
        ps_h3 = [P.ps(L + f"ps_h3{i}", [128, 512]) for i in range(2)]
        ps_y = [P.ps(L + f"ps_y{i}", [128, 512]) for i in range(4)]
        x1t = [P.sb(L + f"x1t{i}", [128, D]) for i in range(2)]
        z = P.sb(L + "z", [128, D])
        zz = P.sb(L + "zz", [128, D])
        st = P.sb(L + "st", [128, 8])
        xo = [P.sb(L + f"xo{i}", [128, D]) for i in range(2)]
        blocks = [(i * 512, 512) for i in range(4)] + ([(2048, 128)] if dst_has_ctx else [])

        def load_w(e):
            b = e % 2
            P.dma(W1[b][:], I["w1"][l, e].rearrange("(k p) n -> p k n", p=128), w=[L + f"W1_{b}"], q="dq_pool")
            P.dma(W3[b][:], I["w3"][l, e].rearrange("(k p) n -> p k n", p=128), w=[L + f"W3_{b}"], q="dq_pool")
            P.dma(W2[b][:], I["w2"][l, e].rearrange("(o p) n -> p o n", p=128), w=[L + f"W2_{b}"], q="dq_pool")

        st_ = dict(cy=0)
        a_of = {}
        pending = []

        def emit_h(e, bb):
            b0, bsz = blocks[bb]
            b = e % 2
            wk1, wk3 = L + f"W1_{b}", L + f"W3_{b}"
            idx = (e * len(blocks) + bb) % 2
            a_, ak = aT[idx], L + f"aT{idx}"
            a_of[(e, bb)] = (a_, ak)
            for o in range(2):
                for k in range(8):
                    P.mm(ps_h1[o][:, 0:bsz], W1[b][:, k, o * 128:(o + 1) * 128], H[:, k, b0:b0 + bsz],
                         k == 0, k == 7, r=[wk1, L + "H"], w=[L + f"ps_h1{o}"])
                for k in range(8):
                    P.mm(ps_h3[o][:, 0:bsz], W3[b][:, k, o * 128:(o + 1) * 128], H[:, k, b0:b0 + bsz],
                         k == 0, k == 7, r=[wk3, L + "H"], w=[L + f"ps_h3{o}"])
                P.act(sg[o][:, 0:bsz], ps_h1[o][:, 0:bsz], AF.Silu, r=[L + f"ps_h1{o}"], w=[L + f"sg{o}"])
                P.tt(a_[:, o, 0:bsz], sg[o][:, 0:bsz], ps_h3[o][:, 0:bsz], ALU.mult,
                     r=[L + f"sg{o}", L + f"ps_h3{o}"], w=[ak])

        def emit_y(e, bb):
            b0, bsz = blocks[bb]
            b = e % 2
            wk2 = L + f"W2_{b}"
            a_, ak = a_of.pop((e, bb))
            for t in range(bsz // 128):
                tile = b0 // 128 + t
                for hf in range(2):
                    cy = st_["cy"]
                    py, pyk = ps_y[cy % 4], L + f"ps_y{cy % 4}"
                    st_["cy"] = cy + 1
                    for o in range(2):
                        P.mm(py[:, :], a_[:, o, t * 128:(t + 1) * 128], W2[b][:, o, hf * 512:(hf + 1) * 512],
                             o == 0, o == 1, r=[ak, wk2], w=[pyk])
                    ya = yacc[:, tile, hf * 512:(hf + 1) * 512]
                    yk = L + f"yacc{tile}_{hf}"
                    if e == 0:
                        P.ts(ya, py[:, :], G[:, tile, e:e + 1], ALU.mult, r=[pyk, L + "G"], w=[yk])
                    else:
                        P.stt(ya, py[:, :], G[:, tile, e:e + 1], ya, ALU.mult, ALU.add,
                              r=[pyk, L + "G", yk], w=[yk])

        for half in range(2):
            T0 = TBASE + half * HT
            P.dma(H[:], self.H2T[:, :, T0:T0 + HT].rearrange("j p t -> p j t"), w=[L + "H"])
            P.dma(G[:], self.GATE[T0:T0 + HT, :].rearrange("(t p) e -> p t e", p=128), w=[L + "G"])
            load_w(0)
            for e in range(32):
                for bb in range(len(blocks)):
                    pending.append((e, bb))
                    emit_h(e, bb)
                    if len(pending) > 1:
                        emit_y(*pending.pop(0))
                    if bb == 0 and e + 1 < 32:
                        load_w(e + 1)
            while pending:
                emit_y(*pending.pop(0))
            for t in range(NT):
                tok0 = TBASE + (half * NT + t) * 128
                rr = 1 if tok0 < NCTX else 0
                xi, xik = x1t[t % 2], L + f"x1t{t % 2}"
                o_, ok = xo[t % 2], L + f"xo{t % 2}"
                P.dma(xi[:], self.X1[tok0:tok0 + 128, :], w=[xik])
                yks = [L + f"yacc{t}_{hf}" for hf in range(2)]
                P.tt(z[:], yacc[:, t, :], rows[:, rr, :], ALU.mult, r=yks + CK, w=[L + "z"], eng="pool")
                P.stt(z[:], xi[:], ALPHA, z[:], ALU.mult, ALU.add, r=[xik, L + "z"], w=[L + "z"])
                self.layernorm(L, z, zz, st, lnr, o_, [L + "z"], [ok], CK)
                if dst_has_ctx:
                    P.dma(dst[tok0:tok0 + 128, :], o_[:], r=[ok], w=["dst"], q="dq_pool")
                else:
                    P.dma(dst[tok0 - NCTX:tok0 - NCTX + 128, :], o_[:], r=[ok], w=["dst"], q="dq_pool")
        P.end_phase()

    def build(self, phases=None, n_layers=2):
        P = self.P
        self.declare_inputs()
        ALL = ("p0", "p1", "p2a", "p2b1", "p2b2", "p2c1", "p2c2", "p2c3", "p2c4", "p3", "p4")
        if phases is None:
            phases = ALL
        self.modrow = [self.scratch(f"modrow{l}", [2, 6 * D]) for l in range(2)]
        self.U = self.scratch("U", [4, 128, T])
        self.QD = self.scratch("QD", [3, 128, T])
        self.KVD = self.scratch("KVD", [2, 128, T])
        self.KR = self.scratch("KR", [32, T])
        self.GQKV = self.scratch("GQKV", [12, 128, T])
        self.OG = self.scratch("OG", [4, 128, T], BF16)
        self.BETA = self.scratch("BETA", [16, T])
        self.GL = self.scratch("GL", [16, T])
        self.MG = self.scratch("MG", [24, 128, T], BF16)
        self.SA = self.scratch("SA", [4, 128, T], BF16)
        self.KT = self.scratch("KT", [8, 96, T], BF16)
        self.QT = self.scratch("QT", [8, 96, T], BF16)
        self.VX = self.scratch("VX", [8, 128, 34, 128], BF16)
        self.OB = self.scratch("OB", [4, 128, T], BF16)
        self.QKN = self.scratch("QKN", [8, 128, T])
        self.KVtok = self.scratch("KVtok", [T, 2, 512])
        self.BGtok = self.scratch("BGtok", [T, 32])
        self.PW = self.scratch("PW", [68, 2, 64, 512], BF16)
        self.PQ = self.scratch("PQ", [68, 2, 64, 512], BF16)
        self.PK = self.scratch("PK", [68, 2, 64, 512], BF16)
        self.PM = self.scratch("PM", [68, 2, 64, 512], BF16)
        self.PU = self.scratch("PU", [68, 2, 64, 512])
        self.PG = self.scratch("PG", [68, 2, 64, 8])
        self.ODIR = self.scratch("ODIR", [2, T, 512])
        self.OC = self.scratch("OC", [4, 128, T], BF16)
        self.X1 = self.scratch("X1", [T, D])
        self.H2T = self.scratch("H2T", [8, 128, T], BF16)
        self.GATE = self.scratch("GATE", [T, 32])
        self.X2 = self.scratch("X2", [T, D])
        self.out = P.dram("out", [NL, D], F32, kind="ExternalOutput")
        kw = getattr(self, "p2c2_kw", {})
        for l in range(n_layers):
            if l == 0:
                xl, xc = self.inp["x"], self.inp["ctx"]
            else:
                xl, xc = self.X2[NCTX:T, :], self.X2[0:NCTX, :]
            last = l == DEPTH - 1
            if "p0" in phases:
                self.phase0(l)
            if "p1" in phases:
                self.phase1(l, xl, xc, "xin")
            if "p2a" in phases:
                self.phase2a(l)
            if "p2b1" in phases:
                self.phase2b1(l)
            if "p2b2" in phases:
                self.phase2b2(l)
            if "p2c1" in phases:
                self.phase2c1(l)
            if "p2c2" in phases:
                self.phase2c2(l, **kw)
            if "p2c3" in phases:
                self.phase2c3(l, **kw)
            if "p2c4" in phases:
                self.phase2c4(l)
            if "p3" in phases:
                self.phase3(l, xl, xc)
            if "p4" in phases:
                if last:
                    self.phase4(l, self.out, False)
                else:
                    self.phase4(l, self.X2, True)
        return P.nc


def host_consts():
    ident = np.eye(128, dtype=np.float32)
    nf = 8
    inv = (10000.0 ** (-np.arange(nf, dtype=np.float32) / nf)).astype(np.float32)
    rows = NL // 64
    r = np.repeat(np.arange(rows, dtype=np.float32), 64)
    col = np.tile(np.arange(64, dtype=np.float32), rows)
    ang = np.stack([r[:, None] * inv, col[:, None] * inv], axis=1)
    cos = np.cos(ang).astype(np.float32)
    sin = np.sin(ang).astype(np.float32)
    cs = np.zeros((2, 32, T), np.float32)
    cs[0, :, :NCTX] = 1.0
    for a in range(2):
        for h in range(2):
            d0 = a * 16 + h * 8
            cs[0, d0:d0 + 8, NCTX:] = cos[:, a, :].T
            cs[1, d0:d0 + 8, NCTX:] = (-sin[:, a, :].T) if h == 0 else sin[:, a, :].T
    perm = np.zeros((32, 32), np.float32)
    for m_ in range(32):
        perm[m_ ^ 8, m_] = 1.0
    pp, ff = np.meshgrid(np.arange(64), np.arange(64), indexing="ij")
    gmask = np.stack([ff <= pp, ff < pp, ff >= pp, ff > pp]).astype(np.float32)
    return ident, cs, perm, gmask


def make_in_maps(inputs, cores):
    ident, cs, perm, gmask = host_consts()
    maps = []
    for b in cores:
        m = {}
        for k, v in inputs.items():
            v = np.asarray(v)
            if k in ("x", "c", "ctx"):
                m[k] = np.ascontiguousarray(v[b])
            elif k in ("a_log", "dt_bias"):
                m[k] = np.ascontiguousarray(v.reshape(2, 16))
            else:
                m[k] = np.ascontiguousarray(v)
        m["ident"] = ident
        m["rope_cs"] = cs
        m["perm32"] = perm
        m["gmask"] = gmask
        maps.append(m)
    return maps


_NC_CACHE = {}


def kernel(**inputs):
    n = 8
    if "nc" not in _NC_CACHE:
        net = Net()
        _NC_CACHE["nc"] = net.build()
    nc = _NC_CACHE["nc"]
    maps = make_in_maps(inputs, list(range(n)))
    res = run_bass_kernel_spmd(nc, maps, core_ids=list(range(n)))
    return np.stack([np.asarray(r["out"]) for r in res.results], axis=0).astype(np.float32)
```
